# Optimizing a Trainium2 kernel written in Bass

```python
import math
import jax, jax.numpy as jnp
from jax import lax
import numpy as np

D_MODEL = 1024
BATCH = 2
SEQ = 8192
DEPTH = 2

D_MIX = D_MODEL
LRU_WIDTH = D_MIX // 4
LRU_BLOCKS = 4
LRU_CONV = 4
LRU_C = 8.0
HG_HEADS = 4
HG_DK = 64
HG_DV = 64
HG_CHUNK = 64
ATT_HEADS = 8
ATT_HD = 64
MOBA_BLOCK = 256
MOBA_TOPK = 3
Q_BLOCK = 64
D_FF = 4 * D_MODEL
EPS = 1e-6
NEG = -1e30
TINY = 1e-30

SPLITS = (LRU_WIDTH, LRU_WIDTH,
          HG_HEADS * HG_DK, HG_HEADS * HG_DK, HG_HEADS * HG_DV, HG_HEADS * HG_DV,
          ATT_HEADS * ATT_HD, ATT_HEADS * ATT_HD, ATT_HEADS * ATT_HD)
D_IN = sum(SPLITS)
D_OUT_CAT = LRU_WIDTH + HG_HEADS * HG_DV + ATT_HEADS * ATT_HD

kernel_name = "hybrid_rglru_hgrn2_moba_parallel_heads"


def rms_norm(x, g):
    xf = x.astype(jnp.float32)
    y = xf * lax.rsqrt(jnp.mean(xf * xf, axis=-1, keepdims=True) + EPS)
    return (y * g.astype(jnp.float32)).astype(x.dtype)


def rglru_group(xb, yb, conv_w, conv_b, wa, ba, wx, bx, lam):
    Bn, S, W = xb.shape
    xc = lax.conv_general_dilated(
        xb, conv_w.reshape(LRU_CONV, 1, W).astype(xb.dtype), window_strides=(1,),
        padding=[(LRU_CONV - 1, 0)], dimension_numbers=('NWC', 'WIO', 'NWC'),
        feature_group_count=W) + conv_b.astype(xb.dtype)
    xg = xc.reshape(Bn, S, LRU_BLOCKS, W // LRU_BLOCKS)
    r = jax.nn.sigmoid(jnp.einsum('bsgi,gij->bsgj', xg, wa).reshape(Bn, S, W).astype(jnp.float32)
                       + ba.astype(jnp.float32))
    i = jax.nn.sigmoid(jnp.einsum('bsgi,gij->bsgj', xg, wx).reshape(Bn, S, W).astype(jnp.float32)
                       + bx.astype(jnp.float32))
    log_a = -LRU_C * r * jax.nn.softplus(-lam.astype(jnp.float32))
    a = jnp.exp(log_a)
    u = jnp.sqrt(-jnp.expm1(2.0 * log_a)) * (i * xc.astype(jnp.float32))

    def combine(c1, c2):
        a1, b1 = c1
        a2, b2 = c2
        return a1 * a2, a2 * b1 + b2

    _, h = lax.associative_scan(combine, (a, u), axis=1)
    return (h * jax.nn.gelu(yb.astype(jnp.float32))).astype(xb.dtype)


def hgrn2_group(q, f, i, g, lb, norm_w):
    Bn, S, _ = q.shape
    nc = S // HG_CHUNK
    lb = lb.astype(jnp.float32)
    fpre = f.astype(jnp.float32)
    log_f = jnp.logaddexp(jnp.log(jnp.maximum(lb, TINY)), jnp.log1p(-lb) + jax.nn.log_sigmoid(fpre))
    k = (1.0 - lb) * jax.nn.sigmoid(-fpre)
    qs = jax.nn.silu(q.astype(jnp.float32))

    def heads(t, d):
        return t.astype(jnp.float32).reshape(Bn, nc, HG_CHUNK, HG_HEADS, d).transpose(1, 0, 3, 2, 4)

    qh, kh, lfh, vh = heads(qs, HG_DK), heads(k, HG_DK), heads(log_f, HG_DK), heads(i, HG_DV)
    mask = jnp.tril(jnp.ones((HG_CHUNK, HG_CHUNK), dtype=bool))

    def step(state, inp):
        qc, kc, lfc, vc = inp
        b = jnp.cumsum(lfc, axis=2)
        diff = b[:, :, :, None, :] - b[:, :, None, :, :]
        decay = jnp.exp(jnp.where(mask[:, :, None], diff, NEG))
        att = jnp.einsum('bhtd,bhsd,bhtsd->bhts', qc, kc, decay)
        o = jnp.einsum('bhts,bhse->bhte', att, vc) + jnp.einsum('bhtd,bhde->bhte', qc * jnp.exp(b), state)
        b_last = b[:, :, -1:, :]
        new_state = jnp.exp(b_last[:, :, 0, :])[..., None] * state + \
            jnp.einsum('bhsd,bhse->bhde', kc * jnp.exp(b_last - b), vc)
        return new_state, o

    s0 = jnp.zeros((Bn, HG_HEADS, HG_DK, HG_DV), jnp.float32)
    _, o = lax.scan(step, s0, (qh, kh, lfh, vh))
    o = o.transpose(1, 0, 3, 2, 4).reshape(Bn, S, HG_HEADS, HG_DV)
    o = rms_norm(o, norm_w) * jax.nn.silu(g.reshape(Bn, S, HG_HEADS, HG_DV).astype(jnp.float32))
    return o.reshape(Bn, S, HG_HEADS * HG_DV).astype(q.dtype)


def moba_group(cq, ck, cv):
    Bn, S, _ = cq.shape
    H, hd = ATT_HEADS, ATT_HD
    s_pad = -(-S // MOBA_BLOCK) * MOBA_BLOCK
    pad = s_pad - S

    def heads(t):
        t = t.reshape(Bn, S, H, hd).transpose(0, 2, 1, 3)
        return jnp.pad(t, ((0, 0), (0, 0), (0, pad), (0, 0)))

    q, k, v = heads(cq), heads(ck), heads(cv)
    nblk = s_pad // MOBA_BLOCK
    topk = min(MOBA_TOPK, nblk)
    kb = k.reshape(Bn, H, nblk, MOBA_BLOCK, hd)
    vb = v.reshape(Bn, H, nblk, MOBA_BLOCK, hd)
    kmean = jnp.mean(kb.astype(jnp.float32), axis=3).astype(k.dtype)
    scale = 1.0 / math.sqrt(hd)
    slopes = jnp.asarray(2.0 ** (-8.0 * np.arange(1, H + 1) / H), dtype=jnp.float32)
    nq = s_pad // Q_BLOCK
    qs = q.reshape(Bn, H, nq, Q_BLOCK, hd).transpose(2, 0, 1, 3, 4)
    bi = jnp.arange(Bn)[:, None, None, None]
    hi = jnp.arange(H)[None, :, None, None]
    blk_pos = jnp.arange(MOBA_BLOCK)

    def step(args):
        qi, q_blk = args
        t = qi * Q_BLOCK + jnp.arange(Q_BLOCK)
        j = (qi * Q_BLOCK) // MOBA_BLOCK
        gate = jnp.einsum('bhqd,bhnd->bhqn', q_blk, kmean).astype(jnp.float32)
        gate = jnp.where(jnp.arange(nblk) < j, gate, NEG)
        _, sel = lax.top_k(gate, topk)
        valid = jnp.arange(topk) < j
        ksel = kb[bi, hi, sel]
        vsel = vb[bi, hi, sel]
        s_sel = jnp.einsum('bhqd,bhqkld->bhqkl', q_blk, ksel).astype(jnp.float32) * scale
        pos_sel = sel[..., None] * MOBA_BLOCK + blk_pos
        s_sel = s_sel - slopes[:, None, None, None] * (t[:, None, None] - pos_sel).astype(jnp.float32)
        s_sel = jnp.where(valid[:, None], s_sel, NEG)
        k_own = lax.dynamic_slice_in_dim(kb, j, 1, axis=2)[:, :, 0]
        v_own = lax.dynamic_slice_in_dim(vb, j, 1, axis=2)[:, :, 0]
        s_own = jnp.einsum('bhqd,bhld->bhql', q_blk, k_own).astype(jnp.float32) * scale
        rel = t[:, None] - (j * MOBA_BLOCK + blk_pos)[None, :]
        s_own = jnp.where(rel >= 0, s_own - slopes[:, None, None] * rel.astype(jnp.float32), NEG)
        scores = jnp.concatenate([s_own, s_sel.reshape(Bn, H, Q_BLOCK, topk * MOBA_BLOCK)], axis=-1)
        p = jax.nn.softmax(scores, axis=-1).astype(v.dtype)
        p_own = p[..., :MOBA_BLOCK]
        p_sel = p[..., MOBA_BLOCK:].reshape(Bn, H, Q_BLOCK, topk, MOBA_BLOCK)
        out = jnp.einsum('bhql,bhld->bhqd', p_own, v_own) + jnp.einsum('bhqkl,bhqkld->bhqd', p_sel, vsel)
        return out.astype(v.dtype)

    o = lax.map(step, (jnp.arange(nq), qs))
    o = o.transpose(1, 2, 0, 3, 4).reshape(Bn, H, s_pad, hd)[:, :, :S]
    return o.transpose(0, 2, 1, 3).reshape(Bn, S, H * hd)


def setup_inputs(seed: int = 0) -> dict:
    key = jax.random.key(seed)
    ks = jax.random.split(key, 20)
    f32 = jnp.float32
    L = DEPTH
    nrm = lambda k, shape, fan: jax.random.normal(k, shape, f32) * (fan ** -0.5)
    gain = lambda k, shape: 1.0 + 0.05 * jax.random.normal(k, shape, f32)
    bw = LRU_WIDTH // LRU_BLOCKS
    u = jax.random.uniform(ks[10], (L, LRU_WIDTH), f32, 0.9, 0.999)
    a_base = u ** (1.0 / LRU_C)
    lam = jnp.log(a_base) - jnp.log1p(-a_base)
    return {
        "x": jax.random.normal(ks[0], (BATCH, SEQ, D_MODEL), f32),
        "w_in": nrm(ks[1], (L, D_MODEL, D_IN), D_MODEL),
        "w_out": nrm(ks[2], (L, D_OUT_CAT, D_MODEL), D_OUT_CAT),
        "norm_mix": gain(ks[3], (L, D_MODEL)),
        "norm_mlp": gain(ks[4], (L, D_MODEL)),
        "lru_conv_w": nrm(ks[5], (L, LRU_CONV, LRU_WIDTH), LRU_CONV),
        "lru_conv_b": 0.01 * jax.random.normal(ks[6], (L, LRU_WIDTH), f32),
        "lru_wa": nrm(ks[7], (L, LRU_BLOCKS, bw, bw), bw),
        "lru_ba": 0.01 * jax.random.normal(ks[8], (L, LRU_WIDTH), f32),
        "lru_wx": nrm(ks[9], (L, LRU_BLOCKS, bw, bw), bw),
        "lru_bx": 0.01 * jax.random.normal(ks[11], (L, LRU_WIDTH), f32),
        "lru_lambda": lam,
        "hg_lower_bounds": 0.1 * jax.random.normal(ks[12], (L, HG_HEADS * HG_DK), f32),
        "hg_norm_w": gain(ks[13], (L, HG_DV)),
        "lru_out_norm": gain(ks[14], (L, LRU_WIDTH)),
        "att_out_norm": gain(ks[15], (L, ATT_HEADS * ATT_HD)),
        "w_ff1": nrm(ks[16], (L, D_MODEL, D_FF), D_MODEL),
        "w_ff2": nrm(ks[17], (L, D_FF, D_MODEL), D_FF),
        "norm_final": gain(ks[18], (D_MODEL,)),
    }


def reference(x, w_in, w_out, norm_mix, norm_mlp, lru_conv_w, lru_conv_b, lru_wa, lru_ba, lru_wx, lru_bx,
              lru_lambda, hg_lower_bounds, hg_norm_w, lru_out_norm, att_out_norm, w_ff1, w_ff2, norm_final):
    lb_soft = jax.nn.softmax(hg_lower_bounds.astype(jnp.float32), axis=0)
    lower_bounds = jnp.cumsum(lb_soft, axis=0) - lb_soft[0]
    split_at = np.cumsum(SPLITS)[:-1].tolist()
    for l in range(DEPTH):
        h = rms_norm(x, norm_mix[l])
        z = jnp.einsum('bsd,de->bse', h, w_in[l])
        a_x, a_y, b_q, b_f, b_i, b_g, c_q, c_k, c_v = jnp.split(z, split_at, axis=-1)
        ya = rglru_group(a_x, a_y, lru_conv_w[l], lru_conv_b[l], lru_wa[l], lru_ba[l],
                         lru_wx[l], lru_bx[l], lru_lambda[l])
        yb = hgrn2_group(b_q, b_f, b_i, b_g, lower_bounds[l], hg_norm_w[l])
        yc = moba_group(c_q, c_k, c_v)
        y = jnp.concatenate([rms_norm(ya, lru_out_norm[l]), yb.astype(x.dtype),
                             rms_norm(yc, att_out_norm[l])], axis=-1)
        x = x + jnp.einsum('bse,ed->bsd', y, w_out[l])
        h = rms_norm(x, norm_mlp[l])
        u = jnp.square(jax.nn.relu(jnp.einsum('bsd,df->bsf', h, w_ff1[l])))
        x = x + jnp.einsum('bsf,fd->bsd', u, w_ff2[l])
    return rms_norm(x, norm_final)
```

```python
import numpy as np
import concourse.bass as bass
import concourse.mybir as mybir

F32 = mybir.dt.float32
BF16 = mybir.dt.bfloat16
AF = mybir.ActivationFunctionType
ALU = mybir.AluOpType
AX = mybir.AxisListType

ENGS = ("pe", "act", "dve", "pool", "sp")


class Buf:
    __slots__ = ("name", "w", "r", "dsem", "excl")

    def __init__(self, name):
        self.name = name
        self.w = None
        self.r = []
        self.dsem = None
        self.excl = False


class DmaSem:
    __slots__ = ("sem", "issued", "group_open", "name")

    def __init__(self, sem, name):
        self.sem = sem
        self.issued = 0
        self.group_open = False
        self.name = name


class Prog:
    def __init__(self, nc, stack):
        self.nc = nc
        self.stack = stack
        self.ops = {e: [] for e in ENGS}
        self.cnt = {e: 0 for e in ENGS}
        self.esem = {}
        for e in ("pe", "act", "dve", "pool"):
            self.esem[e] = stack.enter_context(nc.semaphore("s_" + e))
        self.known = {e: {} for e in ENGS}
        self.dsems = []
        self.nbuf = 0
        import os
        self.limit = int(os.environ.get("MK_LIMIT", "0"))
        self.nrec = 0
        self.lastdesc = None

    def buf(self, name=None):
        self.nbuf += 1
        return Buf(name or f"b{self.nbuf}")

    def sbuf(self, name, shape, dtype):
        t = self.stack.enter_context(self.nc.sbuf_tensor("sb_" + name, list(shape), dtype))
        return t

    def psum(self, name, shape, dtype=F32):
        t = self.stack.enter_context(self.nc.psum_tensor("ps_" + name, list(shape), dtype))
        return t

    def new_dsem(self, name):
        s = self.stack.enter_context(self.nc.semaphore("d_" + name))
        d = DmaSem(s, name)
        self.dsems.append(d)
        return d

    def _need(self, eng, dep, waits):
        if dep is None:
            return
        if dep[0] == "eng":
            _, e, idx = dep
            if e == eng and eng in ("pe",):
                return
            key = ("e", e)
            if self.known[eng].get(key, 0) >= idx:
                return
            if e == eng and False:
                return
            self.known[eng][key] = idx
            waits.append((self.esem[e], idx))
        else:
            _, ds, val = dep
            val = max(val, 16 * ds.issued)
            ds.group_open = False
            key = ("d", id(ds))
            if self.known[eng].get(key, 0) >= val:
                return
            self.known[eng][key] = val
            waits.append((ds.sem, val))

    def _deps(self, eng, reads, writes):
        waits = []
        for b in reads:
            self._need(eng, b.w, waits)
        for b in writes:
            self._need(eng, b.w, waits)
            for r in b.r:
                self._need(eng, r, waits)
        m = {}
        for s, v in waits:
            k = id(s)
            if k not in m or m[k][1] < v:
                m[k] = (s, v)
        return list(m.values())

    def op(self, eng, fn, reads=(), writes=()):
        self.nrec += 1
        if self.limit and self.nrec > self.limit:
            return
        import traceback
        self.lastdesc = (self.nrec, eng, traceback.extract_stack(limit=3)[0].lineno, [b.name for b in reads], [b.name for b in writes])
        writes = list(writes) + [b for b in reads if b.excl and b not in writes]
        waits = self._deps(eng, reads, writes)
        self.cnt[eng] += 1
        idx = self.cnt[eng]
        tag = ("eng", eng, idx)
        for b in reads:
            if b not in writes:
                b.r.append(tag)
        for b in writes:
            b.w = tag
            b.r = []
        self.ops[eng].append((waits, fn, (self.esem[eng], 1)))

    def dma(self, q, out_ap, in_ap, reads=(), writes=(), sem_buf=None, **kw):
        self.nrec += 1
        if self.limit and self.nrec > self.limit:
            return
        sb = sem_buf
        if sb is None:
            for b in list(writes) + list(reads):
                sb = b
                break
        if sb.dsem is None:
            sb.dsem = self.new_dsem(sb.name)
        ds = sb.dsem
        waits = self._deps(q, reads, writes)
        if (not ds.group_open) and ds.issued > 0:
            w2 = []
            self._need(q, ("dma", ds, 16 * ds.issued), w2)
            waits += w2
        ds.issued += 1
        ds.group_open = True
        tag = ("dma", ds, 16 * ds.issued)
        for b in reads:
            b.r.append(tag)
        for b in writes:
            b.w = tag
            b.r = []

        def fn(e, out_ap=out_ap, in_ap=in_ap, kw=kw):
            return e.dma_start(out=out_ap, in_=in_ap, **kw)
        if q in ("pool", "act"):
            pass
        self.ops[q].append((waits, fn, (ds.sem, 16)))

    def finish(self):
        fin = []
        for ds in self.dsems:
            if ds.issued:
                fin.append((ds.sem, 16 * ds.issued))
        nc = self.nc
        ops = self.ops
        with nc.Block() as block:
            def emit(e, lst, final=None):
                for waits, fn, inc in lst:
                    for s, v in waits:
                        e.wait_ge(s, v)
                    ins = fn(e)
                    if inc is not None:
                        ins.then_inc(inc[0], inc[1])
                if final:
                    for s, v in final:
                        e.wait_ge(s, v)

            @block.sync
            def _(e):
                emit(e, ops["sp"], fin)

            @block.tensor
            def _(e):
                emit(e, ops["pe"])

            @block.scalar
            def _(e):
                emit(e, ops["act"])

            @block.vector
            def _(e):
                emit(e, ops["dve"])

            @block.gpsimd
            def _(e):
                emit(e, ops["pool"])


BIG = 30000.0
NEGF = -1.0e30
LN2 = 0.6931471805599453
GC = 0.7978845608028654


def host_consts_B(T, slopes2):
    import ml_dtypes
    bf = ml_dtypes.bfloat16
    nb = T // 256
    c = {}
    oh = np.zeros((33, T), np.float32)
    for n in range(nb):
        oh[n, n * 256:(n + 1) * 256] = 1.0
    oh[32, :] = 1.0
    c["ohk"] = oh.astype(bf)
    t = np.arange(T) % 512
    c["crow"] = np.stack([-8.0 * s * t for s in slopes2]).astype(np.float32).astype(bf)
    NM = T // 128 + 4
    m = np.arange(NM)
    p = np.arange(128)
    tab = np.zeros((128, 2, NM), np.float32)
    for h in range(2):
        tab[:, h, :] = slopes2[h] * (p[:, None] + 128.0 * (m[None, :] - (T // 128)))
    c["abias"] = tab
    c["cmask"] = (np.arange(128)[None, :] >= np.arange(128)[:, None]).astype(np.float32).astype(bf)
    cm = (np.arange(64)[None, :] >= np.arange(64)[:, None]).astype(np.float32)
    bm = np.zeros((128, 128), np.float32)
    bm[:64, :64] = cm
    bm[64:, 64:] = cm
    c["hmask"] = np.tile(bm, (1, 4)).astype(np.float32)
    rm = np.ones((64, 512), np.float32)
    rm[:, ::64] = 0.0
    c["rmask"] = rm
    c["ident"] = np.eye(128, dtype=np.float32).astype(bf)
    c["ones64"] = np.full((64, 64), 1.0 / 64.0, np.float32)
    return c


def build_B(nc, P, T, io, maxdist=(None, None)):
    NB = T // 512
    NM = T // 128 + 4
    MOFF = T // 128
    hT, yT = io["hT"], io["yT"]
    wfm = P.sbuf("wfm", [128, 8, 640], BF16); Bwfm = P.buf("wfm")
    wtm = P.sbuf("wtm", [128, 8, 192], BF16); Bwtm = P.buf("wtm")
    par = P.sbuf("par", [128, 32], F32); Bpar = P.buf("par")
    wg = P.sbuf("wg", [128, 128], BF16); Bwg = P.buf("wg")
    abias = P.sbuf("abias", [128, 2, NM], F32); Bab = P.buf("abias")
    cmask = P.sbuf("cmask", [128, 128], BF16); Bcm = P.buf("cmask")
    hmask = P.sbuf("hmask", [128, 512], F32); Bhm = P.buf("hmask")
    rmask = P.sbuf("rmask", [64, 512], F32); Brm = P.buf("rmask")
    ident = P.sbuf("ident", [128, 128], BF16); Bid = P.buf("ident")
    ones64 = P.sbuf("ones64", [64, 64], F32); Bo64 = P.buf("ones64")
    QA = P.sbuf("QA", [128, T], BF16); QB = P.sbuf("QB", [128, T], BF16)
    KA = P.sbuf("KA", [128, T], BF16); KB = P.sbuf("KB", [128, T], BF16)
    NBK = T // 512
    BQ = [[P.buf(f"Q{h}_{i}") for i in range(NBK)] for h in range(2)]
    BK = [[P.buf(f"K{h}_{i}") for i in range(NBK)] for h in range(2)]
    Qh = [QA, QB]; Kh = [KA, KB]
    VV = P.sbuf("VV", [128, T // 128, 2, 65], BF16); BV = [P.buf(f"VV{i}") for i in range(NBK)]
    VH = P.sbuf("VH", [128, T // 128, 64], BF16); BVH = [P.buf(f"VH{i}") for i in range(NBK)]
    ones64r = P.sbuf("ones64r", [128, 64], F32); Bo64r = P.buf("ones64r")
    kmT = P.sbuf("kmT", [128, 32], BF16); Bkm = P.buf("kmT")
    banks = [P.psum(f"bank{i}", [128, 512]) for i in range(8)]
    Bbank = [P.buf(f"bank{i}") for i in range(8)]
    for b_ in Bbank:
        b_.excl = True
    rr = [0]

    def nbank():
        i = rr[0] % 6
        rr[0] += 1
        return banks[i], Bbank[i]
    pvb = [(banks[6], Bbank[6]), (banks[7], Bbank[7])]

    P.dma("pool", wfm[:], io["wfm"].rearrange("(c p) n -> p c n", p=128), writes=[Bwfm])
    P.dma("pool", wtm[:], io["wtm"].rearrange("(c p) n -> p c n", p=128), writes=[Bwtm])
    P.dma("pool", wg[:], io["wg"], writes=[Bwg])
    P.dma("sp", par[:, 0:16], io["par"], writes=[Bpar])
    P.dma("sp", abias[:], io["abias"], writes=[Bab])
    P.dma("sp", cmask[:], io["cmask"], writes=[Bcm])
    P.dma("sp", hmask[:], io["hmask"], writes=[Bhm])
    P.dma("sp", rmask[:], io["rmask"], writes=[Brm])
    P.dma("sp", ident[:], io["ident"], writes=[Bid])
    P.dma("sp", ones64[:], io["ones64"], writes=[Bo64])
    for h in range(2):
        P.op("pool", lambda e, h=h: e.memset(Qh[h][:], 0.0), writes=BQ[h])
        P.op("pool", lambda e, h=h: e.memset(Kh[h][:], 0.0), writes=BK[h])
    P.dma("sp", KA[64:97, :], io["ohk"], writes=BK[0], sem_buf=BK[0][0])
    P.dma("sp", KB[0:33, :], io["ohk"], writes=BK[1], sem_buf=BK[1][0])
    P.dma("sp", QA[96:97, :], io["crow"][0:1, :], writes=BQ[0], sem_buf=BQ[0][0])
    P.dma("sp", QB[32:33, :], io["crow"][1:2, :], writes=BQ[1], sem_buf=BQ[1][0])
    P.op("pool", lambda e: e.memset(VV[:, :, :, 64:65], 1.0), writes=BV)
    P.op("pool", lambda e: e.memset(ones64r[:], 1.0), writes=[Bo64r])
    P.op("pool", lambda e: e.memset(kmT[:], 0.0), writes=[Bkm])

    L = slice(64, 128)
    H = slice(0, 64)
    def pc(rows, j):
        return par[rows, j:j + 1]
    P.op("dve", lambda e: e.memset(par[:, 24:25], -LN2), reads=[Bpar], writes=[Bpar])
    P.op("dve", lambda e: e.memset(par[:, 25:26], 1e-6), reads=[Bpar], writes=[Bpar])
    P.op("dve", lambda e: e.memset(par[:, 26:27], 1.0), reads=[Bpar], writes=[Bpar])
    P.op("dve", lambda e: e.tensor_scalar(out=par[L, 8:10], in0=par[L, 5:7], scalar1=0.5, scalar2=None, op0=ALU.mult), reads=[Bpar], writes=[Bpar])
    P.op("act", lambda e: e.activation(out=pc(L, 12), in_=pc(L, 7), func=AF.Exp, scale=-1.0), reads=[Bpar], writes=[Bpar])
    P.op("act", lambda e: e.activation(out=pc(L, 13), in_=pc(L, 12), func=AF.Ln, bias=pc(L, 26)), reads=[Bpar], writes=[Bpar])
    P.op("dve", lambda e: e.tensor_scalar(out=pc(L, 10), in0=pc(L, 13), scalar1=-8.0, scalar2=None, op0=ALU.mult), reads=[Bpar], writes=[Bpar])
    P.op("dve", lambda e: e.tensor_scalar(out=pc(L, 11), in0=pc(L, 13), scalar1=-4.0, scalar2=None, op0=ALU.mult), reads=[Bpar], writes=[Bpar])
    P.op("dve", lambda e: e.tensor_tensor(out=pc(H, 20), in0=pc(H, 1), in1=pc(H, 0), op=ALU.subtract), reads=[Bpar], writes=[Bpar])
    P.op("act", lambda e: e.activation(out=pc(H, 21), in_=pc(H, 20), func=AF.Tanh, scale=0.5), reads=[Bpar], writes=[Bpar])
    P.op("dve", lambda e: e.tensor_scalar(out=pc(H, 22), in0=pc(H, 21), scalar1=0.5, scalar2=0.5, op0=ALU.mult, op1=ALU.add), reads=[Bpar], writes=[Bpar])
    P.op("dve", lambda e: e.tensor_tensor(out=pc(H, 19), in0=pc(H, 22), in1=pc(H, 3), op=ALU.mult), reads=[Bpar], writes=[Bpar])
    P.op("dve", lambda e: e.tensor_scalar(out=pc(H, 16), in0=pc(H, 19), scalar1=-0.5, scalar2=0.5, op0=ALU.mult, op1=ALU.add), reads=[Bpar], writes=[Bpar])
    P.op("dve", lambda e: e.tensor_scalar(out=pc(H, 18), in0=pc(H, 16), scalar1=-1.0, scalar2=None, op0=ALU.mult), reads=[Bpar], writes=[Bpar])
    P.op("dve", lambda e: e.tensor_scalar(out=pc(H, 23), in0=pc(H, 19), scalar1=1e-30, scalar2=None, op0=ALU.max), reads=[Bpar], writes=[Bpar])
    P.op("dve", lambda e: e.tensor_tensor(out=pc(H, 17), in0=pc(H, 23), in1=pc(H, 16), op=ALU.add), reads=[Bpar], writes=[Bpar])

    def rot(name, shape, dt, n):
        ts = [P.sbuf(f"{name}{i}", shape, dt) for i in range(n)]
        bs = [P.buf(f"{name}{i}") for i in range(n)]
        return ts, bs
    hblk, Bhblk = rot("hblk", [128, 8, 512], BF16, 2)
    xbuf, Bxbuf = rot("xbuf", [128, 515], F32, 2)
    NW = 2
    W = {}

    class HV:
        def __init__(self, t):
            self.t = t

        def __getitem__(self, key):
            if isinstance(key, tuple):
                return self.t[(slice(0, 64),) + tuple(key[1:])]
            return self.t[0:64, :]
    share = {"thq": "ysb", "thf": "t1", "thg": "t2", "gs2": "xc", "fg": "thr", "kk": "thi", "bb": "aa", "eb": "a2", "enb": "uu",
             "qs2": "hh", "kve": "sh1", "osb": "sh2", "osq": "sh3", "rstd": "sh4"}
    for nm, shp, dt in [("ysb", [128, 512], F32), ("t1", [128, 512], F32), ("t2", [128, 512], F32),
                        ("xc", [128, 512], F32), ("xcb", [128, 512], BF16), ("thr", [128, 512], F32), ("thi", [128, 512], F32),
                        ("aa", [128, 512], F32), ("a2", [128, 512], F32), ("uu", [128, 512], F32), ("hh", [128, 512], F32),
                        ("sh1", [128, 512], F32), ("sh2", [128, 512], F32), ("sh3", [128, 512], F32), ("sh4", [128, 512], F32),
                        ("yo", [128, 512], BF16),
                        ("qt", [64, 512], BF16), ("kt", [64, 512], BF16),
                        ("ktokE", [128, 4, 64], BF16), ("ktokO", [128, 4, 64], BF16), ("attm", [128, 512], BF16), ("ebl", [64, 8], F32),
                        ("hy", [64, 512], BF16),
                        ("gsb", [128, 32], F32), ("top8", [128, 8], F32), ("mp", [128, 32], BF16),
                        ("pt", [128, 512], BF16), ("onum", [65, 512], F32), ("rec", [65, 512], F32), ("my", [64, 512], BF16)]:
        n = {"pt": 4, "gsb": 4, "top8": 4, "mp": 4, "sh1": 1, "sh2": 1, "sh3": 1, "sh4": 1, "rec": 1}.get(nm, NW)
        W[nm] = rot(nm, shp, dt, n)
    for hn, ln in share.items():
        ts, _ = W[ln]
        W[hn] = ([HV(t) for t in ts], [P.buf(f"{hn}{i}") for i in range(len(ts))])
    ctr = {}

    def wt(nm):
        i = ctr.get(nm, 0)
        ctr[nm] = i + 1
        ts, bs = W[nm]
        return ts[i % len(ts)], bs[i % len(bs)]
    for i in range(4):
        P.op("pool", lambda e, i=i: e.memset(W["gsb"][0][i][:], NEGF), writes=[W["gsb"][1][i]])
    for nm_ in ("ktokE", "ktokO"):
        for i in range(NW):
            P.op("pool", lambda e, nm_=nm_, i=i: e.memset(W[nm_][0][i][:], 0.0), writes=[W[nm_][1][i]])
    Sst = P.sbuf("Sst", [64, 64], F32); BS = P.buf("Sst")
    Sbf, BSbf = rot("Sbf", [64, 64], BF16, 4)
    P.op("dve", lambda e: e.memset(Sst[:], 0.0), writes=[BS])
    P.op("dve", lambda e: e.memset(xbuf[1][L, 512:515], 0.0), writes=[Bxbuf[1]])
    hprev = [None]

    S1 = {}

    def stage1(blk):
        c0 = blk * 512
        hb, Bhb = hblk[blk % 2], Bhblk[blk % 2]
        P.dma("sp", hb[:], hT[:, c0:c0 + 512].rearrange("(c p) n -> p c n", p=128), writes=[Bhb])

        def inproj(col0, M, bank, Bb, rows=slice(0, 128)):
            for k in range(8):
                P.op("pe", lambda e, k=k: e.matmul(bank[rows, :], lhsT=wfm[:, k, col0:col0 + M], rhs=hb[:, k, :], start=(k == 0), stop=(k == 7)),
                     reads=[Bwfm, Bhb], writes=[Bb])
        b4, Bb4 = nbank(); inproj(320, 128, b4, Bb4)
        b5, Bb5 = nbank(); inproj(448, 128, b5, Bb5)
        P.op("act", lambda e: e.activation(out=QA[0:64, c0:c0 + 512], in_=b4[0:64, :], func=AF.Identity), reads=[Bb4], writes=[BQ[0][blk]])
        P.op("dve", lambda e: e.tensor_copy(out=QB[64:128, c0:c0 + 512], in_=b4[64:128, :]), reads=[Bb4], writes=[BQ[1][blk]])
        P.op("act", lambda e: e.activation(out=KA[0:64, c0:c0 + 512], in_=b5[0:64, :], func=AF.Identity), reads=[Bb5], writes=[BK[0][blk]])
        P.op("dve", lambda e: e.tensor_copy(out=KB[64:128, c0:c0 + 512], in_=b5[64:128, :]), reads=[Bb5], writes=[BK[1][blk]])
        kms, Bkms = wt("top8")
        P.op("dve", lambda e: e.tensor_reduce(out=kms[:, 0:2], in_=b5[:].rearrange("p (n k) -> p n k", n=2), axis=AX.X, op=ALU.add), reads=[Bb5], writes=[Bkms])
        P.op("dve", lambda e: e.tensor_scalar(out=kmT[:, 2 * blk:2 * blk + 2], in0=kms[:, 0:2], scalar1=1.0 / 256.0, scalar2=None, op0=ALU.mult), reads=[Bkms], writes=[Bkm])
        for pr in range(2):
            bv, Bbv = nbank()
            for tt in (2 * pr, 2 * pr + 1):
                o = (tt % 2) * 192
                for k in range(8):
                    P.op("pe", lambda e, k=k, tt=tt, o=o, bv=bv: e.matmul(bv[:, o:o + 192], lhsT=hb[:, k, tt * 128:(tt + 1) * 128], rhs=wtm[:, k, :], start=(k == 0), stop=(k == 7)),
                         reads=[Bwtm, Bhb], writes=[Bbv])
            for tt in (2 * pr, 2 * pr + 1):
                gi = blk * 4 + tt
                o = (tt % 2) * 192
                P.op("act", lambda e, gi=gi, o=o, bv=bv: e.activation(out=VH[:, gi, :], in_=bv[:, o:o + 64], func=AF.Identity), reads=[Bbv], writes=[BVH[blk]])
                P.op("dve", lambda e, gi=gi, o=o, bv=bv: e.tensor_copy(out=VV[:, gi, :, 0:64], in_=bv[:, o + 64:o + 192].rearrange("p (h d) -> p h d", h=2)), reads=[Bbv], writes=[BV[blk]])
        b1, Bb1 = nbank(); inproj(0, 128, b1, Bb1)
        b2, Bb2 = nbank(); inproj(128, 128, b2, Bb2)
        b3, Bb3 = nbank(); inproj(256, 64, b3, Bb3, rows=slice(0, 64))
        xb, Bxb = xbuf[blk % 2], Bxbuf[blk % 2]
        P.op("act", lambda e: e.activation(out=xb[L, 3:515], in_=b1[L, :], func=AF.Identity), reads=[Bb1], writes=[Bxb])
        ysb, Bysb = wt("ysb")
        P.op("act", lambda e: e.activation(out=ysb[L, :], in_=b2[L, :], func=AF.Identity), reads=[Bb2], writes=[Bysb])
        thq, Bthq = wt("thq"); thf, Bthf = wt("thf"); thg, Bthg = wt("thg")
        P.op("act", lambda e: e.activation(out=thq[:], in_=b1[H, :], func=AF.Tanh, scale=0.5), reads=[Bb1], writes=[Bthq])
        P.op("act", lambda e: e.activation(out=thf[:], in_=b2[H, :], func=AF.Tanh, scale=0.5), reads=[Bb2], writes=[Bthf])
        P.op("act", lambda e: e.activation(out=thg[:], in_=b3[H, :], func=AF.Tanh, scale=0.5), reads=[Bb3], writes=[Bthg])
        qs2, Bqs2 = wt("qs2"); gs2, Bgs2 = wt("gs2")
        P.op("dve", lambda e: e.scalar_tensor_tensor(out=qs2[:], in0=thq[:], scalar=1.0, in1=b1[H, :], op0=ALU.add, op1=ALU.mult), reads=[Bthq, Bb1], writes=[Bqs2])
        P.op("dve", lambda e: e.scalar_tensor_tensor(out=gs2[:], in0=thg[:], scalar=1.0, in1=b3[H, :], op0=ALU.add, op1=ALU.mult), reads=[Bthg, Bb3], writes=[Bgs2])
        S1[blk] = dict(xb=(xb, Bxb), ysb=(ysb, Bysb), thf=(thf, Bthf), qs2=(qs2, Bqs2), gs2=(gs2, Bgs2))

    def stage2(blk):
        c0 = blk * 512
        d = S1.pop(blk)
        xb, Bxb = d["xb"]; ysb, Bysb = d["ysb"]; thf, Bthf = d["thf"]; qs2, Bqs2 = d["qs2"]; gs2, Bgs2 = d["gs2"]
        xo, Bxo = xbuf[(blk + 1) % 2], Bxbuf[(blk + 1) % 2]
        P.op("pool", lambda e: e.tensor_copy(out=xb[L, 0:3], in_=xo[L, 512:515]), reads=[Bxo], writes=[Bxb])
        xc, Bxc = wt("xc")
        P.op("pool", lambda e: e.tensor_scalar(out=xc[L, :], in0=xb[L, 3:515], scalar1=pc(L, 3), scalar2=pc(L, 4), op0=ALU.mult, op1=ALU.add), reads=[Bxb, Bpar], writes=[Bxc])
        for j in (2, 1, 0):
            P.op("dve", lambda e, j=j: e.scalar_tensor_tensor(out=xc[L, :], in0=xb[L, j:j + 512], scalar=pc(L, j), in1=xc[L, :], op0=ALU.mult, op1=ALU.add), reads=[Bxb, Bpar, Bxc], writes=[Bxc])
        xcb, Bxcb = wt("xcb")
        P.op("pool", lambda e: e.tensor_copy(out=xcb[L, :], in_=xc[L, :]), reads=[Bxc], writes=[Bxcb])
        bg, Bbg = nbank()
        bg2, Bbg2 = nbank()
        P.op("pe", lambda e: e.matmul(bg[L, :], lhsT=wg[L, 0:64], rhs=xcb[L, :], start=True, stop=True), reads=[Bwg, Bxcb], writes=[Bbg])
        P.op("pe", lambda e: e.matmul(bg2[L, :], lhsT=wg[L, 64:128], rhs=xcb[L, :], start=True, stop=True), reads=[Bwg, Bxcb], writes=[Bbg2])
        thr, Bthr = wt("thr"); thi, Bthi = wt("thi")
        P.op("act", lambda e: e.activation(out=thr[L, :], in_=bg[L, :], func=AF.Tanh, scale=0.5, bias=pc(L, 8)), reads=[Bbg, Bpar], writes=[Bthr])
        P.op("act", lambda e: e.activation(out=thi[L, :], in_=bg2[L, :], func=AF.Tanh, scale=0.5, bias=pc(L, 9)), reads=[Bbg2, Bpar], writes=[Bthi])
        aa, Baa = wt("aa"); a2, Ba2 = wt("a2")
        P.op("act", lambda e: e.activation(out=aa[L, :], in_=thr[L, :], func=AF.Exp, scale=pc(L, 11), bias=pc(L, 11)), reads=[Bthr, Bpar], writes=[Baa])
        P.op("act", lambda e: e.activation(out=a2[L, :], in_=thr[L, :], func=AF.Exp, scale=pc(L, 10), bias=pc(L, 10)), reads=[Bthr, Bpar], writes=[Ba2])
        t1, Bt1 = wt("t1"); t2, Bt2 = wt("t2")
        P.op("act", lambda e: e.activation(out=t1[L, :], in_=ysb[L, :], func=AF.Square), reads=[Bysb], writes=[Bt1])
        P.op("pool", lambda e: e.tensor_scalar(out=t1[L, :], in0=t1[L, :], scalar1=0.044715, scalar2=1.0, op0=ALU.mult, op1=ALU.add), reads=[Bt1], writes=[Bt1])
        P.op("pool", lambda e: e.tensor_tensor(out=t1[L, :], in0=t1[L, :], in1=ysb[L, :], op=ALU.mult), reads=[Bt1, Bysb], writes=[Bt1])
        P.op("act", lambda e: e.activation(out=t2[L, :], in_=t1[L, :], func=AF.Tanh, scale=GC), reads=[Bt1], writes=[Bt2])
        P.op("dve", lambda e: e.scalar_tensor_tensor(out=t2[L, :], in0=t2[L, :], scalar=1.0, in1=ysb[L, :], op0=ALU.add, op1=ALU.mult), reads=[Bt2, Bysb], writes=[Bt2])
        uu, Buu = wt("uu")
        P.op("dve", lambda e: e.scalar_tensor_tensor(out=uu[L, :], in0=thi[L, :], scalar=1.0, in1=xc[L, :], op0=ALU.add, op1=ALU.mult), reads=[Bthi, Bxc], writes=[Buu])
        fg, Bfg = wt("fg"); kk, Bkk = wt("kk")
        P.op("dve", lambda e: e.tensor_scalar(out=fg[:], in0=thf[:], scalar1=pc(H, 16), scalar2=pc(H, 17), op0=ALU.mult, op1=ALU.add), reads=[Bthf, Bpar], writes=[Bfg])
        P.op("dve", lambda e: e.tensor_scalar(out=kk[:], in0=thf[:], scalar1=pc(H, 18), scalar2=pc(H, 16), op0=ALU.mult, op1=ALU.add), reads=[Bthf, Bpar], writes=[Bkk])
        P.op("act", lambda e: e.activation(out=a2[L, :], in_=a2[L, :], func=AF.Ln, scale=-1.0, bias=pc(L, 26)), reads=[Ba2], writes=[Ba2])
        P.op("act", lambda e: e.activation(out=fg[:], in_=fg[:], func=AF.Ln), reads=[Bfg], writes=[Bfg])
        P.op("act", lambda e: e.activation(out=a2[L, :], in_=a2[L, :], func=AF.Exp, scale=0.5), reads=[Ba2], writes=[Ba2])
        bb, Bbb = wt("bb")
        P.op("dve", lambda e: e.tensor_tensor_scan(out=bb[:], data0=rmask[:], data1=fg[:], initial=0.0, op0=ALU.mult, op1=ALU.add), reads=[Brm, Bfg], writes=[Bbb])
        eb, Beb = wt("eb"); enb, Benb = wt("enb"); ebl, Bebl = wt("ebl")
        P.op("act", lambda e: e.activation(out=eb[:], in_=bb[:], func=AF.Exp, bias=pc(H, 24)), reads=[Bbb, Bpar], writes=[Beb])
        P.op("act", lambda e: e.activation(out=enb[:], in_=bb[:], func=AF.Exp, scale=-1.0), reads=[Bbb], writes=[Benb])
        P.op("act", lambda e: e.activation(out=ebl[:], in_=bb[:, 63:512:64], func=AF.Exp), reads=[Bbb], writes=[Bebl])
        P.op("dve", lambda e: e.scalar_tensor_tensor(out=uu[L, :], in0=uu[L, :], scalar=0.5, in1=a2[L, :], op0=ALU.mult, op1=ALU.mult), reads=[Buu, Ba2], writes=[Buu])
        hh, Bhh = wt("hh")
        if hprev[0] is None:
            P.op("dve", lambda e: e.tensor_tensor_scan(out=hh[L, :], data0=aa[L, :], data1=uu[L, :], initial=0.0, op0=ALU.mult, op1=ALU.add), reads=[Baa, Buu], writes=[Bhh])
        else:
            hp, Bhp = hprev[0]
            P.op("dve", lambda e, hp=hp: e.tensor_tensor_scan(out=hh[L, :], data0=aa[L, :], data1=uu[L, :], initial=hp[L, 511:512], op0=ALU.mult, op1=ALU.add), reads=[Baa, Buu, Bhp], writes=[Bhh])
        hprev[0] = (hh, Bhh)
        yo, Byo = wt("yo")
        P.op("dve", lambda e: e.scalar_tensor_tensor(out=yo[L, :], in0=hh[L, :], scalar=0.5, in1=t2[L, :], op0=ALU.mult, op1=ALU.mult), reads=[Bhh, Bt2], writes=[Byo])
        P.dma("sp", yT[0, :, c0:c0 + 512], yo[L, :], reads=[Byo])
        qt, Bqt = wt("qt"); kt, Bkt = wt("kt")
        P.op("dve", lambda e: e.tensor_tensor(out=qt[:], in0=qs2[:], in1=eb[:], op=ALU.mult), reads=[Bqs2, Beb], writes=[Bqt])
        P.op("dve", lambda e: e.tensor_tensor(out=kt[:], in0=kk[:], in1=enb[:], op=ALU.mult), reads=[Bkk, Benb], writes=[Bkt])
        btr, Bbtr = nbank()
        btr16 = btr[:].bitcast(BF16)
        ktokE, BktokE = wt("ktokE"); ktokO, BktokO = wt("ktokO")
        for tt in range(4):
            P.op("pe", lambda e, tt=tt: e.transpose(btr16[:, tt * 64:(tt + 1) * 64], in_=kt[:, tt * 128:(tt + 1) * 128], identity=ident[0:64, 0:64]), reads=[Bkt, Bid], writes=[Bbtr])
        P.op("act", lambda e: e.activation(out=ktokE[0:64, :, :].rearrange("p t d -> p (t d)"), in_=btr16[0:64, 0:256], func=AF.Identity), reads=[Bbtr], writes=[BktokE])
        P.op("act", lambda e: e.activation(out=ktokO[64:128, :, :].rearrange("p t d -> p (t d)"), in_=btr16[64:128, 0:256], func=AF.Identity), reads=[Bbtr], writes=[BktokO])
        bkv, Bbkv = nbank()
        for c in range(8):
            tt, hf = c // 2, c % 2
            gi = blk * 4 + tt
            kx, Bkx = (ktokE, BktokE) if hf == 0 else (ktokO, BktokO)
            P.op("pe", lambda e, c=c, tt=tt, gi=gi, kx=kx: e.matmul(bkv[0:64, c * 64:(c + 1) * 64], lhsT=kx[:, tt, :], rhs=VH[:, gi, :], start=True, stop=True),
                 reads=[Bkx, BVH[blk]], writes=[Bbkv])
        batt, Bbatt = nbank()
        for tt in range(4):
            P.op("pe", lambda e, tt=tt: e.matmul(batt[:, tt * 128:(tt + 1) * 128], lhsT=kt[:, tt * 128:(tt + 1) * 128], rhs=qt[:, tt * 128:(tt + 1) * 128], start=True, stop=True),
                 reads=[Bkt, Bqt], writes=[Bbatt])
        attm, Battm = wt("attm")
        P.op("dve", lambda e: e.tensor_tensor(out=attm[:], in0=batt[:], in1=hmask[:], op=ALU.mult), reads=[Bbatt, Bhm], writes=[Battm])
        kve, Bkve = wt("kve")
        for c in range(8):
            P.op("act", lambda e, c=c: e.activation(out=kve[:, c * 64:(c + 1) * 64], in_=bkv[0:64, c * 64:(c + 1) * 64], func=AF.Identity, scale=ebl[:, c:c + 1]), reads=[Bbkv, Bebl], writes=[Bkve])
        bo, Bbo = nbank()
        for c in range(8):
            tt, hf = c // 2, c % 2
            gi = blk * 4 + tt
            sb, Bsb = Sbf[(blk * 8 + c) % 4], BSbf[(blk * 8 + c) % 4]
            P.op("act", lambda e, sb=sb: e.activation(out=sb[:], in_=Sst[:], func=AF.Identity), reads=[BS], writes=[Bsb])
            if hf == 0:
                P.op("pe", lambda e, tt=tt, gi=gi: e.matmul(bo[0:64, tt * 128:(tt + 1) * 128], lhsT=VH[:, gi, :], rhs=attm[:, tt * 128:(tt + 1) * 128], start=True, stop=False),
                     reads=[BVH[blk], Battm], writes=[Bbo])
            P.op("pe", lambda e, c=c, sb=sb, hf=hf: e.matmul(bo[0:64, c * 64:(c + 1) * 64], lhsT=sb[:], rhs=qt[:, c * 64:(c + 1) * 64], start=False, stop=(hf == 1)),
                 reads=[Bsb, Bqt], writes=[Bbo])
            P.op("dve", lambda e, c=c: e.scalar_tensor_tensor(out=Sst[:], in0=Sst[:], scalar=ebl[:, c:c + 1], in1=kve[:, c * 64:(c + 1) * 64], op0=ALU.mult, op1=ALU.add), reads=[BS, Bebl, Bkve], writes=[BS])
        osb, Bosb = wt("osb"); osq, Bosq = wt("osq")
        P.op("act", lambda e: e.activation(out=osb[:], in_=bo[0:64, :], func=AF.Identity), reads=[Bbo], writes=[Bosb])
        P.op("act", lambda e: e.activation(out=osq[:], in_=bo[0:64, :], func=AF.Square), reads=[Bbo], writes=[Bosq])
        bms, Bbms = nbank()
        P.op("pe", lambda e: e.matmul(bms[0:64, :], lhsT=ones64[:], rhs=osq[:], start=True, stop=True), reads=[Bo64, Bosq], writes=[Bbms])
        rstd, Brstd = wt("rstd")
        P.op("act", lambda e: e.activation(out=rstd[:], in_=bms[0:64, :], func=AF.Ln, bias=pc(H, 25)), reads=[Bbms, Bpar], writes=[Brstd])
        P.op("act", lambda e: e.activation(out=rstd[:], in_=rstd[:], func=AF.Exp, scale=-0.5), reads=[Brstd], writes=[Brstd])
        P.op("dve", lambda e: e.tensor_tensor(out=osb[:], in0=osb[:], in1=rstd[:], op=ALU.mult), reads=[Bosb, Brstd], writes=[Bosb])
        P.op("dve", lambda e: e.tensor_scalar(out=osb[:], in0=osb[:], scalar1=pc(H, 2), scalar2=0.5, op0=ALU.mult, op1=ALU.mult), reads=[Bosb, Bpar], writes=[Bosb])
        hy, Bhy = wt("hy")
        P.op("dve", lambda e: e.tensor_tensor(out=hy[:], in0=osb[:], in1=gs2[:], op=ALU.mult), reads=[Bosb, Bgs2], writes=[Bhy])
        P.dma("sp", yT[1, :, c0:c0 + 512], hy[:], reads=[Bhy])

    def stage3(blk):
        for h in range(2):
            head3(blk, h)

    def head3(blk, h):
        c0 = blk * 512
        if True:
            Q, K = Qh[h], Kh[h]
            dr = slice(0, 64) if h == 0 else slice(64, 128)
            mr = slice(64, 96) if h == 0 else slice(0, 32)
            bgt, Bbgt = nbank()
            bmp, Bbmp = nbank()
            bmp16 = bmp[:].bitcast(BF16)
            any_mp = False
            for st in range(4):
                j = 2 * blk + (st // 2)
                if j == 0:
                    continue
                any_mp = True
                q0 = c0 + st * 128
                P.op("pe", lambda e, q0=q0, st=st: e.matmul(bgt[:, st * 32:(st + 1) * 32], lhsT=Q[dr, q0:q0 + 128], rhs=kmT[dr, 0:32], start=True, stop=True),
                     reads=[BQ[h][blk], Bkm], writes=[Bbgt])
                gsb, Bgsb = wt("gsb"); top8, Btop8 = wt("top8"); mp, Bmp = wt("mp")
                P.op("dve", lambda e, st=st, j=j, gsb=gsb: e.tensor_copy(out=gsb[:, 0:j], in_=bgt[:, st * 32:st * 32 + j]), reads=[Bbgt], writes=[Bgsb])
                P.op("dve", lambda e, j=j, gsb=gsb, top8=top8: e.max(out=top8[:], in_=gsb[:, 0:max(j, 8)]), reads=[Bgsb], writes=[Btop8])
                P.op("pool", lambda e, mp=mp: e.memset(mp[:], 0.0), writes=[Bmp])
                P.op("dve", lambda e, j=j, gsb=gsb, top8=top8, mp=mp: e.tensor_scalar(out=mp[:, 0:j], in0=gsb[:, 0:j], scalar1=top8[:, 2:3], scalar2=-8.0 * BIG, op0=ALU.is_lt, op1=ALU.mult),
                     reads=[Bgsb, Btop8, Bmp], writes=[Bmp])
                P.op("pe", lambda e, st=st, mp=mp: e.transpose(bmp16[mr, st * 128:(st + 1) * 128], in_=mp[:], identity=ident[:]), reads=[Bmp, Bid], writes=[Bbmp])
            if any_mp:
                s0 = 0 if blk > 0 else 2
                P.op("act", lambda e, s0=s0: e.activation(out=Q[mr, c0 + s0 * 128:c0 + 512], in_=bmp16[mr, s0 * 128:512], func=AF.Identity), reads=[Bbmp], writes=[BQ[h][blk]])
            pv, Bpv = pvb[h]
            tiles = [(kt_, 0) for kt_ in range(4 * blk) if (maxdist[h] is None or (c0 - (kt_ * 128 + 127)) <= maxdist[h])] + [(4 * blk + kk_, kk_) for kk_ in range(4)]
            first = True
            for (kti, own) in tiles:
                isown = kti >= 4 * blk
                n0 = own * 128 if isown else 0
                k0 = kti * 128
                kb = kti // 4
                bs, Bbs = nbank()
                P.op("pe", lambda e, k0=k0, n0=n0, bs=bs: e.matmul(bs[:, n0:512], lhsT=K[:, k0:k0 + 128], rhs=Q[:, c0 + n0:c0 + 512], start=True, stop=True),
                     reads=[BK[h][kb], BQ[h][blk]], writes=[Bbs])
                pt, Bpt = wt("pt")
                m = kti - 4 * blk + MOFF
                P.op("act", lambda e, n0=n0, bs=bs, pt=pt, m=m: e.activation(out=pt[:, n0:512], in_=bs[:, n0:512], func=AF.Exp, scale=0.125, bias=abias[:, h, m:m + 1]),
                     reads=[Bbs, Bab], writes=[Bpt])
                if isown:
                    P.op("dve", lambda e, n0=n0, pt=pt: e.tensor_tensor(out=pt[:, n0:n0 + 128], in0=pt[:, n0:n0 + 128], in1=cmask[:], op=ALU.mult), reads=[Bpt, Bcm], writes=[Bpt])
                last = (kti == tiles[-1][0])
                P.op("pe", lambda e, kti=kti, n0=n0, pt=pt, first=first, last=last: e.matmul(pv[0:65, n0:512], lhsT=VV[:, kti, h, :], rhs=pt[:, n0:512], start=first, stop=last),
                     reads=[BV[kb], Bpt], writes=[Bpv])
                first = False
            onum, Bonum = wt("onum"); rec, Brec = wt("rec"); my, Bmy = wt("my")
            P.op("act", lambda e: e.activation(out=onum[0:65, :], in_=pv[0:65, :], func=AF.Identity), reads=[Bpv], writes=[Bonum])
            P.op("act", lambda e: e.activation(out=rec[64:65, :], in_=onum[64:65, :], func=AF.Ln), reads=[Bonum], writes=[Brec])
            P.op("act", lambda e: e.activation(out=rec[64:65, :], in_=rec[64:65, :], func=AF.Exp, scale=-1.0), reads=[Brec], writes=[Brec])
            bbc, Bbbc = nbank()
            P.op("pe", lambda e: e.matmul(bbc[0:64, :], lhsT=ones64r[64:65, :], rhs=rec[64:65, :], start=True, stop=True), reads=[Bo64r, Brec], writes=[Bbbc])
            P.op("dve", lambda e: e.tensor_tensor(out=my[:], in0=onum[0:64, :], in1=bbc[0:64, :], op=ALU.mult), reads=[Bonum, Bbbc], writes=[Bmy])
            P.dma("sp", yT[2 + h, :, c0:c0 + 512], my[:], reads=[Bmy])

    import os
    stop = os.environ.get("PHB_STOP", "")
    if stop == "init":
        return
    for blk in range(NB):
        stage1(blk)
        if stop == "s1":
            return
        if blk > 0:
            stage3(blk - 1)
        stage2(blk)
        if stop == "s2":
            return
    stage3(NB - 1)


EPS = 1e-6


def host_consts_C():
    c = {}
    oa = np.zeros((128, 128), np.float32)
    oa[:64, :] = 1.0 / 256.0
    c["onesA"] = oa
    c["onesC"] = np.full((128, 128), 1.0 / 512.0, np.float32)
    c["ones1k"] = np.full((128, 128), 1.0 / 1024.0, np.float32)
    return c


def rms_stats(P, banks, Bbanks, src_fn, nchunk, ones_t, Bones, sq_tiles, eps_ap, Bpar, rstd, Brstd, srcbufs):
    bank, Bbank = banks
    for c in range(nchunk):
        sq, Bsq = sq_tiles[c % len(sq_tiles)]
        src = src_fn(c)
        P.op("act", lambda e, sq=sq, src=src: e.activation(out=sq[:], in_=src, func=AF.Square), reads=srcbufs(c), writes=[Bsq])
        P.op("pe", lambda e, sq=sq, c=c: e.matmul(bank[:], lhsT=ones_t[:], rhs=sq[:], start=(c == 0), stop=(c == nchunk - 1)), reads=[Bones, Bsq], writes=[Bbank])
    P.op("act", lambda e: e.activation(out=rstd[:], in_=bank[:], func=AF.Ln, bias=eps_ap), reads=[Bbank, Bpar], writes=[Brstd])
    P.op("act", lambda e: e.activation(out=rstd[:], in_=rstd[:], func=AF.Exp, scale=-0.5), reads=[Brstd], writes=[Brstd])


def build_A(nc, P, io, NT=2048):
    xs = P.sbuf("xs", [128, 8, NT], F32)
    Bxs = [P.buf(f"xs{t}") for t in range(NT // 512)]
    gv = P.sbuf("gv", [128, 16], F32); Bgv = P.buf("gv")
    ones1k = P.sbuf("ones1k", [128, 128], F32); Bo1k = P.buf("ones1k")
    P.dma("sp", gv[:, 0:8], io["gv"], writes=[Bgv])
    P.dma("sp", ones1k[:], io["ones1k"], writes=[Bo1k])
    P.op("dve", lambda e: e.memset(gv[:, 8:9], EPS), reads=[Bgv], writes=[Bgv])
    banks = [P.psum(f"bank{i}", [128, 512]) for i in range(2)]
    Bbank = [P.buf(f"bank{i}") for i in range(2)]
    for b_ in Bbank:
        b_.excl = True
    sqs = [(P.sbuf(f"sq{i}", [128, 512], F32), P.buf(f"sq{i}")) for i in range(2)]
    rs = [(P.sbuf(f"rstd{i}", [128, 512], F32), P.buf(f"rstd{i}")) for i in range(2)]
    ho = [(P.sbuf(f"ho{i}", [128, 8, 512], BF16), P.buf(f"ho{i}")) for i in range(2)]
    xv = io["xT"].rearrange("(c p) n -> p c n", p=128)
    hv = io["hT"].rearrange("(c p) n -> p c n", p=128)
    for t in range(NT // 512):
        P.dma("sp", xs[:, :, t * 512:(t + 1) * 512], xv[:, :, t * 512:(t + 1) * 512], writes=[Bxs[t]])
    for t in range(NT // 512):
        def body(t):
            rstd, Brstd = rs[t % 2]
            rms_stats(P, (banks[t % 2], Bbank[t % 2]), None, lambda c: xs[:, c, t * 512:(t + 1) * 512], 8, ones1k, Bo1k, sqs, gv[:, 8:9], Bgv, rstd, Brstd, lambda c: [Bxs[t]])
            h, Bh = ho[t % 2]
            for c in range(8):
                P.op("dve", lambda e, c=c: e.scalar_tensor_tensor(out=h[:, c, :], in0=xs[:, c, t * 512:(t + 1) * 512], scalar=gv[:, c:c + 1], in1=rstd[:], op0=ALU.mult, op1=ALU.mult),
                     reads=[Bxs[t], Bgv, Brstd], writes=[Bh])
            P.dma("sp", hv[:, :, t * 512:(t + 1) * 512], h[:], reads=[Bh])
        body(t)


def build_C(nc, P, io, final, NT=2048):
    NTC = NT // 512
    xs = P.sbuf("xs", [128, 8, NT], F32)
    Bxs = [P.buf(f"xs{t}") for t in range(NTC)]
    gv = P.sbuf("gv", [128, 40], F32); Bgv = P.buf("gv")
    onesA = P.sbuf("onesA", [128, 128], F32); BoA = P.buf("onesA")
    onesC = P.sbuf("onesC", [128, 128], F32); BoC = P.buf("onesC")
    ones1k = P.sbuf("ones1k", [128, 128], F32); Bo1k = P.buf("ones1k")
    P.dma("sp", gv[:, 0:32], io["gv"], writes=[Bgv])
    P.dma("sp", onesA[:], io["onesA"], writes=[BoA])
    P.dma("sp", onesC[:], io["onesC"], writes=[BoC])
    P.dma("sp", ones1k[:], io["ones1k"], writes=[Bo1k])
    P.op("dve", lambda e: e.memset(gv[:, 32:33], EPS), reads=[Bgv], writes=[Bgv])
    eps_ap = gv[:, 32:33]
    banks = [P.psum(f"bank{i}", [128, 512]) for i in range(8)]
    Bbank = [P.buf(f"bank{i}") for i in range(8)]
    for b_ in Bbank:
        b_.excl = True
    rr = [0]

    def nbank():
        i = rr[0] % 8
        rr[0] += 1
        return banks[i], Bbank[i]
    R = P.sbuf("R", [128, 32768], BF16)
    wout = R[:, 0:8192].rearrange("p (c n) -> p c n", c=8); Bwout = P.buf("wout")
    ysb = [R[:, 8192 + i * 4096:8192 + (i + 1) * 4096].rearrange("p (c n) -> p c n", c=8) for i in range(2)]
    Bysb = [P.buf(f"y{i}") for i in range(2)]
    ynb = [R[:, 16384 + i * 4096:16384 + (i + 1) * 4096].rearrange("p (c n) -> p c n", c=8) for i in range(2)]
    Bynb = [P.buf(f"yn{i}") for i in range(2)]
    u = R[:, :].rearrange("p (f n) -> p f n", f=32)
    Bu = [P.buf(f"u{f}") for f in range(32)]
    h2 = P.sbuf("h2", [128, 8, 1024], BF16); Bh2 = [P.buf("h2_0"), P.buf("h2_1")]
    w1b = [(P.sbuf(f"w1b{i}", [128, 8, 512], BF16), P.buf(f"w1b{i}")) for i in range(2)]
    w2b = [(P.sbuf(f"w2b{i}", [128, 4, 512], BF16), P.buf(f"w2b{i}")) for i in range(2)]
    sqs = [(P.sbuf(f"sq{i}", [128, 512], F32), P.buf(f"sq{i}")) for i in range(3)]
    rsA = [(P.sbuf(f"rsA{i}", [128, 512], F32), P.buf(f"rsA{i}")) for i in range(1)] * 2
    rsC = [(P.sbuf(f"rsC{i}", [128, 512], F32), P.buf(f"rsC{i}")) for i in range(1)] * 2
    rs2 = [(P.sbuf(f"rs2{i}", [128, 512], F32), P.buf(f"rs2{i}")) for i in range(2)]
    rl = [(P.sbuf(f"rl{i}", [128, 512], F32), P.buf(f"rl{i}")) for i in range(3)]
    if final:
        st_f = [(P.sbuf(f"stf{i}", [128, 4, 512], F32), P.buf(f"stf{i}")) for i in range(1)]
    else:
        st_b = [(P.sbuf(f"stb{i}", [128, 8, 512], BF16), P.buf(f"stb{i}")) for i in range(1)]

    xv = io["xT"].rearrange("(c p) n -> p c n", p=128)
    for t in range(NTC):
        P.dma("sp", xs[:, :, t * 512:(t + 1) * 512], xv[:, :, t * 512:(t + 1) * 512], writes=[Bxs[t]])
    P.dma("pool", wout, io["wout"].rearrange("(c p) n -> p c n", p=128), writes=[Bwout])

    def outproj(t):
        tsl = slice(t * 512, (t + 1) * 512)
        y, By = ysb[t % 2], Bysb[t % 2]
        yn, Byn = ynb[t % 2], Bynb[t % 2]
        P.dma("sp", y, io["yT"][:, :, tsl].rearrange("c p n -> p c n"), writes=[By])
        rA, BrA = rsA[t % 2]; rC, BrC = rsC[t % 2]
        bA = nbank()
        rms_stats(P, bA, None, lambda c: y[:, 2 * c, :], 4, onesA, BoA, sqs, eps_ap, Bgv, rA, BrA, lambda c: [By])
        bC = nbank()
        rms_stats(P, bC, None, lambda c: y[:, 2 * c + 1, :], 4, onesC, BoC, sqs, eps_ap, Bgv, rC, BrC, lambda c: [By])
        for g in range(4):
            c0, c1 = 2 * g, 2 * g + 1
            P.op("dve", lambda e, c0=c0: e.scalar_tensor_tensor(out=yn[0:64, c0, :], in0=y[0:64, c0, :], scalar=gv[0:64, 16 + c0:17 + c0], in1=rA[0:64, :], op0=ALU.mult, op1=ALU.mult),
                 reads=[By, Bgv, BrA], writes=[Byn])
            P.op("pool", lambda e, c0=c0: e.tensor_copy(out=yn[64:128, c0, :], in_=y[64:128, c0, :]), reads=[By], writes=[Byn])
            P.op("dve", lambda e, c1=c1: e.scalar_tensor_tensor(out=yn[:, c1, :], in0=y[:, c1, :], scalar=gv[:, 16 + c1:17 + c1], in1=rC[:], op0=ALU.mult, op1=ALU.mult),
                 reads=[By, Bgv, BrC], writes=[Byn])
        for m in range(8):
            bk, Bbk = nbank()
            for c in range(8):
                P.op("pe", lambda e, c=c, m=m, bk=bk: e.matmul(bk[:], lhsT=wout[:, c, m * 128:(m + 1) * 128], rhs=yn[:, c, :], start=(c == 0), stop=(c == 7)),
                     reads=[Bwout, Byn], writes=[Bbk])
            P.op("dve", lambda e, m=m, bk=bk: e.tensor_tensor(out=xs[:, m, tsl], in0=xs[:, m, tsl], in1=bk[:], op=ALU.add), reads=[Bbk, Bxs[t]], writes=[Bxs[t]])
    for t in range(NTC):
        outproj(t)

    w1v = io["w1"].rearrange("(c p) n -> p c n", p=128)
    w2v = io["w2"].rearrange("(f p) n -> p f n", p=128)
    alias_guard = [Bwout] + Bysb + Bynb
    cnt = {"w1": 0, "w2": 0, "rl": 0}

    def ffn_half(hf):
        for tl in range(2):
            t = 2 * hf + tl
            tsl = slice(t * 512, (t + 1) * 512)
            r2, Br2 = rs2[t % 2]
            rms_stats(P, nbank(), None, lambda c, tsl=tsl: xs[:, c, tsl], 8, ones1k, Bo1k, sqs, eps_ap, Bgv, r2, Br2, lambda c, t=t: [Bxs[t]])
            for c in range(8):
                P.op("dve", lambda e, c=c, tsl=tsl, tl=tl, r2=r2: e.scalar_tensor_tensor(out=h2[:, c, tl * 512:(tl + 1) * 512], in0=xs[:, c, tsl], scalar=gv[:, c:c + 1], in1=r2[:], op0=ALU.mult, op1=ALU.mult),
                     reads=[Bxs[t], Bgv, Br2], writes=[Bh2[tl]])
        for fg in range(8):
            w1t, Bw1 = w1b[cnt["w1"] % 2]; cnt["w1"] += 1
            P.dma("pool", w1t[:], w1v[:, :, fg * 512:(fg + 1) * 512], writes=[Bw1])
            for fc in range(4):
                f = fg * 4 + fc
                for tl in range(2):
                    bk, Bbk = nbank()
                    for k in range(8):
                        P.op("pe", lambda e, k=k, fc=fc, tl=tl, bk=bk, w1t=w1t: e.matmul(bk[:], lhsT=w1t[:, k, fc * 128:(fc + 1) * 128], rhs=h2[:, k, tl * 512:(tl + 1) * 512], start=(k == 0), stop=(k == 7)),
                             reads=[Bw1, Bh2[tl]], writes=[Bbk])
                    r, Br = rl[cnt["rl"] % 3]; cnt["rl"] += 1
                    P.op("act", lambda e, bk=bk, r=r: e.activation(out=r[:], in_=bk[:], func=AF.Relu), reads=[Bbk], writes=[Br])
                    extra = alias_guard if (hf == 0) else []
                    eng = "pool" if (cnt["rl"] % 2 == 0) else "dve"
                    P.op(eng, lambda e, f=f, tl=tl, r=r: e.tensor_tensor(out=u[:, f, tl * 512:(tl + 1) * 512], in0=r[:], in1=r[:], op=ALU.mult), reads=[Br], writes=[Bu[f]] + extra)
        for mh in range(2):
            accs = [[nbank() for tl in range(2)] for mm in range(4)]
            for fg in range(8):
                w2t, Bw2 = w2b[cnt["w2"] % 2]; cnt["w2"] += 1
                P.dma("pool", w2t[:], w2v[:, fg * 4:(fg + 1) * 4, mh * 512:(mh + 1) * 512], writes=[Bw2])
                for fc in range(4):
                    f = fg * 4 + fc
                    for mm in range(4):
                        for tl in range(2):
                            bk, Bbk = accs[mm][tl]
                            P.op("pe", lambda e, fc=fc, f=f, mm=mm, tl=tl, bk=bk, w2t=w2t: e.matmul(bk[:], lhsT=w2t[:, fc, mm * 128:(mm + 1) * 128], rhs=u[:, f, tl * 512:(tl + 1) * 512], start=(f == 0), stop=(f == 31)),
                                 reads=[Bw2, Bu[f]], writes=[Bbk])
            for mm in range(4):
                m = mh * 4 + mm
                for tl in range(2):
                    t = 2 * hf + tl
                    tsl = slice(t * 512, (t + 1) * 512)
                    bk, Bbk = accs[mm][tl]
                    P.op("dve", lambda e, m=m, tsl=tsl, bk=bk: e.tensor_tensor(out=xs[:, m, tsl], in0=xs[:, m, tsl], in1=bk[:], op=ALU.add), reads=[Bbk, Bxs[t]], writes=[Bxs[t]])
    for hf in range(NTC // 2):
        ffn_half(hf)

    def tail(t):
        tsl = slice(t * 512, (t + 1) * 512)
        r2, Br2 = rs2[t % 2]
        rms_stats(P, nbank(), None, lambda c: xs[:, c, tsl], 8, ones1k, Bo1k, sqs, eps_ap, Bgv, r2, Br2, lambda c: [Bxs[t]])
        if final:
            so, Bso = st_f[0]
            for ch in range(2):
                for c in range(4 * ch, 4 * ch + 4):
                    P.op("dve", lambda e, c=c: e.scalar_tensor_tensor(out=so[:, c % 4, :], in0=xs[:, c, tsl], scalar=gv[:, 8 + c:9 + c], in1=r2[:], op0=ALU.mult, op1=ALU.mult),
                         reads=[Bxs[t], Bgv, Br2], writes=[Bso])
                P.dma("sp", io["oT"].rearrange("(c p) n -> p c n", p=128)[:, 4 * ch:4 * ch + 4, tsl], so[:], reads=[Bso])
        else:
            so, Bso = st_b[0]
            for c in range(8):
                P.op("dve", lambda e, c=c: e.scalar_tensor_tensor(out=so[:, c, :], in0=xs[:, c, tsl], scalar=gv[:, 8 + c:9 + c], in1=r2[:], op0=ALU.mult, op1=ALU.mult),
                     reads=[Bxs[t], Bgv, Br2], writes=[Bso])
            P.dma("sp", io["hnT"].rearrange("(c p) n -> p c n", p=128)[:, :, tsl], so[:], reads=[Bso])
            P.dma("sp", io["xnT"].rearrange("(c p) n -> p c n", p=128)[:, :, tsl], xs[:, :, tsl], reads=[Bxs[t]], sem_buf=Bso)
    for t in range(NTC):
        tail(t)


import ml_dtypes
from contextlib import ExitStack
from concourse.bass_utils import run_bass_kernel_spmd

_BF = ml_dtypes.bfloat16
SEQ = 8192
NTOK = 2048
ALL_SLOPES = [2.0 ** (-8.0 * (h + 1) / 8) for h in range(8)]
HEAD_A = lambda g: g
HEAD_B = lambda g: 4 + g
MAXDIST = (2432, None)

_cache = {}


def _din(nc, io, name, shape, dt):
    io[name] = nc.dram_tensor(name, list(shape), dt, kind="ExternalInput").ap()


def _dout(nc, io, name, shape, dt):
    io[name] = nc.dram_tensor(name, list(shape), dt, kind="ExternalOutput").ap()


def _build_A():
    nc = bass.Bass("TRN2", target_bir_lowering=False)
    io = {}
    _din(nc, io, "xT", [1024, NTOK], F32); _din(nc, io, "gv", [128, 8], F32); _din(nc, io, "ones1k", [128, 128], F32)
    _dout(nc, io, "hT", [1024, NTOK], BF16)
    with ExitStack() as st:
        P = Prog(nc, st)
        build_A(nc, P, io, NTOK)
        P.finish()
    return nc


def _build_B():
    nc = bass.Bass("TRN2", target_bir_lowering=False)
    io = {}
    T = SEQ
    NM = T // 128 + 4
    _din(nc, io, "hT", [1024, T], BF16); _din(nc, io, "wfm", [1024, 640], F32); _din(nc, io, "wtm", [1024, 192], F32)
    _din(nc, io, "par", [128, 16], F32); _din(nc, io, "wg", [128, 128], F32)
    _din(nc, io, "ohk", [33, T], BF16); _din(nc, io, "crow", [2, T], BF16); _din(nc, io, "abias", [128, 2, NM], F32)
    _din(nc, io, "cmask", [128, 128], BF16); _din(nc, io, "hmask", [128, 512], F32); _din(nc, io, "rmask", [64, 512], F32)
    _din(nc, io, "ident", [128, 128], BF16); _din(nc, io, "ones64", [64, 64], F32)
    _dout(nc, io, "yT", [4, 64, T], BF16)
    with ExitStack() as st:
        P = Prog(nc, st)
        build_B(nc, P, T, io, maxdist=MAXDIST)
        P.finish()
    return nc


def _build_C(final):
    nc = bass.Bass("TRN2", target_bir_lowering=False)
    io = {}
    _din(nc, io, "xT", [1024, NTOK], F32); _din(nc, io, "yT", [8, 128, NTOK], BF16); _din(nc, io, "wout", [1024, 1024], F32)
    _din(nc, io, "w1", [1024, 4096], F32); _din(nc, io, "w2", [4096, 1024], F32); _din(nc, io, "gv", [128, 32], F32)
    _din(nc, io, "onesA", [128, 128], F32); _din(nc, io, "onesC", [128, 128], F32); _din(nc, io, "ones1k", [128, 128], F32)
    if final:
        _dout(nc, io, "oT", [1024, NTOK], F32)
    else:
        _dout(nc, io, "xnT", [1024, NTOK], F32); _dout(nc, io, "hnT", [1024, NTOK], BF16)
    with ExitStack() as st:
        P = Prog(nc, st)
        build_C(nc, P, io, final, NTOK)
        P.finish()
    return nc


def _prog(key, fn):
    if key not in _cache:
        _cache[key] = fn()
    return _cache[key]


def _chunks(v):
    return np.ascontiguousarray(np.asarray(v, np.float32).reshape(8, 128).T)


def _b_inputs(l, g, hT_b, inp, constsB):
    w_in = inp["w_in"][l]
    hA, hB = HEAD_A(g), HEAD_B(g)
    sl = lambda base, w, i: w_in[:, base + w * i: base + w * i + w]
    wfm = np.zeros((1024, 640), np.float32)
    wfm[:, 0:64] = sl(512, 64, g)
    wfm[:, 64:128] = sl(0, 64, g)
    wfm[:, 128:192] = sl(768, 64, g)
    wfm[:, 192:256] = sl(256, 64, g)
    wfm[:, 256:320] = sl(1280, 64, g)
    wfm[:, 320:384] = sl(1536, 64, hA)
    wfm[:, 384:448] = sl(1536, 64, hB)
    wfm[:, 448:512] = sl(2048, 64, hA)
    wfm[:, 512:576] = sl(2048, 64, hB)
    wtm = np.concatenate([sl(1024, 64, g), sl(2560, 64, hA), sl(2560, 64, hB)], axis=1)
    par = np.zeros((128, 16), np.float32)
    ch = slice(64 * g, 64 * g + 64)
    par[64:, 0:4] = inp["lru_conv_w"][l][:, ch].T
    par[64:, 4] = inp["lru_conv_b"][l][ch]
    par[64:, 5] = inp["lru_ba"][l][ch]
    par[64:, 6] = inp["lru_bx"][l][ch]
    par[64:, 7] = inp["lru_lambda"][l][ch]
    par[:64, 0] = inp["hg_lower_bounds"][0][ch]
    par[:64, 1] = inp["hg_lower_bounds"][1][ch]
    par[:64, 2] = inp["hg_norm_w"][l]
    par[:64, 3] = float(l)
    wg = np.zeros((128, 128), np.float32)
    wg[64:, 0:64] = inp["lru_wa"][l][g]
    wg[64:, 64:128] = inp["lru_wx"][l][g]
    d = dict(hT=hT_b, wfm=wfm, wtm=np.ascontiguousarray(wtm), par=par, wg=wg)
    d.update(constsB[g])
    return d


def _c_static(l, inp, next_gain):
    w_out = inp["w_out"][l]
    rows = []
    gy = np.ones((128, 8), np.float32)
    for g in range(4):
        hA, hB = HEAD_A(g), HEAD_B(g)
        rows += list(range(64 * g, 64 * g + 64)) + list(range(256 + 64 * g, 256 + 64 * g + 64))
        rows += list(range(512 + 64 * hA, 512 + 64 * hA + 64)) + list(range(512 + 64 * hB, 512 + 64 * hB + 64))
        gy[0:64, 2 * g] = inp["lru_out_norm"][l][64 * g:64 * g + 64]
        gy[0:64, 2 * g + 1] = inp["att_out_norm"][l][64 * hA:64 * hA + 64]
        gy[64:128, 2 * g + 1] = inp["att_out_norm"][l][64 * hB:64 * hB + 64]
    wout_p = np.ascontiguousarray(w_out[rows, :])
    gv = np.zeros((128, 32), np.float32)
    gv[:, 0:8] = _chunks(inp["norm_mlp"][l])
    gv[:, 8:16] = _chunks(next_gain)
    gv[:, 16:24] = gy
    d = dict(wout=wout_p, w1=np.ascontiguousarray(inp["w_ff1"][l]), w2=np.ascontiguousarray(inp["w_ff2"][l]), gv=gv)
    d.update(host_consts_C())
    return d


def kernel(**inputs):
    inp = {k: np.asarray(v) for k, v in inputs.items()}
    x = inp["x"].astype(np.float32, copy=False)
    B = x.shape[0]
    cores = list(range(8))
    xT = [np.ascontiguousarray(x[c // 4, (c % 4) * NTOK:(c % 4 + 1) * NTOK, :].T) for c in cores]
    constsB = [host_consts_B(SEQ, [ALL_SLOPES[HEAD_A(g)], ALL_SLOPES[HEAD_B(g)]]) for g in range(4)]
    cC = host_consts_C()
    ncA = _prog("A", _build_A)
    gv0 = _chunks(inp["norm_mix"][0])
    resA = run_bass_kernel_spmd(ncA, [dict(xT=xT[c], gv=gv0, ones1k=cC["ones1k"]) for c in cores], core_ids=cores)
    hT = [np.asarray(resA.results[c]["hT"]) for c in cores]
    out = None
    for l in range(2):
        hT_b = [np.ascontiguousarray(np.concatenate([hT[b * 4 + s] for s in range(4)], axis=1)) for b in range(B)]
        ncB = _prog("B", _build_B)
        resB = run_bass_kernel_spmd(ncB, [_b_inputs(l, c % 4, hT_b[c // 4], inp, constsB) for c in cores], core_ids=cores)
        yB = [np.asarray(resB.results[c]["yT"]) for c in cores]
        final = (l == 1)
        next_gain = inp["norm_final"] if final else inp["norm_mix"][l + 1]
        stat = _c_static(l, inp, next_gain)
        insC = []
        for c in cores:
            b, s = c // 4, c % 4
            tsl = slice(s * NTOK, (s + 1) * NTOK)
            yT = np.concatenate([yB[b * 4 + g][:, :, tsl].reshape(2, 128, NTOK) for g in range(4)], axis=0)
            d = dict(xT=xT[c], yT=np.ascontiguousarray(yT))
            d.update(stat)
            insC.append(d)
        ncC = _prog("C1" if final else "C0", lambda: _build_C(final))
        resC = run_bass_kernel_spmd(ncC, insC, core_ids=cores)
        if final:
            out = np.empty((B, SEQ, 1024), np.float32)
            for c in cores:
                b, s = c // 4, c % 4
                out[b, s * NTOK:(s + 1) * NTOK, :] = np.asarray(resC.results[c]["oT"]).T
        else:
            xT = [np.asarray(resC.results[c]["xnT"]) for c in cores]
            hT = [np.asarray(resC.results[c]["hnT"]) for c in cores]
    return out
```

```python
import numpy as np
import concourse.bass as bass
import concourse.mybir as mybir

F32 = mybir.dt.float32
BF16 = mybir.dt.bfloat16
AF = mybir.ActivationFunctionType
ALU = mybir.AluOpType
AX = mybir.AxisListType

ENGS = ("pe", "act", "dve", "pool", "sp")


class Buf:
    __slots__ = ("name", "w", "r", "dsem", "excl")

    def __init__(self, name):
        self.name = name
        self.w = None
        self.r = []
        self.dsem = None
        self.excl = False


class DmaSem:
    __slots__ = ("sem", "issued", "group_open", "name", "unit", "kind")

    def __init__(self, sem, name, unit=16):
        self.sem = sem
        self.issued = 0
        self.group_open = False
        self.name = name
        self.unit = unit
        self.kind = "hw"


class Prog:
    def __init__(self, nc, stack):
        self.nc = nc
        self.stack = stack
        self.ops = {e: [] for e in ENGS}
        self.cnt = {e: 0 for e in ENGS}
        self.esem = {}
        for e in ("pe", "act", "dve", "pool"):
            self.esem[e] = stack.enter_context(nc.semaphore("s_" + e))
        self.known = {e: {} for e in ENGS}
        self.dsems = []
        self.csems = []
        self.free_dsems = []
        self.scopes = []
        self.pending = {}
        self.nbuf = 0
        import os
        self.limit = int(os.environ.get("MK_LIMIT", "0"))
        self.nrec = 0
        self.lastdesc = None

    def buf(self, name=None):
        self.nbuf += 1
        return Buf(name or f"b{self.nbuf}")

    def _st(self):
        return self.scopes[-1][0] if self.scopes else self.stack

    def _nm(self, name):
        return (self.scopes[-1][1] + name) if self.scopes else name

    def push_scope(self, prefix):
        from contextlib import ExitStack
        self.scopes.append((ExitStack(), prefix))

    def pop_scope(self):
        st, _ = self.scopes.pop()
        st.close()

    def sbuf(self, name, shape, dtype):
        t = self._st().enter_context(self.nc.sbuf_tensor("sb_" + self._nm(name), list(shape), dtype))
        return t

    def psum(self, name, shape, dtype=F32):
        t = self._st().enter_context(self.nc.psum_tensor("ps_" + self._nm(name), list(shape), dtype))
        return t

    def new_dsem(self, name, kind="hw"):
        for i, d in enumerate(self.free_dsems):
            if d.kind == kind:
                self.free_dsems.pop(i)
                d.group_open = False
                return d
        s = self.stack.enter_context(self.nc.semaphore("d_" + self._nm(name)))
        d = DmaSem(s, name)
        d.kind = kind
        self.dsems.append(d)
        return d

    def barrier(self):
        pend = []
        for e in ("pe", "act", "dve", "pool"):
            if self.cnt[e] > 0:
                pend.append(("eng", e, self.cnt[e]))
        for ds in self.dsems + self.csems:
            if ds.issued:
                pend.append(("dma", ds, ds.unit * ds.issued))
        for q in ENGS:
            self.pending[q] = list(pend)
        self.free_dsems = list(self.dsems)

    def _pend(self, q, waits):
        for dep in self.pending.pop(q, []):
            if dep[0] == "eng" and dep[1] == q and q == "pe":
                continue
            self._need(q, dep, waits)

    def coll(self, kind, in_ap, out_ap, groups):
        s = self.stack.enter_context(self.nc.semaphore("c_%d" % len(self.csems)))
        d = DmaSem(s, "coll", unit=1)
        waits = []
        self._pend("pool", waits)
        d.issued = 1
        self.csems.append(d)

        def fn(e):
            return e.collective_compute(kind, mybir.AluOpType.bypass, replica_groups=groups, ins=[in_ap], outs=[out_ap])
        self.ops["pool"].append((self._merge(waits), fn, (d.sem, None)))

    def _merge(self, waits):
        m = {}
        for s, v in waits:
            k = id(s)
            if k not in m or m[k][1] < v:
                m[k] = (s, v)
        return list(m.values())

    def _need(self, eng, dep, waits):
        if dep is None:
            return
        if dep[0] == "eng":
            _, e, idx = dep
            if e == eng and eng in ("pe",):
                return
            key = ("e", e)
            if self.known[eng].get(key, 0) >= idx:
                return
            if e == eng and False:
                return
            self.known[eng][key] = idx
            waits.append((self.esem[e], idx))
        else:
            _, ds, val = dep
            val = max(val, ds.unit * ds.issued)
            ds.group_open = False
            key = ("d", id(ds))
            if self.known[eng].get(key, 0) >= val:
                return
            self.known[eng][key] = val
            waits.append((ds.sem, val))

    def _deps(self, eng, reads, writes):
        waits = []
        for b in reads:
            self._need(eng, b.w, waits)
        for b in writes:
            self._need(eng, b.w, waits)
            for r in b.r:
                self._need(eng, r, waits)
        m = {}
        for s, v in waits:
            k = id(s)
            if k not in m or m[k][1] < v:
                m[k] = (s, v)
        return list(m.values())

    def op(self, eng, fn, reads=(), writes=()):
        self.nrec += 1
        if self.limit and self.nrec > self.limit:
            return
        import traceback
        self.lastdesc = (self.nrec, eng, traceback.extract_stack(limit=3)[0].lineno, [b.name for b in reads], [b.name for b in writes])
        writes = list(writes) + [b for b in reads if b.excl and b not in writes]
        waits = []
        self._pend(eng, waits)
        waits = self._merge(waits + self._deps(eng, reads, writes))
        self.cnt[eng] += 1
        idx = self.cnt[eng]
        tag = ("eng", eng, idx)
        for b in reads:
            if b not in writes:
                b.r.append(tag)
        for b in writes:
            b.w = tag
            b.r = []
        self.ops[eng].append((waits, fn, (self.esem[eng], 1)))

    def dma(self, q, out_ap, in_ap, reads=(), writes=(), sem_buf=None, **kw):
        self.nrec += 1
        if self.limit and self.nrec > self.limit:
            return
        sb = sem_buf
        if sb is None:
            for b in list(writes) + list(reads):
                sb = b
                break
        if sb.dsem is None:
            sb.dsem = self.new_dsem(sb.name, "sw" if q == "pool" else "hw")
        ds = sb.dsem
        waits = []
        self._pend(q, waits)
        waits = waits + self._deps(q, reads, writes)
        if (not ds.group_open) and ds.issued > 0:
            w2 = []
            self._need(q, ("dma", ds, 16 * ds.issued), w2)
            waits += w2
        ds.issued += 1
        ds.group_open = True
        tag = ("dma", ds, 16 * ds.issued)
        for b in reads:
            b.r.append(tag)
        for b in writes:
            b.w = tag
            b.r = []

        def fn(e, out_ap=out_ap, in_ap=in_ap, kw=kw):
            return e.dma_start(out=out_ap, in_=in_ap, **kw)
        if q in ("pool", "act"):
            pass
        self.ops[q].append((self._merge(waits), fn, (ds.sem, 16)))

    def finish(self):
        fin = []
        for ds in self.dsems + self.csems:
            if ds.issued:
                fin.append((ds.sem, ds.unit * ds.issued))
        nc = self.nc
        ops = self.ops
        with nc.Block() as block:
            def emit(e, lst, final=None):
                for waits, fn, inc in lst:
                    for s, v in waits:
                        e.wait_ge(s, v)
                    ins = fn(e)
                    if inc is not None:
                        if inc[1] is None:
                            ins.then_inc(inc[0])
                        else:
                            ins.then_inc(inc[0], inc[1])
                if final:
                    for s, v in final:
                        e.wait_ge(s, v)

            @block.sync
            def _(e):
                emit(e, ops["sp"], fin)

            @block.tensor
            def _(e):
                emit(e, ops["pe"])

            @block.scalar
            def _(e):
                emit(e, ops["act"])

            @block.vector
            def _(e):
                emit(e, ops["dve"])

            @block.gpsimd
            def _(e):
                emit(e, ops["pool"])


BIG = 30000.0
NEGF = -1.0e30
LN2 = 0.6931471805599453
GC = 0.7978845608028654


def host_consts_B(T, slopes2):
    import ml_dtypes
    bf = ml_dtypes.bfloat16
    nb = T // 256
    c = {}
    oh = np.zeros((33, T), np.float32)
    for n in range(nb):
        oh[n, n * 256:(n + 1) * 256] = 1.0
    oh[32, :] = 1.0
    c["ohk"] = oh.astype(bf)
    t = np.arange(T) % 512
    c["crow"] = np.stack([-8.0 * s * t for s in slopes2]).astype(np.float32).astype(bf)
    NM = T // 128 + 4
    m = np.arange(NM)
    p = np.arange(128)
    tab = np.zeros((128, 2, NM), np.float32)
    for h in range(2):
        tab[:, h, :] = slopes2[h] * (p[:, None] + 128.0 * (m[None, :] - (T // 128)))
    c["abias"] = tab
    c["cmask"] = (np.arange(128)[None, :] >= np.arange(128)[:, None]).astype(np.float32).astype(bf)
    cm = (np.arange(64)[None, :] >= np.arange(64)[:, None]).astype(np.float32)
    bm = np.zeros((128, 128), np.float32)
    bm[:64, :64] = cm
    bm[64:, 64:] = cm
    c["hmask"] = np.tile(bm, (1, 4)).astype(np.float32)
    rm = np.ones((64, 512), np.float32)
    rm[:, ::64] = 0.0
    c["rmask"] = rm
    c["ident"] = np.eye(128, dtype=np.float32).astype(bf)
    c["ones64"] = np.full((64, 64), 1.0 / 64.0, np.float32)
    return c


def build_B(nc, P, T, io, maxdist=(None, None)):
    NB = T // 512
    NM = T // 128 + 4
    MOFF = T // 128
    hT, yT = io.get("hT"), io["yT"]
    wfm = P.sbuf("wfm", [128, 8, 640], BF16); Bwfm = P.buf("wfm")
    wtm = P.sbuf("wtm", [128, 8, 192], BF16); Bwtm = P.buf("wtm")
    par = P.sbuf("par", [128, 32], F32); Bpar = P.buf("par")
    wg = P.sbuf("wg", [128, 128], BF16); Bwg = P.buf("wg")
    abias = P.sbuf("abias", [128, 2, NM], F32); Bab = P.buf("abias")
    cmask = P.sbuf("cmask", [128, 128], BF16); Bcm = P.buf("cmask")
    hmask = P.sbuf("hmask", [128, 512], F32); Bhm = P.buf("hmask")
    rmask = P.sbuf("rmask", [64, 512], F32); Brm = P.buf("rmask")
    ident = P.sbuf("ident", [128, 128], BF16); Bid = P.buf("ident")
    ones64 = P.sbuf("ones64", [64, 64], F32); Bo64 = P.buf("ones64")
    QA = P.sbuf("QA", [128, T], BF16); QB = P.sbuf("QB", [128, T], BF16)
    KA = P.sbuf("KA", [128, T], BF16); KB = P.sbuf("KB", [128, T], BF16)
    NBK = T // 512
    BQ = [[P.buf(f"Q{h}_{i}") for i in range(NBK)] for h in range(2)]
    BK = [[P.buf(f"K{h}_{i}") for i in range(NBK)] for h in range(2)]
    Qh = [QA, QB]; Kh = [KA, KB]
    VV = P.sbuf("VV", [128, T // 128, 2, 65], BF16); BV = [P.buf(f"VV{i}") for i in range(NBK)]
    VH = P.sbuf("VH", [128, T // 128, 64], BF16); BVH = [P.buf(f"VH{i}") for i in range(NBK)]
    ones64r = P.sbuf("ones64r", [128, 64], F32); Bo64r = P.buf("ones64r")
    kmT = P.sbuf("kmT", [128, 32], BF16); Bkm = P.buf("kmT")
    banks = [P.psum(f"bank{i}", [128, 512]) for i in range(8)]
    Bbank = [P.buf(f"bank{i}") for i in range(8)]
    for b_ in Bbank:
        b_.excl = True
    rr = [0]

    def nbank():
        i = rr[0] % 6
        rr[0] += 1
        return banks[i], Bbank[i]
    pvb = [(banks[6], Bbank[6]), (banks[7], Bbank[7])]

    P.dma("pool", wfm[:], io["wfm"].rearrange("(c p) n -> p c n", p=128), writes=[Bwfm])
    P.dma("pool", wtm[:], io["wtm"].rearrange("(c p) n -> p c n", p=128), writes=[Bwtm])
    P.dma("pool", wg[:], io["wg"], writes=[Bwg])
    P.dma("sp", par[:, 0:16], io["par"], writes=[Bpar])
    P.dma("sp", abias[:], io["abias"], writes=[Bab])
    P.dma("sp", cmask[:], io["cmask"], writes=[Bcm])
    P.dma("sp", hmask[:], io["hmask"], writes=[Bhm])
    P.dma("sp", rmask[:], io["rmask"], writes=[Brm])
    P.dma("sp", ident[:], io["ident"], writes=[Bid])
    P.dma("sp", ones64[:], io["ones64"], writes=[Bo64])
    for h in range(2):
        P.op("pool", lambda e, h=h: e.memset(Qh[h][:], 0.0), writes=BQ[h])
        P.op("pool", lambda e, h=h: e.memset(Kh[h][:], 0.0), writes=BK[h])
    P.dma("sp", KA[64:97, :], io["ohk"], writes=BK[0], sem_buf=BK[0][0])
    P.dma("sp", KB[0:33, :], io["ohk"], writes=BK[1], sem_buf=BK[1][0])
    P.dma("sp", QA[96:97, :], io["crow"][0:1, :], writes=BQ[0], sem_buf=BQ[0][0])
    P.dma("sp", QB[32:33, :], io["crow"][1:2, :], writes=BQ[1], sem_buf=BQ[1][0])
    P.op("pool", lambda e: e.memset(VV[:, :, :, 64:65], 1.0), writes=BV)
    P.op("pool", lambda e: e.memset(ones64r[:], 1.0), writes=[Bo64r])
    P.op("pool", lambda e: e.memset(kmT[:], 0.0), writes=[Bkm])

    L = slice(64, 128)
    H = slice(0, 64)
    def pc(rows, j):
        return par[rows, j:j + 1]
    P.op("dve", lambda e: e.memset(par[:, 24:25], -LN2), reads=[Bpar], writes=[Bpar])
    P.op("dve", lambda e: e.memset(par[:, 25:26], 1e-6), reads=[Bpar], writes=[Bpar])
    P.op("dve", lambda e: e.memset(par[:, 26:27], 1.0), reads=[Bpar], writes=[Bpar])
    P.op("dve", lambda e: e.tensor_scalar(out=par[L, 8:10], in0=par[L, 5:7], scalar1=0.5, scalar2=None, op0=ALU.mult), reads=[Bpar], writes=[Bpar])
    P.op("act", lambda e: e.activation(out=pc(L, 12), in_=pc(L, 7), func=AF.Exp, scale=-1.0), reads=[Bpar], writes=[Bpar])
    P.op("act", lambda e: e.activation(out=pc(L, 13), in_=pc(L, 12), func=AF.Ln, bias=pc(L, 26)), reads=[Bpar], writes=[Bpar])
    P.op("dve", lambda e: e.tensor_scalar(out=pc(L, 10), in0=pc(L, 13), scalar1=-8.0, scalar2=None, op0=ALU.mult), reads=[Bpar], writes=[Bpar])
    P.op("dve", lambda e: e.tensor_scalar(out=pc(L, 11), in0=pc(L, 13), scalar1=-4.0, scalar2=None, op0=ALU.mult), reads=[Bpar], writes=[Bpar])
    P.op("dve", lambda e: e.tensor_tensor(out=pc(H, 20), in0=pc(H, 1), in1=pc(H, 0), op=ALU.subtract), reads=[Bpar], writes=[Bpar])
    P.op("act", lambda e: e.activation(out=pc(H, 21), in_=pc(H, 20), func=AF.Tanh, scale=0.5), reads=[Bpar], writes=[Bpar])
    P.op("dve", lambda e: e.tensor_scalar(out=pc(H, 22), in0=pc(H, 21), scalar1=0.5, scalar2=0.5, op0=ALU.mult, op1=ALU.add), reads=[Bpar], writes=[Bpar])
    P.op("dve", lambda e: e.tensor_tensor(out=pc(H, 19), in0=pc(H, 22), in1=pc(H, 3), op=ALU.mult), reads=[Bpar], writes=[Bpar])
    P.op("dve", lambda e: e.tensor_scalar(out=pc(H, 16), in0=pc(H, 19), scalar1=-0.5, scalar2=0.5, op0=ALU.mult, op1=ALU.add), reads=[Bpar], writes=[Bpar])
    P.op("dve", lambda e: e.tensor_scalar(out=pc(H, 18), in0=pc(H, 16), scalar1=-1.0, scalar2=None, op0=ALU.mult), reads=[Bpar], writes=[Bpar])
    P.op("dve", lambda e: e.tensor_scalar(out=pc(H, 23), in0=pc(H, 19), scalar1=1e-30, scalar2=None, op0=ALU.max), reads=[Bpar], writes=[Bpar])
    P.op("dve", lambda e: e.tensor_tensor(out=pc(H, 17), in0=pc(H, 23), in1=pc(H, 16), op=ALU.add), reads=[Bpar], writes=[Bpar])

    def rot(name, shape, dt, n):
        ts = [P.sbuf(f"{name}{i}", shape, dt) for i in range(n)]
        bs = [P.buf(f"{name}{i}") for i in range(n)]
        return ts, bs
    hblk, Bhblk = rot("hblk", [128, 8, 512], BF16, 2)
    xbuf, Bxbuf = rot("xbuf", [128, 515], F32, 2)
    NW = 2
    W = {}

    class HV:
        def __init__(self, t):
            self.t = t

        def __getitem__(self, key):
            if isinstance(key, tuple):
                return self.t[(slice(0, 64),) + tuple(key[1:])]
            return self.t[0:64, :]
    share = {"thq": "ysb", "thf": "t1", "thg": "t2", "gs2": "xc", "fg": "thr", "kk": "thi", "bb": "aa", "eb": "a2", "enb": "uu",
             "qs2": "hh", "kve": "sh1", "osb": "sh2", "osq": "sh3", "rstd": "sh4"}
    for nm, shp, dt in [("ysb", [128, 512], F32), ("t1", [128, 512], F32), ("t2", [128, 512], F32),
                        ("xc", [128, 512], F32), ("xcb", [128, 512], BF16), ("thr", [128, 512], F32), ("thi", [128, 512], F32),
                        ("aa", [128, 512], F32), ("a2", [128, 512], F32), ("uu", [128, 512], F32), ("hh", [128, 512], F32),
                        ("sh1", [128, 512], F32), ("sh2", [128, 512], F32), ("sh3", [128, 512], F32), ("sh4", [128, 512], F32),
                        ("yo", [128, 512], BF16),
                        ("qt", [64, 512], BF16), ("kt", [64, 512], BF16),
                        ("ktokE", [128, 4, 64], BF16), ("ktokO", [128, 4, 64], BF16), ("attm", [128, 512], BF16), ("ebl", [64, 8], F32),
                        ("hy", [64, 512], BF16),
                        ("gsb", [128, 32], F32), ("top8", [128, 8], F32), ("mp", [128, 32], BF16),
                        ("pt", [128, 512], BF16), ("onum", [65, 512], F32), ("rec", [65, 512], F32), ("my", [64, 512], BF16)]:
        n = {"pt": 4, "gsb": 4, "top8": 4, "mp": 4, "sh1": 1, "sh2": 1, "sh3": 1, "sh4": 1, "rec": 1}.get(nm, NW)
        W[nm] = rot(nm, shp, dt, n)
    for hn, ln in share.items():
        ts, _ = W[ln]
        W[hn] = ([HV(t) for t in ts], [P.buf(f"{hn}{i}") for i in range(len(ts))])
    ctr = {}

    def wt(nm):
        i = ctr.get(nm, 0)
        ctr[nm] = i + 1
        ts, bs = W[nm]
        return ts[i % len(ts)], bs[i % len(bs)]
    for i in range(4):
        P.op("pool", lambda e, i=i: e.memset(W["gsb"][0][i][:], NEGF), writes=[W["gsb"][1][i]])
    for nm_ in ("ktokE", "ktokO"):
        for i in range(NW):
            P.op("pool", lambda e, nm_=nm_, i=i: e.memset(W[nm_][0][i][:], 0.0), writes=[W[nm_][1][i]])
    Sst = P.sbuf("Sst", [64, 64], F32); BS = P.buf("Sst")
    Sbf, BSbf = rot("Sbf", [64, 64], BF16, 4)
    P.op("dve", lambda e: e.memset(Sst[:], 0.0), writes=[BS])
    P.op("dve", lambda e: e.memset(xbuf[1][L, 512:515], 0.0), writes=[Bxbuf[1]])
    hprev = [None]

    S1 = {}

    def stage1(blk):
        c0 = blk * 512
        hb, Bhb = hblk[blk % 2], Bhblk[blk % 2]
        if "hsrc" in io:
            for j in range(4):
                P.dma("sp", hb[:, 2 * j:2 * j + 2, :], io["hsrc"](blk, j), writes=[Bhb])
        else:
            P.dma("sp", hb[:], hT[:, c0:c0 + 512].rearrange("(c p) n -> p c n", p=128), writes=[Bhb])

        def inproj(col0, M, bank, Bb, rows=slice(0, 128)):
            for k in range(8):
                P.op("pe", lambda e, k=k: e.matmul(bank[rows, :], lhsT=wfm[:, k, col0:col0 + M], rhs=hb[:, k, :], start=(k == 0), stop=(k == 7)),
                     reads=[Bwfm, Bhb], writes=[Bb])
        b4, Bb4 = nbank(); inproj(320, 128, b4, Bb4)
        b5, Bb5 = nbank(); inproj(448, 128, b5, Bb5)
        P.op("act", lambda e: e.activation(out=QA[0:64, c0:c0 + 512], in_=b4[0:64, :], func=AF.Identity), reads=[Bb4], writes=[BQ[0][blk]])
        P.op("dve", lambda e: e.tensor_copy(out=QB[64:128, c0:c0 + 512], in_=b4[64:128, :]), reads=[Bb4], writes=[BQ[1][blk]])
        P.op("act", lambda e: e.activation(out=KA[0:64, c0:c0 + 512], in_=b5[0:64, :], func=AF.Identity), reads=[Bb5], writes=[BK[0][blk]])
        P.op("dve", lambda e: e.tensor_copy(out=KB[64:128, c0:c0 + 512], in_=b5[64:128, :]), reads=[Bb5], writes=[BK[1][blk]])
        kms, Bkms = wt("top8")
        P.op("dve", lambda e: e.tensor_reduce(out=kms[:, 0:2], in_=b5[:].rearrange("p (n k) -> p n k", n=2), axis=AX.X, op=ALU.add), reads=[Bb5], writes=[Bkms])
        P.op("dve", lambda e: e.tensor_scalar(out=kmT[:, 2 * blk:2 * blk + 2], in0=kms[:, 0:2], scalar1=1.0 / 256.0, scalar2=None, op0=ALU.mult), reads=[Bkms], writes=[Bkm])
        for pr in range(2):
            bv, Bbv = nbank()
            for tt in (2 * pr, 2 * pr + 1):
                o = (tt % 2) * 192
                for k in range(8):
                    P.op("pe", lambda e, k=k, tt=tt, o=o, bv=bv: e.matmul(bv[:, o:o + 192], lhsT=hb[:, k, tt * 128:(tt + 1) * 128], rhs=wtm[:, k, :], start=(k == 0), stop=(k == 7)),
                         reads=[Bwtm, Bhb], writes=[Bbv])
            for tt in (2 * pr, 2 * pr + 1):
                gi = blk * 4 + tt
                o = (tt % 2) * 192
                P.op("act", lambda e, gi=gi, o=o, bv=bv: e.activation(out=VH[:, gi, :], in_=bv[:, o:o + 64], func=AF.Identity), reads=[Bbv], writes=[BVH[blk]])
                P.op("dve", lambda e, gi=gi, o=o, bv=bv: e.tensor_copy(out=VV[:, gi, :, 0:64], in_=bv[:, o + 64:o + 192].rearrange("p (h d) -> p h d", h=2)), reads=[Bbv], writes=[BV[blk]])
        b1, Bb1 = nbank(); inproj(0, 128, b1, Bb1)
        b2, Bb2 = nbank(); inproj(128, 128, b2, Bb2)
        b3, Bb3 = nbank(); inproj(256, 64, b3, Bb3, rows=slice(0, 64))
        xb, Bxb = xbuf[blk % 2], Bxbuf[blk % 2]
        P.op("act", lambda e: e.activation(out=xb[L, 3:515], in_=b1[L, :], func=AF.Identity), reads=[Bb1], writes=[Bxb])
        ysb, Bysb = wt("ysb")
        P.op("act", lambda e: e.activation(out=ysb[L, :], in_=b2[L, :], func=AF.Identity), reads=[Bb2], writes=[Bysb])
        thq, Bthq = wt("thq"); thf, Bthf = wt("thf"); thg, Bthg = wt("thg")
        P.op("act", lambda e: e.activation(out=thq[:], in_=b1[H, :], func=AF.Tanh, scale=0.5), reads=[Bb1], writes=[Bthq])
        P.op("act", lambda e: e.activation(out=thf[:], in_=b2[H, :], func=AF.Tanh, scale=0.5), reads=[Bb2], writes=[Bthf])
        P.op("act", lambda e: e.activation(out=thg[:], in_=b3[H, :], func=AF.Tanh, scale=0.5), reads=[Bb3], writes=[Bthg])
        qs2, Bqs2 = wt("qs2"); gs2, Bgs2 = wt("gs2")
        P.op("dve", lambda e: e.scalar_tensor_tensor(out=qs2[:], in0=thq[:], scalar=1.0, in1=b1[H, :], op0=ALU.add, op1=ALU.mult), reads=[Bthq, Bb1], writes=[Bqs2])
        P.op("dve", lambda e: e.scalar_tensor_tensor(out=gs2[:], in0=thg[:], scalar=1.0, in1=b3[H, :], op0=ALU.add, op1=ALU.mult), reads=[Bthg, Bb3], writes=[Bgs2])
        S1[blk] = dict(xb=(xb, Bxb), ysb=(ysb, Bysb), thf=(thf, Bthf), qs2=(qs2, Bqs2), gs2=(gs2, Bgs2))

    def stage2(blk):
        c0 = blk * 512
        d = S1.pop(blk)
        xb, Bxb = d["xb"]; ysb, Bysb = d["ysb"]; thf, Bthf = d["thf"]; qs2, Bqs2 = d["qs2"]; gs2, Bgs2 = d["gs2"]
        xo, Bxo = xbuf[(blk + 1) % 2], Bxbuf[(blk + 1) % 2]
        P.op("pool", lambda e: e.tensor_copy(out=xb[L, 0:3], in_=xo[L, 512:515]), reads=[Bxo], writes=[Bxb])
        xc, Bxc = wt("xc")
        P.op("pool", lambda e: e.tensor_scalar(out=xc[L, :], in0=xb[L, 3:515], scalar1=pc(L, 3), scalar2=pc(L, 4), op0=ALU.mult, op1=ALU.add), reads=[Bxb, Bpar], writes=[Bxc])
        for j in (2, 1, 0):
            P.op("dve", lambda e, j=j: e.scalar_tensor_tensor(out=xc[L, :], in0=xb[L, j:j + 512], scalar=pc(L, j), in1=xc[L, :], op0=ALU.mult, op1=ALU.add), reads=[Bxb, Bpar, Bxc], writes=[Bxc])
        xcb, Bxcb = wt("xcb")
        P.op("pool", lambda e: e.tensor_copy(out=xcb[L, :], in_=xc[L, :]), reads=[Bxc], writes=[Bxcb])
        bg, Bbg = nbank()
        bg2, Bbg2 = nbank()
        P.op("pe", lambda e: e.matmul(bg[L, :], lhsT=wg[L, 0:64], rhs=xcb[L, :], start=True, stop=True), reads=[Bwg, Bxcb], writes=[Bbg])
        P.op("pe", lambda e: e.matmul(bg2[L, :], lhsT=wg[L, 64:128], rhs=xcb[L, :], start=True, stop=True), reads=[Bwg, Bxcb], writes=[Bbg2])
        thr, Bthr = wt("thr"); thi, Bthi = wt("thi")
        P.op("act", lambda e: e.activation(out=thr[L, :], in_=bg[L, :], func=AF.Tanh, scale=0.5, bias=pc(L, 8)), reads=[Bbg, Bpar], writes=[Bthr])
        P.op("act", lambda e: e.activation(out=thi[L, :], in_=bg2[L, :], func=AF.Tanh, scale=0.5, bias=pc(L, 9)), reads=[Bbg2, Bpar], writes=[Bthi])
        aa, Baa = wt("aa"); a2, Ba2 = wt("a2")
        P.op("act", lambda e: e.activation(out=aa[L, :], in_=thr[L, :], func=AF.Exp, scale=pc(L, 11), bias=pc(L, 11)), reads=[Bthr, Bpar], writes=[Baa])
        P.op("act", lambda e: e.activation(out=a2[L, :], in_=thr[L, :], func=AF.Exp, scale=pc(L, 10), bias=pc(L, 10)), reads=[Bthr, Bpar], writes=[Ba2])
        t1, Bt1 = wt("t1"); t2, Bt2 = wt("t2")
        P.op("act", lambda e: e.activation(out=t1[L, :], in_=ysb[L, :], func=AF.Square), reads=[Bysb], writes=[Bt1])
        P.op("pool", lambda e: e.tensor_scalar(out=t1[L, :], in0=t1[L, :], scalar1=0.044715, scalar2=1.0, op0=ALU.mult, op1=ALU.add), reads=[Bt1], writes=[Bt1])
        P.op("pool", lambda e: e.tensor_tensor(out=t1[L, :], in0=t1[L, :], in1=ysb[L, :], op=ALU.mult), reads=[Bt1, Bysb], writes=[Bt1])
        P.op("act", lambda e: e.activation(out=t2[L, :], in_=t1[L, :], func=AF.Tanh, scale=GC), reads=[Bt1], writes=[Bt2])
        P.op("dve", lambda e: e.scalar_tensor_tensor(out=t2[L, :], in0=t2[L, :], scalar=1.0, in1=ysb[L, :], op0=ALU.add, op1=ALU.mult), reads=[Bt2, Bysb], writes=[Bt2])
        uu, Buu = wt("uu")
        P.op("dve", lambda e: e.scalar_tensor_tensor(out=uu[L, :], in0=thi[L, :], scalar=1.0, in1=xc[L, :], op0=ALU.add, op1=ALU.mult), reads=[Bthi, Bxc], writes=[Buu])
        fg, Bfg = wt("fg"); kk, Bkk = wt("kk")
        P.op("dve", lambda e: e.tensor_scalar(out=fg[:], in0=thf[:], scalar1=pc(H, 16), scalar2=pc(H, 17), op0=ALU.mult, op1=ALU.add), reads=[Bthf, Bpar], writes=[Bfg])
        P.op("dve", lambda e: e.tensor_scalar(out=kk[:], in0=thf[:], scalar1=pc(H, 18), scalar2=pc(H, 16), op0=ALU.mult, op1=ALU.add), reads=[Bthf, Bpar], writes=[Bkk])
        P.op("act", lambda e: e.activation(out=a2[L, :], in_=a2[L, :], func=AF.Ln, scale=-1.0, bias=pc(L, 26)), reads=[Ba2], writes=[Ba2])
        P.op("act", lambda e: e.activation(out=fg[:], in_=fg[:], func=AF.Ln), reads=[Bfg], writes=[Bfg])
        P.op("act", lambda e: e.activation(out=a2[L, :], in_=a2[L, :], func=AF.Exp, scale=0.5), reads=[Ba2], writes=[Ba2])
        bb, Bbb = wt("bb")
        P.op("dve", lambda e: e.tensor_tensor_scan(out=bb[:], data0=rmask[:], data1=fg[:], initial=0.0, op0=ALU.mult, op1=ALU.add), reads=[Brm, Bfg], writes=[Bbb])
        eb, Beb = wt("eb"); enb, Benb = wt("enb"); ebl, Bebl = wt("ebl")
        P.op("act", lambda e: e.activation(out=eb[:], in_=bb[:], func=AF.Exp, bias=pc(H, 24)), reads=[Bbb, Bpar], writes=[Beb])
        P.op("act", lambda e: e.activation(out=enb[:], in_=bb[:], func=AF.Exp, scale=-1.0), reads=[Bbb], writes=[Benb])
        P.op("act", lambda e: e.activation(out=ebl[:], in_=bb[:, 63:512:64], func=AF.Exp), reads=[Bbb], writes=[Bebl])
        P.op("dve", lambda e: e.scalar_tensor_tensor(out=uu[L, :], in0=uu[L, :], scalar=0.5, in1=a2[L, :], op0=ALU.mult, op1=ALU.mult), reads=[Buu, Ba2], writes=[Buu])
        hh, Bhh = wt("hh")
        if hprev[0] is None:
            P.op("dve", lambda e: e.tensor_tensor_scan(out=hh[L, :], data0=aa[L, :], data1=uu[L, :], initial=0.0, op0=ALU.mult, op1=ALU.add), reads=[Baa, Buu], writes=[Bhh])
        else:
            hp, Bhp = hprev[0]
            P.op("dve", lambda e, hp=hp: e.tensor_tensor_scan(out=hh[L, :], data0=aa[L, :], data1=uu[L, :], initial=hp[L, 511:512], op0=ALU.mult, op1=ALU.add), reads=[Baa, Buu, Bhp], writes=[Bhh])
        hprev[0] = (hh, Bhh)
        yo, Byo = wt("yo")
        P.op("dve", lambda e: e.scalar_tensor_tensor(out=yo[L, :], in0=hh[L, :], scalar=0.5, in1=t2[L, :], op0=ALU.mult, op1=ALU.mult), reads=[Bhh, Bt2], writes=[Byo])
        P.dma("sp", yT[0, :, c0:c0 + 512], yo[L, :], reads=[Byo])
        qt, Bqt = wt("qt"); kt, Bkt = wt("kt")
        P.op("dve", lambda e: e.tensor_tensor(out=qt[:], in0=qs2[:], in1=eb[:], op=ALU.mult), reads=[Bqs2, Beb], writes=[Bqt])
        P.op("dve", lambda e: e.tensor_tensor(out=kt[:], in0=kk[:], in1=enb[:], op=ALU.mult), reads=[Bkk, Benb], writes=[Bkt])
        btr, Bbtr = nbank()
        btr16 = btr[:].bitcast(BF16)
        ktokE, BktokE = wt("ktokE"); ktokO, BktokO = wt("ktokO")
        for tt in range(4):
            P.op("pe", lambda e, tt=tt: e.transpose(btr16[:, tt * 64:(tt + 1) * 64], in_=kt[:, tt * 128:(tt + 1) * 128], identity=ident[0:64, 0:64]), reads=[Bkt, Bid], writes=[Bbtr])
        P.op("act", lambda e: e.activation(out=ktokE[0:64, :, :].rearrange("p t d -> p (t d)"), in_=btr16[0:64, 0:256], func=AF.Identity), reads=[Bbtr], writes=[BktokE])
        P.op("act", lambda e: e.activation(out=ktokO[64:128, :, :].rearrange("p t d -> p (t d)"), in_=btr16[64:128, 0:256], func=AF.Identity), reads=[Bbtr], writes=[BktokO])
        bkv, Bbkv = nbank()
        for c in range(8):
            tt, hf = c // 2, c % 2
            gi = blk * 4 + tt
            kx, Bkx = (ktokE, BktokE) if hf == 0 else (ktokO, BktokO)
            P.op("pe", lambda e, c=c, tt=tt, gi=gi, kx=kx: e.matmul(bkv[0:64, c * 64:(c + 1) * 64], lhsT=kx[:, tt, :], rhs=VH[:, gi, :], start=True, stop=True),
                 reads=[Bkx, BVH[blk]], writes=[Bbkv])
        batt, Bbatt = nbank()
        for tt in range(4):
            P.op("pe", lambda e, tt=tt: e.matmul(batt[:, tt * 128:(tt + 1) * 128], lhsT=kt[:, tt * 128:(tt + 1) * 128], rhs=qt[:, tt * 128:(tt + 1) * 128], start=True, stop=True),
                 reads=[Bkt, Bqt], writes=[Bbatt])
        attm, Battm = wt("attm")
        P.op("dve", lambda e: e.tensor_tensor(out=attm[:], in0=batt[:], in1=hmask[:], op=ALU.mult), reads=[Bbatt, Bhm], writes=[Battm])
        kve, Bkve = wt("kve")
        for c in range(8):
            P.op("act", lambda e, c=c: e.activation(out=kve[:, c * 64:(c + 1) * 64], in_=bkv[0:64, c * 64:(c + 1) * 64], func=AF.Identity, scale=ebl[:, c:c + 1]), reads=[Bbkv, Bebl], writes=[Bkve])
        bo, Bbo = nbank()
        for c in range(8):
            tt, hf = c // 2, c % 2
            gi = blk * 4 + tt
            sb, Bsb = Sbf[(blk * 8 + c) % 4], BSbf[(blk * 8 + c) % 4]
            P.op("act", lambda e, sb=sb: e.activation(out=sb[:], in_=Sst[:], func=AF.Identity), reads=[BS], writes=[Bsb])
            if hf == 0:
                P.op("pe", lambda e, tt=tt, gi=gi: e.matmul(bo[0:64, tt * 128:(tt + 1) * 128], lhsT=VH[:, gi, :], rhs=attm[:, tt * 128:(tt + 1) * 128], start=True, stop=False),
                     reads=[BVH[blk], Battm], writes=[Bbo])
            P.op("pe", lambda e, c=c, sb=sb, hf=hf: e.matmul(bo[0:64, c * 64:(c + 1) * 64], lhsT=sb[:], rhs=qt[:, c * 64:(c + 1) * 64], start=False, stop=(hf == 1)),
                 reads=[Bsb, Bqt], writes=[Bbo])
            P.op("dve", lambda e, c=c: e.scalar_tensor_tensor(out=Sst[:], in0=Sst[:], scalar=ebl[:, c:c + 1], in1=kve[:, c * 64:(c + 1) * 64], op0=ALU.mult, op1=ALU.add), reads=[BS, Bebl, Bkve], writes=[BS])
        osb, Bosb = wt("osb"); osq, Bosq = wt("osq")
        P.op("act", lambda e: e.activation(out=osb[:], in_=bo[0:64, :], func=AF.Identity), reads=[Bbo], writes=[Bosb])
        P.op("act", lambda e: e.activation(out=osq[:], in_=bo[0:64, :], func=AF.Square), reads=[Bbo], writes=[Bosq])
        bms, Bbms = nbank()
        P.op("pe", lambda e: e.matmul(bms[0:64, :], lhsT=ones64[:], rhs=osq[:], start=True, stop=True), reads=[Bo64, Bosq], writes=[Bbms])
        rstd, Brstd = wt("rstd")
        P.op("act", lambda e: e.activation(out=rstd[:], in_=bms[0:64, :], func=AF.Ln, bias=pc(H, 25)), reads=[Bbms, Bpar], writes=[Brstd])
        P.op("act", lambda e: e.activation(out=rstd[:], in_=rstd[:], func=AF.Exp, scale=-0.5), reads=[Brstd], writes=[Brstd])
        P.op("dve", lambda e: e.tensor_tensor(out=osb[:], in0=osb[:], in1=rstd[:], op=ALU.mult), reads=[Bosb, Brstd], writes=[Bosb])
        P.op("dve", lambda e: e.tensor_scalar(out=osb[:], in0=osb[:], scalar1=pc(H, 2), scalar2=0.5, op0=ALU.mult, op1=ALU.mult), reads=[Bosb, Bpar], writes=[Bosb])
        hy, Bhy = wt("hy")
        P.op("dve", lambda e: e.tensor_tensor(out=hy[:], in0=osb[:], in1=gs2[:], op=ALU.mult), reads=[Bosb, Bgs2], writes=[Bhy])
        P.dma("sp", yT[1, :, c0:c0 + 512], hy[:], reads=[Bhy])

    def stage3(blk):
        for h in range(2):
            head3(blk, h)

    def head3(blk, h):
        c0 = blk * 512
        if True:
            Q, K = Qh[h], Kh[h]
            dr = slice(0, 64) if h == 0 else slice(64, 128)
            mr = slice(64, 96) if h == 0 else slice(0, 32)
            bgt, Bbgt = nbank()
            bmp, Bbmp = nbank()
            bmp16 = bmp[:].bitcast(BF16)
            any_mp = False
            for st in range(4):
                j = 2 * blk + (st // 2)
                if j == 0:
                    continue
                any_mp = True
                q0 = c0 + st * 128
                P.op("pe", lambda e, q0=q0, st=st: e.matmul(bgt[:, st * 32:(st + 1) * 32], lhsT=Q[dr, q0:q0 + 128], rhs=kmT[dr, 0:32], start=True, stop=True),
                     reads=[BQ[h][blk], Bkm], writes=[Bbgt])
                gsb, Bgsb = wt("gsb"); top8, Btop8 = wt("top8"); mp, Bmp = wt("mp")
                P.op("dve", lambda e, st=st, j=j, gsb=gsb: e.tensor_copy(out=gsb[:, 0:j], in_=bgt[:, st * 32:st * 32 + j]), reads=[Bbgt], writes=[Bgsb])
                P.op("dve", lambda e, j=j, gsb=gsb, top8=top8: e.max(out=top8[:], in_=gsb[:, 0:max(j, 8)]), reads=[Bgsb], writes=[Btop8])
                P.op("pool", lambda e, mp=mp: e.memset(mp[:], 0.0), writes=[Bmp])
                P.op("dve", lambda e, j=j, gsb=gsb, top8=top8, mp=mp: e.tensor_scalar(out=mp[:, 0:j], in0=gsb[:, 0:j], scalar1=top8[:, 2:3], scalar2=-8.0 * BIG, op0=ALU.is_lt, op1=ALU.mult),
                     reads=[Bgsb, Btop8, Bmp], writes=[Bmp])
                P.op("pe", lambda e, st=st, mp=mp: e.transpose(bmp16[mr, st * 128:(st + 1) * 128], in_=mp[:], identity=ident[:]), reads=[Bmp, Bid], writes=[Bbmp])
            if any_mp:
                s0 = 0 if blk > 0 else 2
                P.op("act", lambda e, s0=s0: e.activation(out=Q[mr, c0 + s0 * 128:c0 + 512], in_=bmp16[mr, s0 * 128:512], func=AF.Identity), reads=[Bbmp], writes=[BQ[h][blk]])
            pv, Bpv = pvb[h]
            tiles = [(kt_, 0) for kt_ in range(4 * blk) if (maxdist[h] is None or (c0 - (kt_ * 128 + 127)) <= maxdist[h])] + [(4 * blk + kk_, kk_) for kk_ in range(4)]
            first = True
            for (kti, own) in tiles:
                isown = kti >= 4 * blk
                n0 = own * 128 if isown else 0
                k0 = kti * 128
                kb = kti // 4
                bs, Bbs = nbank()
                P.op("pe", lambda e, k0=k0, n0=n0, bs=bs: e.matmul(bs[:, n0:512], lhsT=K[:, k0:k0 + 128], rhs=Q[:, c0 + n0:c0 + 512], start=True, stop=True),
                     reads=[BK[h][kb], BQ[h][blk]], writes=[Bbs])
                pt, Bpt = wt("pt")
                m = kti - 4 * blk + MOFF
                P.op("act", lambda e, n0=n0, bs=bs, pt=pt, m=m: e.activation(out=pt[:, n0:512], in_=bs[:, n0:512], func=AF.Exp, scale=0.125, bias=abias[:, h, m:m + 1]),
                     reads=[Bbs, Bab], writes=[Bpt])
                if isown:
                    P.op("dve", lambda e, n0=n0, pt=pt: e.tensor_tensor(out=pt[:, n0:n0 + 128], in0=pt[:, n0:n0 + 128], in1=cmask[:], op=ALU.mult), reads=[Bpt, Bcm], writes=[Bpt])
                last = (kti == tiles[-1][0])
                P.op("pe", lambda e, kti=kti, n0=n0, pt=pt, first=first, last=last: e.matmul(pv[0:65, n0:512], lhsT=VV[:, kti, h, :], rhs=pt[:, n0:512], start=first, stop=last),
                     reads=[BV[kb], Bpt], writes=[Bpv])
                first = False
            onum, Bonum = wt("onum"); rec, Brec = wt("rec"); my, Bmy = wt("my")
            P.op("act", lambda e: e.activation(out=onum[0:65, :], in_=pv[0:65, :], func=AF.Identity), reads=[Bpv], writes=[Bonum])
            P.op("act", lambda e: e.activation(out=rec[64:65, :], in_=onum[64:65, :], func=AF.Ln), reads=[Bonum], writes=[Brec])
            P.op("act", lambda e: e.activation(out=rec[64:65, :], in_=rec[64:65, :], func=AF.Exp, scale=-1.0), reads=[Brec], writes=[Brec])
            bbc, Bbbc = nbank()
            P.op("pe", lambda e: e.matmul(bbc[0:64, :], lhsT=ones64r[64:65, :], rhs=rec[64:65, :], start=True, stop=True), reads=[Bo64r, Brec], writes=[Bbbc])
            P.op("dve", lambda e: e.tensor_tensor(out=my[:], in0=onum[0:64, :], in1=bbc[0:64, :], op=ALU.mult), reads=[Bonum, Bbbc], writes=[Bmy])
            P.dma("sp", yT[2 + h, :, c0:c0 + 512], my[:], reads=[Bmy])

    import os
    stop = os.environ.get("PHB_STOP", "")
    if stop == "init":
        return
    for blk in range(NB):
        stage1(blk)
        if stop == "s1":
            return
        if blk > 0:
            stage3(blk - 1)
        stage2(blk)
        if stop == "s2":
            return
    stage3(NB - 1)


EPS = 1e-6


def host_consts_C():
    c = {}
    oa = np.zeros((128, 128), np.float32)
    oa[:64, :] = 1.0 / 256.0
    c["onesA"] = oa
    c["onesC"] = np.full((128, 128), 1.0 / 512.0, np.float32)
    c["ones1k"] = np.full((128, 128), 1.0 / 1024.0, np.float32)
    return c


def rms_stats(P, banks, Bbanks, src_fn, nchunk, ones_t, Bones, sq_tiles, eps_ap, Bpar, rstd, Brstd, srcbufs):
    bank, Bbank = banks
    for c in range(nchunk):
        sq, Bsq = sq_tiles[c % len(sq_tiles)]
        src = src_fn(c)
        P.op("act", lambda e, sq=sq, src=src: e.activation(out=sq[:], in_=src, func=AF.Square), reads=srcbufs(c), writes=[Bsq])
        P.op("pe", lambda e, sq=sq, c=c: e.matmul(bank[:], lhsT=ones_t[:], rhs=sq[:], start=(c == 0), stop=(c == nchunk - 1)), reads=[Bones, Bsq], writes=[Bbank])
    P.op("act", lambda e: e.activation(out=rstd[:], in_=bank[:], func=AF.Ln, bias=eps_ap), reads=[Bbank, Bpar], writes=[Brstd])
    P.op("act", lambda e: e.activation(out=rstd[:], in_=rstd[:], func=AF.Exp, scale=-0.5), reads=[Brstd], writes=[Brstd])


def build_A(nc, P, io, NT=2048):
    xs = P.sbuf("xs", [128, 8, NT], F32)
    Bxs = [P.buf(f"xs{t}") for t in range(NT // 512)]
    gv = P.sbuf("gv", [128, 16], F32); Bgv = P.buf("gv")
    ones1k = P.sbuf("ones1k", [128, 128], F32); Bo1k = P.buf("ones1k")
    P.dma("sp", gv[:, 0:8], io["gv"], writes=[Bgv])
    P.dma("sp", ones1k[:], io["ones1k"], writes=[Bo1k])
    P.op("dve", lambda e: e.memset(gv[:, 8:9], EPS), reads=[Bgv], writes=[Bgv])
    banks = [P.psum(f"bank{i}", [128, 512]) for i in range(2)]
    Bbank = [P.buf(f"bank{i}") for i in range(2)]
    for b_ in Bbank:
        b_.excl = True
    sqs = [(P.sbuf(f"sq{i}", [128, 512], F32), P.buf(f"sq{i}")) for i in range(2)]
    rs = [(P.sbuf(f"rstd{i}", [128, 512], F32), P.buf(f"rstd{i}")) for i in range(2)]
    ho = [(P.sbuf(f"ho{i}", [128, 8, 512], BF16), P.buf(f"ho{i}")) for i in range(2)]
    xv = io["xT"].rearrange("(c p) n -> p c n", p=128)
    hv = io["hT"].rearrange("(c p) n -> p c n", p=128)
    for t in range(NT // 512):
        P.dma("sp", xs[:, :, t * 512:(t + 1) * 512], xv[:, :, t * 512:(t + 1) * 512], writes=[Bxs[t]])
    for t in range(NT // 512):
        def body(t):
            rstd, Brstd = rs[t % 2]
            rms_stats(P, (banks[t % 2], Bbank[t % 2]), None, lambda c: xs[:, c, t * 512:(t + 1) * 512], 8, ones1k, Bo1k, sqs, gv[:, 8:9], Bgv, rstd, Brstd, lambda c: [Bxs[t]])
            h, Bh = ho[t % 2]
            for c in range(8):
                P.op("dve", lambda e, c=c: e.scalar_tensor_tensor(out=h[:, c, :], in0=xs[:, c, t * 512:(t + 1) * 512], scalar=gv[:, c:c + 1], in1=rstd[:], op0=ALU.mult, op1=ALU.mult),
                     reads=[Bxs[t], Bgv, Brstd], writes=[Bh])
            P.dma("sp", hv[:, :, t * 512:(t + 1) * 512], h[:], reads=[Bh])
        body(t)


def build_C(nc, P, io, final, NT=2048):
    NTC = NT // 512
    xs = P.sbuf("xs", [128, 8, NT], F32)
    Bxs = [P.buf(f"xs{t}") for t in range(NTC)]
    gv = P.sbuf("gv", [128, 40], F32); Bgv = P.buf("gv")
    onesA = P.sbuf("onesA", [128, 128], F32); BoA = P.buf("onesA")
    onesC = P.sbuf("onesC", [128, 128], F32); BoC = P.buf("onesC")
    ones1k = P.sbuf("ones1k", [128, 128], F32); Bo1k = P.buf("ones1k")
    P.dma("sp", gv[:, 0:32], io["gv"], writes=[Bgv])
    P.dma("sp", onesA[:], io["onesA"], writes=[BoA])
    P.dma("sp", onesC[:], io["onesC"], writes=[BoC])
    P.dma("sp", ones1k[:], io["ones1k"], writes=[Bo1k])
    P.op("dve", lambda e: e.memset(gv[:, 32:33], EPS), reads=[Bgv], writes=[Bgv])
    eps_ap = gv[:, 32:33]
    banks = [P.psum(f"bank{i}", [128, 512]) for i in range(8)]
    Bbank = [P.buf(f"bank{i}") for i in range(8)]
    for b_ in Bbank:
        b_.excl = True
    rr = [0]

    def nbank():
        i = rr[0] % 8
        rr[0] += 1
        return banks[i], Bbank[i]
    R = P.sbuf("R", [128, 32768], BF16)
    wout = R[:, 0:8192].rearrange("p (c n) -> p c n", c=8); Bwout = P.buf("wout")
    ysb = [R[:, 8192 + i * 4096:8192 + (i + 1) * 4096].rearrange("p (c n) -> p c n", c=8) for i in range(2)]
    Bysb = [P.buf(f"y{i}") for i in range(2)]
    ynb = [R[:, 16384 + i * 4096:16384 + (i + 1) * 4096].rearrange("p (c n) -> p c n", c=8) for i in range(2)]
    Bynb = [P.buf(f"yn{i}") for i in range(2)]
    u = R[:, :].rearrange("p (f n) -> p f n", f=32)
    Bu = [P.buf(f"u{f}") for f in range(32)]
    h2 = P.sbuf("h2", [128, 8, 1024], BF16); Bh2 = [P.buf("h2_0"), P.buf("h2_1")]
    w1b = [(P.sbuf(f"w1b{i}", [128, 8, 512], BF16), P.buf(f"w1b{i}")) for i in range(2)]
    w2b = [(P.sbuf(f"w2b{i}", [128, 4, 512], BF16), P.buf(f"w2b{i}")) for i in range(2)]
    sqs = [(P.sbuf(f"sq{i}", [128, 512], F32), P.buf(f"sq{i}")) for i in range(3)]
    rsA = [(P.sbuf(f"rsA{i}", [128, 512], F32), P.buf(f"rsA{i}")) for i in range(1)] * 2
    rsC = [(P.sbuf(f"rsC{i}", [128, 512], F32), P.buf(f"rsC{i}")) for i in range(1)] * 2
    rs2 = [(P.sbuf(f"rs2{i}", [128, 512], F32), P.buf(f"rs2{i}")) for i in range(2)]
    rl = [(P.sbuf(f"rl{i}", [128, 512], F32), P.buf(f"rl{i}")) for i in range(3)]
    if final:
        st_f = [(P.sbuf(f"stf{i}", [128, 4, 512], F32), P.buf(f"stf{i}")) for i in range(1)]
    else:
        st_b = [(P.sbuf(f"stb{i}", [128, 8, 512], BF16), P.buf(f"stb{i}")) for i in range(1)]

    xv = io["xT"].rearrange("(c p) n -> p c n", p=128)
    for t in range(NTC):
        P.dma("sp", xs[:, :, t * 512:(t + 1) * 512], xv[:, :, t * 512:(t + 1) * 512], writes=[Bxs[t]])
    P.dma("pool", wout, io["wout"].rearrange("(c p) n -> p c n", p=128), writes=[Bwout])
    if "ysrc" in io:
        cand = [(P.sbuf(f"cand{i}", [128, 8, 512], BF16), P.buf(f"cand{i}")) for i in range(1)] * 2
        cand = [(t_[:], b_) for (t_, b_) in cand]
        ohs = P.sbuf("ohs", [128, 4], F32); Bohs = P.buf("ohs")
        P.dma("sp", ohs[:], io["ohs"], writes=[Bohs])

    def outproj(t):
        tsl = slice(t * 512, (t + 1) * 512)
        y, By = ysb[t % 2], Bysb[t % 2]
        yn, Byn = ynb[t % 2], Bynb[t % 2]
        if "ysrc" in io:
            for sI in range(4):
                cd, Bcd = cand[sI % 2]
                for pp in range(2):
                    for hh in range(2):
                        P.dma("sp", cd[pp * 64:(pp + 1) * 64, hh::2, :], io["ysrc"](sI, t, pp, hh), writes=[Bcd])
                if sI == 0:
                    P.op("dve", lambda e, cd=cd: e.tensor_scalar(out=y, in0=cd, scalar1=ohs[:, 0:1], scalar2=None, op0=ALU.mult), reads=[Bcd, Bohs], writes=[By])
                else:
                    P.op("dve", lambda e, cd=cd, sI=sI: e.scalar_tensor_tensor(out=y, in0=cd, scalar=ohs[:, sI:sI + 1], in1=y, op0=ALU.mult, op1=ALU.add), reads=[Bcd, Bohs, By], writes=[By])
        else:
            P.dma("sp", y, io["yT"][:, :, tsl].rearrange("c p n -> p c n"), writes=[By])
        rA, BrA = rsA[t % 2]; rC, BrC = rsC[t % 2]
        bA = nbank()
        rms_stats(P, bA, None, lambda c: y[:, 2 * c, :], 4, onesA, BoA, sqs, eps_ap, Bgv, rA, BrA, lambda c: [By])
        bC = nbank()
        rms_stats(P, bC, None, lambda c: y[:, 2 * c + 1, :], 4, onesC, BoC, sqs, eps_ap, Bgv, rC, BrC, lambda c: [By])
        for g in range(4):
            c0, c1 = 2 * g, 2 * g + 1
            P.op("dve", lambda e, c0=c0: e.scalar_tensor_tensor(out=yn[0:64, c0, :], in0=y[0:64, c0, :], scalar=gv[0:64, 16 + c0:17 + c0], in1=rA[0:64, :], op0=ALU.mult, op1=ALU.mult),
                 reads=[By, Bgv, BrA], writes=[Byn])
            P.op("pool", lambda e, c0=c0: e.tensor_copy(out=yn[64:128, c0, :], in_=y[64:128, c0, :]), reads=[By], writes=[Byn])
            P.op("dve", lambda e, c1=c1: e.scalar_tensor_tensor(out=yn[:, c1, :], in0=y[:, c1, :], scalar=gv[:, 16 + c1:17 + c1], in1=rC[:], op0=ALU.mult, op1=ALU.mult),
                 reads=[By, Bgv, BrC], writes=[Byn])
        for m in range(8):
            bk, Bbk = nbank()
            for c in range(8):
                P.op("pe", lambda e, c=c, m=m, bk=bk: e.matmul(bk[:], lhsT=wout[:, c, m * 128:(m + 1) * 128], rhs=yn[:, c, :], start=(c == 0), stop=(c == 7)),
                     reads=[Bwout, Byn], writes=[Bbk])
            P.op("dve", lambda e, m=m, bk=bk: e.tensor_tensor(out=xs[:, m, tsl], in0=xs[:, m, tsl], in1=bk[:], op=ALU.add), reads=[Bbk, Bxs[t]], writes=[Bxs[t]])
    for t in range(NTC):
        outproj(t)

    w1v = io["w1"].rearrange("(c p) n -> p c n", p=128)
    w2v = io["w2"].rearrange("(f p) n -> p f n", p=128)
    alias_guard = [Bwout] + Bysb + Bynb
    cnt = {"w1": 0, "w2": 0, "rl": 0}

    def ffn_half(hf):
        for tl in range(2):
            t = 2 * hf + tl
            tsl = slice(t * 512, (t + 1) * 512)
            r2, Br2 = rs2[t % 2]
            rms_stats(P, nbank(), None, lambda c, tsl=tsl: xs[:, c, tsl], 8, ones1k, Bo1k, sqs, eps_ap, Bgv, r2, Br2, lambda c, t=t: [Bxs[t]])
            for c in range(8):
                P.op("dve", lambda e, c=c, tsl=tsl, tl=tl, r2=r2: e.scalar_tensor_tensor(out=h2[:, c, tl * 512:(tl + 1) * 512], in0=xs[:, c, tsl], scalar=gv[:, c:c + 1], in1=r2[:], op0=ALU.mult, op1=ALU.mult),
                     reads=[Bxs[t], Bgv, Br2], writes=[Bh2[tl]])
        for fg in range(8):
            w1t, Bw1 = w1b[cnt["w1"] % 2]; cnt["w1"] += 1
            P.dma("pool", w1t[:], w1v[:, :, fg * 512:(fg + 1) * 512], writes=[Bw1])
            for fc in range(4):
                f = fg * 4 + fc
                for tl in range(2):
                    bk, Bbk = nbank()
                    for k in range(8):
                        P.op("pe", lambda e, k=k, fc=fc, tl=tl, bk=bk, w1t=w1t: e.matmul(bk[:], lhsT=w1t[:, k, fc * 128:(fc + 1) * 128], rhs=h2[:, k, tl * 512:(tl + 1) * 512], start=(k == 0), stop=(k == 7)),
                             reads=[Bw1, Bh2[tl]], writes=[Bbk])
                    r, Br = rl[cnt["rl"] % 3]; cnt["rl"] += 1
                    P.op("act", lambda e, bk=bk, r=r: e.activation(out=r[:], in_=bk[:], func=AF.Relu), reads=[Bbk], writes=[Br])
                    extra = alias_guard if (hf == 0) else []
                    eng = "pool" if (cnt["rl"] % 2 == 0) else "dve"
                    P.op(eng, lambda e, f=f, tl=tl, r=r: e.tensor_tensor(out=u[:, f, tl * 512:(tl + 1) * 512], in0=r[:], in1=r[:], op=ALU.mult), reads=[Br], writes=[Bu[f]] + extra)
        for mh in range(2):
            accs = [[nbank() for tl in range(2)] for mm in range(4)]
            for fg in range(8):
                w2t, Bw2 = w2b[cnt["w2"] % 2]; cnt["w2"] += 1
                P.dma("pool", w2t[:], w2v[:, fg * 4:(fg + 1) * 4, mh * 512:(mh + 1) * 512], writes=[Bw2])
                for fc in range(4):
                    f = fg * 4 + fc
                    for mm in range(4):
                        for tl in range(2):
                            bk, Bbk = accs[mm][tl]
                            P.op("pe", lambda e, fc=fc, f=f, mm=mm, tl=tl, bk=bk, w2t=w2t: e.matmul(bk[:], lhsT=w2t[:, fc, mm * 128:(mm + 1) * 128], rhs=u[:, f, tl * 512:(tl + 1) * 512], start=(f == 0), stop=(f == 31)),
                                 reads=[Bw2, Bu[f]], writes=[Bbk])
            for mm in range(4):
                m = mh * 4 + mm
                for tl in range(2):
                    t = 2 * hf + tl
                    tsl = slice(t * 512, (t + 1) * 512)
                    bk, Bbk = accs[mm][tl]
                    P.op("dve", lambda e, m=m, tsl=tsl, bk=bk: e.tensor_tensor(out=xs[:, m, tsl], in0=xs[:, m, tsl], in1=bk[:], op=ALU.add), reads=[Bbk, Bxs[t]], writes=[Bxs[t]])
    for hf in range(NTC // 2):
        ffn_half(hf)

    def tail(t):
        tsl = slice(t * 512, (t + 1) * 512)
        r2, Br2 = rs2[t % 2]
        rms_stats(P, nbank(), None, lambda c: xs[:, c, tsl], 8, ones1k, Bo1k, sqs, eps_ap, Bgv, r2, Br2, lambda c: [Bxs[t]])
        if final:
            so, Bso = st_f[0]
            for ch in range(2):
                for c in range(4 * ch, 4 * ch + 4):
                    P.op("dve", lambda e, c=c: e.scalar_tensor_tensor(out=so[:, c % 4, :], in0=xs[:, c, tsl], scalar=gv[:, 8 + c:9 + c], in1=r2[:], op0=ALU.mult, op1=ALU.mult),
                         reads=[Bxs[t], Bgv, Br2], writes=[Bso])
                P.dma("sp", io["oT"].rearrange("(c p) n -> p c n", p=128)[:, 4 * ch:4 * ch + 4, tsl], so[:], reads=[Bso])
        else:
            so, Bso = st_b[0]
            for c in range(8):
                P.op("dve", lambda e, c=c: e.scalar_tensor_tensor(out=so[:, c, :], in0=xs[:, c, tsl], scalar=gv[:, 8 + c:9 + c], in1=r2[:], op0=ALU.mult, op1=ALU.mult),
                     reads=[Bxs[t], Bgv, Br2], writes=[Bso])
            P.dma("sp", io["hnT"].rearrange("(c p) n -> p c n", p=128)[:, :, tsl], so[:], reads=[Bso])
            P.dma("sp", io["xnT"].rearrange("(c p) n -> p c n", p=128)[:, :, tsl], xs[:, :, tsl], reads=[Bxs[t]], sem_buf=Bso)
    for t in range(NTC):
        tail(t)


import ml_dtypes
from contextlib import ExitStack
from concourse.bass_utils import run_bass_kernel_spmd

_BF = ml_dtypes.bfloat16
SEQ = 8192
ALL_SLOPES = [2.0 ** (-8.0 * (h + 1) / 8) for h in range(8)]
HEAD_A = lambda g: g
HEAD_B = lambda g: 4 + g
MAXDIST = (2432, None)
GROUPS = [[0, 1, 2, 3], [4, 5, 6, 7]]
NPH = 4
NPY = 4

_cache = {}
B_CONST = ["ohk", "crow", "abias", "cmask", "hmask", "rmask", "ident", "ones64"]
C_CONST = ["onesA", "onesC", "ones1k"]


def _build_fused(seq):
    ntok = seq // 4
    nc = bass.Bass("TRN2", target_bir_lowering=False)
    io = {}

    def din(name, shape, dt):
        io[name] = nc.dram_tensor(name, list(shape), dt, kind="ExternalInput").ap()
    T = seq
    NM = T // 128 + 4
    din("xT", [1024, ntok], F32); din("ohs", [128, 4], F32); din("gvA", [128, 8], F32)
    for l in range(2):
        din(f"wfm{l}", [1024, 640], F32); din(f"wtm{l}", [1024, 192], F32); din(f"par{l}", [128, 16], F32); din(f"wg{l}", [128, 128], F32)
        din(f"wout{l}", [1024, 1024], F32); din(f"w1{l}", [1024, 4096], F32); din(f"w2{l}", [4096, 1024], F32); din(f"gvC{l}", [128, 32], F32)
    din("ohk", [33, T], BF16); din("crow", [2, T], BF16); din("abias", [128, 2, NM], F32)
    din("cmask", [128, 128], BF16); din("hmask", [128, 512], F32); din("rmask", [64, 512], F32)
    din("ident", [128, 128], BF16); din("ones64", [64, 64], F32)
    din("onesA", [128, 128], F32); din("onesC", [128, 128], F32); din("ones1k", [128, 128], F32)
    oT = nc.dram_tensor("oT", [1024, ntok], F32, kind="ExternalOutput").ap()
    hloc = [nc.dram_tensor(f"hloc{l}", [1024, ntok], BF16) for l in range(2)]
    hall = [nc.dram_tensor(f"hall{l}", [NPH, 4 * (1024 // NPH), ntok], BF16) for l in range(2)]
    yloc = [nc.dram_tensor(f"yloc{l}", [256, T], BF16) for l in range(2)]
    yall = [nc.dram_tensor(f"yall{l}", [NPY, 4 * (256 // NPY), T], BF16) for l in range(2)]
    xres = nc.dram_tensor("xres", [1024, ntok], F32)
    with ExitStack() as st:
        P = Prog(nc, st)
        P.push_scope("A_")
        build_A(nc, P, dict(xT=io["xT"], gv=io["gvA"], ones1k=io["ones1k"], hT=hloc[0].ap()), ntok)
        P.pop_scope()
        for l in range(2):
            P.barrier()
            rh = 1024 // NPH
            for j in range(NPH):
                P.coll("AllGather", hloc[l].ap()[j * rh:(j + 1) * rh, :].opt(), hall[l].ap()[j].opt(), GROUPS)
            P.barrier()
            P.push_scope(f"B{l}_")
            ioB = dict(wfm=io[f"wfm{l}"], wtm=io[f"wtm{l}"], par=io[f"par{l}"], wg=io[f"wg{l}"])
            for k in B_CONST:
                ioB[k] = io[k]
            ioB["yT"] = yloc[l].ap().rearrange("(a p) n -> a p n", a=4)
            hv = hall[l].ap().rearrange("j (s cc p) n -> j s p cc n", s=4, p=128)

            def hsrc(blk, j, hv=hv):
                tok = blk * 512
                s, lc = tok // ntok, tok % ntok
                return hv[j][s][:, :, lc:lc + 512]
            ioB["hsrc"] = hsrc
            build_B(nc, P, T, ioB, maxdist=MAXDIST)
            P.pop_scope()
            P.barrier()
            ry = 256 // NPY
            for j in range(NPY):
                P.coll("AllGather", yloc[l].ap()[j * ry:(j + 1) * ry, :].opt(), yall[l].ap()[j].opt(), GROUPS)
            P.barrier()
            P.push_scope(f"C{l}_")
            final = (l == 1)
            ioC = dict(xT=(io["xT"] if l == 0 else xres.ap()), wout=io[f"wout{l}"], w1=io[f"w1{l}"], w2=io[f"w2{l}"], gv=io[f"gvC{l}"], ohs=io["ohs"])
            for k in C_CONST:
                ioC[k] = io[k]
            yv = yall[l].ap().rearrange("j (g r) n -> j r g n", g=4)

            def ysrc(sI, t, pp, h, yv=yv):
                o = sI * ntok + t * 512
                return yv[2 * h + pp][:, :, o:o + 512]
            ioC["ysrc"] = ysrc
            if final:
                ioC["oT"] = oT
            else:
                ioC["xnT"] = xres.ap()
                ioC["hnT"] = hloc[1].ap()
            build_C(nc, P, ioC, final, ntok)
            P.pop_scope()
        n_ops = {e: len(v) for e, v in P.ops.items()}
        print("fused program ops", n_ops, "dsems", len(P.dsems), flush=True)
        P.finish()
    return nc


def _chunks(v):
    return np.ascontiguousarray(np.asarray(v, np.float32).reshape(8, 128).T)


def _b_weights(l, g, inp):
    w_in = inp["w_in"][l]
    hA, hB = HEAD_A(g), HEAD_B(g)
    sl = lambda base, w, i: w_in[:, base + w * i: base + w * i + w]
    wfm = np.zeros((1024, 640), np.float32)
    wfm[:, 0:64] = sl(512, 64, g)
    wfm[:, 64:128] = sl(0, 64, g)
    wfm[:, 128:192] = sl(768, 64, g)
    wfm[:, 192:256] = sl(256, 64, g)
    wfm[:, 256:320] = sl(1280, 64, g)
    wfm[:, 320:384] = sl(1536, 64, hA)
    wfm[:, 384:448] = sl(1536, 64, hB)
    wfm[:, 448:512] = sl(2048, 64, hA)
    wfm[:, 512:576] = sl(2048, 64, hB)
    wtm = np.concatenate([sl(1024, 64, g), sl(2560, 64, hA), sl(2560, 64, hB)], axis=1)
    par = np.zeros((128, 16), np.float32)
    ch = slice(64 * g, 64 * g + 64)
    par[64:, 0:4] = inp["lru_conv_w"][l][:, ch].T
    par[64:, 4] = inp["lru_conv_b"][l][ch]
    par[64:, 5] = inp["lru_ba"][l][ch]
    par[64:, 6] = inp["lru_bx"][l][ch]
    par[64:, 7] = inp["lru_lambda"][l][ch]
    par[:64, 0] = inp["hg_lower_bounds"][0][ch]
    par[:64, 1] = inp["hg_lower_bounds"][1][ch]
    par[:64, 2] = inp["hg_norm_w"][l]
    par[:64, 3] = float(l)
    wg = np.zeros((128, 128), np.float32)
    wg[64:, 0:64] = inp["lru_wa"][l][g]
    wg[64:, 64:128] = inp["lru_wx"][l][g]
    return {f"wfm{l}": wfm, f"wtm{l}": np.ascontiguousarray(wtm), f"par{l}": par, f"wg{l}": wg}


def _c_weights(l, inp, next_gain):
    w_out = inp["w_out"][l]
    rows = []
    gy = np.ones((128, 8), np.float32)
    for g in range(4):
        hA, hB = HEAD_A(g), HEAD_B(g)
        rows += list(range(64 * g, 64 * g + 64)) + list(range(256 + 64 * g, 256 + 64 * g + 64))
        rows += list(range(512 + 64 * hA, 512 + 64 * hA + 64)) + list(range(512 + 64 * hB, 512 + 64 * hB + 64))
        gy[0:64, 2 * g] = inp["lru_out_norm"][l][64 * g:64 * g + 64]
        gy[0:64, 2 * g + 1] = inp["att_out_norm"][l][64 * hA:64 * hA + 64]
        gy[64:128, 2 * g + 1] = inp["att_out_norm"][l][64 * hB:64 * hB + 64]
    gv = np.zeros((128, 32), np.float32)
    gv[:, 0:8] = _chunks(inp["norm_mlp"][l])
    gv[:, 8:16] = _chunks(next_gain)
    gv[:, 16:24] = gy
    return {f"wout{l}": np.ascontiguousarray(w_out[rows, :]), f"w1{l}": np.ascontiguousarray(inp["w_ff1"][l]),
            f"w2{l}": np.ascontiguousarray(inp["w_ff2"][l]), f"gvC{l}": gv}


def kernel(**inputs):
    inp = {k: np.asarray(v) for k, v in inputs.items()}
    x = inp["x"].astype(np.float32, copy=False)
    B, seq = x.shape[0], x.shape[1]
    ntok = seq // 4
    cores = list(range(8))
    if ("F", seq) not in _cache:
        _cache[("F", seq)] = _build_fused(seq)
    nc = _cache[("F", seq)]
    shared = {}
    shared.update(host_consts_C())
    shared["gvA"] = _chunks(inp["norm_mix"][0])
    for l in range(2):
        shared.update(_c_weights(l, inp, inp["norm_final"] if l == 1 else inp["norm_mix"][l + 1]))
    in_maps = []
    for c in cores:
        b, s = c // 4, c % 4
        d = dict(shared)
        d["xT"] = np.ascontiguousarray(x[b, s * ntok:(s + 1) * ntok, :].T)
        oh = np.zeros((128, 4), np.float32)
        oh[:, s] = 1.0
        d["ohs"] = oh
        d.update(host_consts_B(seq, [ALL_SLOPES[HEAD_A(s)], ALL_SLOPES[HEAD_B(s)]]))
        for l in range(2):
            d.update(_b_weights(l, s, inp))
        in_maps.append(d)
    res = run_bass_kernel_spmd(nc, in_maps, core_ids=cores)
    out = np.empty((B, seq, 1024), np.float32)
    for c in cores:
        b, s = c // 4, c % 4
        out[b, s * ntok:(s + 1) * ntok, :] = np.asarray(res.results[c]["oT"]).T
    return out
```

```python
import numpy as np
import concourse.bass as bass
import concourse.mybir as mybir

F32 = mybir.dt.float32
BF16 = mybir.dt.bfloat16
AF = mybir.ActivationFunctionType
ALU = mybir.AluOpType
AX = mybir.AxisListType

ENGS = ("pe", "act", "dve", "pool", "sp")


class Buf:
    __slots__ = ("name", "w", "r", "dsem", "excl")

    def __init__(self, name):
        self.name = name
        self.w = None
        self.r = []
        self.dsem = None
        self.excl = False


class DmaSem:
    __slots__ = ("sem", "issued", "group_open", "name", "unit", "kind")

    def __init__(self, sem, name, unit=16):
        self.sem = sem
        self.issued = 0
        self.group_open = False
        self.name = name
        self.unit = unit
        self.kind = "hw"


class Prog:
    def __init__(self, nc, stack):
        self.nc = nc
        self.stack = stack
        self.ops = {e: [] for e in ENGS}
        self.cnt = {e: 0 for e in ENGS}
        self.esem = {}
        for e in ("pe", "act", "dve", "pool"):
            self.esem[e] = stack.enter_context(nc.semaphore("s_" + e))
        self.known = {e: {} for e in ENGS}
        self.dsems = []
        self.csems = []
        self.free_dsems = []
        self.scopes = []
        self.pending = {}
        self.nbuf = 0
        import os
        self.limit = int(os.environ.get("MK_LIMIT", "0"))
        self.nrec = 0
        self.lastdesc = None

    def buf(self, name=None):
        self.nbuf += 1
        return Buf(name or f"b{self.nbuf}")

    def _st(self):
        return self.scopes[-1][0] if self.scopes else self.stack

    def _nm(self, name):
        return (self.scopes[-1][1] + name) if self.scopes else name

    def push_scope(self, prefix):
        from contextlib import ExitStack
        self.scopes.append((ExitStack(), prefix))

    def pop_scope(self):
        st, _ = self.scopes.pop()
        st.close()

    def sbuf(self, name, shape, dtype):
        t = self._st().enter_context(self.nc.sbuf_tensor("sb_" + self._nm(name), list(shape), dtype))
        return t

    def psum(self, name, shape, dtype=F32):
        t = self._st().enter_context(self.nc.psum_tensor("ps_" + self._nm(name), list(shape), dtype))
        return t

    def new_dsem(self, name, kind="hw"):
        for i, d in enumerate(self.free_dsems):
            if d.kind == kind:
                self.free_dsems.pop(i)
                d.group_open = False
                return d
        s = self.stack.enter_context(self.nc.semaphore("d_" + self._nm(name)))
        d = DmaSem(s, name)
        d.kind = kind
        self.dsems.append(d)
        return d

    def barrier(self):
        pend = []
        for e in ("pe", "act", "dve", "pool"):
            if self.cnt[e] > 0:
                pend.append(("eng", e, self.cnt[e]))
        for ds in self.dsems + self.csems:
            if ds.issued:
                pend.append(("dma", ds, ds.unit * ds.issued))
        for q in ENGS:
            self.pending[q] = list(pend)
        self.free_dsems = list(self.dsems)

    def _pend(self, q, waits):
        for dep in self.pending.pop(q, []):
            if dep[0] == "eng" and dep[1] == q and q == "pe":
                continue
            self._need(q, dep, waits)

    def coll(self, kind, in_ap, out_ap, groups):
        s = self.stack.enter_context(self.nc.semaphore("c_%d" % len(self.csems)))
        d = DmaSem(s, "coll", unit=1)
        waits = []
        self._pend("pool", waits)
        d.issued = 1
        self.csems.append(d)

        def fn(e):
            return e.collective_compute(kind, mybir.AluOpType.bypass, replica_groups=groups, ins=[in_ap], outs=[out_ap])
        self.ops["pool"].append((self._merge(waits), fn, (d.sem, None)))

    def _merge(self, waits):
        m = {}
        for s, v in waits:
            k = id(s)
            if k not in m or m[k][1] < v:
                m[k] = (s, v)
        return list(m.values())

    def _need(self, eng, dep, waits):
        if dep is None:
            return
        if dep[0] == "eng":
            _, e, idx = dep
            if e == eng and eng in ("pe",):
                return
            key = ("e", e)
            if self.known[eng].get(key, 0) >= idx:
                return
            if e == eng and False:
                return
            self.known[eng][key] = idx
            waits.append((self.esem[e], idx))
        else:
            _, ds, val = dep
            val = max(val, ds.unit * ds.issued)
            ds.group_open = False
            key = ("d", id(ds))
            if self.known[eng].get(key, 0) >= val:
                return
            self.known[eng][key] = val
            waits.append((ds.sem, val))

    def _deps(self, eng, reads, writes):
        waits = []
        for b in reads:
            self._need(eng, b.w, waits)
        for b in writes:
            self._need(eng, b.w, waits)
            for r in b.r:
                self._need(eng, r, waits)
        m = {}
        for s, v in waits:
            k = id(s)
            if k not in m or m[k][1] < v:
                m[k] = (s, v)
        return list(m.values())

    def op(self, eng, fn, reads=(), writes=()):
        self.nrec += 1
        if self.limit and self.nrec > self.limit:
            return
        import traceback
        self.lastdesc = (self.nrec, eng, traceback.extract_stack(limit=3)[0].lineno, [b.name for b in reads], [b.name for b in writes])
        writes = list(writes) + [b for b in reads if b.excl and b not in writes]
        waits = []
        self._pend(eng, waits)
        waits = self._merge(waits + self._deps(eng, reads, writes))
        self.cnt[eng] += 1
        idx = self.cnt[eng]
        tag = ("eng", eng, idx)
        for b in reads:
            if b not in writes:
                b.r.append(tag)
        for b in writes:
            b.w = tag
            b.r = []
        self.ops[eng].append((waits, fn, (self.esem[eng], 1)))

    def dma(self, q, out_ap, in_ap, reads=(), writes=(), sem_buf=None, **kw):
        self.nrec += 1
        if self.limit and self.nrec > self.limit:
            return
        sb = sem_buf
        if sb is None:
            for b in list(writes) + list(reads):
                sb = b
                break
        if sb.dsem is None:
            sb.dsem = self.new_dsem(sb.name, "sw" if q == "pool" else "hw")
        ds = sb.dsem
        waits = []
        self._pend(q, waits)
        waits = waits + self._deps(q, reads, writes)
        if (not ds.group_open) and ds.issued > 0:
            w2 = []
            self._need(q, ("dma", ds, 16 * ds.issued), w2)
            waits += w2
        ds.issued += 1
        ds.group_open = True
        tag = ("dma", ds, 16 * ds.issued)
        for b in reads:
            b.r.append(tag)
        for b in writes:
            b.w = tag
            b.r = []

        def fn(e, out_ap=out_ap, in_ap=in_ap, kw=kw):
            return e.dma_start(out=out_ap, in_=in_ap, **kw)
        if q in ("pool", "act"):
            pass
        self.ops[q].append((self._merge(waits), fn, (ds.sem, 16)))

    def finish(self):
        fin = []
        for ds in self.dsems + self.csems:
            if ds.issued:
                fin.append((ds.sem, ds.unit * ds.issued))
        nc = self.nc
        ops = self.ops
        with nc.Block() as block:
            def emit(e, lst, final=None):
                for waits, fn, inc in lst:
                    for s, v in waits:
                        e.wait_ge(s, v)
                    ins = fn(e)
                    if inc is not None:
                        if inc[1] is None:
                            ins.then_inc(inc[0])
                        else:
                            ins.then_inc(inc[0], inc[1])
                if final:
                    for s, v in final:
                        e.wait_ge(s, v)

            @block.sync
            def _(e):
                emit(e, ops["sp"], fin)

            @block.tensor
            def _(e):
                emit(e, ops["pe"])

            @block.scalar
            def _(e):
                emit(e, ops["act"])

            @block.vector
            def _(e):
                emit(e, ops["dve"])

            @block.gpsimd
            def _(e):
                emit(e, ops["pool"])


BIG = 30000.0
NEGF = -1.0e30
LN2 = 0.6931471805599453
GC = 0.7978845608028654


def host_consts_B(T, slopes2):
    import ml_dtypes
    bf = ml_dtypes.bfloat16
    nb = T // 256
    c = {}
    oh = np.zeros((33, T), np.float32)
    for n in range(nb):
        oh[n, n * 256:(n + 1) * 256] = 1.0
    oh[32, :] = 1.0
    c["ohk"] = oh.astype(bf)
    t = np.arange(T) % 512
    c["crow"] = np.stack([-8.0 * s * t for s in slopes2]).astype(np.float32).astype(bf)
    NM = T // 128 + 4
    m = np.arange(NM)
    p = np.arange(128)
    tab = np.zeros((128, 2, NM), np.float32)
    for h in range(2):
        tab[:, h, :] = slopes2[h] * (p[:, None] + 128.0 * (m[None, :] - (T // 128)))
    c["abias"] = tab
    c["cmask"] = (np.arange(128)[None, :] >= np.arange(128)[:, None]).astype(np.float32).astype(bf)
    cm = (np.arange(64)[None, :] >= np.arange(64)[:, None]).astype(np.float32)
    bm = np.zeros((128, 128), np.float32)
    bm[:64, :64] = cm
    bm[64:, 64:] = cm
    c["hmask"] = np.tile(bm, (1, 4)).astype(np.float32)
    rm = np.ones((64, 512), np.float32)
    rm[:, ::64] = 0.0
    c["rmask"] = rm
    c["ident"] = np.eye(128, dtype=np.float32).astype(bf)
    c["ones64"] = np.full((64, 64), 1.0 / 64.0, np.float32)
    return c


def build_B(nc, P, T, io, maxdist=(None, None)):
    NB = T // 512
    NM = T // 128 + 4
    MOFF = T // 128
    hT, yT = io.get("hT"), io["yT"]
    wfm = P.sbuf("wfm", [128, 8, 640], BF16); Bwfm = P.buf("wfm")
    wtm = P.sbuf("wtm", [128, 8, 192], BF16); Bwtm = P.buf("wtm")
    par = P.sbuf("par", [128, 32], F32); Bpar = P.buf("par")
    wg = P.sbuf("wg", [128, 128], BF16); Bwg = P.buf("wg")
    abias = P.sbuf("abias", [128, 2, NM], F32); Bab = P.buf("abias")
    cmask = P.sbuf("cmask", [128, 128], BF16); Bcm = P.buf("cmask")
    hmask = P.sbuf("hmask", [128, 512], F32); Bhm = P.buf("hmask")
    rmask = P.sbuf("rmask", [64, 512], F32); Brm = P.buf("rmask")
    ident = P.sbuf("ident", [128, 128], BF16); Bid = P.buf("ident")
    ones64 = P.sbuf("ones64", [64, 64], F32); Bo64 = P.buf("ones64")
    QA = P.sbuf("QA", [128, T], BF16); QB = P.sbuf("QB", [128, T], BF16)
    KA = P.sbuf("KA", [128, T], BF16); KB = P.sbuf("KB", [128, T], BF16)
    NBK = T // 512
    BQ = [[P.buf(f"Q{h}_{i}") for i in range(NBK)] for h in range(2)]
    BK = [[P.buf(f"K{h}_{i}") for i in range(NBK)] for h in range(2)]
    Qh = [QA, QB]; Kh = [KA, KB]
    VV = P.sbuf("VV", [128, T // 128, 2, 65], BF16); BV = [P.buf(f"VV{i}") for i in range(NBK)]
    VH = P.sbuf("VH", [128, T // 128, 64], BF16); BVH = [P.buf(f"VH{i}") for i in range(NBK)]
    ones64r = P.sbuf("ones64r", [128, 64], F32); Bo64r = P.buf("ones64r")
    kmT = P.sbuf("kmT", [128, 32], BF16); Bkm = P.buf("kmT")
    banks = [P.psum(f"bank{i}", [128, 512]) for i in range(8)]
    Bbank = [P.buf(f"bank{i}") for i in range(8)]
    for b_ in Bbank:
        b_.excl = True
    rr = [0]

    def nbank():
        i = rr[0] % 4
        rr[0] += 1
        return banks[i], Bbank[i]
    rr2 = [0]

    def nbank2():
        i = 4 + rr2[0] % 2
        rr2[0] += 1
        return banks[i], Bbank[i]
    pvb = [(banks[6], Bbank[6]), (banks[7], Bbank[7])]

    P.dma("pool", wfm[:], io["wfm"].rearrange("(c p) n -> p c n", p=128), writes=[Bwfm])
    P.dma("pool", wtm[:], io["wtm"].rearrange("(c p) n -> p c n", p=128), writes=[Bwtm])
    P.dma("pool", wg[:], io["wg"], writes=[Bwg])
    P.dma("sp", par[:, 0:16], io["par"], writes=[Bpar])
    P.dma("sp", abias[:], io["abias"], writes=[Bab])
    P.dma("sp", cmask[:], io["cmask"], writes=[Bcm])
    P.dma("sp", hmask[:], io["hmask"], writes=[Bhm])
    P.dma("sp", rmask[:], io["rmask"], writes=[Brm])
    P.dma("sp", ident[:], io["ident"], writes=[Bid])
    P.dma("sp", ones64[:], io["ones64"], writes=[Bo64])
    for h in range(2):
        P.op("pool", lambda e, h=h: e.memset(Qh[h][:], 0.0), writes=BQ[h])
        P.op("pool", lambda e, h=h: e.memset(Kh[h][:], 0.0), writes=BK[h])
    P.dma("sp", KA[64:97, :], io["ohk"], writes=BK[0], sem_buf=BK[0][0])
    P.dma("sp", KB[0:33, :], io["ohk"], writes=BK[1], sem_buf=BK[1][0])
    P.dma("sp", QA[96:97, :], io["crow"][0:1, :], writes=BQ[0], sem_buf=BQ[0][0])
    P.dma("sp", QB[32:33, :], io["crow"][1:2, :], writes=BQ[1], sem_buf=BQ[1][0])
    P.op("pool", lambda e: e.memset(VV[:, :, :, 64:65], 1.0), writes=BV)
    P.op("pool", lambda e: e.memset(ones64r[:], 1.0), writes=[Bo64r])
    P.op("pool", lambda e: e.memset(kmT[:], 0.0), writes=[Bkm])

    L = slice(64, 128)
    H = slice(0, 64)
    def pc(rows, j):
        return par[rows, j:j + 1]
    P.op("dve", lambda e: e.memset(par[:, 24:25], -LN2), reads=[Bpar], writes=[Bpar])
    P.op("dve", lambda e: e.memset(par[:, 25:26], 1e-6), reads=[Bpar], writes=[Bpar])
    P.op("dve", lambda e: e.memset(par[:, 26:27], 1.0), reads=[Bpar], writes=[Bpar])
    P.op("dve", lambda e: e.tensor_scalar(out=par[L, 8:10], in0=par[L, 5:7], scalar1=0.5, scalar2=None, op0=ALU.mult), reads=[Bpar], writes=[Bpar])
    P.op("act", lambda e: e.activation(out=pc(L, 12), in_=pc(L, 7), func=AF.Exp, scale=-1.0), reads=[Bpar], writes=[Bpar])
    P.op("act", lambda e: e.activation(out=pc(L, 13), in_=pc(L, 12), func=AF.Ln, bias=pc(L, 26)), reads=[Bpar], writes=[Bpar])
    P.op("dve", lambda e: e.tensor_scalar(out=pc(L, 10), in0=pc(L, 13), scalar1=-8.0, scalar2=None, op0=ALU.mult), reads=[Bpar], writes=[Bpar])
    P.op("dve", lambda e: e.tensor_scalar(out=pc(L, 11), in0=pc(L, 13), scalar1=-4.0, scalar2=None, op0=ALU.mult), reads=[Bpar], writes=[Bpar])
    P.op("dve", lambda e: e.tensor_tensor(out=pc(H, 20), in0=pc(H, 1), in1=pc(H, 0), op=ALU.subtract), reads=[Bpar], writes=[Bpar])
    P.op("act", lambda e: e.activation(out=pc(H, 21), in_=pc(H, 20), func=AF.Tanh, scale=0.5), reads=[Bpar], writes=[Bpar])
    P.op("dve", lambda e: e.tensor_scalar(out=pc(H, 22), in0=pc(H, 21), scalar1=0.5, scalar2=0.5, op0=ALU.mult, op1=ALU.add), reads=[Bpar], writes=[Bpar])
    P.op("dve", lambda e: e.tensor_tensor(out=pc(H, 19), in0=pc(H, 22), in1=pc(H, 3), op=ALU.mult), reads=[Bpar], writes=[Bpar])
    P.op("dve", lambda e: e.tensor_scalar(out=pc(H, 16), in0=pc(H, 19), scalar1=-0.5, scalar2=0.5, op0=ALU.mult, op1=ALU.add), reads=[Bpar], writes=[Bpar])
    P.op("dve", lambda e: e.tensor_scalar(out=pc(H, 18), in0=pc(H, 16), scalar1=-1.0, scalar2=None, op0=ALU.mult), reads=[Bpar], writes=[Bpar])
    P.op("dve", lambda e: e.tensor_scalar(out=pc(H, 23), in0=pc(H, 19), scalar1=1e-30, scalar2=None, op0=ALU.max), reads=[Bpar], writes=[Bpar])
    P.op("dve", lambda e: e.tensor_tensor(out=pc(H, 17), in0=pc(H, 23), in1=pc(H, 16), op=ALU.add), reads=[Bpar], writes=[Bpar])

    def rot(name, shape, dt, n):
        ts = [P.sbuf(f"{name}{i}", shape, dt) for i in range(n)]
        bs = [P.buf(f"{name}{i}") for i in range(n)]
        return ts, bs
    hblk, Bhblk = rot("hblk", [128, 8, 512], BF16, 2)
    xbuf, Bxbuf = rot("xbuf", [128, 515], F32, 2)
    NW = 2
    W = {}

    class HV:
        def __init__(self, t):
            self.t = t

        def __getitem__(self, key):
            if isinstance(key, tuple):
                return self.t[(slice(0, 64),) + tuple(key[1:])]
            return self.t[0:64, :]
    share = {"thq": "ysb", "thf": "t1", "thg": "t2", "gs2": "xc", "fg": "thr", "kk": "thi", "bb": "aa", "eb": "a2", "enb": "uu",
             "qs2": "hh", "kve": "sh1", "osb": "sh2", "osq": "sh3", "rstd": "sh4"}
    for nm, shp, dt in [("ysb", [128, 512], F32), ("t1", [128, 512], F32), ("t2", [128, 512], F32),
                        ("xc", [128, 512], F32), ("xcb", [128, 512], BF16), ("thr", [128, 512], F32), ("thi", [128, 512], F32),
                        ("aa", [128, 512], F32), ("a2", [128, 512], F32), ("uu", [128, 512], F32), ("hh", [128, 512], F32),
                        ("sh1", [128, 512], F32), ("sh2", [128, 512], F32), ("sh3", [128, 512], F32), ("sh4", [128, 512], F32),
                        ("yo", [128, 512], BF16),
                        ("qt", [64, 512], BF16), ("kt", [64, 512], BF16),
                        ("ktokE", [128, 4, 64], BF16), ("ktokO", [128, 4, 64], BF16), ("attm", [128, 512], BF16), ("ebl", [64, 8], F32),
                        ("hy", [64, 512], BF16),
                        ("gsb", [128, 32], F32), ("top8", [128, 8], F32), ("mp", [128, 32], BF16),
                        ("pt", [128, 512], BF16), ("onum", [65, 512], F32), ("rec", [65, 512], F32), ("my", [64, 512], BF16)]:
        n = {"pt": 7, "gsb": 4, "top8": 4, "mp": 4, "sh1": 1, "sh2": 1, "sh3": 1, "sh4": 1, "rec": 1}.get(nm, NW)
        W[nm] = rot(nm, shp, dt, n)
    for hn, ln in share.items():
        ts, _ = W[ln]
        W[hn] = ([HV(t) for t in ts], [P.buf(f"{hn}{i}") for i in range(len(ts))])
    ctr = {}

    def wt(nm):
        i = ctr.get(nm, 0)
        ctr[nm] = i + 1
        ts, bs = W[nm]
        return ts[i % len(ts)], bs[i % len(bs)]
    for i in range(4):
        P.op("pool", lambda e, i=i: e.memset(W["gsb"][0][i][:], NEGF), writes=[W["gsb"][1][i]])
    for nm_ in ("ktokE", "ktokO"):
        for i in range(NW):
            P.op("pool", lambda e, nm_=nm_, i=i: e.memset(W[nm_][0][i][:], 0.0), writes=[W[nm_][1][i]])
    Sst = P.sbuf("Sst", [64, 64], F32); BS = P.buf("Sst")
    Sbf, BSbf = rot("Sbf", [64, 64], BF16, 4)
    P.op("dve", lambda e: e.memset(Sst[:], 0.0), writes=[BS])
    P.op("dve", lambda e: e.memset(xbuf[1][L, 512:515], 0.0), writes=[Bxbuf[1]])
    hprev = [None]

    S1 = {}

    class Deferred:
        def __init__(self):
            self.q = []
            self.atomic = None

        def op(self, *a, **k):
            (self.atomic if self.atomic is not None else self.q).append(("op", a, k))

        def dma(self, *a, **k):
            (self.atomic if self.atomic is not None else self.q).append(("dma", a, k))

        def begin(self):
            self.atomic = []

        def end(self):
            self.q.append(("grp", self.atomic, None))
            self.atomic = None

        def flush(self, n=None):
            while self.q and (n is None or n > 0):
                kind, a, k = self.q.pop(0)
                items = a if kind == "grp" else [(kind, a, k)]
                for kd, aa, kk in items:
                    (P.op if kd == "op" else P.dma)(*aa, **kk)
                if n is not None:
                    n -= len(items)
    PQ = Deferred()

    def stage1(blk):
        c0 = blk * 512
        hb, Bhb = hblk[blk % 2], Bhblk[blk % 2]
        if "hsrc" in io:
            for j in range(4):
                P.dma("sp", hb[:, 2 * j:2 * j + 2, :], io["hsrc"](blk, j), writes=[Bhb])
        else:
            P.dma("sp", hb[:], hT[:, c0:c0 + 512].rearrange("(c p) n -> p c n", p=128), writes=[Bhb])

        def inproj(col0, M, bank, Bb, rows=slice(0, 128)):
            for k in range(8):
                P.op("pe", lambda e, k=k: e.matmul(bank[rows, :], lhsT=wfm[:, k, col0:col0 + M], rhs=hb[:, k, :], start=(k == 0), stop=(k == 7)),
                     reads=[Bwfm, Bhb], writes=[Bb])
        b4, Bb4 = nbank(); inproj(320, 128, b4, Bb4)
        b5, Bb5 = nbank(); inproj(448, 128, b5, Bb5)
        P.op("act", lambda e: e.activation(out=QA[0:64, c0:c0 + 512], in_=b4[0:64, :], func=AF.Identity), reads=[Bb4], writes=[BQ[0][blk]])
        P.op("dve", lambda e: e.tensor_copy(out=QB[64:128, c0:c0 + 512], in_=b4[64:128, :]), reads=[Bb4], writes=[BQ[1][blk]])
        P.op("act", lambda e: e.activation(out=KA[0:64, c0:c0 + 512], in_=b5[0:64, :], func=AF.Identity), reads=[Bb5], writes=[BK[0][blk]])
        P.op("dve", lambda e: e.tensor_copy(out=KB[64:128, c0:c0 + 512], in_=b5[64:128, :]), reads=[Bb5], writes=[BK[1][blk]])
        kms, Bkms = wt("top8")
        P.op("dve", lambda e: e.tensor_reduce(out=kms[:, 0:2], in_=b5[:].rearrange("p (n k) -> p n k", n=2), axis=AX.X, op=ALU.add), reads=[Bb5], writes=[Bkms])
        P.op("dve", lambda e: e.tensor_scalar(out=kmT[:, 2 * blk:2 * blk + 2], in0=kms[:, 0:2], scalar1=1.0 / 256.0, scalar2=None, op0=ALU.mult), reads=[Bkms], writes=[Bkm])
        for pr in range(2):
            bv, Bbv = nbank()
            for tt in (2 * pr, 2 * pr + 1):
                o = (tt % 2) * 192
                for k in range(8):
                    P.op("pe", lambda e, k=k, tt=tt, o=o, bv=bv: e.matmul(bv[:, o:o + 192], lhsT=hb[:, k, tt * 128:(tt + 1) * 128], rhs=wtm[:, k, :], start=(k == 0), stop=(k == 7)),
                         reads=[Bwtm, Bhb], writes=[Bbv])
            for tt in (2 * pr, 2 * pr + 1):
                gi = blk * 4 + tt
                o = (tt % 2) * 192
                P.op("act", lambda e, gi=gi, o=o, bv=bv: e.activation(out=VH[:, gi, :], in_=bv[:, o:o + 64], func=AF.Identity), reads=[Bbv], writes=[BVH[blk]])
                P.op("dve", lambda e, gi=gi, o=o, bv=bv: e.tensor_copy(out=VV[:, gi, :, 0:64], in_=bv[:, o + 64:o + 192].rearrange("p (h d) -> p h d", h=2)), reads=[Bbv], writes=[BV[blk]])
        b1, Bb1 = nbank(); inproj(0, 128, b1, Bb1)
        b2, Bb2 = nbank(); inproj(128, 128, b2, Bb2)
        b3, Bb3 = nbank(); inproj(256, 64, b3, Bb3, rows=slice(0, 64))
        xb, Bxb = xbuf[blk % 2], Bxbuf[blk % 2]
        P.op("act", lambda e: e.activation(out=xb[L, 3:515], in_=b1[L, :], func=AF.Identity), reads=[Bb1], writes=[Bxb])
        ysb, Bysb = wt("ysb")
        P.op("act", lambda e: e.activation(out=ysb[L, :], in_=b2[L, :], func=AF.Identity), reads=[Bb2], writes=[Bysb])
        thq, Bthq = wt("thq"); thf, Bthf = wt("thf"); thg, Bthg = wt("thg")
        P.op("act", lambda e: e.activation(out=thq[:], in_=b1[H, :], func=AF.Tanh, scale=0.5), reads=[Bb1], writes=[Bthq])
        P.op("act", lambda e: e.activation(out=thf[:], in_=b2[H, :], func=AF.Tanh, scale=0.5), reads=[Bb2], writes=[Bthf])
        P.op("act", lambda e: e.activation(out=thg[:], in_=b3[H, :], func=AF.Tanh, scale=0.5), reads=[Bb3], writes=[Bthg])
        qs2, Bqs2 = wt("qs2"); gs2, Bgs2 = wt("gs2")
        P.op("dve", lambda e: e.scalar_tensor_tensor(out=qs2[:], in0=thq[:], scalar=1.0, in1=b1[H, :], op0=ALU.add, op1=ALU.mult), reads=[Bthq, Bb1], writes=[Bqs2])
        P.op("dve", lambda e: e.scalar_tensor_tensor(out=gs2[:], in0=thg[:], scalar=1.0, in1=b3[H, :], op0=ALU.add, op1=ALU.mult), reads=[Bthg, Bb3], writes=[Bgs2])
        S1[blk] = dict(xb=(xb, Bxb), ysb=(ysb, Bysb), thf=(thf, Bthf), qs2=(qs2, Bqs2), gs2=(gs2, Bgs2))

    def stage2(blk):
        c0 = blk * 512
        d = S1.pop(blk)
        xb, Bxb = d["xb"]; ysb, Bysb = d["ysb"]; thf, Bthf = d["thf"]; qs2, Bqs2 = d["qs2"]; gs2, Bgs2 = d["gs2"]
        xo, Bxo = xbuf[(blk + 1) % 2], Bxbuf[(blk + 1) % 2]
        PQ.op("pool", lambda e: e.tensor_copy(out=xb[L, 0:3], in_=xo[L, 512:515]), reads=[Bxo], writes=[Bxb])
        xc, Bxc = wt("xc")
        PQ.op("pool", lambda e: e.tensor_scalar(out=xc[L, :], in0=xb[L, 3:515], scalar1=pc(L, 3), scalar2=pc(L, 4), op0=ALU.mult, op1=ALU.add), reads=[Bxb, Bpar], writes=[Bxc])
        for j in (2, 1, 0):
            PQ.op("dve", lambda e, j=j: e.scalar_tensor_tensor(out=xc[L, :], in0=xb[L, j:j + 512], scalar=pc(L, j), in1=xc[L, :], op0=ALU.mult, op1=ALU.add), reads=[Bxb, Bpar, Bxc], writes=[Bxc])
        xcb, Bxcb = wt("xcb")
        PQ.op("pool", lambda e: e.tensor_copy(out=xcb[L, :], in_=xc[L, :]), reads=[Bxc], writes=[Bxcb])
        bg, Bbg = nbank2()
        bg2, Bbg2 = nbank2()
        PQ.op("pe", lambda e: e.matmul(bg[L, :], lhsT=wg[L, 0:64], rhs=xcb[L, :], start=True, stop=True), reads=[Bwg, Bxcb], writes=[Bbg])
        PQ.op("pe", lambda e: e.matmul(bg2[L, :], lhsT=wg[L, 64:128], rhs=xcb[L, :], start=True, stop=True), reads=[Bwg, Bxcb], writes=[Bbg2])
        thr, Bthr = wt("thr"); thi, Bthi = wt("thi")
        PQ.op("act", lambda e: e.activation(out=thr[L, :], in_=bg[L, :], func=AF.Tanh, scale=0.5, bias=pc(L, 8)), reads=[Bbg, Bpar], writes=[Bthr])
        PQ.op("act", lambda e: e.activation(out=thi[L, :], in_=bg2[L, :], func=AF.Tanh, scale=0.5, bias=pc(L, 9)), reads=[Bbg2, Bpar], writes=[Bthi])
        aa, Baa = wt("aa"); a2, Ba2 = wt("a2")
        PQ.op("act", lambda e: e.activation(out=aa[L, :], in_=thr[L, :], func=AF.Exp, scale=pc(L, 11), bias=pc(L, 11)), reads=[Bthr, Bpar], writes=[Baa])
        PQ.op("act", lambda e: e.activation(out=a2[L, :], in_=thr[L, :], func=AF.Exp, scale=pc(L, 10), bias=pc(L, 10)), reads=[Bthr, Bpar], writes=[Ba2])
        t1, Bt1 = wt("t1"); t2, Bt2 = wt("t2")
        PQ.op("act", lambda e: e.activation(out=t1[L, :], in_=ysb[L, :], func=AF.Square), reads=[Bysb], writes=[Bt1])
        PQ.op("pool", lambda e: e.tensor_scalar(out=t1[L, :], in0=t1[L, :], scalar1=0.044715, scalar2=1.0, op0=ALU.mult, op1=ALU.add), reads=[Bt1], writes=[Bt1])
        PQ.op("pool", lambda e: e.tensor_tensor(out=t1[L, :], in0=t1[L, :], in1=ysb[L, :], op=ALU.mult), reads=[Bt1, Bysb], writes=[Bt1])
        PQ.op("act", lambda e: e.activation(out=t2[L, :], in_=t1[L, :], func=AF.Tanh, scale=GC), reads=[Bt1], writes=[Bt2])
        PQ.op("dve", lambda e: e.scalar_tensor_tensor(out=t2[L, :], in0=t2[L, :], scalar=1.0, in1=ysb[L, :], op0=ALU.add, op1=ALU.mult), reads=[Bt2, Bysb], writes=[Bt2])
        uu, Buu = wt("uu")
        PQ.op("dve", lambda e: e.scalar_tensor_tensor(out=uu[L, :], in0=thi[L, :], scalar=1.0, in1=xc[L, :], op0=ALU.add, op1=ALU.mult), reads=[Bthi, Bxc], writes=[Buu])
        fg, Bfg = wt("fg"); kk, Bkk = wt("kk")
        PQ.op("dve", lambda e: e.tensor_scalar(out=fg[:], in0=thf[:], scalar1=pc(H, 16), scalar2=pc(H, 17), op0=ALU.mult, op1=ALU.add), reads=[Bthf, Bpar], writes=[Bfg])
        PQ.op("dve", lambda e: e.tensor_scalar(out=kk[:], in0=thf[:], scalar1=pc(H, 18), scalar2=pc(H, 16), op0=ALU.mult, op1=ALU.add), reads=[Bthf, Bpar], writes=[Bkk])
        PQ.op("act", lambda e: e.activation(out=a2[L, :], in_=a2[L, :], func=AF.Ln, scale=-1.0, bias=pc(L, 26)), reads=[Ba2], writes=[Ba2])
        PQ.op("act", lambda e: e.activation(out=fg[:], in_=fg[:], func=AF.Ln), reads=[Bfg], writes=[Bfg])
        PQ.op("act", lambda e: e.activation(out=a2[L, :], in_=a2[L, :], func=AF.Exp, scale=0.5), reads=[Ba2], writes=[Ba2])
        bb, Bbb = wt("bb")
        PQ.op("dve", lambda e: e.tensor_tensor_scan(out=bb[:], data0=rmask[:], data1=fg[:], initial=0.0, op0=ALU.mult, op1=ALU.add), reads=[Brm, Bfg], writes=[Bbb])
        eb, Beb = wt("eb"); enb, Benb = wt("enb"); ebl, Bebl = wt("ebl")
        PQ.op("act", lambda e: e.activation(out=eb[:], in_=bb[:], func=AF.Exp, bias=pc(H, 24)), reads=[Bbb, Bpar], writes=[Beb])
        PQ.op("act", lambda e: e.activation(out=enb[:], in_=bb[:], func=AF.Exp, scale=-1.0), reads=[Bbb], writes=[Benb])
        PQ.op("act", lambda e: e.activation(out=ebl[:], in_=bb[:, 63:512:64], func=AF.Exp), reads=[Bbb], writes=[Bebl])
        PQ.op("dve", lambda e: e.scalar_tensor_tensor(out=uu[L, :], in0=uu[L, :], scalar=0.5, in1=a2[L, :], op0=ALU.mult, op1=ALU.mult), reads=[Buu, Ba2], writes=[Buu])
        hh, Bhh = wt("hh")
        if hprev[0] is None:
            PQ.op("dve", lambda e: e.tensor_tensor_scan(out=hh[L, :], data0=aa[L, :], data1=uu[L, :], initial=0.0, op0=ALU.mult, op1=ALU.add), reads=[Baa, Buu], writes=[Bhh])
        else:
            hp, Bhp = hprev[0]
            PQ.op("dve", lambda e, hp=hp: e.tensor_tensor_scan(out=hh[L, :], data0=aa[L, :], data1=uu[L, :], initial=hp[L, 511:512], op0=ALU.mult, op1=ALU.add), reads=[Baa, Buu, Bhp], writes=[Bhh])
        hprev[0] = (hh, Bhh)
        yo, Byo = wt("yo")
        PQ.op("dve", lambda e: e.scalar_tensor_tensor(out=yo[L, :], in0=hh[L, :], scalar=0.5, in1=t2[L, :], op0=ALU.mult, op1=ALU.mult), reads=[Bhh, Bt2], writes=[Byo])
        PQ.dma("sp", yT[0, :, c0:c0 + 512], yo[L, :], reads=[Byo])
        qt, Bqt = wt("qt"); kt, Bkt = wt("kt")
        PQ.op("dve", lambda e: e.tensor_tensor(out=qt[:], in0=qs2[:], in1=eb[:], op=ALU.mult), reads=[Bqs2, Beb], writes=[Bqt])
        PQ.op("dve", lambda e: e.tensor_tensor(out=kt[:], in0=kk[:], in1=enb[:], op=ALU.mult), reads=[Bkk, Benb], writes=[Bkt])
        btr, Bbtr = nbank2()
        btr16 = btr[:].bitcast(BF16)
        ktokE, BktokE = wt("ktokE"); ktokO, BktokO = wt("ktokO")
        for tt in range(4):
            PQ.op("pe", lambda e, tt=tt: e.transpose(btr16[:, tt * 64:(tt + 1) * 64], in_=kt[:, tt * 128:(tt + 1) * 128], identity=ident[0:64, 0:64]), reads=[Bkt, Bid], writes=[Bbtr])
        PQ.op("act", lambda e: e.activation(out=ktokE[0:64, :, :].rearrange("p t d -> p (t d)"), in_=btr16[0:64, 0:256], func=AF.Identity), reads=[Bbtr], writes=[BktokE])
        PQ.op("act", lambda e: e.activation(out=ktokO[64:128, :, :].rearrange("p t d -> p (t d)"), in_=btr16[64:128, 0:256], func=AF.Identity), reads=[Bbtr], writes=[BktokO])
        bkv, Bbkv = nbank2()
        for c in range(8):
            tt, hf = c // 2, c % 2
            gi = blk * 4 + tt
            kx, Bkx = (ktokE, BktokE) if hf == 0 else (ktokO, BktokO)
            PQ.op("pe", lambda e, c=c, tt=tt, gi=gi, kx=kx: e.matmul(bkv[0:64, c * 64:(c + 1) * 64], lhsT=kx[:, tt, :], rhs=VH[:, gi, :], start=True, stop=True),
                 reads=[Bkx, BVH[blk]], writes=[Bbkv])
        batt, Bbatt = nbank2()
        for tt in range(4):
            PQ.op("pe", lambda e, tt=tt: e.matmul(batt[:, tt * 128:(tt + 1) * 128], lhsT=kt[:, tt * 128:(tt + 1) * 128], rhs=qt[:, tt * 128:(tt + 1) * 128], start=True, stop=True),
                 reads=[Bkt, Bqt], writes=[Bbatt])
        attm, Battm = wt("attm")
        PQ.op("dve", lambda e: e.tensor_tensor(out=attm[:], in0=batt[:], in1=hmask[:], op=ALU.mult), reads=[Bbatt, Bhm], writes=[Battm])
        kve, Bkve = wt("kve")
        for c in range(8):
            PQ.op("act", lambda e, c=c: e.activation(out=kve[:, c * 64:(c + 1) * 64], in_=bkv[0:64, c * 64:(c + 1) * 64], func=AF.Identity, scale=ebl[:, c:c + 1]), reads=[Bbkv, Bebl], writes=[Bkve])
        bo, Bbo = nbank2()
        for c in range(8):
            tt, hf = c // 2, c % 2
            gi = blk * 4 + tt
            sb, Bsb = Sbf[(blk * 8 + c) % 4], BSbf[(blk * 8 + c) % 4]
            if hf == 0:
                PQ.begin()
            PQ.op("act", lambda e, sb=sb: e.activation(out=sb[:], in_=Sst[:], func=AF.Identity), reads=[BS], writes=[Bsb])
            if hf == 0:
                PQ.op("pe", lambda e, tt=tt, gi=gi: e.matmul(bo[0:64, tt * 128:(tt + 1) * 128], lhsT=VH[:, gi, :], rhs=attm[:, tt * 128:(tt + 1) * 128], start=True, stop=False),
                     reads=[BVH[blk], Battm], writes=[Bbo])
            PQ.op("pe", lambda e, c=c, sb=sb, hf=hf: e.matmul(bo[0:64, c * 64:(c + 1) * 64], lhsT=sb[:], rhs=qt[:, c * 64:(c + 1) * 64], start=False, stop=(hf == 1)),
                 reads=[Bsb, Bqt], writes=[Bbo])
            PQ.op("dve", lambda e, c=c: e.scalar_tensor_tensor(out=Sst[:], in0=Sst[:], scalar=ebl[:, c:c + 1], in1=kve[:, c * 64:(c + 1) * 64], op0=ALU.mult, op1=ALU.add), reads=[BS, Bebl, Bkve], writes=[BS])
            if hf == 1:
                PQ.end()
        osb, Bosb = wt("osb"); osq, Bosq = wt("osq")
        PQ.op("act", lambda e: e.activation(out=osb[:], in_=bo[0:64, :], func=AF.Identity), reads=[Bbo], writes=[Bosb])
        PQ.op("act", lambda e: e.activation(out=osq[:], in_=bo[0:64, :], func=AF.Square), reads=[Bbo], writes=[Bosq])
        bms, Bbms = nbank2()
        PQ.op("pe", lambda e: e.matmul(bms[0:64, :], lhsT=ones64[:], rhs=osq[:], start=True, stop=True), reads=[Bo64, Bosq], writes=[Bbms])
        rstd, Brstd = wt("rstd")
        PQ.op("act", lambda e: e.activation(out=rstd[:], in_=bms[0:64, :], func=AF.Ln, bias=pc(H, 25)), reads=[Bbms, Bpar], writes=[Brstd])
        PQ.op("act", lambda e: e.activation(out=rstd[:], in_=rstd[:], func=AF.Exp, scale=-0.5), reads=[Brstd], writes=[Brstd])
        PQ.op("dve", lambda e: e.tensor_tensor(out=osb[:], in0=osb[:], in1=rstd[:], op=ALU.mult), reads=[Bosb, Brstd], writes=[Bosb])
        PQ.op("dve", lambda e: e.tensor_scalar(out=osb[:], in0=osb[:], scalar1=pc(H, 2), scalar2=0.5, op0=ALU.mult, op1=ALU.mult), reads=[Bosb, Bpar], writes=[Bosb])
        hy, Bhy = wt("hy")
        PQ.op("dve", lambda e: e.tensor_tensor(out=hy[:], in0=osb[:], in1=gs2[:], op=ALU.mult), reads=[Bosb, Bgs2], writes=[Bhy])
        PQ.dma("sp", yT[1, :, c0:c0 + 512], hy[:], reads=[Bhy])

    def stage3(blk, tick=None):
        for h in range(2):
            head3(blk, h, tick)

    def head3(blk, h, tick=None):
        c0 = blk * 512
        if True:
            Q, K = Qh[h], Kh[h]
            dr = slice(0, 64) if h == 0 else slice(64, 128)
            mr = slice(64, 96) if h == 0 else slice(0, 32)
            bgt, Bbgt = nbank()
            bmp, Bbmp = nbank()
            bmp16 = bmp[:].bitcast(BF16)
            any_mp = False
            for st in range(4):
                j = 2 * blk + (st // 2)
                if j == 0:
                    continue
                any_mp = True
                q0 = c0 + st * 128
                P.op("pe", lambda e, q0=q0, st=st: e.matmul(bgt[:, st * 32:(st + 1) * 32], lhsT=Q[dr, q0:q0 + 128], rhs=kmT[dr, 0:32], start=True, stop=True),
                     reads=[BQ[h][blk], Bkm], writes=[Bbgt])
                gsb, Bgsb = wt("gsb"); top8, Btop8 = wt("top8"); mp, Bmp = wt("mp")
                P.op("dve", lambda e, st=st, j=j, gsb=gsb: e.tensor_copy(out=gsb[:, 0:j], in_=bgt[:, st * 32:st * 32 + j]), reads=[Bbgt], writes=[Bgsb])
                P.op("dve", lambda e, j=j, gsb=gsb, top8=top8: e.max(out=top8[:], in_=gsb[:, 0:max(j, 8)]), reads=[Bgsb], writes=[Btop8])
                P.op("pool", lambda e, mp=mp: e.memset(mp[:], 0.0), writes=[Bmp])
                P.op("dve", lambda e, j=j, gsb=gsb, top8=top8, mp=mp: e.tensor_scalar(out=mp[:, 0:j], in0=gsb[:, 0:j], scalar1=top8[:, 2:3], scalar2=-8.0 * BIG, op0=ALU.is_lt, op1=ALU.mult),
                     reads=[Bgsb, Btop8, Bmp], writes=[Bmp])
                P.op("pe", lambda e, st=st, mp=mp: e.transpose(bmp16[mr, st * 128:(st + 1) * 128], in_=mp[:], identity=ident[:]), reads=[Bmp, Bid], writes=[Bbmp])
            if any_mp:
                s0 = 0 if blk > 0 else 2
                P.op("act", lambda e, s0=s0: e.activation(out=Q[mr, c0 + s0 * 128:c0 + 512], in_=bmp16[mr, s0 * 128:512], func=AF.Identity), reads=[Bbmp], writes=[BQ[h][blk]])
            pv, Bpv = pvb[h]
            tiles = [(kt_, 0) for kt_ in range(4 * blk) if (maxdist[h] is None or (c0 - (kt_ * 128 + 127)) <= maxdist[h])] + [(4 * blk + kk_, kk_) for kk_ in range(4)]
            LA = 3
            pend = []
            state = {"first": True}

            def emit_pv(item):
                kti, n0, pt, Bpt, kb, last = item
                first = state["first"]
                P.op("pe", lambda e, kti=kti, n0=n0, pt=pt, first=first, last=last: e.matmul(pv[0:65, n0:512], lhsT=VV[:, kti, h, :], rhs=pt[:, n0:512], start=first, stop=last),
                     reads=[BV[kb], Bpt], writes=[Bpv])
                state["first"] = False
            for (kti, own) in tiles:
                isown = kti >= 4 * blk
                n0 = own * 128 if isown else 0
                k0 = kti * 128
                kb = kti // 4
                bs, Bbs = nbank()
                P.op("pe", lambda e, k0=k0, n0=n0, bs=bs: e.matmul(bs[:, n0:512], lhsT=K[:, k0:k0 + 128], rhs=Q[:, c0 + n0:c0 + 512], start=True, stop=True),
                     reads=[BK[h][kb], BQ[h][blk]], writes=[Bbs])
                pt, Bpt = wt("pt")
                m = kti - 4 * blk + MOFF
                P.op("act", lambda e, n0=n0, bs=bs, pt=pt, m=m: e.activation(out=pt[:, n0:512], in_=bs[:, n0:512], func=AF.Exp, scale=0.125, bias=abias[:, h, m:m + 1]),
                     reads=[Bbs, Bab], writes=[Bpt])
                if isown:
                    P.op("dve", lambda e, n0=n0, pt=pt: e.tensor_tensor(out=pt[:, n0:n0 + 128], in0=pt[:, n0:n0 + 128], in1=cmask[:], op=ALU.mult), reads=[Bpt, Bcm], writes=[Bpt])
                last = (kti == tiles[-1][0])
                pend.append((kti, n0, pt, Bpt, kb, last))
                if len(pend) > LA:
                    emit_pv(pend.pop(0))
                if tick is not None:
                    tick()
            while pend:
                emit_pv(pend.pop(0))
            onum, Bonum = wt("onum"); rec, Brec = wt("rec"); my, Bmy = wt("my")
            P.op("act", lambda e: e.activation(out=onum[0:65, :], in_=pv[0:65, :], func=AF.Identity), reads=[Bpv], writes=[Bonum])
            P.op("act", lambda e: e.activation(out=rec[64:65, :], in_=onum[64:65, :], func=AF.Ln), reads=[Bonum], writes=[Brec])
            P.op("act", lambda e: e.activation(out=rec[64:65, :], in_=rec[64:65, :], func=AF.Exp, scale=-1.0), reads=[Brec], writes=[Brec])
            bbc, Bbbc = nbank()
            P.op("pe", lambda e: e.matmul(bbc[0:64, :], lhsT=ones64r[64:65, :], rhs=rec[64:65, :], start=True, stop=True), reads=[Bo64r, Brec], writes=[Bbbc])
            P.op("dve", lambda e: e.tensor_tensor(out=my[:], in0=onum[0:64, :], in1=bbc[0:64, :], op=ALU.mult), reads=[Bonum, Bbbc], writes=[Bmy])
            P.dma("sp", yT[2 + h, :, c0:c0 + 512], my[:], reads=[Bmy])

    import os
    stop = os.environ.get("PHB_STOP", "")
    if stop == "init":
        return
    for blk in range(NB):
        stage1(blk)
        if stop == "s1":
            return
        stage2(blk)
        if blk > 0:
            ntl = max(1, 2 * (4 * (blk - 1) + 4))
            per = max(2, (len(PQ.q) + ntl - 1) // ntl + 1)
            stage3(blk - 1, tick=lambda per=per: PQ.flush(per))
        PQ.flush()
        if stop == "s2":
            return
    stage3(NB - 1)


EPS = 1e-6


def host_consts_C():
    c = {}
    oa = np.zeros((128, 128), np.float32)
    oa[:64, :] = 1.0 / 256.0
    c["onesA"] = oa
    c["onesC"] = np.full((128, 128), 1.0 / 512.0, np.float32)
    c["ones1k"] = np.full((128, 128), 1.0 / 1024.0, np.float32)
    return c


def rms_stats(P, banks, Bbanks, src_fn, nchunk, ones_t, Bones, sq_tiles, eps_ap, Bpar, rstd, Brstd, srcbufs):
    bank, Bbank = banks
    for c in range(nchunk):
        sq, Bsq = sq_tiles[c % len(sq_tiles)]
        src = src_fn(c)
        P.op("act", lambda e, sq=sq, src=src: e.activation(out=sq[:], in_=src, func=AF.Square), reads=srcbufs(c), writes=[Bsq])
        P.op("pe", lambda e, sq=sq, c=c: e.matmul(bank[:], lhsT=ones_t[:], rhs=sq[:], start=(c == 0), stop=(c == nchunk - 1)), reads=[Bones, Bsq], writes=[Bbank])
    P.op("act", lambda e: e.activation(out=rstd[:], in_=bank[:], func=AF.Ln, bias=eps_ap), reads=[Bbank, Bpar], writes=[Brstd])
    P.op("act", lambda e: e.activation(out=rstd[:], in_=rstd[:], func=AF.Exp, scale=-0.5), reads=[Brstd], writes=[Brstd])


def build_A(nc, P, io, NT=2048):
    xs = P.sbuf("xs", [128, 8, NT], F32)
    Bxs = [P.buf(f"xs{t}") for t in range(NT // 512)]
    gv = P.sbuf("gv", [128, 16], F32); Bgv = P.buf("gv")
    ones1k = P.sbuf("ones1k", [128, 128], F32); Bo1k = P.buf("ones1k")
    P.dma("sp", gv[:, 0:8], io["gv"], writes=[Bgv])
    P.dma("sp", ones1k[:], io["ones1k"], writes=[Bo1k])
    P.op("dve", lambda e: e.memset(gv[:, 8:9], EPS), reads=[Bgv], writes=[Bgv])
    banks = [P.psum(f"bank{i}", [128, 512]) for i in range(2)]
    Bbank = [P.buf(f"bank{i}") for i in range(2)]
    for b_ in Bbank:
        b_.excl = True
    sqs = [(P.sbuf(f"sq{i}", [128, 512], F32), P.buf(f"sq{i}")) for i in range(2)]
    rs = [(P.sbuf(f"rstd{i}", [128, 512], F32), P.buf(f"rstd{i}")) for i in range(2)]
    ho = [(P.sbuf(f"ho{i}", [128, 8, 512], BF16), P.buf(f"ho{i}")) for i in range(2)]
    xv = io["xT"].rearrange("(c p) n -> p c n", p=128)
    hv = io["hT"].rearrange("(c p) n -> p c n", p=128)
    for t in range(NT // 512):
        P.dma("sp", xs[:, :, t * 512:(t + 1) * 512], xv[:, :, t * 512:(t + 1) * 512], writes=[Bxs[t]])
    for t in range(NT // 512):
        def body(t):
            rstd, Brstd = rs[t % 2]
            rms_stats(P, (banks[t % 2], Bbank[t % 2]), None, lambda c: xs[:, c, t * 512:(t + 1) * 512], 8, ones1k, Bo1k, sqs, gv[:, 8:9], Bgv, rstd, Brstd, lambda c: [Bxs[t]])
            h, Bh = ho[t % 2]
            for c in range(8):
                P.op("dve", lambda e, c=c: e.scalar_tensor_tensor(out=h[:, c, :], in0=xs[:, c, t * 512:(t + 1) * 512], scalar=gv[:, c:c + 1], in1=rstd[:], op0=ALU.mult, op1=ALU.mult),
                     reads=[Bxs[t], Bgv, Brstd], writes=[Bh])
            P.dma("sp", hv[:, :, t * 512:(t + 1) * 512], h[:], reads=[Bh])
        body(t)


def build_C(nc, P, io, final, NT=2048):
    NTC = NT // 512
    xs = P.sbuf("xs", [128, 8, NT], F32)
    Bxs = [P.buf(f"xs{t}") for t in range(NTC)]
    gv = P.sbuf("gv", [128, 40], F32); Bgv = P.buf("gv")
    onesA = P.sbuf("onesA", [128, 128], F32); BoA = P.buf("onesA")
    onesC = P.sbuf("onesC", [128, 128], F32); BoC = P.buf("onesC")
    ones1k = P.sbuf("ones1k", [128, 128], F32); Bo1k = P.buf("ones1k")
    P.dma("sp", gv[:, 0:32], io["gv"], writes=[Bgv])
    P.dma("sp", onesA[:], io["onesA"], writes=[BoA])
    P.dma("sp", onesC[:], io["onesC"], writes=[BoC])
    P.dma("sp", ones1k[:], io["ones1k"], writes=[Bo1k])
    P.op("dve", lambda e: e.memset(gv[:, 32:33], EPS), reads=[Bgv], writes=[Bgv])
    eps_ap = gv[:, 32:33]
    banks = [P.psum(f"bank{i}", [128, 512]) for i in range(8)]
    Bbank = [P.buf(f"bank{i}") for i in range(8)]
    for b_ in Bbank:
        b_.excl = True
    rr = [0]

    def nbank():
        i = rr[0] % 8
        rr[0] += 1
        return banks[i], Bbank[i]
    R = P.sbuf("R", [128, 32768], BF16)
    wout = R[:, 0:8192].rearrange("p (c n) -> p c n", c=8); Bwout = P.buf("wout")
    ysb = [R[:, 8192 + i * 4096:8192 + (i + 1) * 4096].rearrange("p (c n) -> p c n", c=8) for i in range(2)]
    Bysb = [P.buf(f"y{i}") for i in range(2)]
    ynb = [R[:, 16384 + i * 4096:16384 + (i + 1) * 4096].rearrange("p (c n) -> p c n", c=8) for i in range(2)]
    Bynb = [P.buf(f"yn{i}") for i in range(2)]
    u = R[:, :].rearrange("p (f n) -> p f n", f=32)
    Bu = [P.buf(f"u{f}") for f in range(32)]
    h2 = P.sbuf("h2", [128, 8, 1024], BF16); Bh2 = [P.buf("h2_0"), P.buf("h2_1")]
    w1b = [(P.sbuf(f"w1b{i}", [128, 8, 512], BF16), P.buf(f"w1b{i}")) for i in range(2)]
    w2b = [(P.sbuf(f"w2b{i}", [128, 4, 512], BF16), P.buf(f"w2b{i}")) for i in range(2)]
    sqs = [(P.sbuf(f"sq{i}", [128, 512], F32), P.buf(f"sq{i}")) for i in range(3)]
    rsA = [(P.sbuf(f"rsA{i}", [128, 512], F32), P.buf(f"rsA{i}")) for i in range(1)] * 2
    rsC = [(P.sbuf(f"rsC{i}", [128, 512], F32), P.buf(f"rsC{i}")) for i in range(1)] * 2
    rs2 = [(P.sbuf(f"rs2{i}", [128, 512], F32), P.buf(f"rs2{i}")) for i in range(2)]
    rl = [(P.sbuf(f"rl{i}", [128, 512], F32), P.buf(f"rl{i}")) for i in range(3)]
    if final:
        st_f = [(P.sbuf(f"stf{i}", [128, 4, 512], F32), P.buf(f"stf{i}")) for i in range(1)]
    else:
        st_b = [(P.sbuf(f"stb{i}", [128, 8, 512], BF16), P.buf(f"stb{i}")) for i in range(1)]

    xv = io["xT"].rearrange("(c p) n -> p c n", p=128)
    for t in range(NTC):
        P.dma("sp", xs[:, :, t * 512:(t + 1) * 512], xv[:, :, t * 512:(t + 1) * 512], writes=[Bxs[t]])
    P.dma("pool", wout, io["wout"].rearrange("(c p) n -> p c n", p=128), writes=[Bwout])
    if "ysrc" in io:
        cand = [(P.sbuf(f"cand{i}", [128, 8, 512], BF16), P.buf(f"cand{i}")) for i in range(1)] * 2
        cand = [(t_[:], b_) for (t_, b_) in cand]
        ohs = P.sbuf("ohs", [128, 4], F32); Bohs = P.buf("ohs")
        P.dma("sp", ohs[:], io["ohs"], writes=[Bohs])

    def outproj(t):
        tsl = slice(t * 512, (t + 1) * 512)
        y, By = ysb[t % 2], Bysb[t % 2]
        yn, Byn = ynb[t % 2], Bynb[t % 2]
        if "ysrc" in io:
            for sI in range(4):
                cd, Bcd = cand[sI % 2]
                for pp in range(2):
                    for hh in range(2):
                        P.dma("sp", cd[pp * 64:(pp + 1) * 64, hh::2, :], io["ysrc"](sI, t, pp, hh), writes=[Bcd])
                if sI == 0:
                    P.op("dve", lambda e, cd=cd: e.tensor_scalar(out=y, in0=cd, scalar1=ohs[:, 0:1], scalar2=None, op0=ALU.mult), reads=[Bcd, Bohs], writes=[By])
                else:
                    P.op("dve", lambda e, cd=cd, sI=sI: e.scalar_tensor_tensor(out=y, in0=cd, scalar=ohs[:, sI:sI + 1], in1=y, op0=ALU.mult, op1=ALU.add), reads=[Bcd, Bohs, By], writes=[By])
        else:
            P.dma("sp", y, io["yT"][:, :, tsl].rearrange("c p n -> p c n"), writes=[By])
        rA, BrA = rsA[t % 2]; rC, BrC = rsC[t % 2]
        bA = nbank()
        rms_stats(P, bA, None, lambda c: y[:, 2 * c, :], 4, onesA, BoA, sqs, eps_ap, Bgv, rA, BrA, lambda c: [By])
        bC = nbank()
        rms_stats(P, bC, None, lambda c: y[:, 2 * c + 1, :], 4, onesC, BoC, sqs, eps_ap, Bgv, rC, BrC, lambda c: [By])
        for g in range(4):
            c0, c1 = 2 * g, 2 * g + 1
            P.op("dve", lambda e, c0=c0: e.scalar_tensor_tensor(out=yn[0:64, c0, :], in0=y[0:64, c0, :], scalar=gv[0:64, 16 + c0:17 + c0], in1=rA[0:64, :], op0=ALU.mult, op1=ALU.mult),
                 reads=[By, Bgv, BrA], writes=[Byn])
            P.op("pool", lambda e, c0=c0: e.tensor_copy(out=yn[64:128, c0, :], in_=y[64:128, c0, :]), reads=[By], writes=[Byn])
            P.op("dve", lambda e, c1=c1: e.scalar_tensor_tensor(out=yn[:, c1, :], in0=y[:, c1, :], scalar=gv[:, 16 + c1:17 + c1], in1=rC[:], op0=ALU.mult, op1=ALU.mult),
                 reads=[By, Bgv, BrC], writes=[Byn])
        for m in range(8):
            bk, Bbk = nbank()
            for c in range(8):
                P.op("pe", lambda e, c=c, m=m, bk=bk: e.matmul(bk[:], lhsT=wout[:, c, m * 128:(m + 1) * 128], rhs=yn[:, c, :], start=(c == 0), stop=(c == 7)),
                     reads=[Bwout, Byn], writes=[Bbk])
            P.op("dve", lambda e, m=m, bk=bk: e.tensor_tensor(out=xs[:, m, tsl], in0=xs[:, m, tsl], in1=bk[:], op=ALU.add), reads=[Bbk, Bxs[t]], writes=[Bxs[t]])
    for t in range(NTC):
        outproj(t)

    w1v = io["w1"].rearrange("(c p) n -> p c n", p=128)
    w2v = io["w2"].rearrange("(f p) n -> p f n", p=128)
    alias_guard = [Bwout] + Bysb + Bynb
    cnt = {"w1": 0, "w2": 0, "rl": 0}

    def ffn_half(hf):
        for tl in range(2):
            t = 2 * hf + tl
            tsl = slice(t * 512, (t + 1) * 512)
            r2, Br2 = rs2[t % 2]
            rms_stats(P, nbank(), None, lambda c, tsl=tsl: xs[:, c, tsl], 8, ones1k, Bo1k, sqs, eps_ap, Bgv, r2, Br2, lambda c, t=t: [Bxs[t]])
            for c in range(8):
                P.op("dve", lambda e, c=c, tsl=tsl, tl=tl, r2=r2: e.scalar_tensor_tensor(out=h2[:, c, tl * 512:(tl + 1) * 512], in0=xs[:, c, tsl], scalar=gv[:, c:c + 1], in1=r2[:], op0=ALU.mult, op1=ALU.mult),
                     reads=[Bxs[t], Bgv, Br2], writes=[Bh2[tl]])
        for fg in range(8):
            w1t, Bw1 = w1b[cnt["w1"] % 2]; cnt["w1"] += 1
            P.dma("pool", w1t[:], w1v[:, :, fg * 512:(fg + 1) * 512], writes=[Bw1])
            for fc in range(4):
                f = fg * 4 + fc
                for tl in range(2):
                    bk, Bbk = nbank()
                    for k in range(8):
                        P.op("pe", lambda e, k=k, fc=fc, tl=tl, bk=bk, w1t=w1t: e.matmul(bk[:], lhsT=w1t[:, k, fc * 128:(fc + 1) * 128], rhs=h2[:, k, tl * 512:(tl + 1) * 512], start=(k == 0), stop=(k == 7)),
                             reads=[Bw1, Bh2[tl]], writes=[Bbk])
                    r, Br = rl[cnt["rl"] % 3]; cnt["rl"] += 1
                    P.op("act", lambda e, bk=bk, r=r: e.activation(out=r[:], in_=bk[:], func=AF.Relu), reads=[Bbk], writes=[Br])
                    extra = alias_guard if (hf == 0) else []
                    eng = "pool" if (cnt["rl"] % 2 == 0) else "dve"
                    P.op(eng, lambda e, f=f, tl=tl, r=r: e.tensor_tensor(out=u[:, f, tl * 512:(tl + 1) * 512], in0=r[:], in1=r[:], op=ALU.mult), reads=[Br], writes=[Bu[f]] + extra)
        for mh in range(2):
            accs = [[nbank() for tl in range(2)] for mm in range(4)]
            for fg in range(8):
                w2t, Bw2 = w2b[cnt["w2"] % 2]; cnt["w2"] += 1
                P.dma("pool", w2t[:], w2v[:, fg * 4:(fg + 1) * 4, mh * 512:(mh + 1) * 512], writes=[Bw2])
                for fc in range(4):
                    f = fg * 4 + fc
                    for mm in range(4):
                        for tl in range(2):
                            bk, Bbk = accs[mm][tl]
                            P.op("pe", lambda e, fc=fc, f=f, mm=mm, tl=tl, bk=bk, w2t=w2t: e.matmul(bk[:], lhsT=w2t[:, fc, mm * 128:(mm + 1) * 128], rhs=u[:, f, tl * 512:(tl + 1) * 512], start=(f == 0), stop=(f == 31)),
                                 reads=[Bw2, Bu[f]], writes=[Bbk])
            for mm in range(4):
                m = mh * 4 + mm
                for tl in range(2):
                    t = 2 * hf + tl
                    tsl = slice(t * 512, (t + 1) * 512)
                    bk, Bbk = accs[mm][tl]
                    P.op("dve", lambda e, m=m, tsl=tsl, bk=bk: e.tensor_tensor(out=xs[:, m, tsl], in0=xs[:, m, tsl], in1=bk[:], op=ALU.add), reads=[Bbk, Bxs[t]], writes=[Bxs[t]])
    for hf in range(NTC // 2):
        ffn_half(hf)

    def tail(t):
        tsl = slice(t * 512, (t + 1) * 512)
        r2, Br2 = rs2[t % 2]
        rms_stats(P, nbank(), None, lambda c: xs[:, c, tsl], 8, ones1k, Bo1k, sqs, eps_ap, Bgv, r2, Br2, lambda c: [Bxs[t]])
        if final:
            so, Bso = st_f[0]
            for ch in range(2):
                for c in range(4 * ch, 4 * ch + 4):
                    P.op("dve", lambda e, c=c: e.scalar_tensor_tensor(out=so[:, c % 4, :], in0=xs[:, c, tsl], scalar=gv[:, 8 + c:9 + c], in1=r2[:], op0=ALU.mult, op1=ALU.mult),
                         reads=[Bxs[t], Bgv, Br2], writes=[Bso])
                P.dma("sp", io["oT"].rearrange("(c p) n -> p c n", p=128)[:, 4 * ch:4 * ch + 4, tsl], so[:], reads=[Bso])
        else:
            so, Bso = st_b[0]
            for c in range(8):
                P.op("dve", lambda e, c=c: e.scalar_tensor_tensor(out=so[:, c, :], in0=xs[:, c, tsl], scalar=gv[:, 8 + c:9 + c], in1=r2[:], op0=ALU.mult, op1=ALU.mult),
                     reads=[Bxs[t], Bgv, Br2], writes=[Bso])
            P.dma("sp", io["hnT"].rearrange("(c p) n -> p c n", p=128)[:, :, tsl], so[:], reads=[Bso])
            P.dma("sp", io["xnT"].rearrange("(c p) n -> p c n", p=128)[:, :, tsl], xs[:, :, tsl], reads=[Bxs[t]], sem_buf=Bso)
    for t in range(NTC):
        tail(t)


import ml_dtypes
from contextlib import ExitStack
from concourse.bass_utils import run_bass_kernel_spmd

_BF = ml_dtypes.bfloat16
SEQ = 8192
ALL_SLOPES = [2.0 ** (-8.0 * (h + 1) / 8) for h in range(8)]
HEAD_A = lambda g: g
HEAD_B = lambda g: 4 + g
MAXDIST = (2432, None)
GROUPS = [[0, 1, 2, 3], [4, 5, 6, 7]]
NPH = 4
NPY = 4

_cache = {}
B_CONST = ["ohk", "crow", "abias", "cmask", "hmask", "rmask", "ident", "ones64"]
C_CONST = ["onesA", "onesC", "ones1k"]


def _build_fused(seq):
    ntok = seq // 4
    nc = bass.Bass("TRN2", target_bir_lowering=False)
    io = {}

    def din(name, shape, dt):
        io[name] = nc.dram_tensor(name, list(shape), dt, kind="ExternalInput").ap()
    T = seq
    NM = T // 128 + 4
    din("xT", [1024, ntok], F32); din("ohs", [128, 4], F32); din("gvA", [128, 8], F32)
    for l in range(2):
        din(f"wfm{l}", [1024, 640], F32); din(f"wtm{l}", [1024, 192], F32); din(f"par{l}", [128, 16], F32); din(f"wg{l}", [128, 128], F32)
        din(f"wout{l}", [1024, 1024], F32); din(f"w1{l}", [1024, 4096], F32); din(f"w2{l}", [4096, 1024], F32); din(f"gvC{l}", [128, 32], F32)
    din("ohk", [33, T], BF16); din("crow", [2, T], BF16); din("abias", [128, 2, NM], F32)
    din("cmask", [128, 128], BF16); din("hmask", [128, 512], F32); din("rmask", [64, 512], F32)
    din("ident", [128, 128], BF16); din("ones64", [64, 64], F32)
    din("onesA", [128, 128], F32); din("onesC", [128, 128], F32); din("ones1k", [128, 128], F32)
    oT = nc.dram_tensor("oT", [1024, ntok], F32, kind="ExternalOutput").ap()
    hloc = [nc.dram_tensor(f"hloc{l}", [1024, ntok], BF16) for l in range(2)]
    hall = [nc.dram_tensor(f"hall{l}", [NPH, 4 * (1024 // NPH), ntok], BF16) for l in range(2)]
    yloc = [nc.dram_tensor(f"yloc{l}", [256, T], BF16) for l in range(2)]
    yall = [nc.dram_tensor(f"yall{l}", [NPY, 4 * (256 // NPY), T], BF16) for l in range(2)]
    xres = nc.dram_tensor("xres", [1024, ntok], F32)
    with ExitStack() as st:
        P = Prog(nc, st)
        P.push_scope("A_")
        build_A(nc, P, dict(xT=io["xT"], gv=io["gvA"], ones1k=io["ones1k"], hT=hloc[0].ap()), ntok)
        P.pop_scope()
        for l in range(2):
            P.barrier()
            rh = 1024 // NPH
            for j in range(NPH):
                P.coll("AllGather", hloc[l].ap()[j * rh:(j + 1) * rh, :].opt(), hall[l].ap()[j].opt(), GROUPS)
            P.barrier()
            P.push_scope(f"B{l}_")
            ioB = dict(wfm=io[f"wfm{l}"], wtm=io[f"wtm{l}"], par=io[f"par{l}"], wg=io[f"wg{l}"])
            for k in B_CONST:
                ioB[k] = io[k]
            ioB["yT"] = yloc[l].ap().rearrange("(a p) n -> a p n", a=4)
            hv = hall[l].ap().rearrange("j (s cc p) n -> j s p cc n", s=4, p=128)

            def hsrc(blk, j, hv=hv):
                tok = blk * 512
                s, lc = tok // ntok, tok % ntok
                return hv[j][s][:, :, lc:lc + 512]
            ioB["hsrc"] = hsrc
            build_B(nc, P, T, ioB, maxdist=MAXDIST)
            P.pop_scope()
            P.barrier()
            ry = 256 // NPY
            for j in range(NPY):
                P.coll("AllGather", yloc[l].ap()[j * ry:(j + 1) * ry, :].opt(), yall[l].ap()[j].opt(), GROUPS)
            P.barrier()
            P.push_scope(f"C{l}_")
            final = (l == 1)
            ioC = dict(xT=(io["xT"] if l == 0 else xres.ap()), wout=io[f"wout{l}"], w1=io[f"w1{l}"], w2=io[f"w2{l}"], gv=io[f"gvC{l}"], ohs=io["ohs"])
            for k in C_CONST:
                ioC[k] = io[k]
            yv = yall[l].ap().rearrange("j (g r) n -> j r g n", g=4)

            def ysrc(sI, t, pp, h, yv=yv):
                o = sI * ntok + t * 512
                return yv[2 * h + pp][:, :, o:o + 512]
            ioC["ysrc"] = ysrc
            if final:
                ioC["oT"] = oT
            else:
                ioC["xnT"] = xres.ap()
                ioC["hnT"] = hloc[1].ap()
            build_C(nc, P, ioC, final, ntok)
            P.pop_scope()
        n_ops = {e: len(v) for e, v in P.ops.items()}
        print("fused program ops", n_ops, "dsems", len(P.dsems), flush=True)
        P.finish()
    return nc


def _chunks(v):
    return np.ascontiguousarray(np.asarray(v, np.float32).reshape(8, 128).T)


def _b_weights(l, g, inp):
    w_in = inp["w_in"][l]
    hA, hB = HEAD_A(g), HEAD_B(g)
    sl = lambda base, w, i: w_in[:, base + w * i: base + w * i + w]
    wfm = np.zeros((1024, 640), np.float32)
    wfm[:, 0:64] = sl(512, 64, g)
    wfm[:, 64:128] = sl(0, 64, g)
    wfm[:, 128:192] = sl(768, 64, g)
    wfm[:, 192:256] = sl(256, 64, g)
    wfm[:, 256:320] = sl(1280, 64, g)
    wfm[:, 320:384] = sl(1536, 64, hA)
    wfm[:, 384:448] = sl(1536, 64, hB)
    wfm[:, 448:512] = sl(2048, 64, hA)
    wfm[:, 512:576] = sl(2048, 64, hB)
    wtm = np.concatenate([sl(1024, 64, g), sl(2560, 64, hA), sl(2560, 64, hB)], axis=1)
    par = np.zeros((128, 16), np.float32)
    ch = slice(64 * g, 64 * g + 64)
    par[64:, 0:4] = inp["lru_conv_w"][l][:, ch].T
    par[64:, 4] = inp["lru_conv_b"][l][ch]
    par[64:, 5] = inp["lru_ba"][l][ch]
    par[64:, 6] = inp["lru_bx"][l][ch]
    par[64:, 7] = inp["lru_lambda"][l][ch]
    par[:64, 0] = inp["hg_lower_bounds"][0][ch]
    par[:64, 1] = inp["hg_lower_bounds"][1][ch]
    par[:64, 2] = inp["hg_norm_w"][l]
    par[:64, 3] = float(l)
    wg = np.zeros((128, 128), np.float32)
    wg[64:, 0:64] = inp["lru_wa"][l][g]
    wg[64:, 64:128] = inp["lru_wx"][l][g]
    return {f"wfm{l}": wfm, f"wtm{l}": np.ascontiguousarray(wtm), f"par{l}": par, f"wg{l}": wg}


def _c_weights(l, inp, next_gain):
    w_out = inp["w_out"][l]
    rows = []
    gy = np.ones((128, 8), np.float32)
    for g in range(4):
        hA, hB = HEAD_A(g), HEAD_B(g)
        rows += list(range(64 * g, 64 * g + 64)) + list(range(256 + 64 * g, 256 + 64 * g + 64))
        rows += list(range(512 + 64 * hA, 512 + 64 * hA + 64)) + list(range(512 + 64 * hB, 512 + 64 * hB + 64))
        gy[0:64, 2 * g] = inp["lru_out_norm"][l][64 * g:64 * g + 64]
        gy[0:64, 2 * g + 1] = inp["att_out_norm"][l][64 * hA:64 * hA + 64]
        gy[64:128, 2 * g + 1] = inp["att_out_norm"][l][64 * hB:64 * hB + 64]
    gv = np.zeros((128, 32), np.float32)
    gv[:, 0:8] = _chunks(inp["norm_mlp"][l])
    gv[:, 8:16] = _chunks(next_gain)
    gv[:, 16:24] = gy
    return {f"wout{l}": np.ascontiguousarray(w_out[rows, :]), f"w1{l}": np.ascontiguousarray(inp["w_ff1"][l]),
            f"w2{l}": np.ascontiguousarray(inp["w_ff2"][l]), f"gvC{l}": gv}


def kernel(**inputs):
    inp = {k: np.asarray(v) for k, v in inputs.items()}
    x = inp["x"].astype(np.float32, copy=False)
    B, seq = x.shape[0], x.shape[1]
    ntok = seq // 4
    cores = list(range(8))
    if ("F", seq) not in _cache:
        _cache[("F", seq)] = _build_fused(seq)
    nc = _cache[("F", seq)]
    shared = {}
    shared.update(host_consts_C())
    shared["gvA"] = _chunks(inp["norm_mix"][0])
    for l in range(2):
        shared.update(_c_weights(l, inp, inp["norm_final"] if l == 1 else inp["norm_mix"][l + 1]))
    in_maps = []
    for c in cores:
        b, s = c // 4, c % 4
        d = dict(shared)
        d["xT"] = np.ascontiguousarray(x[b, s * ntok:(s + 1) * ntok, :].T)
        oh = np.zeros((128, 4), np.float32)
        oh[:, s] = 1.0
        d["ohs"] = oh
        d.update(host_consts_B(seq, [ALL_SLOPES[HEAD_A(s)], ALL_SLOPES[HEAD_B(s)]]))
        for l in range(2):
            d.update(_b_weights(l, s, inp))
        in_maps.append(d)
    res = run_bass_kernel_spmd(nc, in_maps, core_ids=cores)
    out = np.empty((B, seq, 1024), np.float32)
    for c in cores:
        b, s = c // 4, c % 4
        out[b, s * ntok:(s + 1) * ntok, :] = np.asarray(res.results[c]["oT"]).T
    return out
```

```python
import numpy as np
import concourse.bass as bass
import concourse.mybir as mybir

F32 = mybir.dt.float32
BF16 = mybir.dt.bfloat16
AF = mybir.ActivationFunctionType
ALU = mybir.AluOpType
AX = mybir.AxisListType

ENGS = ("pe", "act", "dve", "pool", "sp")


class Buf:
    __slots__ = ("name", "w", "r", "dsem", "excl")

    def __init__(self, name):
        self.name = name
        self.w = None
        self.r = []
        self.dsem = None
        self.excl = False


class DmaSem:
    __slots__ = ("sem", "issued", "group_open", "name", "unit", "kind")

    def __init__(self, sem, name, unit=16):
        self.sem = sem
        self.issued = 0
        self.group_open = False
        self.name = name
        self.unit = unit
        self.kind = "hw"


class Prog:
    def __init__(self, nc, stack):
        self.nc = nc
        self.stack = stack
        self.ops = {e: [] for e in ENGS}
        self.cnt = {e: 0 for e in ENGS}
        self.esem = {}
        for e in ("pe", "act", "dve", "pool"):
            self.esem[e] = stack.enter_context(nc.semaphore("s_" + e))
        self.known = {e: {} for e in ENGS}
        self.dsems = []
        self.csems = []
        self.free_dsems = []
        self.scopes = []
        self.pending = {}
        self.nbuf = 0
        import os
        self.limit = int(os.environ.get("MK_LIMIT", "0"))
        self.nrec = 0
        self.lastdesc = None

    def buf(self, name=None):
        self.nbuf += 1
        return Buf(name or f"b{self.nbuf}")

    def _st(self):
        return self.scopes[-1][0] if self.scopes else self.stack

    def _nm(self, name):
        return (self.scopes[-1][1] + name) if self.scopes else name

    def push_scope(self, prefix):
        from contextlib import ExitStack
        self.scopes.append((ExitStack(), prefix))

    def pop_scope(self):
        st, _ = self.scopes.pop()
        st.close()

    def sbuf(self, name, shape, dtype):
        t = self._st().enter_context(self.nc.sbuf_tensor("sb_" + self._nm(name), list(shape), dtype))
        return t

    def psum(self, name, shape, dtype=F32):
        t = self._st().enter_context(self.nc.psum_tensor("ps_" + self._nm(name), list(shape), dtype))
        return t

    def new_dsem(self, name, kind="hw"):
        for i, d in enumerate(self.free_dsems):
            if d.kind == kind:
                self.free_dsems.pop(i)
                d.group_open = False
                return d
        s = self.stack.enter_context(self.nc.semaphore("d_" + self._nm(name)))
        d = DmaSem(s, name)
        d.kind = kind
        self.dsems.append(d)
        return d

    def barrier(self):
        pend = []
        for e in ("pe", "act", "dve", "pool"):
            if self.cnt[e] > 0:
                pend.append(("eng", e, self.cnt[e]))
        for ds in self.dsems + self.csems:
            if ds.issued:
                pend.append(("dma", ds, ds.unit * ds.issued))
        for q in ENGS:
            self.pending[q] = list(pend)
        self.free_dsems = list(self.dsems)

    def _pend(self, q, waits):
        for dep in self.pending.pop(q, []):
            if dep[0] == "eng" and dep[1] == q and q == "pe":
                continue
            self._need(q, dep, waits)

    def coll(self, kind, in_ap, out_ap, groups):
        s = self.stack.enter_context(self.nc.semaphore("c_%d" % len(self.csems)))
        d = DmaSem(s, "coll", unit=1)
        waits = []
        self._pend("pool", waits)
        d.issued = 1
        self.csems.append(d)

        def fn(e):
            return e.collective_compute(kind, mybir.AluOpType.bypass, replica_groups=groups, ins=[in_ap], outs=[out_ap])
        self.ops["pool"].append((self._merge(waits), fn, (d.sem, None)))

    def _merge(self, waits):
        m = {}
        for s, v in waits:
            k = id(s)
            if k not in m or m[k][1] < v:
                m[k] = (s, v)
        return list(m.values())

    def _need(self, eng, dep, waits):
        if dep is None:
            return
        if dep[0] == "eng":
            _, e, idx = dep
            if e == eng and eng in ("pe",):
                return
            key = ("e", e)
            if self.known[eng].get(key, 0) >= idx:
                return
            if e == eng and False:
                return
            self.known[eng][key] = idx
            waits.append((self.esem[e], idx))
        else:
            _, ds, val = dep
            val = max(val, ds.unit * ds.issued)
            ds.group_open = False
            key = ("d", id(ds))
            if self.known[eng].get(key, 0) >= val:
                return
            self.known[eng][key] = val
            waits.append((ds.sem, val))

    def _deps(self, eng, reads, writes):
        waits = []
        for b in reads:
            self._need(eng, b.w, waits)
        for b in writes:
            self._need(eng, b.w, waits)
            for r in b.r:
                self._need(eng, r, waits)
        m = {}
        for s, v in waits:
            k = id(s)
            if k not in m or m[k][1] < v:
                m[k] = (s, v)
        return list(m.values())

    def op(self, eng, fn, reads=(), writes=()):
        self.nrec += 1
        if self.limit and self.nrec > self.limit:
            return
        import traceback
        self.lastdesc = (self.nrec, eng, traceback.extract_stack(limit=3)[0].lineno, [b.name for b in reads], [b.name for b in writes])
        writes = list(writes) + [b for b in reads if b.excl and b not in writes]
        waits = []
        self._pend(eng, waits)
        waits = self._merge(waits + self._deps(eng, reads, writes))
        self.cnt[eng] += 1
        idx = self.cnt[eng]
        tag = ("eng", eng, idx)
        for b in reads:
            if b not in writes:
                b.r.append(tag)
        for b in writes:
            b.w = tag
            b.r = []
        self.ops[eng].append((waits, fn, (self.esem[eng], 1)))

    def dma(self, q, out_ap, in_ap, reads=(), writes=(), sem_buf=None, **kw):
        self.nrec += 1
        if self.limit and self.nrec > self.limit:
            return
        sb = sem_buf
        if sb is None:
            for b in list(writes) + list(reads):
                sb = b
                break
        if sb.dsem is None:
            sb.dsem = self.new_dsem(sb.name, "sw" if q == "pool" else "hw")
        ds = sb.dsem
        waits = []
        self._pend(q, waits)
        waits = waits + self._deps(q, reads, writes)
        if (not ds.group_open) and ds.issued > 0:
            w2 = []
            self._need(q, ("dma", ds, 16 * ds.issued), w2)
            waits += w2
        ds.issued += 1
        ds.group_open = True
        tag = ("dma", ds, 16 * ds.issued)
        for b in reads:
            b.r.append(tag)
        for b in writes:
            b.w = tag
            b.r = []

        def fn(e, out_ap=out_ap, in_ap=in_ap, kw=kw):
            return e.dma_start(out=out_ap, in_=in_ap, **kw)
        if q in ("pool", "act"):
            pass
        self.ops[q].append((self._merge(waits), fn, (ds.sem, 16)))

    def finish(self):
        fin = []
        for ds in self.dsems + self.csems:
            if ds.issued:
                fin.append((ds.sem, ds.unit * ds.issued))
        nc = self.nc
        ops = self.ops
        with nc.Block() as block:
            def emit(e, lst, final=None):
                for waits, fn, inc in lst:
                    for s, v in waits:
                        e.wait_ge(s, v)
                    ins = fn(e)
                    if inc is not None:
                        if inc[1] is None:
                            ins.then_inc(inc[0])
                        else:
                            ins.then_inc(inc[0], inc[1])
                if final:
                    for s, v in final:
                        e.wait_ge(s, v)

            @block.sync
            def _(e):
                emit(e, ops["sp"], fin)

            @block.tensor
            def _(e):
                emit(e, ops["pe"])

            @block.scalar
            def _(e):
                emit(e, ops["act"])

            @block.vector
            def _(e):
                emit(e, ops["dve"])

            @block.gpsimd
            def _(e):
                emit(e, ops["pool"])


BIG = 30000.0
NEGF = -1.0e30
LN2 = 0.6931471805599453
GC = 0.7978845608028654


def host_consts_B(T, slopes2):
    import ml_dtypes
    bf = ml_dtypes.bfloat16
    nb = T // 256
    c = {}
    oh = np.zeros((33, T), np.float32)
    for n in range(nb):
        oh[n, n * 256:(n + 1) * 256] = 1.0
    oh[32, :] = 1.0
    c["ohk"] = oh.astype(bf)
    t = np.arange(T) % 512
    c["crow"] = np.stack([-8.0 * s * t for s in slopes2]).astype(np.float32).astype(bf)
    NM = T // 128 + 4
    m = np.arange(NM)
    p = np.arange(128)
    tab = np.zeros((128, 2, NM), np.float32)
    for h in range(2):
        tab[:, h, :] = slopes2[h] * (p[:, None] + 128.0 * (m[None, :] - (T // 128)))
    c["abias"] = tab
    c["cmask"] = (np.arange(128)[None, :] >= np.arange(128)[:, None]).astype(np.float32).astype(bf)
    cm = (np.arange(64)[None, :] >= np.arange(64)[:, None]).astype(np.float32)
    bm = np.zeros((128, 128), np.float32)
    bm[:64, :64] = cm
    bm[64:, 64:] = cm
    c["hmask"] = np.tile(bm, (1, 4)).astype(np.float32)
    rm = np.ones((64, 512), np.float32)
    rm[:, ::64] = 0.0
    c["rmask"] = rm
    c["ident"] = np.eye(128, dtype=np.float32).astype(bf)
    c["ones64"] = np.full((64, 64), 1.0 / 64.0, np.float32)
    return c


def build_B(nc, P, T, io, maxdist=(None, None)):
    NB = T // 512
    NM = T // 128 + 4
    MOFF = T // 128
    hT, yT = io.get("hT"), io["yT"]
    wfm = P.sbuf("wfm", [128, 8, 640], BF16); Bwfm = P.buf("wfm")
    wtm = P.sbuf("wtm", [128, 8, 192], BF16); Bwtm = P.buf("wtm")
    par = P.sbuf("par", [128, 32], F32); Bpar = P.buf("par")
    wg = P.sbuf("wg", [128, 128], BF16); Bwg = P.buf("wg")
    abias = P.sbuf("abias", [128, 2, NM], F32); Bab = P.buf("abias")
    cmask = P.sbuf("cmask", [128, 128], BF16); Bcm = P.buf("cmask")
    hmask = P.sbuf("hmask", [128, 512], F32); Bhm = P.buf("hmask")
    rmask = P.sbuf("rmask", [64, 512], F32); Brm = P.buf("rmask")
    ident = P.sbuf("ident", [128, 128], BF16); Bid = P.buf("ident")
    ones64 = P.sbuf("ones64", [64, 64], F32); Bo64 = P.buf("ones64")
    QA = P.sbuf("QA", [128, T], BF16); QB = P.sbuf("QB", [128, T], BF16)
    KA = P.sbuf("KA", [128, T], BF16); KB = P.sbuf("KB", [128, T], BF16)
    NBK = T // 512
    BQ = [[P.buf(f"Q{h}_{i}") for i in range(NBK)] for h in range(2)]
    BK = [[P.buf(f"K{h}_{i}") for i in range(NBK)] for h in range(2)]
    Qh = [QA, QB]; Kh = [KA, KB]
    VV = P.sbuf("VV", [128, T // 128, 2, 65], BF16); BV = [P.buf(f"VV{i}") for i in range(NBK)]
    VH = P.sbuf("VH", [128, T // 128, 64], BF16); BVH = [P.buf(f"VH{i}") for i in range(NBK)]
    ones64r = P.sbuf("ones64r", [128, 64], F32); Bo64r = P.buf("ones64r")
    kmT = P.sbuf("kmT", [128, 32], BF16); Bkm = P.buf("kmT")
    banks = [P.psum(f"bank{i}", [128, 512]) for i in range(8)]
    Bbank = [P.buf(f"bank{i}") for i in range(8)]
    for b_ in Bbank:
        b_.excl = True
    rr = [0]

    def nbank():
        i = rr[0] % 4
        rr[0] += 1
        return banks[i], Bbank[i]
    rr2 = [0]

    def nbank2():
        i = 4 + rr2[0] % 2
        rr2[0] += 1
        return banks[i], Bbank[i]
    pvb = [(banks[6], Bbank[6]), (banks[7], Bbank[7])]

    P.dma("pool", wfm[:], io["wfm"].rearrange("(c p) n -> p c n", p=128), writes=[Bwfm])
    P.dma("pool", wtm[:], io["wtm"].rearrange("(c p) n -> p c n", p=128), writes=[Bwtm])
    P.dma("pool", wg[:], io["wg"], writes=[Bwg])
    P.dma("sp", par[:, 0:16], io["par"], writes=[Bpar])
    P.dma("sp", abias[:], io["abias"], writes=[Bab])
    P.dma("sp", cmask[:], io["cmask"], writes=[Bcm])
    P.dma("sp", hmask[:], io["hmask"], writes=[Bhm])
    P.dma("sp", rmask[:], io["rmask"], writes=[Brm])
    P.dma("sp", ident[:], io["ident"], writes=[Bid])
    P.dma("sp", ones64[:], io["ones64"], writes=[Bo64])
    for h in range(2):
        P.op("pool", lambda e, h=h: e.memset(Qh[h][:], 0.0), writes=BQ[h])
        P.op("pool", lambda e, h=h: e.memset(Kh[h][:], 0.0), writes=BK[h])
    P.dma("sp", KA[64:97, :], io["ohk"], writes=BK[0], sem_buf=BK[0][0])
    P.dma("sp", KB[0:33, :], io["ohk"], writes=BK[1], sem_buf=BK[1][0])
    P.dma("sp", QA[96:97, :], io["crow"][0:1, :], writes=BQ[0], sem_buf=BQ[0][0])
    P.dma("sp", QB[32:33, :], io["crow"][1:2, :], writes=BQ[1], sem_buf=BQ[1][0])
    P.op("pool", lambda e: e.memset(VV[:, :, :, 64:65], 1.0), writes=BV)
    P.op("pool", lambda e: e.memset(ones64r[:], 1.0), writes=[Bo64r])
    P.op("pool", lambda e: e.memset(kmT[:], 0.0), writes=[Bkm])

    L = slice(64, 128)
    H = slice(0, 64)
    def pc(rows, j):
        return par[rows, j:j + 1]
    P.op("dve", lambda e: e.memset(par[:, 24:25], -LN2), reads=[Bpar], writes=[Bpar])
    P.op("dve", lambda e: e.memset(par[:, 25:26], 1e-6), reads=[Bpar], writes=[Bpar])
    P.op("dve", lambda e: e.memset(par[:, 26:27], 1.0), reads=[Bpar], writes=[Bpar])
    P.op("dve", lambda e: e.tensor_scalar(out=par[L, 8:10], in0=par[L, 5:7], scalar1=0.5, scalar2=None, op0=ALU.mult), reads=[Bpar], writes=[Bpar])
    P.op("act", lambda e: e.activation(out=pc(L, 12), in_=pc(L, 7), func=AF.Exp, scale=-1.0), reads=[Bpar], writes=[Bpar])
    P.op("act", lambda e: e.activation(out=pc(L, 13), in_=pc(L, 12), func=AF.Ln, bias=pc(L, 26)), reads=[Bpar], writes=[Bpar])
    P.op("dve", lambda e: e.tensor_scalar(out=pc(L, 10), in0=pc(L, 13), scalar1=-8.0, scalar2=None, op0=ALU.mult), reads=[Bpar], writes=[Bpar])
    P.op("dve", lambda e: e.tensor_scalar(out=pc(L, 11), in0=pc(L, 13), scalar1=-4.0, scalar2=None, op0=ALU.mult), reads=[Bpar], writes=[Bpar])
    P.op("dve", lambda e: e.tensor_tensor(out=pc(H, 20), in0=pc(H, 1), in1=pc(H, 0), op=ALU.subtract), reads=[Bpar], writes=[Bpar])
    P.op("act", lambda e: e.activation(out=pc(H, 21), in_=pc(H, 20), func=AF.Tanh, scale=0.5), reads=[Bpar], writes=[Bpar])
    P.op("dve", lambda e: e.tensor_scalar(out=pc(H, 22), in0=pc(H, 21), scalar1=0.5, scalar2=0.5, op0=ALU.mult, op1=ALU.add), reads=[Bpar], writes=[Bpar])
    P.op("dve", lambda e: e.tensor_tensor(out=pc(H, 19), in0=pc(H, 22), in1=pc(H, 3), op=ALU.mult), reads=[Bpar], writes=[Bpar])
    P.op("dve", lambda e: e.tensor_scalar(out=pc(H, 16), in0=pc(H, 19), scalar1=-0.5, scalar2=0.5, op0=ALU.mult, op1=ALU.add), reads=[Bpar], writes=[Bpar])
    P.op("dve", lambda e: e.tensor_scalar(out=pc(H, 18), in0=pc(H, 16), scalar1=-1.0, scalar2=None, op0=ALU.mult), reads=[Bpar], writes=[Bpar])
    P.op("dve", lambda e: e.tensor_scalar(out=pc(H, 23), in0=pc(H, 19), scalar1=1e-30, scalar2=None, op0=ALU.max), reads=[Bpar], writes=[Bpar])
    P.op("dve", lambda e: e.tensor_tensor(out=pc(H, 17), in0=pc(H, 23), in1=pc(H, 16), op=ALU.add), reads=[Bpar], writes=[Bpar])

    def rot(name, shape, dt, n):
        ts = [P.sbuf(f"{name}{i}", shape, dt) for i in range(n)]
        bs = [P.buf(f"{name}{i}") for i in range(n)]
        return ts, bs
    hblk, Bhblk = rot("hblk", [128, 8, 512], BF16, 2)
    xbuf, Bxbuf = rot("xbuf", [128, 515], F32, 2)
    NW = 2
    W = {}

    class HV:
        def __init__(self, t):
            self.t = t

        def __getitem__(self, key):
            if isinstance(key, tuple):
                return self.t[(slice(0, 64),) + tuple(key[1:])]
            return self.t[0:64, :]
    share = {"thq": "ysb", "thf": "t1", "thg": "t2", "gs2": "xc", "fg": "thr", "kk": "thi", "bb": "aa", "eb": "a2", "enb": "uu",
             "qs2": "hh", "kve": "sh1", "osb": "sh2", "osq": "sh3", "rstd": "sh4"}
    for nm, shp, dt in [("ysb", [128, 512], F32), ("t1", [128, 512], F32), ("t2", [128, 512], F32),
                        ("xc", [128, 512], F32), ("xcb", [128, 512], BF16), ("thr", [128, 512], F32), ("thi", [128, 512], F32),
                        ("aa", [128, 512], F32), ("a2", [128, 512], F32), ("uu", [128, 512], F32), ("hh", [128, 512], F32),
                        ("sh1", [128, 512], F32), ("sh2", [128, 512], F32), ("sh3", [128, 512], F32), ("sh4", [128, 512], F32),
                        ("yo", [128, 512], BF16),
                        ("qt", [64, 512], BF16), ("kt", [64, 512], BF16),
                        ("ktokE", [128, 4, 64], BF16), ("ktokO", [128, 4, 64], BF16), ("attm", [128, 512], BF16), ("ebl", [64, 8], F32),
                        ("hy", [64, 512], BF16),
                        ("gsb", [128, 32], F32), ("top8", [128, 8], F32), ("mp", [128, 32], BF16),
                        ("pt", [128, 512], BF16), ("onum", [65, 512], F32), ("rec", [65, 512], F32), ("my", [64, 512], BF16)]:
        n = {"pt": 7, "gsb": 4, "top8": 4, "mp": 4, "sh1": 1, "sh2": 1, "sh3": 1, "sh4": 1, "rec": 1}.get(nm, NW)
        W[nm] = rot(nm, shp, dt, n)
    for hn, ln in share.items():
        ts, _ = W[ln]
        W[hn] = ([HV(t) for t in ts], [P.buf(f"{hn}{i}") for i in range(len(ts))])
    ctr = {}

    def wt(nm):
        i = ctr.get(nm, 0)
        ctr[nm] = i + 1
        ts, bs = W[nm]
        return ts[i % len(ts)], bs[i % len(bs)]
    for i in range(4):
        P.op("pool", lambda e, i=i: e.memset(W["gsb"][0][i][:], NEGF), writes=[W["gsb"][1][i]])
    for nm_ in ("ktokE", "ktokO"):
        for i in range(NW):
            P.op("pool", lambda e, nm_=nm_, i=i: e.memset(W[nm_][0][i][:], 0.0), writes=[W[nm_][1][i]])
    Sst = P.sbuf("Sst", [64, 64], F32); BS = P.buf("Sst")
    Sbf, BSbf = rot("Sbf", [64, 64], BF16, 4)
    P.op("dve", lambda e: e.memset(Sst[:], 0.0), writes=[BS])
    P.op("dve", lambda e: e.memset(xbuf[1][L, 512:515], 0.0), writes=[Bxbuf[1]])
    hprev = [None]

    S1 = {}

    class Deferred:
        def __init__(self):
            self.q = []
            self.atomic = None
            self.tag = 0

        def op(self, *a, **k):
            (self.atomic if self.atomic is not None else self.q).append(("op", a, k, self.tag))

        def dma(self, *a, **k):
            (self.atomic if self.atomic is not None else self.q).append(("dma", a, k, self.tag))

        def begin(self):
            self.atomic = []

        def end(self):
            self.q.append(("grp", self.atomic, None, self.tag))
            self.atomic = None

        def _emit(self):
            kind, a, k, _ = self.q.pop(0)
            items = a if kind == "grp" else [(kind, a, k, 0)]
            for kd, aa, kk, _t in items:
                (P.op if kd == "op" else P.dma)(*aa, **kk)
            return len(items)

        def flush(self, n=None):
            while self.q and (n is None or n > 0):
                c = self._emit()
                if n is not None:
                    n -= c

        def flush_upto(self, tag):
            while self.q and self.q[0][3] <= tag:
                self._emit()

        def count_upto(self, tag):
            return sum(1 for it in self.q if it[3] <= tag)
    PQ = Deferred()

    def stage1(blk):
        c0 = blk * 512
        hb, Bhb = hblk[blk % 2], Bhblk[blk % 2]
        if "hsrc" in io:
            for j in range(4):
                P.dma("sp", hb[:, 2 * j:2 * j + 2, :], io["hsrc"](blk, j), writes=[Bhb])
        else:
            P.dma("sp", hb[:], hT[:, c0:c0 + 512].rearrange("(c p) n -> p c n", p=128), writes=[Bhb])

        def inproj(col0, M, bank, Bb, rows=slice(0, 128)):
            for k in range(8):
                P.op("pe", lambda e, k=k: e.matmul(bank[rows, :], lhsT=wfm[:, k, col0:col0 + M], rhs=hb[:, k, :], start=(k == 0), stop=(k == 7)),
                     reads=[Bwfm, Bhb], writes=[Bb])
        b4, Bb4 = nbank(); inproj(320, 128, b4, Bb4)
        b5, Bb5 = nbank(); inproj(448, 128, b5, Bb5)
        P.op("act", lambda e: e.activation(out=QA[0:64, c0:c0 + 512], in_=b4[0:64, :], func=AF.Identity), reads=[Bb4], writes=[BQ[0][blk]])
        P.op("dve", lambda e: e.tensor_copy(out=QB[64:128, c0:c0 + 512], in_=b4[64:128, :]), reads=[Bb4], writes=[BQ[1][blk]])
        P.op("act", lambda e: e.activation(out=KA[0:64, c0:c0 + 512], in_=b5[0:64, :], func=AF.Identity), reads=[Bb5], writes=[BK[0][blk]])
        P.op("dve", lambda e: e.tensor_copy(out=KB[64:128, c0:c0 + 512], in_=b5[64:128, :]), reads=[Bb5], writes=[BK[1][blk]])
        kms, Bkms = wt("top8")
        P.op("dve", lambda e: e.tensor_reduce(out=kms[:, 0:2], in_=b5[:].rearrange("p (n k) -> p n k", n=2), axis=AX.X, op=ALU.add), reads=[Bb5], writes=[Bkms])
        P.op("dve", lambda e: e.tensor_scalar(out=kmT[:, 2 * blk:2 * blk + 2], in0=kms[:, 0:2], scalar1=1.0 / 256.0, scalar2=None, op0=ALU.mult), reads=[Bkms], writes=[Bkm])
        for pr in range(2):
            bv, Bbv = nbank()
            for tt in (2 * pr, 2 * pr + 1):
                o = (tt % 2) * 192
                for k in range(8):
                    P.op("pe", lambda e, k=k, tt=tt, o=o, bv=bv: e.matmul(bv[:, o:o + 192], lhsT=hb[:, k, tt * 128:(tt + 1) * 128], rhs=wtm[:, k, :], start=(k == 0), stop=(k == 7)),
                         reads=[Bwtm, Bhb], writes=[Bbv])
            for tt in (2 * pr, 2 * pr + 1):
                gi = blk * 4 + tt
                o = (tt % 2) * 192
                P.op("act", lambda e, gi=gi, o=o, bv=bv: e.activation(out=VH[:, gi, :], in_=bv[:, o:o + 64], func=AF.Identity), reads=[Bbv], writes=[BVH[blk]])
                P.op("dve", lambda e, gi=gi, o=o, bv=bv: e.tensor_copy(out=VV[:, gi, :, 0:64], in_=bv[:, o + 64:o + 192].rearrange("p (h d) -> p h d", h=2)), reads=[Bbv], writes=[BV[blk]])
        b1, Bb1 = nbank(); inproj(0, 128, b1, Bb1)
        b2, Bb2 = nbank(); inproj(128, 128, b2, Bb2)
        b3, Bb3 = nbank(); inproj(256, 64, b3, Bb3, rows=slice(0, 64))
        xb, Bxb = xbuf[blk % 2], Bxbuf[blk % 2]
        P.op("act", lambda e: e.activation(out=xb[L, 3:515], in_=b1[L, :], func=AF.Identity), reads=[Bb1], writes=[Bxb])
        xo_, Bxo_ = xbuf[(blk + 1) % 2], Bxbuf[(blk + 1) % 2]
        P.op("pool", lambda e: e.tensor_copy(out=xb[L, 0:3], in_=xo_[L, 512:515]), reads=[Bxo_], writes=[Bxb])
        ysb, Bysb = wt("ysb")
        P.op("act", lambda e: e.activation(out=ysb[L, :], in_=b2[L, :], func=AF.Identity), reads=[Bb2], writes=[Bysb])
        thq, Bthq = wt("thq"); thf, Bthf = wt("thf"); thg, Bthg = wt("thg")
        P.op("act", lambda e: e.activation(out=thq[:], in_=b1[H, :], func=AF.Tanh, scale=0.5), reads=[Bb1], writes=[Bthq])
        P.op("act", lambda e: e.activation(out=thf[:], in_=b2[H, :], func=AF.Tanh, scale=0.5), reads=[Bb2], writes=[Bthf])
        P.op("act", lambda e: e.activation(out=thg[:], in_=b3[H, :], func=AF.Tanh, scale=0.5), reads=[Bb3], writes=[Bthg])
        qs2, Bqs2 = wt("qs2"); gs2, Bgs2 = wt("gs2")
        P.op("dve", lambda e: e.scalar_tensor_tensor(out=qs2[:], in0=thq[:], scalar=1.0, in1=b1[H, :], op0=ALU.add, op1=ALU.mult), reads=[Bthq, Bb1], writes=[Bqs2])
        P.op("dve", lambda e: e.scalar_tensor_tensor(out=gs2[:], in0=thg[:], scalar=1.0, in1=b3[H, :], op0=ALU.add, op1=ALU.mult), reads=[Bthg, Bb3], writes=[Bgs2])
        S1[blk] = dict(xb=(xb, Bxb), ysb=(ysb, Bysb), thf=(thf, Bthf), qs2=(qs2, Bqs2), gs2=(gs2, Bgs2))

    def stage2(blk):
        c0 = blk * 512
        d = S1.pop(blk)
        xb, Bxb = d["xb"]; ysb, Bysb = d["ysb"]; thf, Bthf = d["thf"]; qs2, Bqs2 = d["qs2"]; gs2, Bgs2 = d["gs2"]
        xo, Bxo = xbuf[(blk + 1) % 2], Bxbuf[(blk + 1) % 2]
        xc, Bxc = wt("xc")
        PQ.op("pool", lambda e: e.tensor_scalar(out=xc[L, :], in0=xb[L, 3:515], scalar1=pc(L, 3), scalar2=pc(L, 4), op0=ALU.mult, op1=ALU.add), reads=[Bxb, Bpar], writes=[Bxc])
        for j in (2, 1, 0):
            PQ.op("dve", lambda e, j=j: e.scalar_tensor_tensor(out=xc[L, :], in0=xb[L, j:j + 512], scalar=pc(L, j), in1=xc[L, :], op0=ALU.mult, op1=ALU.add), reads=[Bxb, Bpar, Bxc], writes=[Bxc])
        xcb, Bxcb = wt("xcb")
        PQ.op("pool", lambda e: e.tensor_copy(out=xcb[L, :], in_=xc[L, :]), reads=[Bxc], writes=[Bxcb])
        bg, Bbg = nbank2()
        bg2, Bbg2 = nbank2()
        PQ.op("pe", lambda e: e.matmul(bg[L, :], lhsT=wg[L, 0:64], rhs=xcb[L, :], start=True, stop=True), reads=[Bwg, Bxcb], writes=[Bbg])
        PQ.op("pe", lambda e: e.matmul(bg2[L, :], lhsT=wg[L, 64:128], rhs=xcb[L, :], start=True, stop=True), reads=[Bwg, Bxcb], writes=[Bbg2])
        thr, Bthr = wt("thr"); thi, Bthi = wt("thi")
        PQ.op("act", lambda e: e.activation(out=thr[L, :], in_=bg[L, :], func=AF.Tanh, scale=0.5, bias=pc(L, 8)), reads=[Bbg, Bpar], writes=[Bthr])
        PQ.op("act", lambda e: e.activation(out=thi[L, :], in_=bg2[L, :], func=AF.Tanh, scale=0.5, bias=pc(L, 9)), reads=[Bbg2, Bpar], writes=[Bthi])
        aa, Baa = wt("aa"); a2, Ba2 = wt("a2")
        PQ.op("act", lambda e: e.activation(out=aa[L, :], in_=thr[L, :], func=AF.Exp, scale=pc(L, 11), bias=pc(L, 11)), reads=[Bthr, Bpar], writes=[Baa])
        PQ.op("act", lambda e: e.activation(out=a2[L, :], in_=thr[L, :], func=AF.Exp, scale=pc(L, 10), bias=pc(L, 10)), reads=[Bthr, Bpar], writes=[Ba2])
        t1, Bt1 = wt("t1"); t2, Bt2 = wt("t2")
        PQ.op("act", lambda e: e.activation(out=t1[L, :], in_=ysb[L, :], func=AF.Square), reads=[Bysb], writes=[Bt1])
        PQ.op("pool", lambda e: e.tensor_scalar(out=t1[L, :], in0=t1[L, :], scalar1=0.044715, scalar2=1.0, op0=ALU.mult, op1=ALU.add), reads=[Bt1], writes=[Bt1])
        PQ.op("pool", lambda e: e.tensor_tensor(out=t1[L, :], in0=t1[L, :], in1=ysb[L, :], op=ALU.mult), reads=[Bt1, Bysb], writes=[Bt1])
        PQ.op("act", lambda e: e.activation(out=t2[L, :], in_=t1[L, :], func=AF.Tanh, scale=GC), reads=[Bt1], writes=[Bt2])
        PQ.op("dve", lambda e: e.scalar_tensor_tensor(out=t2[L, :], in0=t2[L, :], scalar=1.0, in1=ysb[L, :], op0=ALU.add, op1=ALU.mult), reads=[Bt2, Bysb], writes=[Bt2])
        uu, Buu = wt("uu")
        PQ.op("dve", lambda e: e.scalar_tensor_tensor(out=uu[L, :], in0=thi[L, :], scalar=1.0, in1=xc[L, :], op0=ALU.add, op1=ALU.mult), reads=[Bthi, Bxc], writes=[Buu])
        fg, Bfg = wt("fg"); kk, Bkk = wt("kk")
        PQ.op("dve", lambda e: e.tensor_scalar(out=fg[:], in0=thf[:], scalar1=pc(H, 16), scalar2=pc(H, 17), op0=ALU.mult, op1=ALU.add), reads=[Bthf, Bpar], writes=[Bfg])
        PQ.op("dve", lambda e: e.tensor_scalar(out=kk[:], in0=thf[:], scalar1=pc(H, 18), scalar2=pc(H, 16), op0=ALU.mult, op1=ALU.add), reads=[Bthf, Bpar], writes=[Bkk])
        PQ.op("act", lambda e: e.activation(out=a2[L, :], in_=a2[L, :], func=AF.Ln, scale=-1.0, bias=pc(L, 26)), reads=[Ba2], writes=[Ba2])
        PQ.op("act", lambda e: e.activation(out=fg[:], in_=fg[:], func=AF.Ln), reads=[Bfg], writes=[Bfg])
        PQ.op("act", lambda e: e.activation(out=a2[L, :], in_=a2[L, :], func=AF.Exp, scale=0.5), reads=[Ba2], writes=[Ba2])
        bb, Bbb = wt("bb")
        PQ.op("dve", lambda e: e.tensor_tensor_scan(out=bb[:], data0=rmask[:], data1=fg[:], initial=0.0, op0=ALU.mult, op1=ALU.add), reads=[Brm, Bfg], writes=[Bbb])
        eb, Beb = wt("eb"); enb, Benb = wt("enb"); ebl, Bebl = wt("ebl")
        PQ.op("act", lambda e: e.activation(out=eb[:], in_=bb[:], func=AF.Exp, bias=pc(H, 24)), reads=[Bbb, Bpar], writes=[Beb])
        PQ.op("act", lambda e: e.activation(out=enb[:], in_=bb[:], func=AF.Exp, scale=-1.0), reads=[Bbb], writes=[Benb])
        PQ.op("act", lambda e: e.activation(out=ebl[:], in_=bb[:, 63:512:64], func=AF.Exp), reads=[Bbb], writes=[Bebl])
        PQ.op("dve", lambda e: e.scalar_tensor_tensor(out=uu[L, :], in0=uu[L, :], scalar=0.5, in1=a2[L, :], op0=ALU.mult, op1=ALU.mult), reads=[Buu, Ba2], writes=[Buu])
        hh, Bhh = wt("hh")
        if hprev[0] is None:
            PQ.op("dve", lambda e: e.tensor_tensor_scan(out=hh[L, :], data0=aa[L, :], data1=uu[L, :], initial=0.0, op0=ALU.mult, op1=ALU.add), reads=[Baa, Buu], writes=[Bhh])
        else:
            hp, Bhp = hprev[0]
            PQ.op("dve", lambda e, hp=hp: e.tensor_tensor_scan(out=hh[L, :], data0=aa[L, :], data1=uu[L, :], initial=hp[L, 511:512], op0=ALU.mult, op1=ALU.add), reads=[Baa, Buu, Bhp], writes=[Bhh])
        hprev[0] = (hh, Bhh)
        yo, Byo = wt("yo")
        PQ.op("dve", lambda e: e.scalar_tensor_tensor(out=yo[L, :], in0=hh[L, :], scalar=0.5, in1=t2[L, :], op0=ALU.mult, op1=ALU.mult), reads=[Bhh, Bt2], writes=[Byo])
        PQ.dma("sp", yT[0, :, c0:c0 + 512], yo[L, :], reads=[Byo])
        qt, Bqt = wt("qt"); kt, Bkt = wt("kt")
        PQ.op("dve", lambda e: e.tensor_tensor(out=qt[:], in0=qs2[:], in1=eb[:], op=ALU.mult), reads=[Bqs2, Beb], writes=[Bqt])
        PQ.op("dve", lambda e: e.tensor_tensor(out=kt[:], in0=kk[:], in1=enb[:], op=ALU.mult), reads=[Bkk, Benb], writes=[Bkt])
        btr, Bbtr = nbank2()
        btr16 = btr[:].bitcast(BF16)
        ktokE, BktokE = wt("ktokE"); ktokO, BktokO = wt("ktokO")
        for tt in range(4):
            PQ.op("pe", lambda e, tt=tt: e.transpose(btr16[:, tt * 64:(tt + 1) * 64], in_=kt[:, tt * 128:(tt + 1) * 128], identity=ident[0:64, 0:64]), reads=[Bkt, Bid], writes=[Bbtr])
        PQ.op("act", lambda e: e.activation(out=ktokE[0:64, :, :].rearrange("p t d -> p (t d)"), in_=btr16[0:64, 0:256], func=AF.Identity), reads=[Bbtr], writes=[BktokE])
        PQ.op("act", lambda e: e.activation(out=ktokO[64:128, :, :].rearrange("p t d -> p (t d)"), in_=btr16[64:128, 0:256], func=AF.Identity), reads=[Bbtr], writes=[BktokO])
        bkv, Bbkv = nbank2()
        for c in range(8):
            tt, hf = c // 2, c % 2
            gi = blk * 4 + tt
            kx, Bkx = (ktokE, BktokE) if hf == 0 else (ktokO, BktokO)
            PQ.op("pe", lambda e, c=c, tt=tt, gi=gi, kx=kx: e.matmul(bkv[0:64, c * 64:(c + 1) * 64], lhsT=kx[:, tt, :], rhs=VH[:, gi, :], start=True, stop=True),
                 reads=[Bkx, BVH[blk]], writes=[Bbkv])
        batt, Bbatt = nbank2()
        for tt in range(4):
            PQ.op("pe", lambda e, tt=tt: e.matmul(batt[:, tt * 128:(tt + 1) * 128], lhsT=kt[:, tt * 128:(tt + 1) * 128], rhs=qt[:, tt * 128:(tt + 1) * 128], start=True, stop=True),
                 reads=[Bkt, Bqt], writes=[Bbatt])
        attm, Battm = wt("attm")
        PQ.op("dve", lambda e: e.tensor_tensor(out=attm[:], in0=batt[:], in1=hmask[:], op=ALU.mult), reads=[Bbatt, Bhm], writes=[Battm])
        kve, Bkve = wt("kve")
        for c in range(8):
            PQ.op("act", lambda e, c=c: e.activation(out=kve[:, c * 64:(c + 1) * 64], in_=bkv[0:64, c * 64:(c + 1) * 64], func=AF.Identity, scale=ebl[:, c:c + 1]), reads=[Bbkv, Bebl], writes=[Bkve])
        bo, Bbo = nbank2()
        for c in range(8):
            tt, hf = c // 2, c % 2
            gi = blk * 4 + tt
            sb, Bsb = Sbf[(blk * 8 + c) % 4], BSbf[(blk * 8 + c) % 4]
            if hf == 0:
                PQ.begin()
            PQ.op("act", lambda e, sb=sb: e.activation(out=sb[:], in_=Sst[:], func=AF.Identity), reads=[BS], writes=[Bsb])
            if hf == 0:
                PQ.op("pe", lambda e, tt=tt, gi=gi: e.matmul(bo[0:64, tt * 128:(tt + 1) * 128], lhsT=VH[:, gi, :], rhs=attm[:, tt * 128:(tt + 1) * 128], start=True, stop=False),
                     reads=[BVH[blk], Battm], writes=[Bbo])
            PQ.op("pe", lambda e, c=c, sb=sb, hf=hf: e.matmul(bo[0:64, c * 64:(c + 1) * 64], lhsT=sb[:], rhs=qt[:, c * 64:(c + 1) * 64], start=False, stop=(hf == 1)),
                 reads=[Bsb, Bqt], writes=[Bbo])
            PQ.op("dve", lambda e, c=c: e.scalar_tensor_tensor(out=Sst[:], in0=Sst[:], scalar=ebl[:, c:c + 1], in1=kve[:, c * 64:(c + 1) * 64], op0=ALU.mult, op1=ALU.add), reads=[BS, Bebl, Bkve], writes=[BS])
            if hf == 1:
                PQ.end()
        osb, Bosb = wt("osb"); osq, Bosq = wt("osq")
        PQ.op("act", lambda e: e.activation(out=osb[:], in_=bo[0:64, :], func=AF.Identity), reads=[Bbo], writes=[Bosb])
        PQ.op("act", lambda e: e.activation(out=osq[:], in_=bo[0:64, :], func=AF.Square), reads=[Bbo], writes=[Bosq])
        bms, Bbms = nbank2()
        PQ.op("pe", lambda e: e.matmul(bms[0:64, :], lhsT=ones64[:], rhs=osq[:], start=True, stop=True), reads=[Bo64, Bosq], writes=[Bbms])
        rstd, Brstd = wt("rstd")
        PQ.op("act", lambda e: e.activation(out=rstd[:], in_=bms[0:64, :], func=AF.Ln, bias=pc(H, 25)), reads=[Bbms, Bpar], writes=[Brstd])
        PQ.op("act", lambda e: e.activation(out=rstd[:], in_=rstd[:], func=AF.Exp, scale=-0.5), reads=[Brstd], writes=[Brstd])
        PQ.op("dve", lambda e: e.tensor_tensor(out=osb[:], in0=osb[:], in1=rstd[:], op=ALU.mult), reads=[Bosb, Brstd], writes=[Bosb])
        PQ.op("dve", lambda e: e.tensor_scalar(out=osb[:], in0=osb[:], scalar1=pc(H, 2), scalar2=0.5, op0=ALU.mult, op1=ALU.mult), reads=[Bosb, Bpar], writes=[Bosb])
        hy, Bhy = wt("hy")
        PQ.op("dve", lambda e: e.tensor_tensor(out=hy[:], in0=osb[:], in1=gs2[:], op=ALU.mult), reads=[Bosb, Bgs2], writes=[Bhy])
        PQ.dma("sp", yT[1, :, c0:c0 + 512], hy[:], reads=[Bhy])

    def stage3(blk, tick=None):
        for h in range(2):
            head3(blk, h, tick)

    def head3(blk, h, tick=None):
        c0 = blk * 512
        if True:
            Q, K = Qh[h], Kh[h]
            dr = slice(0, 64) if h == 0 else slice(64, 128)
            mr = slice(64, 96) if h == 0 else slice(0, 32)
            bgt, Bbgt = nbank()
            bmp, Bbmp = nbank()
            bmp16 = bmp[:].bitcast(BF16)
            any_mp = False
            for st in range(4):
                j = 2 * blk + (st // 2)
                if j == 0:
                    continue
                any_mp = True
                q0 = c0 + st * 128
                P.op("pe", lambda e, q0=q0, st=st: e.matmul(bgt[:, st * 32:(st + 1) * 32], lhsT=Q[dr, q0:q0 + 128], rhs=kmT[dr, 0:32], start=True, stop=True),
                     reads=[BQ[h][blk], Bkm], writes=[Bbgt])
                gsb, Bgsb = wt("gsb"); top8, Btop8 = wt("top8"); mp, Bmp = wt("mp")
                P.op("dve", lambda e, st=st, j=j, gsb=gsb: e.tensor_copy(out=gsb[:, 0:j], in_=bgt[:, st * 32:st * 32 + j]), reads=[Bbgt], writes=[Bgsb])
                P.op("dve", lambda e, j=j, gsb=gsb, top8=top8: e.max(out=top8[:], in_=gsb[:, 0:max(j, 8)]), reads=[Bgsb], writes=[Btop8])
                P.op("pool", lambda e, mp=mp: e.memset(mp[:], 0.0), writes=[Bmp])
                P.op("dve", lambda e, j=j, gsb=gsb, top8=top8, mp=mp: e.tensor_scalar(out=mp[:, 0:j], in0=gsb[:, 0:j], scalar1=top8[:, 2:3], scalar2=-8.0 * BIG, op0=ALU.is_lt, op1=ALU.mult),
                     reads=[Bgsb, Btop8, Bmp], writes=[Bmp])
                P.op("pe", lambda e, st=st, mp=mp: e.transpose(bmp16[mr, st * 128:(st + 1) * 128], in_=mp[:], identity=ident[:]), reads=[Bmp, Bid], writes=[Bbmp])
            if any_mp:
                s0 = 0 if blk > 0 else 2
                P.op("act", lambda e, s0=s0: e.activation(out=Q[mr, c0 + s0 * 128:c0 + 512], in_=bmp16[mr, s0 * 128:512], func=AF.Identity), reads=[Bbmp], writes=[BQ[h][blk]])
            pv, Bpv = pvb[h]
            tiles = [(kt_, 0) for kt_ in range(4 * blk) if (maxdist[h] is None or (c0 - (kt_ * 128 + 127)) <= maxdist[h])] + [(4 * blk + kk_, kk_) for kk_ in range(4)]
            LA = 3
            pend = []
            state = {"first": True}

            def emit_pv(item):
                kti, n0, pt, Bpt, kb, last = item
                first = state["first"]
                P.op("pe", lambda e, kti=kti, n0=n0, pt=pt, first=first, last=last: e.matmul(pv[0:65, n0:512], lhsT=VV[:, kti, h, :], rhs=pt[:, n0:512], start=first, stop=last),
                     reads=[BV[kb], Bpt], writes=[Bpv])
                state["first"] = False
            for (kti, own) in tiles:
                isown = kti >= 4 * blk
                n0 = own * 128 if isown else 0
                k0 = kti * 128
                kb = kti // 4
                bs, Bbs = nbank()
                P.op("pe", lambda e, k0=k0, n0=n0, bs=bs: e.matmul(bs[:, n0:512], lhsT=K[:, k0:k0 + 128], rhs=Q[:, c0 + n0:c0 + 512], start=True, stop=True),
                     reads=[BK[h][kb], BQ[h][blk]], writes=[Bbs])
                pt, Bpt = wt("pt")
                m = kti - 4 * blk + MOFF
                P.op("act", lambda e, n0=n0, bs=bs, pt=pt, m=m: e.activation(out=pt[:, n0:512], in_=bs[:, n0:512], func=AF.Exp, scale=0.125, bias=abias[:, h, m:m + 1]),
                     reads=[Bbs, Bab], writes=[Bpt])
                if isown:
                    P.op("dve", lambda e, n0=n0, pt=pt: e.tensor_tensor(out=pt[:, n0:n0 + 128], in0=pt[:, n0:n0 + 128], in1=cmask[:], op=ALU.mult), reads=[Bpt, Bcm], writes=[Bpt])
                last = (kti == tiles[-1][0])
                pend.append((kti, n0, pt, Bpt, kb, last))
                if len(pend) > LA:
                    emit_pv(pend.pop(0))
                if tick is not None:
                    tick()
            while pend:
                emit_pv(pend.pop(0))
            onum, Bonum = wt("onum"); rec, Brec = wt("rec"); my, Bmy = wt("my")
            P.op("act", lambda e: e.activation(out=onum[0:65, :], in_=pv[0:65, :], func=AF.Identity), reads=[Bpv], writes=[Bonum])
            P.op("act", lambda e: e.activation(out=rec[64:65, :], in_=onum[64:65, :], func=AF.Ln), reads=[Bonum], writes=[Brec])
            P.op("act", lambda e: e.activation(out=rec[64:65, :], in_=rec[64:65, :], func=AF.Exp, scale=-1.0), reads=[Brec], writes=[Brec])
            bbc, Bbbc = nbank()
            P.op("pe", lambda e: e.matmul(bbc[0:64, :], lhsT=ones64r[64:65, :], rhs=rec[64:65, :], start=True, stop=True), reads=[Bo64r, Brec], writes=[Bbbc])
            P.op("dve", lambda e: e.tensor_tensor(out=my[:], in0=onum[0:64, :], in1=bbc[0:64, :], op=ALU.mult), reads=[Bonum, Bbbc], writes=[Bmy])
            P.dma("sp", yT[2 + h, :, c0:c0 + 512], my[:], reads=[Bmy])

    import os
    stop = os.environ.get("PHB_STOP", "")
    if stop == "init":
        return
    for blk in range(NB):
        stage1(blk)
        if stop == "s1":
            return
        PQ.tag = blk
        stage2(blk)
        if blk > 0:
            ntl = max(1, 2 * (4 * (blk - 1) + 4))
            must = PQ.count_upto(blk - 1)
            per = max(1, (must + len(PQ.q) // 2 + ntl - 1) // ntl)
            stage3(blk - 1, tick=lambda per=per: PQ.flush(per))
            PQ.flush_upto(blk - 1)
        if stop == "s2":
            PQ.flush()
            return
    stage3(NB - 1, tick=lambda: PQ.flush(2))
    PQ.flush()


EPS = 1e-6


def host_consts_C():
    c = {}
    oa = np.zeros((128, 128), np.float32)
    oa[:64, :] = 1.0 / 256.0
    c["onesA"] = oa
    c["onesC"] = np.full((128, 128), 1.0 / 512.0, np.float32)
    c["ones1k"] = np.full((128, 128), 1.0 / 1024.0, np.float32)
    return c


def rms_stats(P, banks, Bbanks, src_fn, nchunk, ones_t, Bones, sq_tiles, eps_ap, Bpar, rstd, Brstd, srcbufs):
    bank, Bbank = banks
    for c in range(nchunk):
        sq, Bsq = sq_tiles[c % len(sq_tiles)]
        src = src_fn(c)
        P.op("act", lambda e, sq=sq, src=src: e.activation(out=sq[:], in_=src, func=AF.Square), reads=srcbufs(c), writes=[Bsq])
        P.op("pe", lambda e, sq=sq, c=c: e.matmul(bank[:], lhsT=ones_t[:], rhs=sq[:], start=(c == 0), stop=(c == nchunk - 1)), reads=[Bones, Bsq], writes=[Bbank])
    P.op("act", lambda e: e.activation(out=rstd[:], in_=bank[:], func=AF.Ln, bias=eps_ap), reads=[Bbank, Bpar], writes=[Brstd])
    P.op("act", lambda e: e.activation(out=rstd[:], in_=rstd[:], func=AF.Exp, scale=-0.5), reads=[Brstd], writes=[Brstd])


def build_A(nc, P, io, NT=2048):
    xs = P.sbuf("xs", [128, 8, NT], F32)
    Bxs = [P.buf(f"xs{t}") for t in range(NT // 512)]
    gv = P.sbuf("gv", [128, 16], F32); Bgv = P.buf("gv")
    ones1k = P.sbuf("ones1k", [128, 128], F32); Bo1k = P.buf("ones1k")
    P.dma("sp", gv[:, 0:8], io["gv"], writes=[Bgv])
    P.dma("sp", ones1k[:], io["ones1k"], writes=[Bo1k])
    P.op("dve", lambda e: e.memset(gv[:, 8:9], EPS), reads=[Bgv], writes=[Bgv])
    banks = [P.psum(f"bank{i}", [128, 512]) for i in range(2)]
    Bbank = [P.buf(f"bank{i}") for i in range(2)]
    for b_ in Bbank:
        b_.excl = True
    sqs = [(P.sbuf(f"sq{i}", [128, 512], F32), P.buf(f"sq{i}")) for i in range(2)]
    rs = [(P.sbuf(f"rstd{i}", [128, 512], F32), P.buf(f"rstd{i}")) for i in range(2)]
    ho = [(P.sbuf(f"ho{i}", [128, 8, 512], BF16), P.buf(f"ho{i}")) for i in range(2)]
    xv = io["xT"].rearrange("(c p) n -> p c n", p=128)
    hv = io["hT"].rearrange("(c p) n -> p c n", p=128)
    for t in range(NT // 512):
        P.dma("sp", xs[:, :, t * 512:(t + 1) * 512], xv[:, :, t * 512:(t + 1) * 512], writes=[Bxs[t]])
    for t in range(NT // 512):
        def body(t):
            rstd, Brstd = rs[t % 2]
            rms_stats(P, (banks[t % 2], Bbank[t % 2]), None, lambda c: xs[:, c, t * 512:(t + 1) * 512], 8, ones1k, Bo1k, sqs, gv[:, 8:9], Bgv, rstd, Brstd, lambda c: [Bxs[t]])
            h, Bh = ho[t % 2]
            for c in range(8):
                P.op("dve", lambda e, c=c: e.scalar_tensor_tensor(out=h[:, c, :], in0=xs[:, c, t * 512:(t + 1) * 512], scalar=gv[:, c:c + 1], in1=rstd[:], op0=ALU.mult, op1=ALU.mult),
                     reads=[Bxs[t], Bgv, Brstd], writes=[Bh])
            P.dma("sp", hv[:, :, t * 512:(t + 1) * 512], h[:], reads=[Bh])
        body(t)


def build_C(nc, P, io, final, NT=2048):
    NTC = NT // 512
    xs = P.sbuf("xs", [128, 8, NT], F32)
    Bxs = [P.buf(f"xs{t}") for t in range(NTC)]
    gv = P.sbuf("gv", [128, 40], F32); Bgv = P.buf("gv")
    onesA = P.sbuf("onesA", [128, 128], F32); BoA = P.buf("onesA")
    onesC = P.sbuf("onesC", [128, 128], F32); BoC = P.buf("onesC")
    ones1k = P.sbuf("ones1k", [128, 128], F32); Bo1k = P.buf("ones1k")
    P.dma("sp", gv[:, 0:32], io["gv"], writes=[Bgv])
    P.dma("sp", onesA[:], io["onesA"], writes=[BoA])
    P.dma("sp", onesC[:], io["onesC"], writes=[BoC])
    P.dma("sp", ones1k[:], io["ones1k"], writes=[Bo1k])
    P.op("dve", lambda e: e.memset(gv[:, 32:33], EPS), reads=[Bgv], writes=[Bgv])
    eps_ap = gv[:, 32:33]
    banks = [P.psum(f"bank{i}", [128, 512]) for i in range(8)]
    Bbank = [P.buf(f"bank{i}") for i in range(8)]
    for b_ in Bbank:
        b_.excl = True
    rr = [0]

    def nbank():
        i = rr[0] % 8
        rr[0] += 1
        return banks[i], Bbank[i]
    R = P.sbuf("R", [128, 32768], BF16)
    wout = R[:, 0:8192].rearrange("p (c n) -> p c n", c=8); Bwout = P.buf("wout")
    ysb = [R[:, 8192 + i * 4096:8192 + (i + 1) * 4096].rearrange("p (c n) -> p c n", c=8) for i in range(2)]
    Bysb = [P.buf(f"y{i}") for i in range(2)]
    ynb = [R[:, 16384 + i * 4096:16384 + (i + 1) * 4096].rearrange("p (c n) -> p c n", c=8) for i in range(2)]
    Bynb = [P.buf(f"yn{i}") for i in range(2)]
    u = R[:, :].rearrange("p (f n) -> p f n", f=32)
    Bu = [P.buf(f"u{f}") for f in range(32)]
    h2 = P.sbuf("h2", [128, 8, 1024], BF16); Bh2 = [P.buf("h2_0"), P.buf("h2_1")]
    w1b = [(P.sbuf(f"w1b{i}", [128, 8, 512], BF16), P.buf(f"w1b{i}")) for i in range(2)]
    w2b = [(P.sbuf(f"w2b{i}", [128, 4, 512], BF16), P.buf(f"w2b{i}")) for i in range(2)]
    sqs = [(P.sbuf(f"sq{i}", [128, 512], F32), P.buf(f"sq{i}")) for i in range(3)]
    rsA = [(P.sbuf(f"rsA{i}", [128, 512], F32), P.buf(f"rsA{i}")) for i in range(1)] * 2
    rsC = [(P.sbuf(f"rsC{i}", [128, 512], F32), P.buf(f"rsC{i}")) for i in range(1)] * 2
    rs2 = [(P.sbuf(f"rs2{i}", [128, 512], F32), P.buf(f"rs2{i}")) for i in range(2)]
    rl = [(P.sbuf(f"rl{i}", [128, 512], F32), P.buf(f"rl{i}")) for i in range(3)]
    if final:
        st_f = [(P.sbuf(f"stf{i}", [128, 4, 512], F32), P.buf(f"stf{i}")) for i in range(1)]
    else:
        st_b = [(P.sbuf(f"stb{i}", [128, 8, 512], BF16), P.buf(f"stb{i}")) for i in range(1)]

    xv = io["xT"].rearrange("(c p) n -> p c n", p=128)
    for t in range(NTC):
        P.dma("sp", xs[:, :, t * 512:(t + 1) * 512], xv[:, :, t * 512:(t + 1) * 512], writes=[Bxs[t]])
    P.dma("pool", wout, io["wout"].rearrange("(c p) n -> p c n", p=128), writes=[Bwout])
    if "ysrc" in io:
        cand = [(P.sbuf(f"cand{i}", [128, 8, 512], BF16), P.buf(f"cand{i}")) for i in range(1)] * 2
        cand = [(t_[:], b_) for (t_, b_) in cand]
        ohs = P.sbuf("ohs", [128, 4], F32); Bohs = P.buf("ohs")
        P.dma("sp", ohs[:], io["ohs"], writes=[Bohs])

    def outproj(t):
        tsl = slice(t * 512, (t + 1) * 512)
        y, By = ysb[t % 2], Bysb[t % 2]
        yn, Byn = ynb[t % 2], Bynb[t % 2]
        if "ysrc" in io:
            for sI in range(4):
                cd, Bcd = cand[sI % 2]
                for pp in range(2):
                    for hh in range(2):
                        P.dma("sp", cd[pp * 64:(pp + 1) * 64, hh::2, :], io["ysrc"](sI, t, pp, hh), writes=[Bcd])
                if sI == 0:
                    P.op("dve", lambda e, cd=cd: e.tensor_scalar(out=y, in0=cd, scalar1=ohs[:, 0:1], scalar2=None, op0=ALU.mult), reads=[Bcd, Bohs], writes=[By])
                else:
                    P.op("dve", lambda e, cd=cd, sI=sI: e.scalar_tensor_tensor(out=y, in0=cd, scalar=ohs[:, sI:sI + 1], in1=y, op0=ALU.mult, op1=ALU.add), reads=[Bcd, Bohs, By], writes=[By])
        else:
            P.dma("sp", y, io["yT"][:, :, tsl].rearrange("c p n -> p c n"), writes=[By])
        rA, BrA = rsA[t % 2]; rC, BrC = rsC[t % 2]
        bA = nbank()
        rms_stats(P, bA, None, lambda c: y[:, 2 * c, :], 4, onesA, BoA, sqs, eps_ap, Bgv, rA, BrA, lambda c: [By])
        bC = nbank()
        rms_stats(P, bC, None, lambda c: y[:, 2 * c + 1, :], 4, onesC, BoC, sqs, eps_ap, Bgv, rC, BrC, lambda c: [By])
        for g in range(4):
            c0, c1 = 2 * g, 2 * g + 1
            P.op("dve", lambda e, c0=c0: e.scalar_tensor_tensor(out=yn[0:64, c0, :], in0=y[0:64, c0, :], scalar=gv[0:64, 16 + c0:17 + c0], in1=rA[0:64, :], op0=ALU.mult, op1=ALU.mult),
                 reads=[By, Bgv, BrA], writes=[Byn])
            P.op("pool", lambda e, c0=c0: e.tensor_copy(out=yn[64:128, c0, :], in_=y[64:128, c0, :]), reads=[By], writes=[Byn])
            P.op("dve", lambda e, c1=c1: e.scalar_tensor_tensor(out=yn[:, c1, :], in0=y[:, c1, :], scalar=gv[:, 16 + c1:17 + c1], in1=rC[:], op0=ALU.mult, op1=ALU.mult),
                 reads=[By, Bgv, BrC], writes=[Byn])
        for m in range(8):
            bk, Bbk = nbank()
            for c in range(8):
                P.op("pe", lambda e, c=c, m=m, bk=bk: e.matmul(bk[:], lhsT=wout[:, c, m * 128:(m + 1) * 128], rhs=yn[:, c, :], start=(c == 0), stop=(c == 7)),
                     reads=[Bwout, Byn], writes=[Bbk])
            P.op("dve", lambda e, m=m, bk=bk: e.tensor_tensor(out=xs[:, m, tsl], in0=xs[:, m, tsl], in1=bk[:], op=ALU.add), reads=[Bbk, Bxs[t]], writes=[Bxs[t]])
    for t in range(NTC):
        outproj(t)

    w1v = io["w1"].rearrange("(c p) n -> p c n", p=128)
    w2v = io["w2"].rearrange("(f p) n -> p f n", p=128)
    alias_guard = [Bwout] + Bysb + Bynb
    cnt = {"w1": 0, "w2": 0, "rl": 0}

    def ffn_half(hf):
        for tl in range(2):
            t = 2 * hf + tl
            tsl = slice(t * 512, (t + 1) * 512)
            r2, Br2 = rs2[t % 2]
            rms_stats(P, nbank(), None, lambda c, tsl=tsl: xs[:, c, tsl], 8, ones1k, Bo1k, sqs, eps_ap, Bgv, r2, Br2, lambda c, t=t: [Bxs[t]])
            for c in range(8):
                P.op("dve", lambda e, c=c, tsl=tsl, tl=tl, r2=r2: e.scalar_tensor_tensor(out=h2[:, c, tl * 512:(tl + 1) * 512], in0=xs[:, c, tsl], scalar=gv[:, c:c + 1], in1=r2[:], op0=ALU.mult, op1=ALU.mult),
                     reads=[Bxs[t], Bgv, Br2], writes=[Bh2[tl]])
        for fg in range(8):
            w1t, Bw1 = w1b[cnt["w1"] % 2]; cnt["w1"] += 1
            P.dma("pool", w1t[:], w1v[:, :, fg * 512:(fg + 1) * 512], writes=[Bw1])
            for fc in range(4):
                f = fg * 4 + fc
                for tl in range(2):
                    bk, Bbk = nbank()
                    for k in range(8):
                        P.op("pe", lambda e, k=k, fc=fc, tl=tl, bk=bk, w1t=w1t: e.matmul(bk[:], lhsT=w1t[:, k, fc * 128:(fc + 1) * 128], rhs=h2[:, k, tl * 512:(tl + 1) * 512], start=(k == 0), stop=(k == 7)),
                             reads=[Bw1, Bh2[tl]], writes=[Bbk])
                    r, Br = rl[cnt["rl"] % 3]; cnt["rl"] += 1
                    P.op("act", lambda e, bk=bk, r=r: e.activation(out=r[:], in_=bk[:], func=AF.Relu), reads=[Bbk], writes=[Br])
                    extra = alias_guard if (hf == 0) else []
                    eng = "pool" if (cnt["rl"] % 2 == 0) else "dve"
                    P.op(eng, lambda e, f=f, tl=tl, r=r: e.tensor_tensor(out=u[:, f, tl * 512:(tl + 1) * 512], in0=r[:], in1=r[:], op=ALU.mult), reads=[Br], writes=[Bu[f]] + extra)
        for mh in range(2):
            accs = [[nbank() for tl in range(2)] for mm in range(4)]
            for fg in range(8):
                w2t, Bw2 = w2b[cnt["w2"] % 2]; cnt["w2"] += 1
                P.dma("pool", w2t[:], w2v[:, fg * 4:(fg + 1) * 4, mh * 512:(mh + 1) * 512], writes=[Bw2])
                for fc in range(4):
                    f = fg * 4 + fc
                    for mm in range(4):
                        for tl in range(2):
                            bk, Bbk = accs[mm][tl]
                            P.op("pe", lambda e, fc=fc, f=f, mm=mm, tl=tl, bk=bk, w2t=w2t: e.matmul(bk[:], lhsT=w2t[:, fc, mm * 128:(mm + 1) * 128], rhs=u[:, f, tl * 512:(tl + 1) * 512], start=(f == 0), stop=(f == 31)),
                                 reads=[Bw2, Bu[f]], writes=[Bbk])
            for mm in range(4):
                m = mh * 4 + mm
                for tl in range(2):
                    t = 2 * hf + tl
                    tsl = slice(t * 512, (t + 1) * 512)
                    bk, Bbk = accs[mm][tl]
                    P.op("dve", lambda e, m=m, tsl=tsl, bk=bk: e.tensor_tensor(out=xs[:, m, tsl], in0=xs[:, m, tsl], in1=bk[:], op=ALU.add), reads=[Bbk, Bxs[t]], writes=[Bxs[t]])
    for hf in range(NTC // 2):
        ffn_half(hf)

    def tail(t):
        tsl = slice(t * 512, (t + 1) * 512)
        r2, Br2 = rs2[t % 2]
        rms_stats(P, nbank(), None, lambda c: xs[:, c, tsl], 8, ones1k, Bo1k, sqs, eps_ap, Bgv, r2, Br2, lambda c: [Bxs[t]])
        if final:
            so, Bso = st_f[0]
            for ch in range(2):
                for c in range(4 * ch, 4 * ch + 4):
                    P.op("dve", lambda e, c=c: e.scalar_tensor_tensor(out=so[:, c % 4, :], in0=xs[:, c, tsl], scalar=gv[:, 8 + c:9 + c], in1=r2[:], op0=ALU.mult, op1=ALU.mult),
                         reads=[Bxs[t], Bgv, Br2], writes=[Bso])
                P.dma("sp", io["oT"].rearrange("(c p) n -> p c n", p=128)[:, 4 * ch:4 * ch + 4, tsl], so[:], reads=[Bso])
        else:
            so, Bso = st_b[0]
            for c in range(8):
                P.op("dve", lambda e, c=c: e.scalar_tensor_tensor(out=so[:, c, :], in0=xs[:, c, tsl], scalar=gv[:, 8 + c:9 + c], in1=r2[:], op0=ALU.mult, op1=ALU.mult),
                     reads=[Bxs[t], Bgv, Br2], writes=[Bso])
            P.dma("sp", io["hnT"].rearrange("(c p) n -> p c n", p=128)[:, :, tsl], so[:], reads=[Bso])
            P.dma("sp", io["xnT"].rearrange("(c p) n -> p c n", p=128)[:, :, tsl], xs[:, :, tsl], reads=[Bxs[t]], sem_buf=Bso)
    for t in range(NTC):
        tail(t)


import ml_dtypes
from contextlib import ExitStack
from concourse.bass_utils import run_bass_kernel_spmd

_BF = ml_dtypes.bfloat16
SEQ = 8192
ALL_SLOPES = [2.0 ** (-8.0 * (h + 1) / 8) for h in range(8)]
HEAD_A = lambda g: g
HEAD_B = lambda g: 4 + g
MAXDIST = (2432, None)
GROUPS = [[0, 1, 2, 3], [4, 5, 6, 7]]
NPH = 4
NPY = 4

_cache = {}
B_CONST = ["ohk", "crow", "abias", "cmask", "hmask", "rmask", "ident", "ones64"]
C_CONST = ["onesA", "onesC", "ones1k"]


def _build_fused(seq):
    ntok = seq // 4
    nc = bass.Bass("TRN2", target_bir_lowering=False)
    io = {}

    def din(name, shape, dt):
        io[name] = nc.dram_tensor(name, list(shape), dt, kind="ExternalInput").ap()
    T = seq
    NM = T // 128 + 4
    din("xT", [1024, ntok], F32); din("ohs", [128, 4], F32); din("gvA", [128, 8], F32)
    for l in range(2):
        din(f"wfm{l}", [1024, 640], F32); din(f"wtm{l}", [1024, 192], F32); din(f"par{l}", [128, 16], F32); din(f"wg{l}", [128, 128], F32)
        din(f"wout{l}", [1024, 1024], F32); din(f"w1{l}", [1024, 4096], F32); din(f"w2{l}", [4096, 1024], F32); din(f"gvC{l}", [128, 32], F32)
    din("ohk", [33, T], BF16); din("crow", [2, T], BF16); din("abias", [128, 2, NM], F32)
    din("cmask", [128, 128], BF16); din("hmask", [128, 512], F32); din("rmask", [64, 512], F32)
    din("ident", [128, 128], BF16); din("ones64", [64, 64], F32)
    din("onesA", [128, 128], F32); din("onesC", [128, 128], F32); din("ones1k", [128, 128], F32)
    oT = nc.dram_tensor("oT", [1024, ntok], F32, kind="ExternalOutput").ap()
    hloc = [nc.dram_tensor(f"hloc{l}", [1024, ntok], BF16) for l in range(2)]
    hall = [nc.dram_tensor(f"hall{l}", [NPH, 4 * (1024 // NPH), ntok], BF16) for l in range(2)]
    yloc = [nc.dram_tensor(f"yloc{l}", [256, T], BF16) for l in range(2)]
    yall = [nc.dram_tensor(f"yall{l}", [NPY, 4 * (256 // NPY), T], BF16) for l in range(2)]
    xres = nc.dram_tensor("xres", [1024, ntok], F32)
    with ExitStack() as st:
        P = Prog(nc, st)
        P.push_scope("A_")
        build_A(nc, P, dict(xT=io["xT"], gv=io["gvA"], ones1k=io["ones1k"], hT=hloc[0].ap()), ntok)
        P.pop_scope()
        for l in range(2):
            P.barrier()
            rh = 1024 // NPH
            for j in range(NPH):
                P.coll("AllGather", hloc[l].ap()[j * rh:(j + 1) * rh, :].opt(), hall[l].ap()[j].opt(), GROUPS)
            P.barrier()
            P.push_scope(f"B{l}_")
            ioB = dict(wfm=io[f"wfm{l}"], wtm=io[f"wtm{l}"], par=io[f"par{l}"], wg=io[f"wg{l}"])
            for k in B_CONST:
                ioB[k] = io[k]
            ioB["yT"] = yloc[l].ap().rearrange("(a p) n -> a p n", a=4)
            hv = hall[l].ap().rearrange("j (s cc p) n -> j s p cc n", s=4, p=128)

            def hsrc(blk, j, hv=hv):
                tok = blk * 512
                s, lc = tok // ntok, tok % ntok
                return hv[j][s][:, :, lc:lc + 512]
            ioB["hsrc"] = hsrc
            build_B(nc, P, T, ioB, maxdist=MAXDIST)
            P.pop_scope()
            P.barrier()
            ry = 256 // NPY
            for j in range(NPY):
                P.coll("AllGather", yloc[l].ap()[j * ry:(j + 1) * ry, :].opt(), yall[l].ap()[j].opt(), GROUPS)
            P.barrier()
            P.push_scope(f"C{l}_")
            final = (l == 1)
            ioC = dict(xT=(io["xT"] if l == 0 else xres.ap()), wout=io[f"wout{l}"], w1=io[f"w1{l}"], w2=io[f"w2{l}"], gv=io[f"gvC{l}"], ohs=io["ohs"])
            for k in C_CONST:
                ioC[k] = io[k]
            yv = yall[l].ap().rearrange("j (g r) n -> j r g n", g=4)

            def ysrc(sI, t, pp, h, yv=yv):
                o = sI * ntok + t * 512
                return yv[2 * h + pp][:, :, o:o + 512]
            ioC["ysrc"] = ysrc
            if final:
                ioC["oT"] = oT
            else:
                ioC["xnT"] = xres.ap()
                ioC["hnT"] = hloc[1].ap()
            build_C(nc, P, ioC, final, ntok)
            P.pop_scope()
        n_ops = {e: len(v) for e, v in P.ops.items()}
        print("fused program ops", n_ops, "dsems", len(P.dsems), flush=True)
        P.finish()
    return nc


def _chunks(v):
    return np.ascontiguousarray(np.asarray(v, np.float32).reshape(8, 128).T)


def _b_weights(l, g, inp):
    w_in = inp["w_in"][l]
    hA, hB = HEAD_A(g), HEAD_B(g)
    sl = lambda base, w, i: w_in[:, base + w * i: base + w * i + w]
    wfm = np.zeros((1024, 640), np.float32)
    wfm[:, 0:64] = sl(512, 64, g)
    wfm[:, 64:128] = sl(0, 64, g)
    wfm[:, 128:192] = sl(768, 64, g)
    wfm[:, 192:256] = sl(256, 64, g)
    wfm[:, 256:320] = sl(1280, 64, g)
    wfm[:, 320:384] = sl(1536, 64, hA)
    wfm[:, 384:448] = sl(1536, 64, hB)
    wfm[:, 448:512] = sl(2048, 64, hA)
    wfm[:, 512:576] = sl(2048, 64, hB)
    wtm = np.concatenate([sl(1024, 64, g), sl(2560, 64, hA), sl(2560, 64, hB)], axis=1)
    par = np.zeros((128, 16), np.float32)
    ch = slice(64 * g, 64 * g + 64)
    par[64:, 0:4] = inp["lru_conv_w"][l][:, ch].T
    par[64:, 4] = inp["lru_conv_b"][l][ch]
    par[64:, 5] = inp["lru_ba"][l][ch]
    par[64:, 6] = inp["lru_bx"][l][ch]
    par[64:, 7] = inp["lru_lambda"][l][ch]
    par[:64, 0] = inp["hg_lower_bounds"][0][ch]
    par[:64, 1] = inp["hg_lower_bounds"][1][ch]
    par[:64, 2] = inp["hg_norm_w"][l]
    par[:64, 3] = float(l)
    wg = np.zeros((128, 128), np.float32)
    wg[64:, 0:64] = inp["lru_wa"][l][g]
    wg[64:, 64:128] = inp["lru_wx"][l][g]
    return {f"wfm{l}": wfm, f"wtm{l}": np.ascontiguousarray(wtm), f"par{l}": par, f"wg{l}": wg}


def _c_weights(l, inp, next_gain):
    w_out = inp["w_out"][l]
    rows = []
    gy = np.ones((128, 8), np.float32)
    for g in range(4):
        hA, hB = HEAD_A(g), HEAD_B(g)
        rows += list(range(64 * g, 64 * g + 64)) + list(range(256 + 64 * g, 256 + 64 * g + 64))
        rows += list(range(512 + 64 * hA, 512 + 64 * hA + 64)) + list(range(512 + 64 * hB, 512 + 64 * hB + 64))
        gy[0:64, 2 * g] = inp["lru_out_norm"][l][64 * g:64 * g + 64]
        gy[0:64, 2 * g + 1] = inp["att_out_norm"][l][64 * hA:64 * hA + 64]
        gy[64:128, 2 * g + 1] = inp["att_out_norm"][l][64 * hB:64 * hB + 64]
    gv = np.zeros((128, 32), np.float32)
    gv[:, 0:8] = _chunks(inp["norm_mlp"][l])
    gv[:, 8:16] = _chunks(next_gain)
    gv[:, 16:24] = gy
    return {f"wout{l}": np.ascontiguousarray(w_out[rows, :]), f"w1{l}": np.ascontiguousarray(inp["w_ff1"][l]),
            f"w2{l}": np.ascontiguousarray(inp["w_ff2"][l]), f"gvC{l}": gv}


def kernel(**inputs):
    inp = {k: np.asarray(v) for k, v in inputs.items()}
    x = inp["x"].astype(np.float32, copy=False)
    B, seq = x.shape[0], x.shape[1]
    ntok = seq // 4
    cores = list(range(8))
    if ("F", seq) not in _cache:
        _cache[("F", seq)] = _build_fused(seq)
    nc = _cache[("F", seq)]
    shared = {}
    shared.update(host_consts_C())
    shared["gvA"] = _chunks(inp["norm_mix"][0])
    for l in range(2):
        shared.update(_c_weights(l, inp, inp["norm_final"] if l == 1 else inp["norm_mix"][l + 1]))
    in_maps = []
    for c in cores:
        b, s = c // 4, c % 4
        d = dict(shared)
        d["xT"] = np.ascontiguousarray(x[b, s * ntok:(s + 1) * ntok, :].T)
        oh = np.zeros((128, 4), np.float32)
        oh[:, s] = 1.0
        d["ohs"] = oh
        d.update(host_consts_B(seq, [ALL_SLOPES[HEAD_A(s)], ALL_SLOPES[HEAD_B(s)]]))
        for l in range(2):
            d.update(_b_weights(l, s, inp))
        in_maps.append(d)
    res = run_bass_kernel_spmd(nc, in_maps, core_ids=cores)
    out = np.empty((B, seq, 1024), np.float32)
    for c in cores:
        b, s = c // 4, c % 4
        out[b, s * ntok:(s + 1) * ntok, :] = np.asarray(res.results[c]["oT"]).T
    return out
```

```python
import numpy as np
import concourse.bass as bass
import concourse.mybir as mybir

F32 = mybir.dt.float32
BF16 = mybir.dt.bfloat16
AF = mybir.ActivationFunctionType
ALU = mybir.AluOpType
AX = mybir.AxisListType

ENGS = ("pe", "act", "dve", "pool", "sp")


class Buf:
    __slots__ = ("name", "w", "r", "dsem", "excl")

    def __init__(self, name):
        self.name = name
        self.w = None
        self.r = []
        self.dsem = None
        self.excl = False


class DmaSem:
    __slots__ = ("sem", "issued", "group_open", "name", "unit", "kind")

    def __init__(self, sem, name, unit=16):
        self.sem = sem
        self.issued = 0
        self.group_open = False
        self.name = name
        self.unit = unit
        self.kind = "hw"


class Prog:
    def __init__(self, nc, stack):
        self.nc = nc
        self.stack = stack
        self.ops = {e: [] for e in ENGS}
        self.cnt = {e: 0 for e in ENGS}
        self.esem = {}
        for e in ("pe", "act", "dve", "pool"):
            self.esem[e] = stack.enter_context(nc.semaphore("s_" + e))
        self.known = {e: {} for e in ENGS}
        self.dsems = []
        self.csems = []
        self.free_dsems = []
        self.scopes = []
        self.pending = {}
        self.nbuf = 0
        import os
        self.limit = int(os.environ.get("MK_LIMIT", "0"))
        self.nrec = 0
        self.lastdesc = None

    def buf(self, name=None):
        self.nbuf += 1
        return Buf(name or f"b{self.nbuf}")

    def _st(self):
        return self.scopes[-1][0] if self.scopes else self.stack

    def _nm(self, name):
        return (self.scopes[-1][1] + name) if self.scopes else name

    def push_scope(self, prefix):
        from contextlib import ExitStack
        self.scopes.append((ExitStack(), prefix))

    def pop_scope(self):
        st, _ = self.scopes.pop()
        st.close()

    def sbuf(self, name, shape, dtype):
        t = self._st().enter_context(self.nc.sbuf_tensor("sb_" + self._nm(name), list(shape), dtype))
        return t

    def psum(self, name, shape, dtype=F32):
        t = self._st().enter_context(self.nc.psum_tensor("ps_" + self._nm(name), list(shape), dtype))
        return t

    def new_dsem(self, name, kind="hw"):
        for i, d in enumerate(self.free_dsems):
            if d.kind == kind:
                self.free_dsems.pop(i)
                d.group_open = False
                return d
        s = self.stack.enter_context(self.nc.semaphore("d_" + self._nm(name)))
        d = DmaSem(s, name)
        d.kind = kind
        self.dsems.append(d)
        return d

    def barrier(self):
        pend = []
        for e in ("pe", "act", "dve", "pool"):
            if self.cnt[e] > 0:
                pend.append(("eng", e, self.cnt[e]))
        for ds in self.dsems + self.csems:
            if ds.issued:
                pend.append(("dma", ds, ds.unit * ds.issued))
        for q in ENGS:
            self.pending[q] = list(pend)
        self.free_dsems = list(self.dsems)

    def _pend(self, q, waits):
        for dep in self.pending.pop(q, []):
            if dep[0] == "eng" and dep[1] == q and q == "pe":
                continue
            self._need(q, dep, waits)

    def coll(self, kind, in_ap, out_ap, groups, reads=()):
        s = self.stack.enter_context(self.nc.semaphore("c_%d" % len(self.csems)))
        d = DmaSem(s, "coll", unit=1)
        waits = []
        self._pend("pool", waits)
        waits = waits + self._deps("pool", list(reads), [])
        d.issued = 1
        self.csems.append(d)

        def fn(e):
            return e.collective_compute(kind, mybir.AluOpType.bypass, replica_groups=groups, ins=[in_ap], outs=[out_ap])
        self.ops["pool"].append((self._merge(waits), fn, (d.sem, None)))

    def _merge(self, waits):
        m = {}
        for s, v in waits:
            k = id(s)
            if k not in m or m[k][1] < v:
                m[k] = (s, v)
        return list(m.values())

    def _need(self, eng, dep, waits):
        if dep is None:
            return
        if dep[0] == "eng":
            _, e, idx = dep
            if e == eng and eng in ("pe",):
                return
            key = ("e", e)
            if self.known[eng].get(key, 0) >= idx:
                return
            if e == eng and False:
                return
            self.known[eng][key] = idx
            waits.append((self.esem[e], idx))
        else:
            _, ds, val = dep
            val = max(val, ds.unit * ds.issued)
            ds.group_open = False
            key = ("d", id(ds))
            if self.known[eng].get(key, 0) >= val:
                return
            self.known[eng][key] = val
            waits.append((ds.sem, val))

    def _deps(self, eng, reads, writes):
        waits = []
        for b in reads:
            self._need(eng, b.w, waits)
        for b in writes:
            self._need(eng, b.w, waits)
            for r in b.r:
                self._need(eng, r, waits)
        m = {}
        for s, v in waits:
            k = id(s)
            if k not in m or m[k][1] < v:
                m[k] = (s, v)
        return list(m.values())

    def op(self, eng, fn, reads=(), writes=()):
        self.nrec += 1
        if self.limit and self.nrec > self.limit:
            return
        import traceback
        self.lastdesc = (self.nrec, eng, traceback.extract_stack(limit=3)[0].lineno, [b.name for b in reads], [b.name for b in writes])
        writes = list(writes) + [b for b in reads if b.excl and b not in writes]
        waits = []
        self._pend(eng, waits)
        waits = self._merge(waits + self._deps(eng, reads, writes))
        self.cnt[eng] += 1
        idx = self.cnt[eng]
        tag = ("eng", eng, idx)
        for b in reads:
            if b not in writes:
                b.r.append(tag)
        for b in writes:
            b.w = tag
            b.r = []
        self.ops[eng].append((waits, fn, (self.esem[eng], 1)))

    def dma(self, q, out_ap, in_ap, reads=(), writes=(), sem_buf=None, **kw):
        self.nrec += 1
        if self.limit and self.nrec > self.limit:
            return
        sb = sem_buf
        if sb is None:
            for b in list(writes) + list(reads):
                sb = b
                break
        if sb.dsem is None:
            sb.dsem = self.new_dsem(sb.name, "sw" if q == "pool" else "hw")
        ds = sb.dsem
        waits = []
        self._pend(q, waits)
        waits = waits + self._deps(q, reads, writes)
        if (not ds.group_open) and ds.issued > 0:
            w2 = []
            self._need(q, ("dma", ds, 16 * ds.issued), w2)
            waits += w2
        ds.issued += 1
        ds.group_open = True
        tag = ("dma", ds, 16 * ds.issued)
        for b in reads:
            b.r.append(tag)
        for b in writes:
            b.w = tag
            b.r = []

        def fn(e, out_ap=out_ap, in_ap=in_ap, kw=kw):
            return e.dma_start(out=out_ap, in_=in_ap, **kw)
        if q in ("pool", "act"):
            pass
        self.ops[q].append((self._merge(waits), fn, (ds.sem, 16)))

    def finish(self):
        fin = []
        for ds in self.dsems + self.csems:
            if ds.issued:
                fin.append((ds.sem, ds.unit * ds.issued))
        nc = self.nc
        ops = self.ops
        with nc.Block() as block:
            def emit(e, lst, final=None):
                for waits, fn, inc in lst:
                    for s, v in waits:
                        e.wait_ge(s, v)
                    ins = fn(e)
                    if inc is not None:
                        if inc[1] is None:
                            ins.then_inc(inc[0])
                        else:
                            ins.then_inc(inc[0], inc[1])
                if final:
                    for s, v in final:
                        e.wait_ge(s, v)

            @block.sync
            def _(e):
                emit(e, ops["sp"], fin)

            @block.tensor
            def _(e):
                emit(e, ops["pe"])

            @block.scalar
            def _(e):
                emit(e, ops["act"])

            @block.vector
            def _(e):
                emit(e, ops["dve"])

            @block.gpsimd
            def _(e):
                emit(e, ops["pool"])


BIG = 30000.0
NEGF = -1.0e30
LN2 = 0.6931471805599453
GC = 0.7978845608028654


def host_consts_B(T, slopes2):
    import ml_dtypes
    bf = ml_dtypes.bfloat16
    nb = T // 256
    c = {}
    oh = np.zeros((33, T), np.float32)
    for n in range(nb):
        oh[n, n * 256:(n + 1) * 256] = 1.0
    oh[32, :] = 1.0
    c["ohk"] = oh.astype(bf)
    t = np.arange(T) % 512
    c["crow"] = np.stack([-8.0 * s * t for s in slopes2]).astype(np.float32).astype(bf)
    NM = T // 128 + 4
    m = np.arange(NM)
    p = np.arange(128)
    tab = np.zeros((128, 2, NM), np.float32)
    for h in range(2):
        tab[:, h, :] = slopes2[h] * (p[:, None] + 128.0 * (m[None, :] - (T // 128)))
    c["abias"] = tab
    c["cmask"] = (np.arange(128)[None, :] >= np.arange(128)[:, None]).astype(np.float32).astype(bf)
    cm = (np.arange(64)[None, :] >= np.arange(64)[:, None]).astype(np.float32)
    bm = np.zeros((128, 128), np.float32)
    bm[:64, :64] = cm
    bm[64:, 64:] = cm
    c["hmask"] = np.tile(bm, (1, 4)).astype(np.float32)
    rm = np.ones((64, 512), np.float32)
    rm[:, ::64] = 0.0
    c["rmask"] = rm
    c["ident"] = np.eye(128, dtype=np.float32).astype(bf)
    c["ones64"] = np.full((64, 64), 1.0 / 64.0, np.float32)
    return c


def build_B(nc, P, T, io, maxdist=(None, None)):
    NB = T // 512
    NM = T // 128 + 4
    MOFF = T // 128
    hT, yT = io.get("hT"), io["yT"]
    wfm = P.sbuf("wfm", [128, 8, 640], BF16); Bwfm = P.buf("wfm")
    wtm = P.sbuf("wtm", [128, 8, 192], BF16); Bwtm = P.buf("wtm")
    par = P.sbuf("par", [128, 32], F32); Bpar = P.buf("par")
    wg = P.sbuf("wg", [128, 128], BF16); Bwg = P.buf("wg")
    abias = P.sbuf("abias", [128, 2, NM], F32); Bab = P.buf("abias")
    cmask = P.sbuf("cmask", [128, 128], BF16); Bcm = P.buf("cmask")
    hmask = P.sbuf("hmask", [128, 512], F32); Bhm = P.buf("hmask")
    rmask = P.sbuf("rmask", [64, 512], F32); Brm = P.buf("rmask")
    ident = P.sbuf("ident", [128, 128], BF16); Bid = P.buf("ident")
    ones64 = P.sbuf("ones64", [64, 64], F32); Bo64 = P.buf("ones64")
    QA = P.sbuf("QA", [128, T], BF16); QB = P.sbuf("QB", [128, T], BF16)
    KA = P.sbuf("KA", [128, T], BF16); KB = P.sbuf("KB", [128, T], BF16)
    NBK = T // 512
    BQ = [[P.buf(f"Q{h}_{i}") for i in range(NBK)] for h in range(2)]
    BK = [[P.buf(f"K{h}_{i}") for i in range(NBK)] for h in range(2)]
    Qh = [QA, QB]; Kh = [KA, KB]
    VV = P.sbuf("VV", [128, T // 128, 2, 65], BF16); BV = [P.buf(f"VV{i}") for i in range(NBK)]
    VH = P.sbuf("VH", [128, T // 128, 64], BF16); BVH = [P.buf(f"VH{i}") for i in range(NBK)]
    ones64r = P.sbuf("ones64r", [128, 64], F32); Bo64r = P.buf("ones64r")
    kmT = P.sbuf("kmT", [128, 32], BF16); Bkm = P.buf("kmT")
    banks = [P.psum(f"bank{i}", [128, 512]) for i in range(8)]
    Bbank = [P.buf(f"bank{i}") for i in range(8)]
    for b_ in Bbank:
        b_.excl = True
    rr = [0]

    def nbank():
        i = rr[0] % 4
        rr[0] += 1
        return banks[i], Bbank[i]
    rr2 = [0]

    def nbank2():
        i = 4 + rr2[0] % 2
        rr2[0] += 1
        return banks[i], Bbank[i]
    pvb = [(banks[6], Bbank[6]), (banks[7], Bbank[7])]

    P.dma("pool", wfm[:], io["wfm"].rearrange("(c p) n -> p c n", p=128), writes=[Bwfm])
    P.dma("pool", wtm[:], io["wtm"].rearrange("(c p) n -> p c n", p=128), writes=[Bwtm])
    P.dma("pool", wg[:], io["wg"], writes=[Bwg])
    P.dma("sp", par[:, 0:16], io["par"], writes=[Bpar])
    P.dma("sp", abias[:], io["abias"], writes=[Bab])
    P.dma("sp", cmask[:], io["cmask"], writes=[Bcm])
    P.dma("sp", hmask[:], io["hmask"], writes=[Bhm])
    P.dma("sp", rmask[:], io["rmask"], writes=[Brm])
    P.dma("sp", ident[:], io["ident"], writes=[Bid])
    P.dma("sp", ones64[:], io["ones64"], writes=[Bo64])
    for h in range(2):
        P.op("pool", lambda e, h=h: e.memset(Qh[h][:], 0.0), writes=BQ[h])
        P.op("pool", lambda e, h=h: e.memset(Kh[h][:], 0.0), writes=BK[h])
    P.dma("sp", KA[64:97, :], io["ohk"], writes=BK[0], sem_buf=BK[0][0])
    P.dma("sp", KB[0:33, :], io["ohk"], writes=BK[1], sem_buf=BK[1][0])
    P.dma("sp", QA[96:97, :], io["crow"][0:1, :], writes=BQ[0], sem_buf=BQ[0][0])
    P.dma("sp", QB[32:33, :], io["crow"][1:2, :], writes=BQ[1], sem_buf=BQ[1][0])
    P.op("pool", lambda e: e.memset(VV[:, :, :, 64:65], 1.0), writes=BV)
    P.op("pool", lambda e: e.memset(ones64r[:], 1.0), writes=[Bo64r])
    P.op("pool", lambda e: e.memset(kmT[:], 0.0), writes=[Bkm])

    L = slice(64, 128)
    H = slice(0, 64)
    def pc(rows, j):
        return par[rows, j:j + 1]
    P.op("dve", lambda e: e.memset(par[:, 24:25], -LN2), reads=[Bpar], writes=[Bpar])
    P.op("dve", lambda e: e.memset(par[:, 25:26], 1e-6), reads=[Bpar], writes=[Bpar])
    P.op("dve", lambda e: e.memset(par[:, 26:27], 1.0), reads=[Bpar], writes=[Bpar])
    P.op("dve", lambda e: e.tensor_scalar(out=par[L, 8:10], in0=par[L, 5:7], scalar1=0.5, scalar2=None, op0=ALU.mult), reads=[Bpar], writes=[Bpar])
    P.op("act", lambda e: e.activation(out=pc(L, 12), in_=pc(L, 7), func=AF.Exp, scale=-1.0), reads=[Bpar], writes=[Bpar])
    P.op("act", lambda e: e.activation(out=pc(L, 13), in_=pc(L, 12), func=AF.Ln, bias=pc(L, 26)), reads=[Bpar], writes=[Bpar])
    P.op("dve", lambda e: e.tensor_scalar(out=pc(L, 10), in0=pc(L, 13), scalar1=-8.0, scalar2=None, op0=ALU.mult), reads=[Bpar], writes=[Bpar])
    P.op("dve", lambda e: e.tensor_scalar(out=pc(L, 11), in0=pc(L, 13), scalar1=-4.0, scalar2=None, op0=ALU.mult), reads=[Bpar], writes=[Bpar])
    P.op("dve", lambda e: e.tensor_tensor(out=pc(H, 20), in0=pc(H, 1), in1=pc(H, 0), op=ALU.subtract), reads=[Bpar], writes=[Bpar])
    P.op("act", lambda e: e.activation(out=pc(H, 21), in_=pc(H, 20), func=AF.Tanh, scale=0.5), reads=[Bpar], writes=[Bpar])
    P.op("dve", lambda e: e.tensor_scalar(out=pc(H, 22), in0=pc(H, 21), scalar1=0.5, scalar2=0.5, op0=ALU.mult, op1=ALU.add), reads=[Bpar], writes=[Bpar])
    P.op("dve", lambda e: e.tensor_tensor(out=pc(H, 19), in0=pc(H, 22), in1=pc(H, 3), op=ALU.mult), reads=[Bpar], writes=[Bpar])
    P.op("dve", lambda e: e.tensor_scalar(out=pc(H, 16), in0=pc(H, 19), scalar1=-0.5, scalar2=0.5, op0=ALU.mult, op1=ALU.add), reads=[Bpar], writes=[Bpar])
    P.op("dve", lambda e: e.tensor_scalar(out=pc(H, 18), in0=pc(H, 16), scalar1=-1.0, scalar2=None, op0=ALU.mult), reads=[Bpar], writes=[Bpar])
    P.op("dve", lambda e: e.tensor_scalar(out=pc(H, 23), in0=pc(H, 19), scalar1=1e-30, scalar2=None, op0=ALU.max), reads=[Bpar], writes=[Bpar])
    P.op("dve", lambda e: e.tensor_tensor(out=pc(H, 17), in0=pc(H, 23), in1=pc(H, 16), op=ALU.add), reads=[Bpar], writes=[Bpar])

    def rot(name, shape, dt, n):
        ts = [P.sbuf(f"{name}{i}", shape, dt) for i in range(n)]
        bs = [P.buf(f"{name}{i}") for i in range(n)]
        return ts, bs
    hblk, Bhblk = rot("hblk", [128, 8, 512], BF16, 2)
    xbuf, Bxbuf = rot("xbuf", [128, 515], F32, 2)
    NW = 2
    W = {}

    class HV:
        def __init__(self, t):
            self.t = t

        def __getitem__(self, key):
            if isinstance(key, tuple):
                return self.t[(slice(0, 64),) + tuple(key[1:])]
            return self.t[0:64, :]
    share = {"thq": "ysb", "thf": "t1", "thg": "t2", "gs2": "xc", "fg": "thr", "kk": "thi", "bb": "aa", "eb": "a2", "enb": "uu",
             "qs2": "hh", "kve": "sh1", "osb": "sh2", "osq": "sh3", "rstd": "sh4"}
    for nm, shp, dt in [("ysb", [128, 512], F32), ("t1", [128, 512], F32), ("t2", [128, 512], F32),
                        ("xc", [128, 512], F32), ("xcb", [128, 512], BF16), ("thr", [128, 512], F32), ("thi", [128, 512], F32),
                        ("aa", [128, 512], F32), ("a2", [128, 512], F32), ("uu", [128, 512], F32), ("hh", [128, 512], F32),
                        ("sh1", [128, 512], F32), ("sh2", [128, 512], F32), ("sh3", [128, 512], F32), ("sh4", [128, 512], F32),
                        ("yo", [128, 512], BF16),
                        ("qt", [64, 512], BF16), ("kt", [64, 512], BF16),
                        ("ktokE", [128, 4, 64], BF16), ("ktokO", [128, 4, 64], BF16), ("attm", [128, 512], BF16), ("ebl", [64, 8], F32),
                        ("hy", [64, 512], BF16),
                        ("gsb", [128, 32], F32), ("top8", [128, 8], F32), ("top8g", [128, 8], F32), ("mp", [128, 32], BF16),
                        ("pt", [128, 512], BF16), ("onum", [65, 512], F32), ("rec", [65, 512], F32), ("my", [64, 512], BF16)]:
        n = {"pt": 7, "gsb": 4, "top8": 4, "top8g": 4, "mp": 4, "sh1": 1, "sh2": 1, "sh3": 1, "sh4": 1, "rec": 1}.get(nm, NW)
        W[nm] = rot(nm, shp, dt, n)
    for hn, ln in share.items():
        ts, _ = W[ln]
        W[hn] = ([HV(t) for t in ts], [P.buf(f"{hn}{i}") for i in range(len(ts))])
    ctr = {}

    def wt(nm):
        i = ctr.get(nm, 0)
        ctr[nm] = i + 1
        ts, bs = W[nm]
        return ts[i % len(ts)], bs[i % len(bs)]
    for i in range(4):
        P.op("pool", lambda e, i=i: e.memset(W["gsb"][0][i][:], NEGF), writes=[W["gsb"][1][i]])
    for nm_ in ("ktokE", "ktokO"):
        for i in range(NW):
            P.op("pool", lambda e, nm_=nm_, i=i: e.memset(W[nm_][0][i][:], 0.0), writes=[W[nm_][1][i]])
    Sst = P.sbuf("Sst", [64, 64], F32); BS = P.buf("Sst")
    Sbf, BSbf = rot("Sbf", [64, 64], BF16, 4)
    P.op("dve", lambda e: e.memset(Sst[:], 0.0), writes=[BS])
    P.op("dve", lambda e: e.memset(xbuf[1][L, 512:515], 0.0), writes=[Bxbuf[1]])
    hprev = [None]

    S1 = {}

    class Deferred:
        def __init__(self):
            self.q = []
            self.atomic = None
            self.tag = 0

        def op(self, *a, **k):
            (self.atomic if self.atomic is not None else self.q).append(("op", a, k, self.tag))

        def dma(self, *a, **k):
            (self.atomic if self.atomic is not None else self.q).append(("dma", a, k, self.tag))

        def begin(self):
            self.atomic = []

        def end(self):
            self.q.append(("grp", self.atomic, None, self.tag))
            self.atomic = None

        def _emit(self):
            kind, a, k, _ = self.q.pop(0)
            items = a if kind == "grp" else [(kind, a, k, 0)]
            for kd, aa, kk, _t in items:
                (P.op if kd == "op" else P.dma)(*aa, **kk)
            return len(items)

        def flush(self, n=None):
            while self.q and (n is None or n > 0):
                c = self._emit()
                if n is not None:
                    n -= c

        def flush_upto(self, tag):
            while self.q and self.q[0][3] <= tag:
                self._emit()

        def count_upto(self, tag):
            return sum(1 for it in self.q if it[3] <= tag)
    PQ = Deferred()

    def stage1(blk):
        c0 = blk * 512
        hb, Bhb = hblk[blk % 2], Bhblk[blk % 2]
        if "hsrc" in io:
            P.dma("sp", hb[:], io["hsrc"](blk), writes=[Bhb])
        else:
            P.dma("sp", hb[:], hT[:, c0:c0 + 512].rearrange("(c p) n -> p c n", p=128), writes=[Bhb])

        def inproj(col0, M, bank, Bb, rows=slice(0, 128)):
            for k in range(8):
                P.op("pe", lambda e, k=k: e.matmul(bank[rows, :], lhsT=wfm[:, k, col0:col0 + M], rhs=hb[:, k, :], start=(k == 0), stop=(k == 7)),
                     reads=[Bwfm, Bhb], writes=[Bb])
        b4, Bb4 = nbank(); inproj(320, 128, b4, Bb4)
        b5, Bb5 = nbank(); inproj(448, 128, b5, Bb5)
        P.op("dve", lambda e: e.tensor_copy(out=QA[0:64, c0:c0 + 512], in_=b4[0:64, :]), reads=[Bb4], writes=[BQ[0][blk]])
        P.op("dve", lambda e: e.tensor_copy(out=QB[64:128, c0:c0 + 512], in_=b4[64:128, :]), reads=[Bb4], writes=[BQ[1][blk]])
        P.op("act", lambda e: e.activation(out=KA[0:64, c0:c0 + 512], in_=b5[0:64, :], func=AF.Identity), reads=[Bb5], writes=[BK[0][blk]])
        P.op("dve", lambda e: e.tensor_copy(out=KB[64:128, c0:c0 + 512], in_=b5[64:128, :]), reads=[Bb5], writes=[BK[1][blk]])
        kms, Bkms = wt("top8")
        P.op("dve", lambda e: e.tensor_reduce(out=kms[:, 0:2], in_=b5[:].rearrange("p (n k) -> p n k", n=2), axis=AX.X, op=ALU.add), reads=[Bb5], writes=[Bkms])
        P.op("dve", lambda e: e.tensor_scalar(out=kmT[:, 2 * blk:2 * blk + 2], in0=kms[:, 0:2], scalar1=1.0 / 256.0, scalar2=None, op0=ALU.mult), reads=[Bkms], writes=[Bkm])
        for pr in range(2):
            bv, Bbv = nbank()
            for tt in (2 * pr, 2 * pr + 1):
                o = (tt % 2) * 192
                for k in range(8):
                    P.op("pe", lambda e, k=k, tt=tt, o=o, bv=bv: e.matmul(bv[:, o:o + 192], lhsT=hb[:, k, tt * 128:(tt + 1) * 128], rhs=wtm[:, k, :], start=(k == 0), stop=(k == 7)),
                         reads=[Bwtm, Bhb], writes=[Bbv])
            for tt in (2 * pr, 2 * pr + 1):
                gi = blk * 4 + tt
                o = (tt % 2) * 192
                P.op("dve", lambda e, gi=gi, o=o, bv=bv: e.tensor_copy(out=VH[:, gi, :], in_=bv[:, o:o + 64]), reads=[Bbv], writes=[BVH[blk]])
                P.op("dve", lambda e, gi=gi, o=o, bv=bv: e.tensor_copy(out=VV[:, gi, :, 0:64], in_=bv[:, o + 64:o + 192].rearrange("p (h d) -> p h d", h=2)), reads=[Bbv], writes=[BV[blk]])
        b1, Bb1 = nbank(); inproj(0, 128, b1, Bb1)
        b2, Bb2 = nbank(); inproj(128, 128, b2, Bb2)
        b3, Bb3 = nbank(); inproj(256, 64, b3, Bb3, rows=slice(0, 64))
        xb, Bxb = xbuf[blk % 2], Bxbuf[blk % 2]
        P.op("act", lambda e: e.activation(out=xb[L, 3:515], in_=b1[L, :], func=AF.Identity), reads=[Bb1], writes=[Bxb])
        xo_, Bxo_ = xbuf[(blk + 1) % 2], Bxbuf[(blk + 1) % 2]
        P.op("pool", lambda e: e.tensor_copy(out=xb[L, 0:3], in_=xo_[L, 512:515]), reads=[Bxo_], writes=[Bxb])
        ysb, Bysb = wt("ysb")
        P.op("dve", lambda e: e.tensor_copy(out=ysb[L, :], in_=b2[L, :]), reads=[Bb2], writes=[Bysb])
        thq, Bthq = wt("thq"); thf, Bthf = wt("thf"); thg, Bthg = wt("thg")
        P.op("act", lambda e: e.activation(out=thq[:], in_=b1[H, :], func=AF.Tanh, scale=0.5), reads=[Bb1], writes=[Bthq])
        P.op("act", lambda e: e.activation(out=thf[:], in_=b2[H, :], func=AF.Tanh, scale=0.5), reads=[Bb2], writes=[Bthf])
        P.op("act", lambda e: e.activation(out=thg[:], in_=b3[H, :], func=AF.Tanh, scale=0.5), reads=[Bb3], writes=[Bthg])
        qs2, Bqs2 = wt("qs2"); gs2, Bgs2 = wt("gs2")
        P.op("dve", lambda e: e.scalar_tensor_tensor(out=qs2[:], in0=thq[:], scalar=1.0, in1=b1[H, :], op0=ALU.add, op1=ALU.mult), reads=[Bthq, Bb1], writes=[Bqs2])
        P.op("dve", lambda e: e.scalar_tensor_tensor(out=gs2[:], in0=thg[:], scalar=1.0, in1=b3[H, :], op0=ALU.add, op1=ALU.mult), reads=[Bthg, Bb3], writes=[Bgs2])
        S1[blk] = dict(xb=(xb, Bxb), ysb=(ysb, Bysb), thf=(thf, Bthf), qs2=(qs2, Bqs2), gs2=(gs2, Bgs2))

    def stage2(blk):
        c0 = blk * 512
        d = S1.pop(blk)
        xb, Bxb = d["xb"]; ysb, Bysb = d["ysb"]; thf, Bthf = d["thf"]; qs2, Bqs2 = d["qs2"]; gs2, Bgs2 = d["gs2"]
        xo, Bxo = xbuf[(blk + 1) % 2], Bxbuf[(blk + 1) % 2]
        xc, Bxc = wt("xc")
        PQ.op("pool", lambda e: e.tensor_scalar(out=xc[L, :], in0=xb[L, 3:515], scalar1=pc(L, 3), scalar2=pc(L, 4), op0=ALU.mult, op1=ALU.add), reads=[Bxb, Bpar], writes=[Bxc])
        for j in (2, 1, 0):
            PQ.op("dve", lambda e, j=j: e.scalar_tensor_tensor(out=xc[L, :], in0=xb[L, j:j + 512], scalar=pc(L, j), in1=xc[L, :], op0=ALU.mult, op1=ALU.add), reads=[Bxb, Bpar, Bxc], writes=[Bxc])
        xcb, Bxcb = wt("xcb")
        PQ.op("pool", lambda e: e.tensor_copy(out=xcb[L, :], in_=xc[L, :]), reads=[Bxc], writes=[Bxcb])
        bg, Bbg = nbank2()
        bg2, Bbg2 = nbank2()
        PQ.op("pe", lambda e: e.matmul(bg[L, :], lhsT=wg[L, 0:64], rhs=xcb[L, :], start=True, stop=True), reads=[Bwg, Bxcb], writes=[Bbg])
        PQ.op("pe", lambda e: e.matmul(bg2[L, :], lhsT=wg[L, 64:128], rhs=xcb[L, :], start=True, stop=True), reads=[Bwg, Bxcb], writes=[Bbg2])
        thr, Bthr = wt("thr"); thi, Bthi = wt("thi")
        PQ.op("act", lambda e: e.activation(out=thr[L, :], in_=bg[L, :], func=AF.Tanh, scale=0.5, bias=pc(L, 8)), reads=[Bbg, Bpar], writes=[Bthr])
        PQ.op("act", lambda e: e.activation(out=thi[L, :], in_=bg2[L, :], func=AF.Tanh, scale=0.5, bias=pc(L, 9)), reads=[Bbg2, Bpar], writes=[Bthi])
        aa, Baa = wt("aa"); a2, Ba2 = wt("a2")
        PQ.op("act", lambda e: e.activation(out=aa[L, :], in_=thr[L, :], func=AF.Exp, scale=pc(L, 11), bias=pc(L, 11)), reads=[Bthr, Bpar], writes=[Baa])
        PQ.op("act", lambda e: e.activation(out=a2[L, :], in_=thr[L, :], func=AF.Exp, scale=pc(L, 10), bias=pc(L, 10)), reads=[Bthr, Bpar], writes=[Ba2])
        t1, Bt1 = wt("t1"); t2, Bt2 = wt("t2")
        PQ.op("act", lambda e: e.activation(out=t1[L, :], in_=ysb[L, :], func=AF.Square), reads=[Bysb], writes=[Bt1])
        PQ.op("pool", lambda e: e.tensor_scalar(out=t1[L, :], in0=t1[L, :], scalar1=0.044715, scalar2=1.0, op0=ALU.mult, op1=ALU.add), reads=[Bt1], writes=[Bt1])
        PQ.op("pool", lambda e: e.tensor_tensor(out=t1[L, :], in0=t1[L, :], in1=ysb[L, :], op=ALU.mult), reads=[Bt1, Bysb], writes=[Bt1])
        PQ.op("act", lambda e: e.activation(out=t2[L, :], in_=t1[L, :], func=AF.Tanh, scale=GC), reads=[Bt1], writes=[Bt2])
        PQ.op("dve", lambda e: e.scalar_tensor_tensor(out=t2[L, :], in0=t2[L, :], scalar=1.0, in1=ysb[L, :], op0=ALU.add, op1=ALU.mult), reads=[Bt2, Bysb], writes=[Bt2])
        uu, Buu = wt("uu")
        PQ.op("dve", lambda e: e.scalar_tensor_tensor(out=uu[L, :], in0=thi[L, :], scalar=1.0, in1=xc[L, :], op0=ALU.add, op1=ALU.mult), reads=[Bthi, Bxc], writes=[Buu])
        fg, Bfg = wt("fg"); kk, Bkk = wt("kk")
        PQ.op("dve", lambda e: e.tensor_scalar(out=fg[:], in0=thf[:], scalar1=pc(H, 16), scalar2=pc(H, 17), op0=ALU.mult, op1=ALU.add), reads=[Bthf, Bpar], writes=[Bfg])
        PQ.op("dve", lambda e: e.tensor_scalar(out=kk[:], in0=thf[:], scalar1=pc(H, 18), scalar2=pc(H, 16), op0=ALU.mult, op1=ALU.add), reads=[Bthf, Bpar], writes=[Bkk])
        PQ.op("act", lambda e: e.activation(out=a2[L, :], in_=a2[L, :], func=AF.Ln, scale=-1.0, bias=pc(L, 26)), reads=[Ba2], writes=[Ba2])
        PQ.op("act", lambda e: e.activation(out=fg[:], in_=fg[:], func=AF.Ln), reads=[Bfg], writes=[Bfg])
        PQ.op("act", lambda e: e.activation(out=a2[L, :], in_=a2[L, :], func=AF.Exp, scale=0.5), reads=[Ba2], writes=[Ba2])
        bb, Bbb = wt("bb")
        PQ.op("dve", lambda e: e.tensor_tensor_scan(out=bb[:], data0=rmask[:], data1=fg[:], initial=0.0, op0=ALU.mult, op1=ALU.add), reads=[Brm, Bfg], writes=[Bbb])
        eb, Beb = wt("eb"); enb, Benb = wt("enb"); ebl, Bebl = wt("ebl")
        PQ.op("act", lambda e: e.activation(out=eb[:], in_=bb[:], func=AF.Exp, bias=pc(H, 24)), reads=[Bbb, Bpar], writes=[Beb])
        PQ.op("act", lambda e: e.activation(out=enb[:], in_=bb[:], func=AF.Exp, scale=-1.0), reads=[Bbb], writes=[Benb])
        PQ.op("act", lambda e: e.activation(out=ebl[:], in_=bb[:, 63:512:64], func=AF.Exp), reads=[Bbb], writes=[Bebl])
        PQ.op("dve", lambda e: e.scalar_tensor_tensor(out=uu[L, :], in0=uu[L, :], scalar=0.5, in1=a2[L, :], op0=ALU.mult, op1=ALU.mult), reads=[Buu, Ba2], writes=[Buu])
        hh, Bhh = wt("hh")
        if hprev[0] is None:
            PQ.op("dve", lambda e: e.tensor_tensor_scan(out=hh[L, :], data0=aa[L, :], data1=uu[L, :], initial=0.0, op0=ALU.mult, op1=ALU.add), reads=[Baa, Buu], writes=[Bhh])
        else:
            hp, Bhp = hprev[0]
            PQ.op("dve", lambda e, hp=hp: e.tensor_tensor_scan(out=hh[L, :], data0=aa[L, :], data1=uu[L, :], initial=hp[L, 511:512], op0=ALU.mult, op1=ALU.add), reads=[Baa, Buu, Bhp], writes=[Bhh])
        hprev[0] = (hh, Bhh)
        yo, Byo = wt("yo")
        PQ.op("dve", lambda e: e.scalar_tensor_tensor(out=yo[L, :], in0=hh[L, :], scalar=0.5, in1=t2[L, :], op0=ALU.mult, op1=ALU.mult), reads=[Bhh, Bt2], writes=[Byo])
        PQ.dma("sp", yT[0, :, c0:c0 + 512], yo[L, :], reads=[Byo])
        qt, Bqt = wt("qt"); kt, Bkt = wt("kt")
        PQ.op("dve", lambda e: e.tensor_tensor(out=qt[:], in0=qs2[:], in1=eb[:], op=ALU.mult), reads=[Bqs2, Beb], writes=[Bqt])
        PQ.op("dve", lambda e: e.tensor_tensor(out=kt[:], in0=kk[:], in1=enb[:], op=ALU.mult), reads=[Bkk, Benb], writes=[Bkt])
        btr, Bbtr = nbank2()
        btr16 = btr[:].bitcast(BF16)
        ktokE, BktokE = wt("ktokE"); ktokO, BktokO = wt("ktokO")
        for tt in range(4):
            PQ.op("pe", lambda e, tt=tt: e.transpose(btr16[:, tt * 64:(tt + 1) * 64], in_=kt[:, tt * 128:(tt + 1) * 128], identity=ident[0:64, 0:64]), reads=[Bkt, Bid], writes=[Bbtr])
        PQ.op("act", lambda e: e.activation(out=ktokE[0:64, :, :].rearrange("p t d -> p (t d)"), in_=btr16[0:64, 0:256], func=AF.Identity), reads=[Bbtr], writes=[BktokE])
        PQ.op("act", lambda e: e.activation(out=ktokO[64:128, :, :].rearrange("p t d -> p (t d)"), in_=btr16[64:128, 0:256], func=AF.Identity), reads=[Bbtr], writes=[BktokO])
        bkv, Bbkv = nbank2()
        for c in range(8):
            tt, hf = c // 2, c % 2
            gi = blk * 4 + tt
            kx, Bkx = (ktokE, BktokE) if hf == 0 else (ktokO, BktokO)
            PQ.op("pe", lambda e, c=c, tt=tt, gi=gi, kx=kx: e.matmul(bkv[0:64, c * 64:(c + 1) * 64], lhsT=kx[:, tt, :], rhs=VH[:, gi, :], start=True, stop=True),
                 reads=[Bkx, BVH[blk]], writes=[Bbkv])
        batt, Bbatt = nbank2()
        for tt in range(4):
            PQ.op("pe", lambda e, tt=tt: e.matmul(batt[:, tt * 128:(tt + 1) * 128], lhsT=kt[:, tt * 128:(tt + 1) * 128], rhs=qt[:, tt * 128:(tt + 1) * 128], start=True, stop=True),
                 reads=[Bkt, Bqt], writes=[Bbatt])
        attm, Battm = wt("attm")
        PQ.op("dve", lambda e: e.tensor_tensor(out=attm[:], in0=batt[:], in1=hmask[:], op=ALU.mult), reads=[Bbatt, Bhm], writes=[Battm])
        kve, Bkve = wt("kve")
        for c in range(8):
            PQ.op("act", lambda e, c=c: e.activation(out=kve[:, c * 64:(c + 1) * 64], in_=bkv[0:64, c * 64:(c + 1) * 64], func=AF.Identity, scale=ebl[:, c:c + 1]), reads=[Bbkv, Bebl], writes=[Bkve])
        bo, Bbo = nbank2()
        for c in range(8):
            tt, hf = c // 2, c % 2
            gi = blk * 4 + tt
            sb, Bsb = Sbf[(blk * 8 + c) % 4], BSbf[(blk * 8 + c) % 4]
            if hf == 0:
                PQ.begin()
            PQ.op("act", lambda e, sb=sb: e.activation(out=sb[:], in_=Sst[:], func=AF.Identity), reads=[BS], writes=[Bsb])
            if hf == 0:
                PQ.op("pe", lambda e, tt=tt, gi=gi: e.matmul(bo[0:64, tt * 128:(tt + 1) * 128], lhsT=VH[:, gi, :], rhs=attm[:, tt * 128:(tt + 1) * 128], start=True, stop=False),
                     reads=[BVH[blk], Battm], writes=[Bbo])
            PQ.op("pe", lambda e, c=c, sb=sb, hf=hf: e.matmul(bo[0:64, c * 64:(c + 1) * 64], lhsT=sb[:], rhs=qt[:, c * 64:(c + 1) * 64], start=False, stop=(hf == 1)),
                 reads=[Bsb, Bqt], writes=[Bbo])
            PQ.op("dve", lambda e, c=c: e.scalar_tensor_tensor(out=Sst[:], in0=Sst[:], scalar=ebl[:, c:c + 1], in1=kve[:, c * 64:(c + 1) * 64], op0=ALU.mult, op1=ALU.add), reads=[BS, Bebl, Bkve], writes=[BS])
            if hf == 1:
                PQ.end()
        osb, Bosb = wt("osb"); osq, Bosq = wt("osq")
        PQ.op("act", lambda e: e.activation(out=osb[:], in_=bo[0:64, :], func=AF.Identity), reads=[Bbo], writes=[Bosb])
        PQ.op("act", lambda e: e.activation(out=osq[:], in_=bo[0:64, :], func=AF.Square), reads=[Bbo], writes=[Bosq])
        bms, Bbms = nbank2()
        PQ.op("pe", lambda e: e.matmul(bms[0:64, :], lhsT=ones64[:], rhs=osq[:], start=True, stop=True), reads=[Bo64, Bosq], writes=[Bbms])
        rstd, Brstd = wt("rstd")
        PQ.op("act", lambda e: e.activation(out=rstd[:], in_=bms[0:64, :], func=AF.Ln, bias=pc(H, 25)), reads=[Bbms, Bpar], writes=[Brstd])
        PQ.op("act", lambda e: e.activation(out=rstd[:], in_=rstd[:], func=AF.Exp, scale=-0.5), reads=[Brstd], writes=[Brstd])
        PQ.op("dve", lambda e: e.tensor_tensor(out=osb[:], in0=osb[:], in1=rstd[:], op=ALU.mult), reads=[Bosb, Brstd], writes=[Bosb])
        PQ.op("dve", lambda e: e.tensor_scalar(out=osb[:], in0=osb[:], scalar1=pc(H, 2), scalar2=0.5, op0=ALU.mult, op1=ALU.mult), reads=[Bosb, Bpar], writes=[Bosb])
        hy, Bhy = wt("hy")
        PQ.op("dve", lambda e: e.tensor_tensor(out=hy[:], in0=osb[:], in1=gs2[:], op=ALU.mult), reads=[Bosb, Bgs2], writes=[Bhy])
        PQ.dma("sp", yT[1, :, c0:c0 + 512], hy[:], reads=[Bhy])

    def stage3(blk, tick=None):
        for h in range(2):
            head3(blk, h, tick)

    def gating(blk, h):
        c0 = blk * 512
        Q = Qh[h]
        dr = slice(0, 64) if h == 0 else slice(64, 128)
        mr = slice(64, 96) if h == 0 else slice(0, 32)
        bgt, Bbgt = nbank2()
        bmp, Bbmp = nbank2()
        bmp16 = bmp[:].bitcast(BF16)
        any_mp = False
        for st in range(4):
            j = 2 * blk + (st // 2)
            if j == 0:
                continue
            any_mp = True
            q0 = c0 + st * 128
            PQ.op("pe", lambda e, q0=q0, st=st: e.matmul(bgt[:, st * 32:(st + 1) * 32], lhsT=Q[dr, q0:q0 + 128], rhs=kmT[dr, 0:32], start=True, stop=True),
                  reads=[BQ[h][blk], Bkm], writes=[Bbgt])
            gsb, Bgsb = wt("gsb"); top8, Btop8 = wt("top8g"); mp, Bmp = wt("mp")
            PQ.op("dve", lambda e, st=st, j=j, gsb=gsb: e.tensor_copy(out=gsb[:, 0:j], in_=bgt[:, st * 32:st * 32 + j]), reads=[Bbgt], writes=[Bgsb])
            PQ.op("dve", lambda e, j=j, gsb=gsb, top8=top8: e.max(out=top8[:], in_=gsb[:, 0:max(j, 8)]), reads=[Bgsb], writes=[Btop8])
            PQ.op("pool", lambda e, mp=mp: e.memset(mp[:], 0.0), writes=[Bmp])
            PQ.op("dve", lambda e, j=j, gsb=gsb, top8=top8, mp=mp: e.tensor_scalar(out=mp[:, 0:j], in0=gsb[:, 0:j], scalar1=top8[:, 2:3], scalar2=-8.0 * BIG, op0=ALU.is_lt, op1=ALU.mult),
                  reads=[Bgsb, Btop8, Bmp], writes=[Bmp])
            PQ.op("pe", lambda e, st=st, mp=mp: e.transpose(bmp16[mr, st * 128:(st + 1) * 128], in_=mp[:], identity=ident[:]), reads=[Bmp, Bid], writes=[Bbmp])
        if any_mp:
            s0 = 0 if blk > 0 else 2
            PQ.op("act", lambda e, s0=s0: e.activation(out=Q[mr, c0 + s0 * 128:c0 + 512], in_=bmp16[mr, s0 * 128:512], func=AF.Identity), reads=[Bbmp], writes=[BQ[h][blk]])

    postponed = []

    def head3(blk, h, tick=None):
        c0 = blk * 512
        if True:
            Q, K = Qh[h], Kh[h]
            pv, Bpv = pvb[h]
            tiles = [(kt_, 0) for kt_ in range(4 * blk) if (maxdist[h] is None or (c0 - (kt_ * 128 + 127)) <= maxdist[h])] + [(4 * blk + kk_, kk_) for kk_ in range(4)]
            LA = 3
            pend = []
            ntile_done = [0]
            prev_fin = list(postponed)
            del postponed[:]
            state = {"first": True}

            def emit_pv(item):
                kti, n0, pt, Bpt, kb, last = item
                first = state["first"]
                P.op("pe", lambda e, kti=kti, n0=n0, pt=pt, first=first, last=last: e.matmul(pv[0:65, n0:512], lhsT=VV[:, kti, h, :], rhs=pt[:, n0:512], start=first, stop=last),
                     reads=[BV[kb], Bpt], writes=[Bpv])
                state["first"] = False
            for (kti, own) in tiles:
                isown = kti >= 4 * blk
                n0 = own * 128 if isown else 0
                k0 = kti * 128
                kb = kti // 4
                bs, Bbs = nbank()
                P.op("pe", lambda e, k0=k0, n0=n0, bs=bs: e.matmul(bs[:, n0:512], lhsT=K[:, k0:k0 + 128], rhs=Q[:, c0 + n0:c0 + 512], start=True, stop=True),
                     reads=[BK[h][kb], BQ[h][blk]], writes=[Bbs])
                pt, Bpt = wt("pt")
                m = kti - 4 * blk + MOFF
                P.op("act", lambda e, n0=n0, bs=bs, pt=pt, m=m: e.activation(out=pt[:, n0:512], in_=bs[:, n0:512], func=AF.Exp, scale=0.125, bias=abias[:, h, m:m + 1]),
                     reads=[Bbs, Bab], writes=[Bpt])
                if isown:
                    P.op("dve", lambda e, n0=n0, pt=pt: e.tensor_tensor(out=pt[:, n0:n0 + 128], in0=pt[:, n0:n0 + 128], in1=cmask[:], op=ALU.mult), reads=[Bpt, Bcm], writes=[Bpt])
                last = (kti == tiles[-1][0])
                pend.append((kti, n0, pt, Bpt, kb, last))
                if len(pend) > LA:
                    emit_pv(pend.pop(0))
                ntile_done[0] += 1
                if ntile_done[0] == 5 and prev_fin:
                    prev_fin.pop(0)()
                if tick is not None:
                    tick()
            while pend:
                emit_pv(pend.pop(0))
            while prev_fin:
                prev_fin.pop(0)()
            onum, Bonum = wt("onum"); rec, Brec = wt("rec"); my, Bmy = wt("my")
            P.op("act", lambda e: e.activation(out=onum[0:65, :], in_=pv[0:65, :], func=AF.Identity), reads=[Bpv], writes=[Bonum])

            def finalize():
                P.op("act", lambda e: e.activation(out=rec[64:65, :], in_=onum[64:65, :], func=AF.Ln), reads=[Bonum], writes=[Brec])
                P.op("act", lambda e: e.activation(out=rec[64:65, :], in_=rec[64:65, :], func=AF.Exp, scale=-1.0), reads=[Brec], writes=[Brec])
                bbc, Bbbc = nbank()
                P.op("pe", lambda e: e.matmul(bbc[0:64, :], lhsT=ones64r[64:65, :], rhs=rec[64:65, :], start=True, stop=True), reads=[Bo64r, Brec], writes=[Bbbc])
                P.op("dve", lambda e: e.tensor_tensor(out=my[:], in0=onum[0:64, :], in1=bbc[0:64, :], op=ALU.mult), reads=[Bonum, Bbbc], writes=[Bmy])
                P.dma("sp", yT[2 + h, :, c0:c0 + 512], my[:], reads=[Bmy])
            postponed.append(finalize)

    import os
    stop = os.environ.get("PHB_STOP", "")
    if stop == "init":
        return
    for blk in range(NB):
        stage1(blk)
        if stop == "s1":
            return
        PQ.tag = blk - 1
        for h_ in range(2):
            gating(blk, h_)
        PQ.tag = blk
        stage2(blk)
        if blk > 0:
            ntl = max(1, 2 * (4 * (blk - 1) + 4))
            must = PQ.count_upto(blk - 1)
            per = max(1, (must + len(PQ.q) // 2 + ntl - 1) // ntl)
            stage3(blk - 1, tick=lambda per=per: PQ.flush(per))
        PQ.flush_upto(blk - 1)
        if stop == "s2":
            PQ.flush()
            return
    PQ.flush_upto(NB - 2)
    stage3(NB - 1, tick=lambda: PQ.flush(2))
    PQ.flush()
    while postponed:
        postponed.pop(0)()


EPS = 1e-6


def host_consts_C():
    c = {}
    oa = np.zeros((128, 128), np.float32)
    oa[:64, :] = 1.0 / 256.0
    c["onesA"] = oa
    c["onesC"] = np.full((128, 128), 1.0 / 512.0, np.float32)
    c["ones1k"] = np.full((128, 128), 1.0 / 1024.0, np.float32)
    return c


def rms_stats(P, banks, Bbanks, src_fn, nchunk, ones_t, Bones, sq_tiles, eps_ap, Bpar, rstd, Brstd, srcbufs):
    bank, Bbank = banks
    for c in range(nchunk):
        sq, Bsq = sq_tiles[c % len(sq_tiles)]
        src = src_fn(c)
        P.op("act", lambda e, sq=sq, src=src: e.activation(out=sq[:], in_=src, func=AF.Square), reads=srcbufs(c), writes=[Bsq])
        P.op("pe", lambda e, sq=sq, c=c: e.matmul(bank[:], lhsT=ones_t[:], rhs=sq[:], start=(c == 0), stop=(c == nchunk - 1)), reads=[Bones, Bsq], writes=[Bbank])
    P.op("act", lambda e: e.activation(out=rstd[:], in_=bank[:], func=AF.Ln, bias=eps_ap), reads=[Bbank, Bpar], writes=[Brstd])
    P.op("act", lambda e: e.activation(out=rstd[:], in_=rstd[:], func=AF.Exp, scale=-0.5), reads=[Brstd], writes=[Brstd])


def build_A(nc, P, io, NT=2048):
    xs = P.sbuf("xs", [128, 8, NT], F32)
    Bxs = [P.buf(f"xs{t}") for t in range(NT // 512)]
    gv = P.sbuf("gv", [128, 16], F32); Bgv = P.buf("gv")
    ones1k = P.sbuf("ones1k", [128, 128], F32); Bo1k = P.buf("ones1k")
    P.dma("sp", gv[:, 0:8], io["gv"], writes=[Bgv])
    P.dma("sp", ones1k[:], io["ones1k"], writes=[Bo1k])
    P.op("dve", lambda e: e.memset(gv[:, 8:9], EPS), reads=[Bgv], writes=[Bgv])
    banks = [P.psum(f"bank{i}", [128, 512]) for i in range(2)]
    Bbank = [P.buf(f"bank{i}") for i in range(2)]
    for b_ in Bbank:
        b_.excl = True
    sqs = [(P.sbuf(f"sq{i}", [128, 512], F32), P.buf(f"sq{i}")) for i in range(2)]
    rs = [(P.sbuf(f"rstd{i}", [128, 512], F32), P.buf(f"rstd{i}")) for i in range(2)]
    ho = [(P.sbuf(f"ho{i}", [128, 8, 512], BF16), P.buf(f"ho{i}")) for i in range(2)]
    xv = io["xT"].rearrange("(c p) n -> p c n", p=128)
    hv = io["hT"].rearrange("(c p) n -> p c n", p=128) if "hT" in io else None
    for t in range(NT // 512):
        P.dma("sp", xs[:, :, t * 512:(t + 1) * 512], xv[:, :, t * 512:(t + 1) * 512], writes=[Bxs[t]])
    for t in range(NT // 512):
        def body(t):
            rstd, Brstd = rs[t % 2]
            rms_stats(P, (banks[t % 2], Bbank[t % 2]), None, lambda c: xs[:, c, t * 512:(t + 1) * 512], 8, ones1k, Bo1k, sqs, gv[:, 8:9], Bgv, rstd, Brstd, lambda c: [Bxs[t]])
            h, Bh = ho[t % 2]
            for c in range(8):
                P.op("dve", lambda e, c=c: e.scalar_tensor_tensor(out=h[:, c, :], in0=xs[:, c, t * 512:(t + 1) * 512], scalar=gv[:, c:c + 1], in1=rstd[:], op0=ALU.mult, op1=ALU.mult),
                     reads=[Bxs[t], Bgv, Brstd], writes=[Bh])
            if "hn_tc" in io:
                P.dma("sp", io["hn_tc"](t), h[:], reads=[Bh])
            else:
                P.dma("sp", hv[:, :, t * 512:(t + 1) * 512], h[:], reads=[Bh])
        body(t)


def build_C(nc, P, io, final, NT=2048):
    NTC = NT // 512
    xs = P.sbuf("xs", [128, 8, NT], F32)
    Bxs = [P.buf(f"xs{t}") for t in range(NTC)]
    gv = P.sbuf("gv", [128, 40], F32); Bgv = P.buf("gv")
    onesA = P.sbuf("onesA", [128, 128], F32); BoA = P.buf("onesA")
    onesC = P.sbuf("onesC", [128, 128], F32); BoC = P.buf("onesC")
    ones1k = P.sbuf("ones1k", [128, 128], F32); Bo1k = P.buf("ones1k")
    P.dma("sp", gv[:, 0:32], io["gv"], writes=[Bgv])
    P.dma("sp", onesA[:], io["onesA"], writes=[BoA])
    P.dma("sp", onesC[:], io["onesC"], writes=[BoC])
    P.dma("sp", ones1k[:], io["ones1k"], writes=[Bo1k])
    P.op("dve", lambda e: e.memset(gv[:, 32:33], EPS), reads=[Bgv], writes=[Bgv])
    eps_ap = gv[:, 32:33]
    banks = [P.psum(f"bank{i}", [128, 512]) for i in range(8)]
    Bbank = [P.buf(f"bank{i}") for i in range(8)]
    for b_ in Bbank:
        b_.excl = True
    rr = [0]

    def nbank():
        i = rr[0] % 8
        rr[0] += 1
        return banks[i], Bbank[i]
    R = P.sbuf("R", [128, 32768], BF16)
    wout = R[:, 0:8192].rearrange("p (c n) -> p c n", c=8); Bwout = P.buf("wout")
    ysb = [R[:, 8192 + i * 4096:8192 + (i + 1) * 4096].rearrange("p (c n) -> p c n", c=8) for i in range(2)]
    Bysb = [P.buf(f"y{i}") for i in range(2)]
    ynb = [R[:, 16384 + i * 4096:16384 + (i + 1) * 4096].rearrange("p (c n) -> p c n", c=8) for i in range(2)]
    Bynb = [P.buf(f"yn{i}") for i in range(2)]
    u = R[:, :].rearrange("p (f n) -> p f n", f=32)
    Bu = [P.buf(f"u{f}") for f in range(32)]
    h2 = P.sbuf("h2", [128, 8, 1024], BF16); Bh2 = [P.buf("h2_0"), P.buf("h2_1")]
    w1b = [(P.sbuf(f"w1b{i}", [128, 8, 512], BF16), P.buf(f"w1b{i}")) for i in range(2)]
    w2b = [(P.sbuf(f"w2b{i}", [128, 4, 512], BF16), P.buf(f"w2b{i}")) for i in range(2)]
    sqs = [(P.sbuf(f"sq{i}", [128, 512], F32), P.buf(f"sq{i}")) for i in range(3)]
    rsA = [(P.sbuf(f"rsA{i}", [128, 512], F32), P.buf(f"rsA{i}")) for i in range(1)] * 2
    rsC = [(P.sbuf(f"rsC{i}", [128, 512], F32), P.buf(f"rsC{i}")) for i in range(1)] * 2
    rs2 = [(P.sbuf(f"rs2{i}", [128, 512], F32), P.buf(f"rs2{i}")) for i in range(2)]
    rl = [(P.sbuf(f"rl{i}", [128, 512], F32), P.buf(f"rl{i}")) for i in range(3)]
    if final:
        st_f = [(P.sbuf(f"stf{i}", [128, 4, 512], F32), P.buf(f"stf{i}")) for i in range(1)]
    else:
        st_b = [(P.sbuf(f"stb{i}", [128, 8, 512], BF16), P.buf(f"stb{i}")) for i in range(1)]

    xv = io["xT"].rearrange("(c p) n -> p c n", p=128)
    for t in range(NTC):
        P.dma("sp", xs[:, :, t * 512:(t + 1) * 512], xv[:, :, t * 512:(t + 1) * 512], writes=[Bxs[t]])
    P.dma("pool", wout, io["wout"].rearrange("(c p) n -> p c n", p=128), writes=[Bwout])
    if "ysrc" in io:
        cand = [(P.sbuf(f"cand{i}", [128, 8, 512], BF16), P.buf(f"cand{i}")) for i in range(1)] * 2
        cand = [(t_[:], b_) for (t_, b_) in cand]
        ohs = P.sbuf("ohs", [128, 4], F32); Bohs = P.buf("ohs")
        P.dma("sp", ohs[:], io["ohs"], writes=[Bohs])

    def ysel(t):
        tsl = slice(t * 512, (t + 1) * 512)
        y, By = ysb[t % 2], Bysb[t % 2]
        if "ysrc" in io:
            for sI in range(4):
                cd, Bcd = cand[sI % 2]
                for pp in range(2):
                    for hh in range(2):
                        P.dma("sp", cd[pp * 64:(pp + 1) * 64, hh::2, :], io["ysrc"](sI, t, pp, hh), writes=[Bcd])
                if sI == 0:
                    P.op("dve", lambda e, cd=cd: e.tensor_scalar(out=y, in0=cd, scalar1=ohs[:, 0:1], scalar2=None, op0=ALU.mult), reads=[Bcd, Bohs], writes=[By])
                else:
                    P.op("dve", lambda e, cd=cd, sI=sI: e.scalar_tensor_tensor(out=y, in0=cd, scalar=ohs[:, sI:sI + 1], in1=y, op0=ALU.mult, op1=ALU.add), reads=[Bcd, Bohs, By], writes=[By])
        else:
            P.dma("sp", y, io["yT"][:, :, tsl].rearrange("c p n -> p c n"), writes=[By])

    def outproj(t):
        tsl = slice(t * 512, (t + 1) * 512)
        y, By = ysb[t % 2], Bysb[t % 2]
        yn, Byn = ynb[t % 2], Bynb[t % 2]
        rA, BrA = rsA[t % 2]; rC, BrC = rsC[t % 2]
        bA = nbank()
        rms_stats(P, bA, None, lambda c: y[:, 2 * c, :], 4, onesA, BoA, sqs, eps_ap, Bgv, rA, BrA, lambda c: [By])
        bC = nbank()
        rms_stats(P, bC, None, lambda c: y[:, 2 * c + 1, :], 4, onesC, BoC, sqs, eps_ap, Bgv, rC, BrC, lambda c: [By])
        for g in range(4):
            c0, c1 = 2 * g, 2 * g + 1
            P.op("dve", lambda e, c0=c0: e.scalar_tensor_tensor(out=yn[0:64, c0, :], in0=y[0:64, c0, :], scalar=gv[0:64, 16 + c0:17 + c0], in1=rA[0:64, :], op0=ALU.mult, op1=ALU.mult),
                 reads=[By, Bgv, BrA], writes=[Byn])
            P.op("pool", lambda e, c0=c0: e.tensor_copy(out=yn[64:128, c0, :], in_=y[64:128, c0, :]), reads=[By], writes=[Byn])
            P.op("dve", lambda e, c1=c1: e.scalar_tensor_tensor(out=yn[:, c1, :], in0=y[:, c1, :], scalar=gv[:, 16 + c1:17 + c1], in1=rC[:], op0=ALU.mult, op1=ALU.mult),
                 reads=[By, Bgv, BrC], writes=[Byn])
        for m in range(8):
            bk, Bbk = nbank()
            for c in range(8):
                P.op("pe", lambda e, c=c, m=m, bk=bk: e.matmul(bk[:], lhsT=wout[:, c, m * 128:(m + 1) * 128], rhs=yn[:, c, :], start=(c == 0), stop=(c == 7)),
                     reads=[Bwout, Byn], writes=[Bbk])
            P.op("dve", lambda e, m=m, bk=bk: e.tensor_tensor(out=xs[:, m, tsl], in0=xs[:, m, tsl], in1=bk[:], op=ALU.add), reads=[Bbk, Bxs[t]], writes=[Bxs[t]])
    ysel(0)
    for t in range(NTC):
        if t + 1 < NTC:
            ysel(t + 1)
        outproj(t)

    w1v = io["w1"].rearrange("(c p) n -> p c n", p=128)
    w2v = io["w2"].rearrange("(f p) n -> p f n", p=128)
    alias_guard = [Bwout] + Bysb + Bynb
    cnt = {"w1": 0, "w2": 0, "rl": 0}

    def ffn_half(hf):
        for tl in range(2):
            t = 2 * hf + tl
            tsl = slice(t * 512, (t + 1) * 512)
            r2, Br2 = rs2[t % 2]
            rms_stats(P, nbank(), None, lambda c, tsl=tsl: xs[:, c, tsl], 8, ones1k, Bo1k, sqs, eps_ap, Bgv, r2, Br2, lambda c, t=t: [Bxs[t]])
            for c in range(8):
                P.op("dve", lambda e, c=c, tsl=tsl, tl=tl, r2=r2: e.scalar_tensor_tensor(out=h2[:, c, tl * 512:(tl + 1) * 512], in0=xs[:, c, tsl], scalar=gv[:, c:c + 1], in1=r2[:], op0=ALU.mult, op1=ALU.mult),
                     reads=[Bxs[t], Bgv, Br2], writes=[Bh2[tl]])
        for fg in range(8):
            w1t, Bw1 = w1b[cnt["w1"] % 2]; cnt["w1"] += 1
            P.dma("pool", w1t[:], w1v[:, :, fg * 512:(fg + 1) * 512], writes=[Bw1])
            for fc in range(4):
                f = fg * 4 + fc
                for tl in range(2):
                    bk, Bbk = nbank()
                    for k in range(8):
                        P.op("pe", lambda e, k=k, fc=fc, tl=tl, bk=bk, w1t=w1t: e.matmul(bk[:], lhsT=w1t[:, k, fc * 128:(fc + 1) * 128], rhs=h2[:, k, tl * 512:(tl + 1) * 512], start=(k == 0), stop=(k == 7)),
                             reads=[Bw1, Bh2[tl]], writes=[Bbk])
                    r, Br = rl[cnt["rl"] % 3]; cnt["rl"] += 1
                    P.op("act", lambda e, bk=bk, r=r: e.activation(out=r[:], in_=bk[:], func=AF.Relu), reads=[Bbk], writes=[Br])
                    extra = alias_guard if (hf == 0) else []
                    eng = "pool" if (cnt["rl"] % 2 == 0) else "dve"
                    P.op(eng, lambda e, f=f, tl=tl, r=r: e.tensor_tensor(out=u[:, f, tl * 512:(tl + 1) * 512], in0=r[:], in1=r[:], op=ALU.mult), reads=[Br], writes=[Bu[f]] + extra)
        for mh in range(2):
            accs = [[nbank() for tl in range(2)] for mm in range(4)]
            for fg in range(8):
                w2t, Bw2 = w2b[cnt["w2"] % 2]; cnt["w2"] += 1
                P.dma("pool", w2t[:], w2v[:, fg * 4:(fg + 1) * 4, mh * 512:(mh + 1) * 512], writes=[Bw2])
                for fc in range(4):
                    f = fg * 4 + fc
                    for mm in range(4):
                        for tl in range(2):
                            bk, Bbk = accs[mm][tl]
                            P.op("pe", lambda e, fc=fc, f=f, mm=mm, tl=tl, bk=bk, w2t=w2t: e.matmul(bk[:], lhsT=w2t[:, fc, mm * 128:(mm + 1) * 128], rhs=u[:, f, tl * 512:(tl + 1) * 512], start=(f == 0), stop=(f == 31)),
                                 reads=[Bw2, Bu[f]], writes=[Bbk])
            for mm in range(4):
                m = mh * 4 + mm
                for tl in range(2):
                    t = 2 * hf + tl
                    tsl = slice(t * 512, (t + 1) * 512)
                    bk, Bbk = accs[mm][tl]
                    P.op("dve", lambda e, m=m, tsl=tsl, bk=bk: e.tensor_tensor(out=xs[:, m, tsl], in0=xs[:, m, tsl], in1=bk[:], op=ALU.add), reads=[Bbk, Bxs[t]], writes=[Bxs[t]])
    def tail(t):
        tsl = slice(t * 512, (t + 1) * 512)
        r2, Br2 = rs2[t % 2]
        rms_stats(P, nbank(), None, lambda c: xs[:, c, tsl], 8, ones1k, Bo1k, sqs, eps_ap, Bgv, r2, Br2, lambda c: [Bxs[t]])
        if final:
            so, Bso = st_f[0]
            for ch in range(2):
                for c in range(4 * ch, 4 * ch + 4):
                    P.op("dve", lambda e, c=c: e.scalar_tensor_tensor(out=so[:, c % 4, :], in0=xs[:, c, tsl], scalar=gv[:, 8 + c:9 + c], in1=r2[:], op0=ALU.mult, op1=ALU.mult),
                         reads=[Bxs[t], Bgv, Br2], writes=[Bso])
                P.dma("sp", io["oT"].rearrange("(c p) n -> p c n", p=128)[:, 4 * ch:4 * ch + 4, tsl], so[:], reads=[Bso])
        else:
            so, Bso = st_b[0]
            for c in range(8):
                P.op("dve", lambda e, c=c: e.scalar_tensor_tensor(out=so[:, c, :], in0=xs[:, c, tsl], scalar=gv[:, 8 + c:9 + c], in1=r2[:], op0=ALU.mult, op1=ALU.mult),
                     reads=[Bxs[t], Bgv, Br2], writes=[Bso])
            if "hn_tc" in io:
                Bhn = P.buf(f"hn_dram{t}")
                P.dma("sp", io["hn_tc"](t), so[:], reads=[Bso], writes=[Bhn], sem_buf=Bso)
                P.dma("sp", io["xnT"].rearrange("(c p) n -> p c n", p=128)[:, :, tsl], xs[:, :, tsl], reads=[Bxs[t]], sem_buf=Bso)
                io["coll_h"](t, [Bhn])
            else:
                P.dma("sp", io["hnT"].rearrange("(c p) n -> p c n", p=128)[:, :, tsl], so[:], reads=[Bso])
                P.dma("sp", io["xnT"].rearrange("(c p) n -> p c n", p=128)[:, :, tsl], xs[:, :, tsl], reads=[Bxs[t]], sem_buf=Bso)
    early = bool(io.get("early_tail"))
    for hf in range(NTC // 2):
        ffn_half(hf)
        if early:
            tail(2 * hf); tail(2 * hf + 1)

    if not early:
        for t in range(NTC):
            tail(t)


import ml_dtypes
from contextlib import ExitStack
from concourse.bass_utils import run_bass_kernel_spmd

_BF = ml_dtypes.bfloat16
SEQ = 8192
ALL_SLOPES = [2.0 ** (-8.0 * (h + 1) / 8) for h in range(8)]
HEAD_A = lambda g: g
HEAD_B = lambda g: 4 + g
MAXDIST = (2432, None)
GROUPS = [[0, 1, 2, 3], [4, 5, 6, 7]]
NPH = 4
NPY = 4

_cache = {}
B_CONST = ["ohk", "crow", "abias", "cmask", "hmask", "rmask", "ident", "ones64"]
C_CONST = ["onesA", "onesC", "ones1k"]


def _build_fused(seq):
    ntok = seq // 4
    nc = bass.Bass("TRN2", target_bir_lowering=False)
    io = {}

    def din(name, shape, dt):
        io[name] = nc.dram_tensor(name, list(shape), dt, kind="ExternalInput").ap()
    T = seq
    NM = T // 128 + 4
    din("xT", [1024, ntok], F32); din("ohs", [128, 4], F32); din("gvA", [128, 8], F32)
    for l in range(2):
        din(f"wfm{l}", [1024, 640], F32); din(f"wtm{l}", [1024, 192], F32); din(f"par{l}", [128, 16], F32); din(f"wg{l}", [128, 128], F32)
        din(f"wout{l}", [1024, 1024], F32); din(f"w1{l}", [1024, 4096], F32); din(f"w2{l}", [4096, 1024], F32); din(f"gvC{l}", [128, 32], F32)
    din("ohk", [33, T], BF16); din("crow", [2, T], BF16); din("abias", [128, 2, NM], F32)
    din("cmask", [128, 128], BF16); din("hmask", [128, 512], F32); din("rmask", [64, 512], F32)
    din("ident", [128, 128], BF16); din("ones64", [64, 64], F32)
    din("onesA", [128, 128], F32); din("onesC", [128, 128], F32); din("ones1k", [128, 128], F32)
    oT = nc.dram_tensor("oT", [1024, ntok], F32, kind="ExternalOutput").ap()
    NTC = ntok // 512
    hloc = [nc.dram_tensor(f"hloc{l}", [NTC, 1024, 512], BF16) for l in range(2)]
    hall = [nc.dram_tensor(f"hall{l}", [NTC, 4 * 1024, 512], BF16) for l in range(2)]
    yloc = [nc.dram_tensor(f"yloc{l}", [256, T], BF16) for l in range(2)]
    yall = [nc.dram_tensor(f"yall{l}", [NPY, 4 * (256 // NPY), T], BF16) for l in range(2)]
    xres = nc.dram_tensor("xres", [1024, ntok], F32)
    with ExitStack() as st:
        P = Prog(nc, st)
        done_h = [set(), set()]
        P.push_scope("A_")
        hn0 = hloc[0].ap()
        build_A(nc, P, dict(xT=io["xT"], gv=io["gvA"], ones1k=io["ones1k"], hn_tc=lambda t: hn0[t].rearrange("(c p) n -> p c n", p=128)), ntok)
        P.pop_scope()
        for l in range(2):
            P.barrier()
            for t in range(NTC):
                if t not in done_h[l]:
                    P.coll("AllGather", hloc[l].ap()[t].opt(), hall[l].ap()[t].opt(), GROUPS)
            P.barrier()
            P.push_scope(f"B{l}_")
            ioB = dict(wfm=io[f"wfm{l}"], wtm=io[f"wtm{l}"], par=io[f"par{l}"], wg=io[f"wg{l}"])
            for k in B_CONST:
                ioB[k] = io[k]
            ioB["yT"] = yloc[l].ap().rearrange("(a p) n -> a p n", a=4)
            hv = hall[l].ap()

            def hsrc(blk, hv=hv):
                tok = blk * 512
                s, tc = tok // ntok, (tok % ntok) // 512
                return hv[tc][s * 1024:(s + 1) * 1024, :].rearrange("(c p) n -> p c n", p=128)
            ioB["hsrc"] = hsrc
            build_B(nc, P, T, ioB, maxdist=MAXDIST)
            P.pop_scope()
            P.barrier()
            ry = 256 // NPY
            for j in range(NPY):
                P.coll("AllGather", yloc[l].ap()[j * ry:(j + 1) * ry, :].opt(), yall[l].ap()[j].opt(), GROUPS)
            P.barrier()
            P.push_scope(f"C{l}_")
            final = (l == 1)
            ioC = dict(xT=(io["xT"] if l == 0 else xres.ap()), wout=io[f"wout{l}"], w1=io[f"w1{l}"], w2=io[f"w2{l}"], gv=io[f"gvC{l}"], ohs=io["ohs"])
            for k in C_CONST:
                ioC[k] = io[k]
            yv = yall[l].ap().rearrange("j (g r) n -> j r g n", g=4)

            def ysrc(sI, t, pp, h, yv=yv):
                o = sI * ntok + t * 512
                return yv[2 * h + pp][:, :, o:o + 512]
            ioC["ysrc"] = ysrc
            if final:
                ioC["oT"] = oT
            else:
                ioC["xnT"] = xres.ap()
                hn1 = hloc[1].ap()
                ioC["hn_tc"] = lambda t, hn1=hn1: hn1[t].rearrange("(c p) n -> p c n", p=128)

                def coll_h(t, bufs, l=l):
                    if t < NTC - 2:
                        P.coll("AllGather", hloc[l + 1].ap()[t].opt(), hall[l + 1].ap()[t].opt(), GROUPS, reads=bufs)
                        done_h[l + 1].add(t)
                ioC["coll_h"] = coll_h
            ioC["early_tail"] = True
            build_C(nc, P, ioC, final, ntok)
            P.pop_scope()
        n_ops = {e: len(v) for e, v in P.ops.items()}
        print("fused program ops", n_ops, "dsems", len(P.dsems), flush=True)
        P.finish()
    return nc


def _chunks(v):
    return np.ascontiguousarray(np.asarray(v, np.float32).reshape(8, 128).T)


def _b_weights(l, g, inp):
    w_in = inp["w_in"][l]
    hA, hB = HEAD_A(g), HEAD_B(g)
    sl = lambda base, w, i: w_in[:, base + w * i: base + w * i + w]
    wfm = np.zeros((1024, 640), np.float32)
    wfm[:, 0:64] = sl(512, 64, g)
    wfm[:, 64:128] = sl(0, 64, g)
    wfm[:, 128:192] = sl(768, 64, g)
    wfm[:, 192:256] = sl(256, 64, g)
    wfm[:, 256:320] = sl(1280, 64, g)
    wfm[:, 320:384] = sl(1536, 64, hA)
    wfm[:, 384:448] = sl(1536, 64, hB)
    wfm[:, 448:512] = sl(2048, 64, hA)
    wfm[:, 512:576] = sl(2048, 64, hB)
    wtm = np.concatenate([sl(1024, 64, g), sl(2560, 64, hA), sl(2560, 64, hB)], axis=1)
    par = np.zeros((128, 16), np.float32)
    ch = slice(64 * g, 64 * g + 64)
    par[64:, 0:4] = inp["lru_conv_w"][l][:, ch].T
    par[64:, 4] = inp["lru_conv_b"][l][ch]
    par[64:, 5] = inp["lru_ba"][l][ch]
    par[64:, 6] = inp["lru_bx"][l][ch]
    par[64:, 7] = inp["lru_lambda"][l][ch]
    par[:64, 0] = inp["hg_lower_bounds"][0][ch]
    par[:64, 1] = inp["hg_lower_bounds"][1][ch]
    par[:64, 2] = inp["hg_norm_w"][l]
    par[:64, 3] = float(l)
    wg = np.zeros((128, 128), np.float32)
    wg[64:, 0:64] = inp["lru_wa"][l][g]
    wg[64:, 64:128] = inp["lru_wx"][l][g]
    return {f"wfm{l}": wfm, f"wtm{l}": np.ascontiguousarray(wtm), f"par{l}": par, f"wg{l}": wg}


def _c_weights(l, inp, next_gain):
    w_out = inp["w_out"][l]
    rows = []
    gy = np.ones((128, 8), np.float32)
    for g in range(4):
        hA, hB = HEAD_A(g), HEAD_B(g)
        rows += list(range(64 * g, 64 * g + 64)) + list(range(256 + 64 * g, 256 + 64 * g + 64))
        rows += list(range(512 + 64 * hA, 512 + 64 * hA + 64)) + list(range(512 + 64 * hB, 512 + 64 * hB + 64))
        gy[0:64, 2 * g] = inp["lru_out_norm"][l][64 * g:64 * g + 64]
        gy[0:64, 2 * g + 1] = inp["att_out_norm"][l][64 * hA:64 * hA + 64]
        gy[64:128, 2 * g + 1] = inp["att_out_norm"][l][64 * hB:64 * hB + 64]
    gv = np.zeros((128, 32), np.float32)
    gv[:, 0:8] = _chunks(inp["norm_mlp"][l])
    gv[:, 8:16] = _chunks(next_gain)
    gv[:, 16:24] = gy
    return {f"wout{l}": np.ascontiguousarray(w_out[rows, :]), f"w1{l}": np.ascontiguousarray(inp["w_ff1"][l]),
            f"w2{l}": np.ascontiguousarray(inp["w_ff2"][l]), f"gvC{l}": gv}


def kernel(**inputs):
    inp = {k: np.asarray(v) for k, v in inputs.items()}
    x = inp["x"].astype(np.float32, copy=False)
    B, seq = x.shape[0], x.shape[1]
    ntok = seq // 4
    cores = list(range(8))
    if ("F", seq) not in _cache:
        _cache[("F", seq)] = _build_fused(seq)
    nc = _cache[("F", seq)]
    shared = {}
    shared.update(host_consts_C())
    shared["gvA"] = _chunks(inp["norm_mix"][0])
    for l in range(2):
        shared.update(_c_weights(l, inp, inp["norm_final"] if l == 1 else inp["norm_mix"][l + 1]))
    in_maps = []
    for c in cores:
        b, s = c // 4, c % 4
        d = dict(shared)
        d["xT"] = np.ascontiguousarray(x[b, s * ntok:(s + 1) * ntok, :].T)
        oh = np.zeros((128, 4), np.float32)
        oh[:, s] = 1.0
        d["ohs"] = oh
        d.update(host_consts_B(seq, [ALL_SLOPES[HEAD_A(s)], ALL_SLOPES[HEAD_B(s)]]))
        for l in range(2):
            d.update(_b_weights(l, s, inp))
        in_maps.append(d)
    res = run_bass_kernel_spmd(nc, in_maps, core_ids=cores)
    out = np.empty((B, seq, 1024), np.float32)
    for c in cores:
        b, s = c // 4, c % 4
        out[b, s * ntok:(s + 1) * ntok, :] = np.asarray(res.results[c]["oT"]).T
    return out
```

```python
import numpy as np
import concourse.bass as bass
import concourse.mybir as mybir

F32 = mybir.dt.float32
BF16 = mybir.dt.bfloat16
AF = mybir.ActivationFunctionType
ALU = mybir.AluOpType
AX = mybir.AxisListType

ENGS = ("pe", "act", "dve", "pool", "sp")


class Buf:
    __slots__ = ("name", "w", "r", "dsem", "excl")

    def __init__(self, name):
        self.name = name
        self.w = None
        self.r = []
        self.dsem = None
        self.excl = False


class DmaSem:
    __slots__ = ("sem", "issued", "group_open", "name", "unit", "kind")

    def __init__(self, sem, name, unit=16):
        self.sem = sem
        self.issued = 0
        self.group_open = False
        self.name = name
        self.unit = unit
        self.kind = "hw"


class Prog:
    def __init__(self, nc, stack):
        self.nc = nc
        self.stack = stack
        self.ops = {e: [] for e in ENGS}
        self.cnt = {e: 0 for e in ENGS}
        self.esem = {}
        for e in ("pe", "act", "dve", "pool"):
            self.esem[e] = stack.enter_context(nc.semaphore("s_" + e))
        self.known = {e: {} for e in ENGS}
        self.dsems = []
        self.csems = []
        self.free_dsems = []
        self.scopes = []
        self.pending = {}
        self.nbuf = 0
        import os
        self.limit = int(os.environ.get("MK_LIMIT", "0"))
        self.nrec = 0
        self.lastdesc = None

    def buf(self, name=None):
        self.nbuf += 1
        return Buf(name or f"b{self.nbuf}")

    def _st(self):
        return self.scopes[-1][0] if self.scopes else self.stack

    def _nm(self, name):
        return (self.scopes[-1][1] + name) if self.scopes else name

    def push_scope(self, prefix):
        from contextlib import ExitStack
        self.scopes.append((ExitStack(), prefix))

    def pop_scope(self):
        st, _ = self.scopes.pop()
        st.close()

    def sbuf(self, name, shape, dtype):
        t = self._st().enter_context(self.nc.sbuf_tensor("sb_" + self._nm(name), list(shape), dtype))
        return t

    def psum(self, name, shape, dtype=F32):
        t = self._st().enter_context(self.nc.psum_tensor("ps_" + self._nm(name), list(shape), dtype))
        return t

    def new_dsem(self, name, kind="hw"):
        for i, d in enumerate(self.free_dsems):
            if d.kind == kind:
                self.free_dsems.pop(i)
                d.group_open = False
                return d
        s = self.stack.enter_context(self.nc.semaphore("d_" + self._nm(name)))
        d = DmaSem(s, name)
        d.kind = kind
        self.dsems.append(d)
        return d

    def barrier(self):
        pend = []
        for e in ("pe", "act", "dve", "pool"):
            if self.cnt[e] > 0:
                pend.append(("eng", e, self.cnt[e]))
        for ds in self.dsems + self.csems:
            if ds.issued:
                pend.append(("dma", ds, ds.unit * ds.issued))
        for q in ENGS:
            self.pending[q] = list(pend)
        self.free_dsems = list(self.dsems)

    def _pend(self, q, waits):
        for dep in self.pending.pop(q, []):
            if dep[0] == "eng" and dep[1] == q and q == "pe":
                continue
            self._need(q, dep, waits)

    def coll(self, kind, in_ap, out_ap, groups, reads=(), dsems=()):
        s = self.stack.enter_context(self.nc.semaphore("c_%d" % len(self.csems)))
        d = DmaSem(s, "coll", unit=1)
        waits = []
        self._pend("pool", waits)
        waits = waits + self._deps("pool", list(reads), [])
        for ds_ in dsems:
            self._need("pool", ("dma", ds_, ds_.unit * ds_.issued), waits)
        d.issued = 1
        self.csems.append(d)

        def fn(e):
            return e.collective_compute(kind, mybir.AluOpType.bypass, replica_groups=groups, ins=[in_ap], outs=[out_ap])
        self.ops["pool"].append((self._merge(waits), fn, (d.sem, None)))

    def _merge(self, waits):
        m = {}
        for s, v in waits:
            k = id(s)
            if k not in m or m[k][1] < v:
                m[k] = (s, v)
        return list(m.values())

    def _need(self, eng, dep, waits):
        if dep is None:
            return
        if dep[0] == "eng":
            _, e, idx = dep
            if e == eng and eng in ("pe",):
                return
            key = ("e", e)
            if self.known[eng].get(key, 0) >= idx:
                return
            if e == eng and False:
                return
            self.known[eng][key] = idx
            waits.append((self.esem[e], idx))
        else:
            _, ds, val = dep
            val = max(val, ds.unit * ds.issued)
            ds.group_open = False
            key = ("d", id(ds))
            if self.known[eng].get(key, 0) >= val:
                return
            self.known[eng][key] = val
            waits.append((ds.sem, val))

    def _deps(self, eng, reads, writes):
        waits = []
        for b in reads:
            self._need(eng, b.w, waits)
        for b in writes:
            self._need(eng, b.w, waits)
            for r in b.r:
                self._need(eng, r, waits)
        m = {}
        for s, v in waits:
            k = id(s)
            if k not in m or m[k][1] < v:
                m[k] = (s, v)
        return list(m.values())

    def op(self, eng, fn, reads=(), writes=()):
        self.nrec += 1
        if self.limit and self.nrec > self.limit:
            return
        import traceback
        self.lastdesc = (self.nrec, eng, traceback.extract_stack(limit=3)[0].lineno, [b.name for b in reads], [b.name for b in writes])
        writes = list(writes) + [b for b in reads if b.excl and b not in writes]
        waits = []
        self._pend(eng, waits)
        waits = self._merge(waits + self._deps(eng, reads, writes))
        self.cnt[eng] += 1
        idx = self.cnt[eng]
        tag = ("eng", eng, idx)
        for b in reads:
            if b not in writes:
                b.r.append(tag)
        for b in writes:
            b.w = tag
            b.r = []
        self.ops[eng].append((waits, fn, (self.esem[eng], 1)))

    def dma(self, q, out_ap, in_ap, reads=(), writes=(), sem_buf=None, **kw):
        self.nrec += 1
        if self.limit and self.nrec > self.limit:
            return
        sb = sem_buf
        if sb is None:
            for b in list(writes) + list(reads):
                sb = b
                break
        if sb.dsem is None:
            sb.dsem = self.new_dsem(sb.name, "sw" if q == "pool" else "hw")
        ds = sb.dsem
        waits = []
        self._pend(q, waits)
        waits = waits + self._deps(q, reads, writes)
        if (not ds.group_open) and ds.issued > 0:
            w2 = []
            self._need(q, ("dma", ds, 16 * ds.issued), w2)
            waits += w2
        ds.issued += 1
        ds.group_open = True
        tag = ("dma", ds, 16 * ds.issued)
        for b in reads:
            b.r.append(tag)
        for b in writes:
            b.w = tag
            b.r = []

        def fn(e, out_ap=out_ap, in_ap=in_ap, kw=kw):
            return e.dma_start(out=out_ap, in_=in_ap, **kw)
        if q in ("pool", "act"):
            pass
        self.ops[q].append((self._merge(waits), fn, (ds.sem, 16)))

    def finish(self):
        fin = []
        for ds in self.dsems + self.csems:
            if ds.issued:
                fin.append((ds.sem, ds.unit * ds.issued))
        nc = self.nc
        ops = self.ops
        with nc.Block() as block:
            def emit(e, lst, final=None):
                for waits, fn, inc in lst:
                    for s, v in waits:
                        e.wait_ge(s, v)
                    ins = fn(e)
                    if inc is not None:
                        if inc[1] is None:
                            ins.then_inc(inc[0])
                        else:
                            ins.then_inc(inc[0], inc[1])
                if final:
                    for s, v in final:
                        e.wait_ge(s, v)

            @block.sync
            def _(e):
                emit(e, ops["sp"], fin)

            @block.tensor
            def _(e):
                emit(e, ops["pe"])

            @block.scalar
            def _(e):
                emit(e, ops["act"])

            @block.vector
            def _(e):
                emit(e, ops["dve"])

            @block.gpsimd
            def _(e):
                emit(e, ops["pool"])


BIG = 30000.0
NEGF = -1.0e30
LN2 = 0.6931471805599453
GC = 0.7978845608028654


def host_consts_B(T, slopes2):
    import ml_dtypes
    bf = ml_dtypes.bfloat16
    nb = T // 256
    c = {}
    oh = np.zeros((33, T), np.float32)
    for n in range(nb):
        oh[n, n * 256:(n + 1) * 256] = 1.0
    oh[32, :] = 1.0
    c["ohk"] = oh.astype(bf)
    t = np.arange(T) % 512
    c["crow"] = np.stack([-8.0 * s * t for s in slopes2]).astype(np.float32).astype(bf)
    NM = T // 128 + 4
    m = np.arange(NM)
    p = np.arange(128)
    tab = np.zeros((128, 2, NM), np.float32)
    for h in range(2):
        tab[:, h, :] = slopes2[h] * (p[:, None] + 128.0 * (m[None, :] - (T // 128)))
    c["abias"] = tab
    c["cmask"] = (np.arange(128)[None, :] >= np.arange(128)[:, None]).astype(np.float32).astype(bf)
    cm = (np.arange(64)[None, :] >= np.arange(64)[:, None]).astype(np.float32)
    bm = np.zeros((128, 128), np.float32)
    bm[:64, :64] = cm
    bm[64:, 64:] = cm
    c["hmask"] = np.tile(bm, (1, 4)).astype(np.float32)
    rm = np.ones((64, 512), np.float32)
    rm[:, ::64] = 0.0
    c["rmask"] = rm
    c["ident"] = np.eye(128, dtype=np.float32).astype(bf)
    c["ones64"] = np.full((64, 64), 1.0 / 64.0, np.float32)
    return c


def build_B(nc, P, T, io, maxdist=(None, None)):
    NB = T // 512
    NM = T // 128 + 4
    MOFF = T // 128
    hT, yT = io.get("hT"), io.get("yT")

    def ydst(i, blk_):
        if "ydst" in io:
            return io["ydst"](i, blk_)
        return yT[i, :, blk_ * 512:blk_ * 512 + 512]
    wfm = P.sbuf("wfm", [128, 8, 640], BF16); Bwfm = P.buf("wfm")
    wtm = P.sbuf("wtm", [128, 8, 192], BF16); Bwtm = P.buf("wtm")
    par = P.sbuf("par", [128, 32], F32); Bpar = P.buf("par")
    wg = P.sbuf("wg", [128, 128], BF16); Bwg = P.buf("wg")
    abias = P.sbuf("abias", [128, 2, NM], F32); Bab = P.buf("abias")
    cmask = P.sbuf("cmask", [128, 128], BF16); Bcm = P.buf("cmask")
    hmask = P.sbuf("hmask", [128, 512], F32); Bhm = P.buf("hmask")
    rmask = P.sbuf("rmask", [64, 512], F32); Brm = P.buf("rmask")
    ident = P.sbuf("ident", [128, 128], BF16); Bid = P.buf("ident")
    ones64 = P.sbuf("ones64", [64, 64], F32); Bo64 = P.buf("ones64")
    QA = P.sbuf("QA", [128, T], BF16); QB = P.sbuf("QB", [128, T], BF16)
    KA = P.sbuf("KA", [128, T], BF16); KB = P.sbuf("KB", [128, T], BF16)
    NBK = T // 512
    BQ = [[P.buf(f"Q{h}_{i}") for i in range(NBK)] for h in range(2)]
    BK = [[P.buf(f"K{h}_{i}") for i in range(NBK)] for h in range(2)]
    Qh = [QA, QB]; Kh = [KA, KB]
    VV = P.sbuf("VV", [128, T // 128, 2, 65], BF16); BV = [P.buf(f"VV{i}") for i in range(NBK)]
    VH = P.sbuf("VH", [128, T // 128, 64], BF16); BVH = [P.buf(f"VH{i}") for i in range(NBK)]
    ones64r = P.sbuf("ones64r", [128, 64], F32); Bo64r = P.buf("ones64r")
    kmT = P.sbuf("kmT", [128, 32], BF16); Bkm = P.buf("kmT")
    banks = [P.psum(f"bank{i}", [128, 512]) for i in range(8)]
    Bbank = [P.buf(f"bank{i}") for i in range(8)]
    for b_ in Bbank:
        b_.excl = True
    rr = [0]

    def nbank():
        i = rr[0] % 4
        rr[0] += 1
        return banks[i], Bbank[i]
    rr2 = [0]

    def nbank2():
        i = 4 + rr2[0] % 2
        rr2[0] += 1
        return banks[i], Bbank[i]
    pvb = [(banks[6], Bbank[6]), (banks[7], Bbank[7])]

    P.dma("pool", wfm[:], io["wfm"].rearrange("(c p) n -> p c n", p=128), writes=[Bwfm])
    P.dma("pool", wtm[:], io["wtm"].rearrange("(c p) n -> p c n", p=128), writes=[Bwtm])
    P.dma("pool", wg[:], io["wg"], writes=[Bwg])
    P.dma("sp", par[:, 0:16], io["par"], writes=[Bpar])
    P.dma("sp", abias[:], io["abias"], writes=[Bab])
    P.dma("sp", cmask[:], io["cmask"], writes=[Bcm])
    P.dma("sp", hmask[:], io["hmask"], writes=[Bhm])
    P.dma("sp", rmask[:], io["rmask"], writes=[Brm])
    P.dma("sp", ident[:], io["ident"], writes=[Bid])
    P.dma("sp", ones64[:], io["ones64"], writes=[Bo64])
    for h in range(2):
        P.op("pool", lambda e, h=h: e.memset(Qh[h][:], 0.0), writes=BQ[h])
        P.op("pool", lambda e, h=h: e.memset(Kh[h][:], 0.0), writes=BK[h])
    P.dma("sp", KA[64:97, :], io["ohk"], writes=BK[0], sem_buf=BK[0][0])
    P.dma("sp", KB[0:33, :], io["ohk"], writes=BK[1], sem_buf=BK[1][0])
    P.dma("sp", QA[96:97, :], io["crow"][0:1, :], writes=BQ[0], sem_buf=BQ[0][0])
    P.dma("sp", QB[32:33, :], io["crow"][1:2, :], writes=BQ[1], sem_buf=BQ[1][0])
    P.op("pool", lambda e: e.memset(VV[:, :, :, 64:65], 1.0), writes=BV)
    P.op("pool", lambda e: e.memset(ones64r[:], 1.0), writes=[Bo64r])
    P.op("pool", lambda e: e.memset(kmT[:], 0.0), writes=[Bkm])

    L = slice(64, 128)
    H = slice(0, 64)
    def pc(rows, j):
        return par[rows, j:j + 1]
    P.op("dve", lambda e: e.memset(par[:, 24:25], -LN2), reads=[Bpar], writes=[Bpar])
    P.op("dve", lambda e: e.memset(par[:, 25:26], 1e-6), reads=[Bpar], writes=[Bpar])
    P.op("dve", lambda e: e.memset(par[:, 26:27], 1.0), reads=[Bpar], writes=[Bpar])
    P.op("dve", lambda e: e.tensor_scalar(out=par[L, 8:10], in0=par[L, 5:7], scalar1=0.5, scalar2=None, op0=ALU.mult), reads=[Bpar], writes=[Bpar])
    P.op("act", lambda e: e.activation(out=pc(L, 12), in_=pc(L, 7), func=AF.Exp, scale=-1.0), reads=[Bpar], writes=[Bpar])
    P.op("act", lambda e: e.activation(out=pc(L, 13), in_=pc(L, 12), func=AF.Ln, bias=pc(L, 26)), reads=[Bpar], writes=[Bpar])
    P.op("dve", lambda e: e.tensor_scalar(out=pc(L, 10), in0=pc(L, 13), scalar1=-8.0, scalar2=None, op0=ALU.mult), reads=[Bpar], writes=[Bpar])
    P.op("dve", lambda e: e.tensor_scalar(out=pc(L, 11), in0=pc(L, 13), scalar1=-4.0, scalar2=None, op0=ALU.mult), reads=[Bpar], writes=[Bpar])
    P.op("dve", lambda e: e.tensor_tensor(out=pc(H, 20), in0=pc(H, 1), in1=pc(H, 0), op=ALU.subtract), reads=[Bpar], writes=[Bpar])
    P.op("act", lambda e: e.activation(out=pc(H, 21), in_=pc(H, 20), func=AF.Tanh, scale=0.5), reads=[Bpar], writes=[Bpar])
    P.op("dve", lambda e: e.tensor_scalar(out=pc(H, 22), in0=pc(H, 21), scalar1=0.5, scalar2=0.5, op0=ALU.mult, op1=ALU.add), reads=[Bpar], writes=[Bpar])
    P.op("dve", lambda e: e.tensor_tensor(out=pc(H, 19), in0=pc(H, 22), in1=pc(H, 3), op=ALU.mult), reads=[Bpar], writes=[Bpar])
    P.op("dve", lambda e: e.tensor_scalar(out=pc(H, 16), in0=pc(H, 19), scalar1=-0.5, scalar2=0.5, op0=ALU.mult, op1=ALU.add), reads=[Bpar], writes=[Bpar])
    P.op("dve", lambda e: e.tensor_scalar(out=pc(H, 18), in0=pc(H, 16), scalar1=-1.0, scalar2=None, op0=ALU.mult), reads=[Bpar], writes=[Bpar])
    P.op("dve", lambda e: e.tensor_scalar(out=pc(H, 23), in0=pc(H, 19), scalar1=1e-30, scalar2=None, op0=ALU.max), reads=[Bpar], writes=[Bpar])
    P.op("dve", lambda e: e.tensor_tensor(out=pc(H, 17), in0=pc(H, 23), in1=pc(H, 16), op=ALU.add), reads=[Bpar], writes=[Bpar])

    def rot(name, shape, dt, n):
        ts = [P.sbuf(f"{name}{i}", shape, dt) for i in range(n)]
        bs = [P.buf(f"{name}{i}") for i in range(n)]
        return ts, bs
    hblk, Bhblk = rot("hblk", [128, 8, 512], BF16, 2)
    xbuf, Bxbuf = rot("xbuf", [128, 515], F32, 2)
    NW = 2
    W = {}

    class HV:
        def __init__(self, t):
            self.t = t

        def __getitem__(self, key):
            if isinstance(key, tuple):
                return self.t[(slice(0, 64),) + tuple(key[1:])]
            return self.t[0:64, :]
    share = {"thq": "ysb", "thf": "t1", "thg": "t2", "gs2": "xc", "fg": "thr", "kk": "thi", "bb": "aa", "eb": "a2", "enb": "uu",
             "qs2": "hh", "kve": "sh1", "osb": "sh2", "osq": "sh3", "rstd": "sh4"}
    for nm, shp, dt in [("ysb", [128, 512], F32), ("t1", [128, 512], F32), ("t2", [128, 512], F32),
                        ("xc", [128, 512], F32), ("xcb", [128, 512], BF16), ("thr", [128, 512], F32), ("thi", [128, 512], F32),
                        ("aa", [128, 512], F32), ("a2", [128, 512], F32), ("uu", [128, 512], F32), ("hh", [128, 512], F32),
                        ("sh1", [128, 512], F32), ("sh2", [128, 512], F32), ("sh3", [128, 512], F32), ("sh4", [128, 512], F32),
                        ("yo", [128, 512], BF16),
                        ("qt", [64, 512], BF16), ("kt", [64, 512], BF16),
                        ("ktokE", [128, 4, 64], BF16), ("ktokO", [128, 4, 64], BF16), ("attm", [128, 512], BF16), ("ebl", [64, 8], F32),
                        ("hy", [64, 512], BF16),
                        ("gsb", [128, 32], F32), ("top8", [128, 8], F32), ("top8g", [128, 8], F32), ("mp", [128, 32], BF16),
                        ("pt", [128, 512], BF16), ("onum", [65, 512], F32), ("rec", [65, 512], F32), ("my", [64, 512], BF16)]:
        n = {"pt": 7, "gsb": 4, "top8": 4, "top8g": 4, "mp": 4, "sh1": 1, "sh2": 1, "sh3": 1, "sh4": 1, "rec": 1}.get(nm, NW)
        W[nm] = rot(nm, shp, dt, n)
    for hn, ln in share.items():
        ts, _ = W[ln]
        W[hn] = ([HV(t) for t in ts], [P.buf(f"{hn}{i}") for i in range(len(ts))])
    ctr = {}

    def wt(nm):
        i = ctr.get(nm, 0)
        ctr[nm] = i + 1
        ts, bs = W[nm]
        return ts[i % len(ts)], bs[i % len(bs)]
    for i in range(4):
        P.op("pool", lambda e, i=i: e.memset(W["gsb"][0][i][:], NEGF), writes=[W["gsb"][1][i]])
    for nm_ in ("ktokE", "ktokO"):
        for i in range(NW):
            P.op("pool", lambda e, nm_=nm_, i=i: e.memset(W[nm_][0][i][:], 0.0), writes=[W[nm_][1][i]])
    Sst = P.sbuf("Sst", [64, 64], F32); BS = P.buf("Sst")
    Sbf, BSbf = rot("Sbf", [64, 64], BF16, 4)
    P.op("dve", lambda e: e.memset(Sst[:], 0.0), writes=[BS])
    P.op("dve", lambda e: e.memset(xbuf[1][L, 512:515], 0.0), writes=[Bxbuf[1]])
    hprev = [None]

    S1 = {}

    class Deferred:
        def __init__(self):
            self.q = []
            self.atomic = None
            self.tag = 0

        def op(self, *a, **k):
            (self.atomic if self.atomic is not None else self.q).append(("op", a, k, self.tag))

        def dma(self, *a, **k):
            (self.atomic if self.atomic is not None else self.q).append(("dma", a, k, self.tag))

        def begin(self):
            self.atomic = []

        def end(self):
            self.q.append(("grp", self.atomic, None, self.tag))
            self.atomic = None

        def _emit(self):
            kind, a, k, _ = self.q.pop(0)
            items = a if kind == "grp" else [(kind, a, k, 0)]
            for kd, aa, kk, _t in items:
                (P.op if kd == "op" else P.dma)(*aa, **kk)
            return len(items)

        def flush(self, n=None):
            while self.q and (n is None or n > 0):
                c = self._emit()
                if n is not None:
                    n -= c

        def flush_upto(self, tag):
            while self.q and self.q[0][3] <= tag:
                self._emit()

        def count_upto(self, tag):
            return sum(1 for it in self.q if it[3] <= tag)
    PQ = Deferred()

    def stage1(blk):
        c0 = blk * 512
        hb, Bhb = hblk[blk % 2], Bhblk[blk % 2]
        if "hsrc" in io:
            P.dma("sp", hb[:], io["hsrc"](blk), writes=[Bhb])
        else:
            P.dma("sp", hb[:], hT[:, c0:c0 + 512].rearrange("(c p) n -> p c n", p=128), writes=[Bhb])

        def inproj(col0, M, bank, Bb, rows=slice(0, 128)):
            for k in range(8):
                P.op("pe", lambda e, k=k: e.matmul(bank[rows, :], lhsT=wfm[:, k, col0:col0 + M], rhs=hb[:, k, :], start=(k == 0), stop=(k == 7)),
                     reads=[Bwfm, Bhb], writes=[Bb])
        b4, Bb4 = nbank(); inproj(320, 128, b4, Bb4)
        b5, Bb5 = nbank(); inproj(448, 128, b5, Bb5)
        P.op("dve", lambda e: e.tensor_copy(out=QA[0:64, c0:c0 + 512], in_=b4[0:64, :]), reads=[Bb4], writes=[BQ[0][blk]])
        P.op("dve", lambda e: e.tensor_copy(out=QB[64:128, c0:c0 + 512], in_=b4[64:128, :]), reads=[Bb4], writes=[BQ[1][blk]])
        P.op("act", lambda e: e.activation(out=KA[0:64, c0:c0 + 512], in_=b5[0:64, :], func=AF.Identity), reads=[Bb5], writes=[BK[0][blk]])
        P.op("dve", lambda e: e.tensor_copy(out=KB[64:128, c0:c0 + 512], in_=b5[64:128, :]), reads=[Bb5], writes=[BK[1][blk]])
        kms, Bkms = wt("top8")
        P.op("dve", lambda e: e.tensor_reduce(out=kms[:, 0:2], in_=b5[:].rearrange("p (n k) -> p n k", n=2), axis=AX.X, op=ALU.add), reads=[Bb5], writes=[Bkms])
        P.op("dve", lambda e: e.tensor_scalar(out=kmT[:, 2 * blk:2 * blk + 2], in0=kms[:, 0:2], scalar1=1.0 / 256.0, scalar2=None, op0=ALU.mult), reads=[Bkms], writes=[Bkm])
        for pr in range(2):
            bv, Bbv = nbank()
            for tt in (2 * pr, 2 * pr + 1):
                o = (tt % 2) * 192
                for k in range(8):
                    P.op("pe", lambda e, k=k, tt=tt, o=o, bv=bv: e.matmul(bv[:, o:o + 192], lhsT=hb[:, k, tt * 128:(tt + 1) * 128], rhs=wtm[:, k, :], start=(k == 0), stop=(k == 7)),
                         reads=[Bwtm, Bhb], writes=[Bbv])
            for tt in (2 * pr, 2 * pr + 1):
                gi = blk * 4 + tt
                o = (tt % 2) * 192
                P.op("dve", lambda e, gi=gi, o=o, bv=bv: e.tensor_copy(out=VH[:, gi, :], in_=bv[:, o:o + 64]), reads=[Bbv], writes=[BVH[blk]])
                P.op("dve", lambda e, gi=gi, o=o, bv=bv: e.tensor_copy(out=VV[:, gi, :, 0:64], in_=bv[:, o + 64:o + 192].rearrange("p (h d) -> p h d", h=2)), reads=[Bbv], writes=[BV[blk]])
        b1, Bb1 = nbank(); inproj(0, 128, b1, Bb1)
        b2, Bb2 = nbank(); inproj(128, 128, b2, Bb2)
        b3, Bb3 = nbank(); inproj(256, 64, b3, Bb3, rows=slice(0, 64))
        xb, Bxb = xbuf[blk % 2], Bxbuf[blk % 2]
        P.op("act", lambda e: e.activation(out=xb[L, 3:515], in_=b1[L, :], func=AF.Identity), reads=[Bb1], writes=[Bxb])
        xo_, Bxo_ = xbuf[(blk + 1) % 2], Bxbuf[(blk + 1) % 2]
        P.op("pool", lambda e: e.tensor_copy(out=xb[L, 0:3], in_=xo_[L, 512:515]), reads=[Bxo_], writes=[Bxb])
        ysb, Bysb = wt("ysb")
        P.op("dve", lambda e: e.tensor_copy(out=ysb[L, :], in_=b2[L, :]), reads=[Bb2], writes=[Bysb])
        thq, Bthq = wt("thq"); thf, Bthf = wt("thf"); thg, Bthg = wt("thg")
        P.op("act", lambda e: e.activation(out=thq[:], in_=b1[H, :], func=AF.Tanh, scale=0.5), reads=[Bb1], writes=[Bthq])
        P.op("act", lambda e: e.activation(out=thf[:], in_=b2[H, :], func=AF.Tanh, scale=0.5), reads=[Bb2], writes=[Bthf])
        P.op("act", lambda e: e.activation(out=thg[:], in_=b3[H, :], func=AF.Tanh, scale=0.5), reads=[Bb3], writes=[Bthg])
        qs2, Bqs2 = wt("qs2"); gs2, Bgs2 = wt("gs2")
        P.op("dve", lambda e: e.scalar_tensor_tensor(out=qs2[:], in0=thq[:], scalar=1.0, in1=b1[H, :], op0=ALU.add, op1=ALU.mult), reads=[Bthq, Bb1], writes=[Bqs2])
        P.op("dve", lambda e: e.scalar_tensor_tensor(out=gs2[:], in0=thg[:], scalar=1.0, in1=b3[H, :], op0=ALU.add, op1=ALU.mult), reads=[Bthg, Bb3], writes=[Bgs2])
        S1[blk] = dict(xb=(xb, Bxb), ysb=(ysb, Bysb), thf=(thf, Bthf), qs2=(qs2, Bqs2), gs2=(gs2, Bgs2))

    def stage2(blk):
        c0 = blk * 512
        d = S1.pop(blk)
        xb, Bxb = d["xb"]; ysb, Bysb = d["ysb"]; thf, Bthf = d["thf"]; qs2, Bqs2 = d["qs2"]; gs2, Bgs2 = d["gs2"]
        xo, Bxo = xbuf[(blk + 1) % 2], Bxbuf[(blk + 1) % 2]
        xc, Bxc = wt("xc")
        PQ.op("pool", lambda e: e.tensor_scalar(out=xc[L, :], in0=xb[L, 3:515], scalar1=pc(L, 3), scalar2=pc(L, 4), op0=ALU.mult, op1=ALU.add), reads=[Bxb, Bpar], writes=[Bxc])
        for j in (2, 1, 0):
            PQ.op("dve", lambda e, j=j: e.scalar_tensor_tensor(out=xc[L, :], in0=xb[L, j:j + 512], scalar=pc(L, j), in1=xc[L, :], op0=ALU.mult, op1=ALU.add), reads=[Bxb, Bpar, Bxc], writes=[Bxc])
        xcb, Bxcb = wt("xcb")
        PQ.op("pool", lambda e: e.tensor_copy(out=xcb[L, :], in_=xc[L, :]), reads=[Bxc], writes=[Bxcb])
        bg, Bbg = nbank2()
        bg2, Bbg2 = nbank2()
        PQ.op("pe", lambda e: e.matmul(bg[L, :], lhsT=wg[L, 0:64], rhs=xcb[L, :], start=True, stop=True), reads=[Bwg, Bxcb], writes=[Bbg])
        PQ.op("pe", lambda e: e.matmul(bg2[L, :], lhsT=wg[L, 64:128], rhs=xcb[L, :], start=True, stop=True), reads=[Bwg, Bxcb], writes=[Bbg2])
        thr, Bthr = wt("thr"); thi, Bthi = wt("thi")
        PQ.op("act", lambda e: e.activation(out=thr[L, :], in_=bg[L, :], func=AF.Tanh, scale=0.5, bias=pc(L, 8)), reads=[Bbg, Bpar], writes=[Bthr])
        PQ.op("act", lambda e: e.activation(out=thi[L, :], in_=bg2[L, :], func=AF.Tanh, scale=0.5, bias=pc(L, 9)), reads=[Bbg2, Bpar], writes=[Bthi])
        aa, Baa = wt("aa"); a2, Ba2 = wt("a2")
        PQ.op("act", lambda e: e.activation(out=aa[L, :], in_=thr[L, :], func=AF.Exp, scale=pc(L, 11), bias=pc(L, 11)), reads=[Bthr, Bpar], writes=[Baa])
        PQ.op("act", lambda e: e.activation(out=a2[L, :], in_=thr[L, :], func=AF.Exp, scale=pc(L, 10), bias=pc(L, 10)), reads=[Bthr, Bpar], writes=[Ba2])
        t1, Bt1 = wt("t1"); t2, Bt2 = wt("t2")
        PQ.op("act", lambda e: e.activation(out=t1[L, :], in_=ysb[L, :], func=AF.Square), reads=[Bysb], writes=[Bt1])
        PQ.op("pool", lambda e: e.tensor_scalar(out=t1[L, :], in0=t1[L, :], scalar1=0.044715, scalar2=1.0, op0=ALU.mult, op1=ALU.add), reads=[Bt1], writes=[Bt1])
        PQ.op("pool", lambda e: e.tensor_tensor(out=t1[L, :], in0=t1[L, :], in1=ysb[L, :], op=ALU.mult), reads=[Bt1, Bysb], writes=[Bt1])
        PQ.op("act", lambda e: e.activation(out=t2[L, :], in_=t1[L, :], func=AF.Tanh, scale=GC), reads=[Bt1], writes=[Bt2])
        PQ.op("dve", lambda e: e.scalar_tensor_tensor(out=t2[L, :], in0=t2[L, :], scalar=1.0, in1=ysb[L, :], op0=ALU.add, op1=ALU.mult), reads=[Bt2, Bysb], writes=[Bt2])
        uu, Buu = wt("uu")
        PQ.op("dve", lambda e: e.scalar_tensor_tensor(out=uu[L, :], in0=thi[L, :], scalar=1.0, in1=xc[L, :], op0=ALU.add, op1=ALU.mult), reads=[Bthi, Bxc], writes=[Buu])
        fg, Bfg = wt("fg"); kk, Bkk = wt("kk")
        PQ.op("dve", lambda e: e.tensor_scalar(out=fg[:], in0=thf[:], scalar1=pc(H, 16), scalar2=pc(H, 17), op0=ALU.mult, op1=ALU.add), reads=[Bthf, Bpar], writes=[Bfg])
        PQ.op("dve", lambda e: e.tensor_scalar(out=kk[:], in0=thf[:], scalar1=pc(H, 18), scalar2=pc(H, 16), op0=ALU.mult, op1=ALU.add), reads=[Bthf, Bpar], writes=[Bkk])
        PQ.op("act", lambda e: e.activation(out=a2[L, :], in_=a2[L, :], func=AF.Ln, scale=-1.0, bias=pc(L, 26)), reads=[Ba2], writes=[Ba2])
        PQ.op("act", lambda e: e.activation(out=fg[:], in_=fg[:], func=AF.Ln), reads=[Bfg], writes=[Bfg])
        PQ.op("act", lambda e: e.activation(out=a2[L, :], in_=a2[L, :], func=AF.Exp, scale=0.5), reads=[Ba2], writes=[Ba2])
        bb, Bbb = wt("bb")
        PQ.op("dve", lambda e: e.tensor_tensor_scan(out=bb[:], data0=rmask[:], data1=fg[:], initial=0.0, op0=ALU.mult, op1=ALU.add), reads=[Brm, Bfg], writes=[Bbb])
        eb, Beb = wt("eb"); enb, Benb = wt("enb"); ebl, Bebl = wt("ebl")
        PQ.op("act", lambda e: e.activation(out=eb[:], in_=bb[:], func=AF.Exp, bias=pc(H, 24)), reads=[Bbb, Bpar], writes=[Beb])
        PQ.op("act", lambda e: e.activation(out=enb[:], in_=bb[:], func=AF.Exp, scale=-1.0), reads=[Bbb], writes=[Benb])
        PQ.op("act", lambda e: e.activation(out=ebl[:], in_=bb[:, 63:512:64], func=AF.Exp), reads=[Bbb], writes=[Bebl])
        PQ.op("dve", lambda e: e.scalar_tensor_tensor(out=uu[L, :], in0=uu[L, :], scalar=0.5, in1=a2[L, :], op0=ALU.mult, op1=ALU.mult), reads=[Buu, Ba2], writes=[Buu])
        hh, Bhh = wt("hh")
        if hprev[0] is None:
            PQ.op("dve", lambda e: e.tensor_tensor_scan(out=hh[L, :], data0=aa[L, :], data1=uu[L, :], initial=0.0, op0=ALU.mult, op1=ALU.add), reads=[Baa, Buu], writes=[Bhh])
        else:
            hp, Bhp = hprev[0]
            PQ.op("dve", lambda e, hp=hp: e.tensor_tensor_scan(out=hh[L, :], data0=aa[L, :], data1=uu[L, :], initial=hp[L, 511:512], op0=ALU.mult, op1=ALU.add), reads=[Baa, Buu, Bhp], writes=[Bhh])
        hprev[0] = (hh, Bhh)
        yo, Byo = wt("yo")
        PQ.op("dve", lambda e: e.scalar_tensor_tensor(out=yo[L, :], in0=hh[L, :], scalar=0.5, in1=t2[L, :], op0=ALU.mult, op1=ALU.mult), reads=[Bhh, Bt2], writes=[Byo])
        PQ.dma("sp", ydst(0, blk), yo[L, :], reads=[Byo])
        qt, Bqt = wt("qt"); kt, Bkt = wt("kt")
        PQ.op("dve", lambda e: e.tensor_tensor(out=qt[:], in0=qs2[:], in1=eb[:], op=ALU.mult), reads=[Bqs2, Beb], writes=[Bqt])
        PQ.op("dve", lambda e: e.tensor_tensor(out=kt[:], in0=kk[:], in1=enb[:], op=ALU.mult), reads=[Bkk, Benb], writes=[Bkt])
        btr, Bbtr = nbank2()
        btr16 = btr[:].bitcast(BF16)
        ktokE, BktokE = wt("ktokE"); ktokO, BktokO = wt("ktokO")
        for tt in range(4):
            PQ.op("pe", lambda e, tt=tt: e.transpose(btr16[:, tt * 64:(tt + 1) * 64], in_=kt[:, tt * 128:(tt + 1) * 128], identity=ident[0:64, 0:64]), reads=[Bkt, Bid], writes=[Bbtr])
        PQ.op("act", lambda e: e.activation(out=ktokE[0:64, :, :].rearrange("p t d -> p (t d)"), in_=btr16[0:64, 0:256], func=AF.Identity), reads=[Bbtr], writes=[BktokE])
        PQ.op("act", lambda e: e.activation(out=ktokO[64:128, :, :].rearrange("p t d -> p (t d)"), in_=btr16[64:128, 0:256], func=AF.Identity), reads=[Bbtr], writes=[BktokO])
        bkv, Bbkv = nbank2()
        for c in range(8):
            tt, hf = c // 2, c % 2
            gi = blk * 4 + tt
            kx, Bkx = (ktokE, BktokE) if hf == 0 else (ktokO, BktokO)
            PQ.op("pe", lambda e, c=c, tt=tt, gi=gi, kx=kx: e.matmul(bkv[0:64, c * 64:(c + 1) * 64], lhsT=kx[:, tt, :], rhs=VH[:, gi, :], start=True, stop=True),
                 reads=[Bkx, BVH[blk]], writes=[Bbkv])
        batt, Bbatt = nbank2()
        for tt in range(4):
            PQ.op("pe", lambda e, tt=tt: e.matmul(batt[:, tt * 128:(tt + 1) * 128], lhsT=kt[:, tt * 128:(tt + 1) * 128], rhs=qt[:, tt * 128:(tt + 1) * 128], start=True, stop=True),
                 reads=[Bkt, Bqt], writes=[Bbatt])
        attm, Battm = wt("attm")
        PQ.op("dve", lambda e: e.tensor_tensor(out=attm[:], in0=batt[:], in1=hmask[:], op=ALU.mult), reads=[Bbatt, Bhm], writes=[Battm])
        kve, Bkve = wt("kve")
        for c in range(8):
            PQ.op("act", lambda e, c=c: e.activation(out=kve[:, c * 64:(c + 1) * 64], in_=bkv[0:64, c * 64:(c + 1) * 64], func=AF.Identity, scale=ebl[:, c:c + 1]), reads=[Bbkv, Bebl], writes=[Bkve])
        bo, Bbo = nbank2()
        for c in range(8):
            tt, hf = c // 2, c % 2
            gi = blk * 4 + tt
            sb, Bsb = Sbf[(blk * 8 + c) % 4], BSbf[(blk * 8 + c) % 4]
            if hf == 0:
                PQ.begin()
            PQ.op("act", lambda e, sb=sb: e.activation(out=sb[:], in_=Sst[:], func=AF.Identity), reads=[BS], writes=[Bsb])
            if hf == 0:
                PQ.op("pe", lambda e, tt=tt, gi=gi: e.matmul(bo[0:64, tt * 128:(tt + 1) * 128], lhsT=VH[:, gi, :], rhs=attm[:, tt * 128:(tt + 1) * 128], start=True, stop=False),
                     reads=[BVH[blk], Battm], writes=[Bbo])
            PQ.op("pe", lambda e, c=c, sb=sb, hf=hf: e.matmul(bo[0:64, c * 64:(c + 1) * 64], lhsT=sb[:], rhs=qt[:, c * 64:(c + 1) * 64], start=False, stop=(hf == 1)),
                 reads=[Bsb, Bqt], writes=[Bbo])
            PQ.op("dve", lambda e, c=c: e.scalar_tensor_tensor(out=Sst[:], in0=Sst[:], scalar=ebl[:, c:c + 1], in1=kve[:, c * 64:(c + 1) * 64], op0=ALU.mult, op1=ALU.add), reads=[BS, Bebl, Bkve], writes=[BS])
            if hf == 1:
                PQ.end()
        osb, Bosb = wt("osb"); osq, Bosq = wt("osq")
        PQ.op("act", lambda e: e.activation(out=osb[:], in_=bo[0:64, :], func=AF.Identity), reads=[Bbo], writes=[Bosb])
        PQ.op("act", lambda e: e.activation(out=osq[:], in_=bo[0:64, :], func=AF.Square), reads=[Bbo], writes=[Bosq])
        bms, Bbms = nbank2()
        PQ.op("pe", lambda e: e.matmul(bms[0:64, :], lhsT=ones64[:], rhs=osq[:], start=True, stop=True), reads=[Bo64, Bosq], writes=[Bbms])
        rstd, Brstd = wt("rstd")
        PQ.op("act", lambda e: e.activation(out=rstd[:], in_=bms[0:64, :], func=AF.Ln, bias=pc(H, 25)), reads=[Bbms, Bpar], writes=[Brstd])
        PQ.op("act", lambda e: e.activation(out=rstd[:], in_=rstd[:], func=AF.Exp, scale=-0.5), reads=[Brstd], writes=[Brstd])
        PQ.op("dve", lambda e: e.tensor_tensor(out=osb[:], in0=osb[:], in1=rstd[:], op=ALU.mult), reads=[Bosb, Brstd], writes=[Bosb])
        PQ.op("dve", lambda e: e.tensor_scalar(out=osb[:], in0=osb[:], scalar1=pc(H, 2), scalar2=0.5, op0=ALU.mult, op1=ALU.mult), reads=[Bosb, Bpar], writes=[Bosb])
        hy, Bhy = wt("hy")
        PQ.op("dve", lambda e: e.tensor_tensor(out=hy[:], in0=osb[:], in1=gs2[:], op=ALU.mult), reads=[Bosb, Bgs2], writes=[Bhy])
        PQ.dma("sp", ydst(1, blk), hy[:], reads=[Bhy])

    def stage3(blk, tick=None):
        for h in range(2):
            head3(blk, h, tick)

    def gating(blk, h):
        c0 = blk * 512
        Q = Qh[h]
        dr = slice(0, 64) if h == 0 else slice(64, 128)
        mr = slice(64, 96) if h == 0 else slice(0, 32)
        bgt, Bbgt = nbank2()
        bmp, Bbmp = nbank2()
        bmp16 = bmp[:].bitcast(BF16)
        any_mp = False
        for st in range(4):
            j = 2 * blk + (st // 2)
            if j == 0:
                continue
            any_mp = True
            q0 = c0 + st * 128
            PQ.op("pe", lambda e, q0=q0, st=st: e.matmul(bgt[:, st * 32:(st + 1) * 32], lhsT=Q[dr, q0:q0 + 128], rhs=kmT[dr, 0:32], start=True, stop=True),
                  reads=[BQ[h][blk], Bkm], writes=[Bbgt])
            gsb, Bgsb = wt("gsb"); top8, Btop8 = wt("top8g"); mp, Bmp = wt("mp")
            PQ.op("dve", lambda e, st=st, j=j, gsb=gsb: e.tensor_copy(out=gsb[:, 0:j], in_=bgt[:, st * 32:st * 32 + j]), reads=[Bbgt], writes=[Bgsb])
            PQ.op("dve", lambda e, j=j, gsb=gsb, top8=top8: e.max(out=top8[:], in_=gsb[:, 0:max(j, 8)]), reads=[Bgsb], writes=[Btop8])
            PQ.op("pool", lambda e, mp=mp: e.memset(mp[:], 0.0), writes=[Bmp])
            PQ.op("dve", lambda e, j=j, gsb=gsb, top8=top8, mp=mp: e.tensor_scalar(out=mp[:, 0:j], in0=gsb[:, 0:j], scalar1=top8[:, 2:3], scalar2=-8.0 * BIG, op0=ALU.is_lt, op1=ALU.mult),
                  reads=[Bgsb, Btop8, Bmp], writes=[Bmp])
            PQ.op("pe", lambda e, st=st, mp=mp: e.transpose(bmp16[mr, st * 128:(st + 1) * 128], in_=mp[:], identity=ident[:]), reads=[Bmp, Bid], writes=[Bbmp])
        if any_mp:
            s0 = 0 if blk > 0 else 2
            PQ.op("act", lambda e, s0=s0: e.activation(out=Q[mr, c0 + s0 * 128:c0 + 512], in_=bmp16[mr, s0 * 128:512], func=AF.Identity), reads=[Bbmp], writes=[BQ[h][blk]])

    postponed = []

    def head3(blk, h, tick=None):
        c0 = blk * 512
        if True:
            Q, K = Qh[h], Kh[h]
            pv, Bpv = pvb[h]
            tiles = [(kt_, 0) for kt_ in range(4 * blk) if (maxdist[h] is None or (c0 - (kt_ * 128 + 127)) <= maxdist[h])] + [(4 * blk + kk_, kk_) for kk_ in range(4)]
            LA = 3
            pend = []
            ntile_done = [0]
            prev_fin = list(postponed)
            del postponed[:]
            state = {"first": True}

            def emit_pv(item):
                kti, n0, pt, Bpt, kb, last = item
                first = state["first"]
                P.op("pe", lambda e, kti=kti, n0=n0, pt=pt, first=first, last=last: e.matmul(pv[0:65, n0:512], lhsT=VV[:, kti, h, :], rhs=pt[:, n0:512], start=first, stop=last),
                     reads=[BV[kb], Bpt], writes=[Bpv])
                state["first"] = False
            for (kti, own) in tiles:
                isown = kti >= 4 * blk
                n0 = own * 128 if isown else 0
                k0 = kti * 128
                kb = kti // 4
                bs, Bbs = nbank()
                P.op("pe", lambda e, k0=k0, n0=n0, bs=bs: e.matmul(bs[:, n0:512], lhsT=K[:, k0:k0 + 128], rhs=Q[:, c0 + n0:c0 + 512], start=True, stop=True),
                     reads=[BK[h][kb], BQ[h][blk]], writes=[Bbs])
                pt, Bpt = wt("pt")
                m = kti - 4 * blk + MOFF
                P.op("act", lambda e, n0=n0, bs=bs, pt=pt, m=m: e.activation(out=pt[:, n0:512], in_=bs[:, n0:512], func=AF.Exp, scale=0.125, bias=abias[:, h, m:m + 1]),
                     reads=[Bbs, Bab], writes=[Bpt])
                if isown:
                    P.op("dve", lambda e, n0=n0, pt=pt: e.tensor_tensor(out=pt[:, n0:n0 + 128], in0=pt[:, n0:n0 + 128], in1=cmask[:], op=ALU.mult), reads=[Bpt, Bcm], writes=[Bpt])
                last = (kti == tiles[-1][0])
                pend.append((kti, n0, pt, Bpt, kb, last))
                if len(pend) > LA:
                    emit_pv(pend.pop(0))
                ntile_done[0] += 1
                if ntile_done[0] == 5 and prev_fin:
                    prev_fin.pop(0)()
                if tick is not None:
                    tick()
            while pend:
                emit_pv(pend.pop(0))
            while prev_fin:
                prev_fin.pop(0)()
            onum, Bonum = wt("onum"); rec, Brec = wt("rec"); my, Bmy = wt("my")
            P.op("act", lambda e: e.activation(out=onum[0:65, :], in_=pv[0:65, :], func=AF.Identity), reads=[Bpv], writes=[Bonum])

            def finalize():
                P.op("act", lambda e: e.activation(out=rec[64:65, :], in_=onum[64:65, :], func=AF.Ln), reads=[Bonum], writes=[Brec])
                P.op("act", lambda e: e.activation(out=rec[64:65, :], in_=rec[64:65, :], func=AF.Exp, scale=-1.0), reads=[Brec], writes=[Brec])
                bbc, Bbbc = nbank()
                P.op("pe", lambda e: e.matmul(bbc[0:64, :], lhsT=ones64r[64:65, :], rhs=rec[64:65, :], start=True, stop=True), reads=[Bo64r, Brec], writes=[Bbbc])
                P.op("dve", lambda e: e.tensor_tensor(out=my[:], in0=onum[0:64, :], in1=bbc[0:64, :], op=ALU.mult), reads=[Bonum, Bbbc], writes=[Bmy])
                P.dma("sp", ydst(2 + h, blk), my[:], reads=[Bmy])
            postponed.append(finalize)

    import os
    stop = os.environ.get("PHB_STOP", "")
    if stop == "init":
        return
    for blk in range(NB):
        stage1(blk)
        if stop == "s1":
            return
        PQ.tag = blk - 1
        for h_ in range(2):
            gating(blk, h_)
        PQ.tag = blk
        stage2(blk)
        if blk > 0:
            ntl = max(1, 2 * (4 * (blk - 1) + 4))
            must = PQ.count_upto(blk - 1)
            per = max(1, (must + len(PQ.q) // 2 + ntl - 1) // ntl)
            stage3(blk - 1, tick=lambda per=per: PQ.flush(per))
        PQ.flush_upto(blk - 1)
        if "coll_y" in io and blk >= 2:
            ybufs = W["yo"][1] + W["hy"][1] + W["my"][1]
            io["coll_y"](blk - 2, [b_.dsem for b_ in ybufs if b_.dsem is not None])
        if stop == "s2":
            PQ.flush()
            return
    PQ.flush_upto(NB - 2)
    stage3(NB - 1, tick=lambda: PQ.flush(2))
    PQ.flush()
    while postponed:
        postponed.pop(0)()


EPS = 1e-6


def host_consts_C():
    c = {}
    oa = np.zeros((128, 128), np.float32)
    oa[:64, :] = 1.0 / 256.0
    c["onesA"] = oa
    c["onesC"] = np.full((128, 128), 1.0 / 512.0, np.float32)
    c["ones1k"] = np.full((128, 128), 1.0 / 1024.0, np.float32)
    return c


def rms_stats(P, banks, Bbanks, src_fn, nchunk, ones_t, Bones, sq_tiles, eps_ap, Bpar, rstd, Brstd, srcbufs):
    bank, Bbank = banks
    for c in range(nchunk):
        sq, Bsq = sq_tiles[c % len(sq_tiles)]
        src = src_fn(c)
        P.op("act", lambda e, sq=sq, src=src: e.activation(out=sq[:], in_=src, func=AF.Square), reads=srcbufs(c), writes=[Bsq])
        P.op("pe", lambda e, sq=sq, c=c: e.matmul(bank[:], lhsT=ones_t[:], rhs=sq[:], start=(c == 0), stop=(c == nchunk - 1)), reads=[Bones, Bsq], writes=[Bbank])
    P.op("act", lambda e: e.activation(out=rstd[:], in_=bank[:], func=AF.Ln, bias=eps_ap), reads=[Bbank, Bpar], writes=[Brstd])
    P.op("act", lambda e: e.activation(out=rstd[:], in_=rstd[:], func=AF.Exp, scale=-0.5), reads=[Brstd], writes=[Brstd])


def build_A(nc, P, io, NT=2048):
    xs = P.sbuf("xs", [128, 8, NT], F32)
    Bxs = [P.buf(f"xs{t}") for t in range(NT // 512)]
    gv = P.sbuf("gv", [128, 16], F32); Bgv = P.buf("gv")
    ones1k = P.sbuf("ones1k", [128, 128], F32); Bo1k = P.buf("ones1k")
    P.dma("sp", gv[:, 0:8], io["gv"], writes=[Bgv])
    P.dma("sp", ones1k[:], io["ones1k"], writes=[Bo1k])
    P.op("dve", lambda e: e.memset(gv[:, 8:9], EPS), reads=[Bgv], writes=[Bgv])
    banks = [P.psum(f"bank{i}", [128, 512]) for i in range(2)]
    Bbank = [P.buf(f"bank{i}") for i in range(2)]
    for b_ in Bbank:
        b_.excl = True
    sqs = [(P.sbuf(f"sq{i}", [128, 512], F32), P.buf(f"sq{i}")) for i in range(2)]
    rs = [(P.sbuf(f"rstd{i}", [128, 512], F32), P.buf(f"rstd{i}")) for i in range(2)]
    ho = [(P.sbuf(f"ho{i}", [128, 8, 512], BF16), P.buf(f"ho{i}")) for i in range(2)]
    xv = io["xT"].rearrange("(c p) n -> p c n", p=128)
    hv = io["hT"].rearrange("(c p) n -> p c n", p=128) if "hT" in io else None
    for t in range(NT // 512):
        P.dma("sp", xs[:, :, t * 512:(t + 1) * 512], xv[:, :, t * 512:(t + 1) * 512], writes=[Bxs[t]])
    for t in range(NT // 512):
        def body(t):
            rstd, Brstd = rs[t % 2]
            rms_stats(P, (banks[t % 2], Bbank[t % 2]), None, lambda c: xs[:, c, t * 512:(t + 1) * 512], 8, ones1k, Bo1k, sqs, gv[:, 8:9], Bgv, rstd, Brstd, lambda c: [Bxs[t]])
            h, Bh = ho[t % 2]
            for c in range(8):
                P.op("dve", lambda e, c=c: e.scalar_tensor_tensor(out=h[:, c, :], in0=xs[:, c, t * 512:(t + 1) * 512], scalar=gv[:, c:c + 1], in1=rstd[:], op0=ALU.mult, op1=ALU.mult),
                     reads=[Bxs[t], Bgv, Brstd], writes=[Bh])
            if "hn_tc" in io:
                P.dma("sp", io["hn_tc"](t), h[:], reads=[Bh])
            else:
                P.dma("sp", hv[:, :, t * 512:(t + 1) * 512], h[:], reads=[Bh])
        body(t)


def build_C(nc, P, io, final, NT=2048):
    NTC = NT // 512
    xs = P.sbuf("xs", [128, 8, NT], F32)
    Bxs = [P.buf(f"xs{t}") for t in range(NTC)]
    gv = P.sbuf("gv", [128, 40], F32); Bgv = P.buf("gv")
    onesA = P.sbuf("onesA", [128, 128], F32); BoA = P.buf("onesA")
    onesC = P.sbuf("onesC", [128, 128], F32); BoC = P.buf("onesC")
    ones1k = P.sbuf("ones1k", [128, 128], F32); Bo1k = P.buf("ones1k")
    P.dma("sp", gv[:, 0:32], io["gv"], writes=[Bgv])
    P.dma("sp", onesA[:], io["onesA"], writes=[BoA])
    P.dma("sp", onesC[:], io["onesC"], writes=[BoC])
    P.dma("sp", ones1k[:], io["ones1k"], writes=[Bo1k])
    P.op("dve", lambda e: e.memset(gv[:, 32:33], EPS), reads=[Bgv], writes=[Bgv])
    eps_ap = gv[:, 32:33]
    banks = [P.psum(f"bank{i}", [128, 512]) for i in range(8)]
    Bbank = [P.buf(f"bank{i}") for i in range(8)]
    for b_ in Bbank:
        b_.excl = True
    rr = [0]

    def nbank():
        i = rr[0] % 8
        rr[0] += 1
        return banks[i], Bbank[i]
    R = P.sbuf("R", [128, 32768], BF16)
    wout = R[:, 0:8192].rearrange("p (c n) -> p c n", c=8); Bwout = P.buf("wout")
    ysb = [R[:, 8192 + i * 4096:8192 + (i + 1) * 4096].rearrange("p (c n) -> p c n", c=8) for i in range(2)]
    Bysb = [P.buf(f"y{i}") for i in range(2)]
    ynb = [R[:, 16384 + i * 4096:16384 + (i + 1) * 4096].rearrange("p (c n) -> p c n", c=8) for i in range(2)]
    Bynb = [P.buf(f"yn{i}") for i in range(2)]
    u = R[:, :].rearrange("p (f n) -> p f n", f=32)
    Bu = [P.buf(f"u{f}") for f in range(32)]
    h2 = P.sbuf("h2", [128, 8, 1024], BF16); Bh2 = [P.buf("h2_0"), P.buf("h2_1")]
    w1b = [(P.sbuf(f"w1b{i}", [128, 8, 512], BF16), P.buf(f"w1b{i}")) for i in range(2)]
    w2b = [(P.sbuf(f"w2b{i}", [128, 4, 512], BF16), P.buf(f"w2b{i}")) for i in range(2)]
    sqs = [(P.sbuf(f"sq{i}", [128, 512], F32), P.buf(f"sq{i}")) for i in range(3)]
    rsA = [(P.sbuf(f"rsA{i}", [128, 512], F32), P.buf(f"rsA{i}")) for i in range(1)] * 2
    rsC = [(P.sbuf(f"rsC{i}", [128, 512], F32), P.buf(f"rsC{i}")) for i in range(1)] * 2
    rs2 = [(P.sbuf(f"rs2{i}", [128, 512], F32), P.buf(f"rs2{i}")) for i in range(2)]
    rl = [(P.sbuf(f"rl{i}", [128, 512], F32), P.buf(f"rl{i}")) for i in range(3)]
    if final:
        st_f = [(P.sbuf(f"stf{i}", [128, 4, 512], F32), P.buf(f"stf{i}")) for i in range(1)]
    else:
        st_b = [(P.sbuf(f"stb{i}", [128, 8, 512], BF16), P.buf(f"stb{i}")) for i in range(1)]

    xv = io["xT"].rearrange("(c p) n -> p c n", p=128)
    for t in range(NTC):
        P.dma("sp", xs[:, :, t * 512:(t + 1) * 512], xv[:, :, t * 512:(t + 1) * 512], writes=[Bxs[t]])
    P.dma("pool", wout, io["wout"].rearrange("(c p) n -> p c n", p=128), writes=[Bwout])
    if "ysrc" in io:
        cand = [(P.sbuf(f"cand{i}", [128, 8, 512], BF16), P.buf(f"cand{i}")) for i in range(1)] * 2
        cand = [(t_[:], b_) for (t_, b_) in cand]
        ohs = P.sbuf("ohs", [128, 4], F32); Bohs = P.buf("ohs")
        P.dma("sp", ohs[:], io["ohs"], writes=[Bohs])

    def ysel(t):
        tsl = slice(t * 512, (t + 1) * 512)
        y, By = ysb[t % 2], Bysb[t % 2]
        if "ysrc" in io:
            for sI in range(4):
                cd, Bcd = cand[sI % 2]
                for pp in range(2):
                    for hh in range(2):
                        P.dma("sp", cd[pp * 64:(pp + 1) * 64, hh::2, :], io["ysrc"](sI, t, pp, hh), writes=[Bcd])
                if sI == 0:
                    P.op("dve", lambda e, cd=cd: e.tensor_scalar(out=y, in0=cd, scalar1=ohs[:, 0:1], scalar2=None, op0=ALU.mult), reads=[Bcd, Bohs], writes=[By])
                else:
                    P.op("dve", lambda e, cd=cd, sI=sI: e.scalar_tensor_tensor(out=y, in0=cd, scalar=ohs[:, sI:sI + 1], in1=y, op0=ALU.mult, op1=ALU.add), reads=[Bcd, Bohs, By], writes=[By])
        else:
            P.dma("sp", y, io["yT"][:, :, tsl].rearrange("c p n -> p c n"), writes=[By])

    def outproj(t):
        tsl = slice(t * 512, (t + 1) * 512)
        y, By = ysb[t % 2], Bysb[t % 2]
        yn, Byn = ynb[t % 2], Bynb[t % 2]
        rA, BrA = rsA[t % 2]; rC, BrC = rsC[t % 2]
        bA = nbank()
        rms_stats(P, bA, None, lambda c: y[:, 2 * c, :], 4, onesA, BoA, sqs, eps_ap, Bgv, rA, BrA, lambda c: [By])
        bC = nbank()
        rms_stats(P, bC, None, lambda c: y[:, 2 * c + 1, :], 4, onesC, BoC, sqs, eps_ap, Bgv, rC, BrC, lambda c: [By])
        for g in range(4):
            c0, c1 = 2 * g, 2 * g + 1
            P.op("dve", lambda e, c0=c0: e.scalar_tensor_tensor(out=yn[0:64, c0, :], in0=y[0:64, c0, :], scalar=gv[0:64, 16 + c0:17 + c0], in1=rA[0:64, :], op0=ALU.mult, op1=ALU.mult),
                 reads=[By, Bgv, BrA], writes=[Byn])
            P.op("dve", lambda e, c0=c0: e.tensor_copy(out=yn[64:128, c0, :], in_=y[64:128, c0, :]), reads=[By], writes=[Byn])
            P.op("dve", lambda e, c1=c1: e.scalar_tensor_tensor(out=yn[:, c1, :], in0=y[:, c1, :], scalar=gv[:, 16 + c1:17 + c1], in1=rC[:], op0=ALU.mult, op1=ALU.mult),
                 reads=[By, Bgv, BrC], writes=[Byn])
        for m in range(8):
            bk, Bbk = nbank()
            for c in range(8):
                P.op("pe", lambda e, c=c, m=m, bk=bk: e.matmul(bk[:], lhsT=wout[:, c, m * 128:(m + 1) * 128], rhs=yn[:, c, :], start=(c == 0), stop=(c == 7)),
                     reads=[Bwout, Byn], writes=[Bbk])
            P.op("dve", lambda e, m=m, bk=bk: e.tensor_tensor(out=xs[:, m, tsl], in0=xs[:, m, tsl], in1=bk[:], op=ALU.add), reads=[Bbk, Bxs[t]], writes=[Bxs[t]])
    ysel(0)
    for t in range(NTC):
        if t + 1 < NTC:
            ysel(t + 1)
        outproj(t)

    w1v = io["w1"].rearrange("(c p) n -> p c n", p=128)
    w2v = io["w2"].rearrange("(f p) n -> p f n", p=128)
    alias_guard = [Bwout] + Bysb + Bynb
    cnt = {"w1": 0, "w2": 0, "rl": 0}

    def ffn_half(hf):
        for tl in range(2):
            t = 2 * hf + tl
            tsl = slice(t * 512, (t + 1) * 512)
            r2, Br2 = rs2[t % 2]
            rms_stats(P, nbank(), None, lambda c, tsl=tsl: xs[:, c, tsl], 8, ones1k, Bo1k, sqs, eps_ap, Bgv, r2, Br2, lambda c, t=t: [Bxs[t]])
            for c in range(8):
                P.op("dve", lambda e, c=c, tsl=tsl, tl=tl, r2=r2: e.scalar_tensor_tensor(out=h2[:, c, tl * 512:(tl + 1) * 512], in0=xs[:, c, tsl], scalar=gv[:, c:c + 1], in1=r2[:], op0=ALU.mult, op1=ALU.mult),
                     reads=[Bxs[t], Bgv, Br2], writes=[Bh2[tl]])
        for fg in range(8):
            w1t, Bw1 = w1b[cnt["w1"] % 2]; cnt["w1"] += 1
            P.dma("pool", w1t[:], w1v[:, :, fg * 512:(fg + 1) * 512], writes=[Bw1])
            for fc in range(4):
                f = fg * 4 + fc
                for tl in range(2):
                    bk, Bbk = nbank()
                    for k in range(8):
                        P.op("pe", lambda e, k=k, fc=fc, tl=tl, bk=bk, w1t=w1t: e.matmul(bk[:], lhsT=w1t[:, k, fc * 128:(fc + 1) * 128], rhs=h2[:, k, tl * 512:(tl + 1) * 512], start=(k == 0), stop=(k == 7)),
                             reads=[Bw1, Bh2[tl]], writes=[Bbk])
                    r, Br = rl[cnt["rl"] % 3]; cnt["rl"] += 1
                    P.op("act", lambda e, bk=bk, r=r: e.activation(out=r[:], in_=bk[:], func=AF.Relu), reads=[Bbk], writes=[Br])
                    extra = alias_guard if (hf == 0) else []
                    eng = "dve"
                    P.op(eng, lambda e, f=f, tl=tl, r=r: e.tensor_tensor(out=u[:, f, tl * 512:(tl + 1) * 512], in0=r[:], in1=r[:], op=ALU.mult), reads=[Br], writes=[Bu[f]] + extra)
        for mh in range(2):
            accs = [[nbank() for tl in range(2)] for mm in range(4)]
            for fg in range(8):
                w2t, Bw2 = w2b[cnt["w2"] % 2]; cnt["w2"] += 1
                P.dma("pool", w2t[:], w2v[:, fg * 4:(fg + 1) * 4, mh * 512:(mh + 1) * 512], writes=[Bw2])
                for fc in range(4):
                    f = fg * 4 + fc
                    for mm in range(4):
                        for tl in range(2):
                            bk, Bbk = accs[mm][tl]
                            P.op("pe", lambda e, fc=fc, f=f, mm=mm, tl=tl, bk=bk, w2t=w2t: e.matmul(bk[:], lhsT=w2t[:, fc, mm * 128:(mm + 1) * 128], rhs=u[:, f, tl * 512:(tl + 1) * 512], start=(f == 0), stop=(f == 31)),
                                 reads=[Bw2, Bu[f]], writes=[Bbk])
            for mm in range(4):
                m = mh * 4 + mm
                for tl in range(2):
                    t = 2 * hf + tl
                    tsl = slice(t * 512, (t + 1) * 512)
                    bk, Bbk = accs[mm][tl]
                    P.op("dve", lambda e, m=m, tsl=tsl, bk=bk: e.tensor_tensor(out=xs[:, m, tsl], in0=xs[:, m, tsl], in1=bk[:], op=ALU.add), reads=[Bbk, Bxs[t]], writes=[Bxs[t]])
    def tail(t):
        tsl = slice(t * 512, (t + 1) * 512)
        r2, Br2 = rs2[t % 2]
        rms_stats(P, nbank(), None, lambda c: xs[:, c, tsl], 8, ones1k, Bo1k, sqs, eps_ap, Bgv, r2, Br2, lambda c: [Bxs[t]])
        if final:
            so, Bso = st_f[0]
            for ch in range(2):
                for c in range(4 * ch, 4 * ch + 4):
                    P.op("dve", lambda e, c=c: e.scalar_tensor_tensor(out=so[:, c % 4, :], in0=xs[:, c, tsl], scalar=gv[:, 8 + c:9 + c], in1=r2[:], op0=ALU.mult, op1=ALU.mult),
                         reads=[Bxs[t], Bgv, Br2], writes=[Bso])
                P.dma("sp", io["oT"].rearrange("(c p) n -> p c n", p=128)[:, 4 * ch:4 * ch + 4, tsl], so[:], reads=[Bso])
        else:
            so, Bso = st_b[0]
            for c in range(8):
                P.op("dve", lambda e, c=c: e.scalar_tensor_tensor(out=so[:, c, :], in0=xs[:, c, tsl], scalar=gv[:, 8 + c:9 + c], in1=r2[:], op0=ALU.mult, op1=ALU.mult),
                     reads=[Bxs[t], Bgv, Br2], writes=[Bso])
            if "hn_tc" in io:
                Bhn = P.buf(f"hn_dram{t}")
                P.dma("sp", io["hn_tc"](t), so[:], reads=[Bso], writes=[Bhn], sem_buf=Bso)
                P.dma("sp", io["xnT"].rearrange("(c p) n -> p c n", p=128)[:, :, tsl], xs[:, :, tsl], reads=[Bxs[t]], sem_buf=Bso)
                io["coll_h"](t, [Bhn])
            else:
                P.dma("sp", io["hnT"].rearrange("(c p) n -> p c n", p=128)[:, :, tsl], so[:], reads=[Bso])
                P.dma("sp", io["xnT"].rearrange("(c p) n -> p c n", p=128)[:, :, tsl], xs[:, :, tsl], reads=[Bxs[t]], sem_buf=Bso)
    early = bool(io.get("early_tail"))
    for hf in range(NTC // 2):
        ffn_half(hf)
        if early:
            tail(2 * hf); tail(2 * hf + 1)

    if not early:
        for t in range(NTC):
            tail(t)


import ml_dtypes
from contextlib import ExitStack
from concourse.bass_utils import run_bass_kernel_spmd

_BF = ml_dtypes.bfloat16
SEQ = 8192
ALL_SLOPES = [2.0 ** (-8.0 * (h + 1) / 8) for h in range(8)]
HEAD_A = lambda g: g
HEAD_B = lambda g: 4 + g
MAXDIST = (2432, None)
GROUPS = [[0, 1, 2, 3], [4, 5, 6, 7]]
NPH = 4
NPY = 4

_cache = {}
B_CONST = ["ohk", "crow", "abias", "cmask", "hmask", "rmask", "ident", "ones64"]
C_CONST = ["onesA", "onesC", "ones1k"]


def _build_fused(seq):
    ntok = seq // 4
    nc = bass.Bass("TRN2", target_bir_lowering=False)
    io = {}

    def din(name, shape, dt):
        io[name] = nc.dram_tensor(name, list(shape), dt, kind="ExternalInput").ap()
    T = seq
    NM = T // 128 + 4
    din("xT", [1024, ntok], F32); din("ohs", [128, 4], F32); din("gvA", [128, 8], F32)
    for l in range(2):
        din(f"wfm{l}", [1024, 640], F32); din(f"wtm{l}", [1024, 192], F32); din(f"par{l}", [128, 16], F32); din(f"wg{l}", [128, 128], F32)
        din(f"wout{l}", [1024, 1024], F32); din(f"w1{l}", [1024, 4096], F32); din(f"w2{l}", [4096, 1024], F32); din(f"gvC{l}", [128, 32], F32)
    din("ohk", [33, T], BF16); din("crow", [2, T], BF16); din("abias", [128, 2, NM], F32)
    din("cmask", [128, 128], BF16); din("hmask", [128, 512], F32); din("rmask", [64, 512], F32)
    din("ident", [128, 128], BF16); din("ones64", [64, 64], F32)
    din("onesA", [128, 128], F32); din("onesC", [128, 128], F32); din("ones1k", [128, 128], F32)
    oT = nc.dram_tensor("oT", [1024, ntok], F32, kind="ExternalOutput").ap()
    NTC = ntok // 512
    hloc = [nc.dram_tensor(f"hloc{l}", [NTC, 1024, 512], BF16) for l in range(2)]
    hall = [nc.dram_tensor(f"hall{l}", [NTC, 4 * 1024, 512], BF16) for l in range(2)]
    yloc = [nc.dram_tensor(f"yloc{l}", [4, 256, ntok], BF16) for l in range(2)]
    yall = [nc.dram_tensor(f"yall{l}", [4, 4 * 256, ntok], BF16) for l in range(2)]
    xres = nc.dram_tensor("xres", [1024, ntok], F32)
    with ExitStack() as st:
        P = Prog(nc, st)
        done_h = [set(), set()]
        P.push_scope("A_")
        hn0 = hloc[0].ap()
        build_A(nc, P, dict(xT=io["xT"], gv=io["gvA"], ones1k=io["ones1k"], hn_tc=lambda t: hn0[t].rearrange("(c p) n -> p c n", p=128)), ntok)
        P.pop_scope()
        for l in range(2):
            P.barrier()
            for t in range(NTC):
                if t not in done_h[l]:
                    P.coll("AllGather", hloc[l].ap()[t].opt(), hall[l].ap()[t].opt(), GROUPS)
            P.barrier()
            P.push_scope(f"B{l}_")
            ioB = dict(wfm=io[f"wfm{l}"], wtm=io[f"wtm{l}"], par=io[f"par{l}"], wg=io[f"wg{l}"])
            for k in B_CONST:
                ioB[k] = io[k]
            ylv = yloc[l].ap().rearrange("q (a p) n -> q a p n", a=4)

            def ydst(i, blk, ylv=ylv):
                tok = blk * 512
                q, lc = tok // ntok, tok % ntok
                return ylv[q][i][:, lc:lc + 512]
            ioB["ydst"] = ydst
            done_y = set()
            bpq = ntok // 512

            def coll_y(blk_done, sems, l=l, done_y=done_y):
                q = (blk_done + 1) // bpq - 1
                if (blk_done + 1) % bpq == 0 and q >= 0 and q not in done_y:
                    P.coll("AllGather", yloc[l].ap()[q].opt(), yall[l].ap()[q].opt(), GROUPS, dsems=sems)
                    done_y.add(q)
            ioB["coll_y"] = coll_y
            hv = hall[l].ap()

            def hsrc(blk, hv=hv):
                tok = blk * 512
                s, tc = tok // ntok, (tok % ntok) // 512
                return hv[tc][s * 1024:(s + 1) * 1024, :].rearrange("(c p) n -> p c n", p=128)
            ioB["hsrc"] = hsrc
            build_B(nc, P, T, ioB, maxdist=MAXDIST)
            P.pop_scope()
            P.barrier()
            for q in range(4):
                if q not in done_y:
                    P.coll("AllGather", yloc[l].ap()[q].opt(), yall[l].ap()[q].opt(), GROUPS)
            P.barrier()
            P.push_scope(f"C{l}_")
            final = (l == 1)
            ioC = dict(xT=(io["xT"] if l == 0 else xres.ap()), wout=io[f"wout{l}"], w1=io[f"w1{l}"], w2=io[f"w2{l}"], gv=io[f"gvC{l}"], ohs=io["ohs"])
            for k in C_CONST:
                ioC[k] = io[k]
            yv = yall[l].ap().rearrange("q (g j r) n -> q j r g n", g=4, j=4)

            def ysrc(sI, t, pp, h, yv=yv):
                return yv[sI][2 * h + pp][:, :, t * 512:(t + 1) * 512]
            ioC["ysrc"] = ysrc
            if final:
                ioC["oT"] = oT
            else:
                ioC["xnT"] = xres.ap()
                hn1 = hloc[1].ap()
                ioC["hn_tc"] = lambda t, hn1=hn1: hn1[t].rearrange("(c p) n -> p c n", p=128)

                def coll_h(t, bufs, l=l):
                    if t < NTC - 2:
                        P.coll("AllGather", hloc[l + 1].ap()[t].opt(), hall[l + 1].ap()[t].opt(), GROUPS, reads=bufs)
                        done_h[l + 1].add(t)
                ioC["coll_h"] = coll_h
            ioC["early_tail"] = True
            build_C(nc, P, ioC, final, ntok)
            P.pop_scope()
        n_ops = {e: len(v) for e, v in P.ops.items()}
        print("fused program ops", n_ops, "dsems", len(P.dsems), flush=True)
        P.finish()
    return nc


def _chunks(v):
    return np.ascontiguousarray(np.asarray(v, np.float32).reshape(8, 128).T)


def _b_weights(l, g, inp):
    w_in = inp["w_in"][l]
    hA, hB = HEAD_A(g), HEAD_B(g)
    sl = lambda base, w, i: w_in[:, base + w * i: base + w * i + w]
    wfm = np.zeros((1024, 640), np.float32)
    wfm[:, 0:64] = sl(512, 64, g)
    wfm[:, 64:128] = sl(0, 64, g)
    wfm[:, 128:192] = sl(768, 64, g)
    wfm[:, 192:256] = sl(256, 64, g)
    wfm[:, 256:320] = sl(1280, 64, g)
    wfm[:, 320:384] = sl(1536, 64, hA)
    wfm[:, 384:448] = sl(1536, 64, hB)
    wfm[:, 448:512] = sl(2048, 64, hA)
    wfm[:, 512:576] = sl(2048, 64, hB)
    wtm = np.concatenate([sl(1024, 64, g), sl(2560, 64, hA), sl(2560, 64, hB)], axis=1)
    par = np.zeros((128, 16), np.float32)
    ch = slice(64 * g, 64 * g + 64)
    par[64:, 0:4] = inp["lru_conv_w"][l][:, ch].T
    par[64:, 4] = inp["lru_conv_b"][l][ch]
    par[64:, 5] = inp["lru_ba"][l][ch]
    par[64:, 6] = inp["lru_bx"][l][ch]
    par[64:, 7] = inp["lru_lambda"][l][ch]
    par[:64, 0] = inp["hg_lower_bounds"][0][ch]
    par[:64, 1] = inp["hg_lower_bounds"][1][ch]
    par[:64, 2] = inp["hg_norm_w"][l]
    par[:64, 3] = float(l)
    wg = np.zeros((128, 128), np.float32)
    wg[64:, 0:64] = inp["lru_wa"][l][g]
    wg[64:, 64:128] = inp["lru_wx"][l][g]
    return {f"wfm{l}": wfm, f"wtm{l}": np.ascontiguousarray(wtm), f"par{l}": par, f"wg{l}": wg}


def _c_weights(l, inp, next_gain):
    w_out = inp["w_out"][l]
    rows = []
    gy = np.ones((128, 8), np.float32)
    for g in range(4):
        hA, hB = HEAD_A(g), HEAD_B(g)
        rows += list(range(64 * g, 64 * g + 64)) + list(range(256 + 64 * g, 256 + 64 * g + 64))
        rows += list(range(512 + 64 * hA, 512 + 64 * hA + 64)) + list(range(512 + 64 * hB, 512 + 64 * hB + 64))
        gy[0:64, 2 * g] = inp["lru_out_norm"][l][64 * g:64 * g + 64]
        gy[0:64, 2 * g + 1] = inp["att_out_norm"][l][64 * hA:64 * hA + 64]
        gy[64:128, 2 * g + 1] = inp["att_out_norm"][l][64 * hB:64 * hB + 64]
    gv = np.zeros((128, 32), np.float32)
    gv[:, 0:8] = _chunks(inp["norm_mlp"][l])
    gv[:, 8:16] = _chunks(next_gain)
    gv[:, 16:24] = gy
    return {f"wout{l}": np.ascontiguousarray(w_out[rows, :]), f"w1{l}": np.ascontiguousarray(inp["w_ff1"][l]),
            f"w2{l}": np.ascontiguousarray(inp["w_ff2"][l]), f"gvC{l}": gv}


def kernel(**inputs):
    inp = {k: np.asarray(v) for k, v in inputs.items()}
    x = inp["x"].astype(np.float32, copy=False)
    B, seq = x.shape[0], x.shape[1]
    ntok = seq // 4
    cores = list(range(8))
    if ("F", seq) not in _cache:
        _cache[("F", seq)] = _build_fused(seq)
    nc = _cache[("F", seq)]
    shared = {}
    shared.update(host_consts_C())
    shared["gvA"] = _chunks(inp["norm_mix"][0])
    for l in range(2):
        shared.update(_c_weights(l, inp, inp["norm_final"] if l == 1 else inp["norm_mix"][l + 1]))
    in_maps = []
    for c in cores:
        b, s = c // 4, c % 4
        d = dict(shared)
        d["xT"] = np.ascontiguousarray(x[b, s * ntok:(s + 1) * ntok, :].T)
        oh = np.zeros((128, 4), np.float32)
        oh[:, s] = 1.0
        d["ohs"] = oh
        d.update(host_consts_B(seq, [ALL_SLOPES[HEAD_A(s)], ALL_SLOPES[HEAD_B(s)]]))
        for l in range(2):
            d.update(_b_weights(l, s, inp))
        in_maps.append(d)
    res = run_bass_kernel_spmd(nc, in_maps, core_ids=cores)
    out = np.empty((B, seq, 1024), np.float32)
    for c in cores:
        b, s = c // 4, c % 4
        out[b, s * ntok:(s + 1) * ntok, :] = np.asarray(res.results[c]["oT"]).T
    return out
```
